# Optimizing a Trainium2 kernel written in Bass

```python
import jax, jax.numpy as jnp
from jax import lax
import numpy as np

D_MODEL = 2048
BATCH = 2
SEQ = 8192
DEPTH = 4

GRID_W = 64
CTX_LEN = 256
MLSTM_WIDTH = D_MODEL // 2
MLSTM_HEADS = 8
MLSTM_HEAD_DIM = MLSTM_WIDTH // MLSTM_HEADS
MLSTM_CHUNK = 128
FORGET_BIAS_LO = 3.0
FORGET_BIAS_HI = 6.0
POOL_WIDTH = D_MODEL // 2
POOL_WINDOWS = (2, 4, 8, 16)
POOL_GROUPS = len(POOL_WINDOWS)
POOL_GROUP_DIM = POOL_WIDTH // POOL_GROUPS
EVEN_IN = 4 * MLSTM_WIDTH + 4 * MLSTM_HEADS + POOL_WIDTH
EVEN_MIX = MLSTM_WIDTH + POOL_WIDTH
NA_HEADS = 16
NA_HEAD_DIM = D_MODEL // NA_HEADS
NA_WIDTH = NA_HEADS * NA_HEAD_DIM
NA_ROWS = 8
NA_COLS = 16
RPB_R = 2 * NA_ROWS - 1
RPB_C = 2 * NA_COLS - 1
N_EXPERTS = 16
EXPERT_FF = D_MODEL // 2
CAPACITY_FACTOR = 2
ROPE_BASE = 10000.0
EPS = 1e-6

kernel_name = "hybrid_mlstm_pool_natten_ec_moe_dit"


def rmsnorm(x, g):
    x32 = x.astype(jnp.float32)
    y = x32 * lax.rsqrt(jnp.mean(x32 * x32, axis=-1, keepdims=True) + EPS)
    return (y * g.astype(jnp.float32)).astype(x.dtype)


def modulate(h, shift, scale):
    return h * (1 + scale) + shift


def rope_1d(x, pos):
    half = x.shape[-1] // 2
    inv = ROPE_BASE ** (-jnp.arange(half, dtype=jnp.float32) / half)
    ang = pos.astype(jnp.float32)[:, None] * inv[None, :]
    cos = jnp.cos(ang).astype(x.dtype)
    sin = jnp.sin(ang).astype(x.dtype)
    x1, x2 = x[..., :half], x[..., half:]
    return jnp.concatenate([x1 * cos - x2 * sin, x1 * sin + x2 * cos], axis=-1)


def axial_rope(x, rows, cols):
    half = x.shape[-1] // 2
    return jnp.concatenate([rope_1d(x[..., :half], rows), rope_1d(x[..., half:], cols)], axis=-1)


def zero_state(batch):
    H, d = MLSTM_HEADS, MLSTM_HEAD_DIM
    return (jnp.zeros((batch, H, d, d), jnp.float32), jnp.zeros((batch, H, d), jnp.float32),
            jnp.zeros((batch, H), jnp.float32))


def mlstm_chunked(q, k, v, li, lf, state0):
    f32 = jnp.float32
    B, H, N, dk = q.shape
    dv = v.shape[-1]
    L = min(MLSTM_CHUNK, N)
    NC = N // L
    q = q.astype(f32).reshape(B, H, NC, L, dk)
    k = k.astype(f32).reshape(B, H, NC, L, dk)
    v = v.astype(f32).reshape(B, H, NC, L, dv)
    li = li.astype(f32).reshape(B, H, NC, L)
    lf = lf.astype(f32).reshape(B, H, NC, L)
    b = jnp.cumsum(lf, axis=-1)
    g = b[..., -1]
    a = g[..., None] - b + li
    m_loc = jnp.max(a, axis=-1)
    wa = jnp.exp(a - m_loc[..., None])
    C_loc = jnp.einsum('bhcl,bhclv,bhclk->bhcvk', wa, v, k)
    n_loc = jnp.einsum('bhcl,bhclk->bhck', wa, k)

    def step(carry, inp):
        C, n, m = carry
        Cl, nl, ml, gc = inp
        m_new = jnp.maximum(gc + m, ml)
        sp = jnp.exp(gc + m - m_new)
        sl = jnp.exp(ml - m_new)
        C_new = sp[..., None, None] * C + sl[..., None, None] * Cl
        n_new = sp[..., None] * n + sl[..., None] * nl
        return (C_new, n_new, m_new), (C, n, m)

    xs = tuple(jnp.moveaxis(t, 2, 0) for t in (C_loc, n_loc, m_loc, g))
    final, starts = lax.scan(step, state0, xs)
    Cs, ns, ms = (jnp.moveaxis(t, 0, 2) for t in starts)
    lower = np.tril(np.ones((L, L), dtype=bool))
    Dm = jnp.where(lower, b[..., :, None] - b[..., None, :] + li[..., None, :], -jnp.inf)
    inter = b + ms[..., None]
    m_j = jnp.maximum(inter, jnp.max(Dm, axis=-1))
    S = jnp.einsum('bhcjd,bhcsd->bhcjs', q, k) * jnp.exp(Dm - m_j[..., None])
    w_int = jnp.exp(inter - m_j)
    num = jnp.einsum('bhcjs,bhcsv->bhcjv', S, v) + w_int[..., None] * jnp.einsum('bhcjk,bhcvk->bhcjv', q, Cs)
    den = jnp.sum(S, axis=-1) + w_int * jnp.einsum('bhcjk,bhck->bhcj', q, ns)
    h = num / jnp.maximum(jnp.abs(den), jnp.exp(-m_j))[..., None]
    return h.reshape(B, H, N, dv), final


def mlstm_bidir(q, k, v, gates, state_fwd, state_bwd):
    flip = lambda t: jnp.flip(t, axis=2)
    h_f, s_f = mlstm_chunked(q, k, v, gates[0], jax.nn.log_sigmoid(gates[1]), state_fwd)
    h_b, s_b = mlstm_chunked(flip(q), flip(k), flip(v), flip(gates[2]), flip(jax.nn.log_sigmoid(gates[3])), state_bwd)
    return h_f + flip(h_b), s_f, s_b


def multiscale_pool(u, pool_w, pool_scale):
    B, N, _ = u.shape
    u32 = u.astype(jnp.float32)
    cs = jnp.concatenate([jnp.zeros((B, 1, POOL_WIDTH), jnp.float32), jnp.cumsum(u32, axis=1)], axis=1)
    t = np.arange(N)
    diffs = []
    for gi, w in enumerate(POOL_WINDOWS):
        lo = np.clip(t - w // 2, 0, N)
        hi = np.clip(t + w // 2, 0, N)
        sl = slice(gi * POOL_GROUP_DIM, (gi + 1) * POOL_GROUP_DIM)
        csg = cs[..., sl]
        cnt = jnp.asarray((hi - lo)[None, :, None], jnp.float32)
        diffs.append((csg[:, hi] - csg[:, lo]) / cnt - u32[..., sl])
    d = jnp.stack(diffs, axis=2).astype(u.dtype)
    y = jnp.einsum('bngc,gce->bnge', d, pool_w).reshape(B, N, POOL_WIDTH)
    return y * pool_scale


def mlstm_pool_mixer(hc, hl, w_in, gate_b, head_g, pool_w, pool_scale, w_out, rows, cols, ctx_out):
    H, dh = MLSTM_HEADS, MLSTM_HEAD_DIM
    cuts = [MLSTM_WIDTH, 2 * MLSTM_WIDTH, 3 * MLSTM_WIDTH, 4 * MLSTM_WIDTH, 4 * MLSTM_WIDTH + 4 * H]

    def project(h):
        B, N, _ = h.shape
        q, k, v, o, gts, u = jnp.split(h @ w_in, cuts, axis=-1)
        heads = lambda t: t.reshape(B, N, H, dh).transpose(0, 2, 1, 3)
        gts = (gts.astype(jnp.float32).reshape(B, N, 4, H) + gate_b.astype(jnp.float32)).transpose(2, 0, 3, 1)
        return heads(q), heads(k) * (dh ** -0.5), heads(v), o, gts, u

    def finish(h, o, u):
        B, _, N, _ = h.shape
        gn = head_g.astype(jnp.float32).reshape(H, dh)[None, :, None, :]
        hn = h * lax.rsqrt(jnp.mean(h * h, axis=-1, keepdims=True) + EPS) * gn
        hn = hn.transpose(0, 2, 1, 3).reshape(B, N, MLSTM_WIDTH).astype(o.dtype) * jax.nn.sigmoid(o)
        return jnp.concatenate([hn, multiscale_pool(u, pool_w, pool_scale)], axis=-1) @ w_out

    qc, kc, vc, oc, gc, uc = project(hc)
    ql, kl, vl, ol, gl, ul = project(hl)
    ql = axial_rope(ql, rows, cols)
    kl = axial_rope(kl, rows, cols)
    z = zero_state(hc.shape[0])
    h_c, s_f, s_b = mlstm_bidir(qc, kc, vc, gc, z, z)
    h_l, _, _ = mlstm_bidir(ql, kl, vl, gl, s_f, s_b)
    out_c = finish(h_c, oc, uc) if ctx_out else None
    return out_c, finish(h_l, ol, ul)


def na_indices(n):
    R = n // GRID_W
    WR = min(NA_ROWS, R)
    WC = NA_COLS
    r = np.arange(R)
    c = np.arange(GRID_W)
    rs = np.clip(r - WR // 2, 0, R - WR)
    cs = np.clip(c - WC // 2, 0, GRID_W - WC)
    kr = rs[:, None] + np.arange(WR)[None, :]
    kc = cs[:, None] + np.arange(WC)[None, :]
    idx = kr[:, None, :, None] * GRID_W + kc[None, :, None, :]
    dr = kr - r[:, None]
    dc = kc - c[:, None]
    bidx = (dr[:, None, :, None] + NA_ROWS - 1) * RPB_C + (dc[None, :, None, :] + NA_COLS - 1)
    return (jnp.asarray(idx.reshape(R, GRID_W, WR * WC), jnp.int32),
            jnp.asarray(bidx.reshape(R, GRID_W, WR * WC), jnp.int32))


def na_mixer(hc, hl, w_in, rpb, w_out, ctx_out):
    H, dh = NA_HEADS, NA_HEAD_DIM
    scale = dh ** -0.5

    def qkv(h):
        B, N, _ = h.shape
        return [t.reshape(B, N, H, dh).transpose(0, 2, 1, 3) for t in jnp.split(h @ w_in, 3, axis=-1)]

    def merge(o):
        B, _, N, _ = o.shape
        return o.transpose(0, 2, 1, 3).reshape(B, N, NA_WIDTH) @ w_out

    qc, kc, vc = qkv(hc)
    ql, kl, vl = qkv(hl)
    out_c = None
    if ctx_out:
        pc = jax.nn.softmax(jnp.einsum('bhqd,bhkd->bhqk', qc * scale, kc).astype(jnp.float32), axis=-1)
        out_c = merge(jnp.einsum('bhqk,bhkd->bhqd', pc.astype(vc.dtype), vc))
    B, _, N, _ = ql.shape
    R = N // GRID_W
    idx, bidx = na_indices(N)
    rpb_flat = rpb.reshape(H, RPB_R * RPB_C)
    qrows = ql.reshape(B, H, R, GRID_W, dh).transpose(2, 0, 1, 3, 4)

    def row_block(args):
        qb, ib, bb = args
        kn = kl[:, :, ib]
        vn = vl[:, :, ib]
        s_nb = jnp.einsum('bhqd,bhqkd->bhqk', qb * scale, kn).astype(jnp.float32) + rpb_flat[:, bb].astype(jnp.float32)
        s_cx = jnp.einsum('bhqd,bhkd->bhqk', qb * scale, kc).astype(jnp.float32)
        p = jax.nn.softmax(jnp.concatenate([s_nb, s_cx], axis=-1), axis=-1).astype(vl.dtype)
        K = ib.shape[-1]
        return (jnp.einsum('bhqk,bhqkd->bhqd', p[..., :K], vn)
                + jnp.einsum('bhqk,bhkd->bhqd', p[..., K:], vc))

    o = lax.map(row_block, (qrows, idx, bidx))
    ol = o.transpose(1, 2, 0, 3, 4).reshape(B, H, N, dh)
    return out_c, merge(ol)


def expert_choice_ffn(h, w_router, w_gate, w_up, w_down):
    B, N, D = h.shape
    cap = CAPACITY_FACTOR * N // N_EXPERTS
    aff = jax.nn.softmax((h @ w_router).astype(jnp.float32), axis=-1)
    gate, idx = lax.top_k(jnp.swapaxes(aff, 1, 2), cap)
    xin = jax.vmap(lambda hb, ib: hb[ib])(h, idx)
    a = jnp.einsum('becd,edf->becf', xin, w_gate)
    u = jnp.einsum('becd,edf->becf', xin, w_up)
    y = jnp.einsum('becf,efd->becd', jax.nn.silu(a) * u, w_down) * gate[..., None].astype(h.dtype)
    bi = jnp.arange(B)[:, None, None]
    return jnp.zeros_like(h).at[bi, idx].add(y)


def setup_inputs(seed: int = 0) -> dict:
    key = jax.random.key(seed)
    ks = jax.random.split(key, 24)
    D = D_MODEL
    n_even = (DEPTH + 1) // 2
    n_odd = DEPTH // 2
    nrm = lambda k, shape, std: jax.random.normal(k, shape, jnp.float32) * std
    fb = jnp.linspace(FORGET_BIAS_LO, FORGET_BIAS_HI, MLSTM_HEADS, dtype=jnp.float32)
    zb = jnp.zeros((MLSTM_HEADS,), jnp.float32)
    gate_base = jnp.stack([zb, fb, zb, fb])
    return {
        "x": nrm(ks[0], (BATCH, SEQ, D), 1.0),
        "c": nrm(ks[1], (BATCH, D), 1.0),
        "ctx": nrm(ks[2], (BATCH, CTX_LEN, D), 1.0),
        "c_ctx": nrm(ks[3], (D,), 1.0),
        "ada_w": nrm(ks[4], (DEPTH, D, 6 * D), 0.5 * D ** -0.5),
        "ada_b": nrm(ks[5], (DEPTH, 6 * D), 0.02),
        "norm_g": 1.0 + nrm(ks[6], (DEPTH, 2, D), 0.02),
        "final_g": 1.0 + nrm(ks[7], (D,), 0.02),
        "ev_w_in": nrm(ks[8], (n_even, D, EVEN_IN), D ** -0.5),
        "ev_gate_b": gate_base[None] + nrm(ks[9], (n_even, 4, MLSTM_HEADS), 0.1),
        "ev_head_g": 1.0 + nrm(ks[10], (n_even, MLSTM_WIDTH), 0.02),
        "ev_pool_w": nrm(ks[11], (n_even, POOL_GROUPS, POOL_GROUP_DIM, POOL_GROUP_DIM), POOL_GROUP_DIM ** -0.5),
        "ev_pool_scale": 1.0 + nrm(ks[12], (n_even, POOL_WIDTH), 0.1),
        "ev_w_out": nrm(ks[13], (n_even, EVEN_MIX, D), EVEN_MIX ** -0.5),
        "na_w_in": nrm(ks[14], (n_odd, D, 3 * NA_WIDTH), D ** -0.5),
        "na_rpb": nrm(ks[15], (n_odd, NA_HEADS, RPB_R, RPB_C), 0.1),
        "na_w_out": nrm(ks[16], (n_odd, NA_WIDTH, D), NA_WIDTH ** -0.5),
        "moe_w_router": nrm(ks[17], (DEPTH, D, N_EXPERTS), D ** -0.5),
        "moe_w_gate": nrm(ks[18], (DEPTH, N_EXPERTS, D, EXPERT_FF), D ** -0.5),
        "moe_w_up": nrm(ks[19], (DEPTH, N_EXPERTS, D, EXPERT_FF), D ** -0.5),
        "moe_w_down": nrm(ks[20], (DEPTH, N_EXPERTS, EXPERT_FF, D), EXPERT_FF ** -0.5),
    }


def reference(x, c, ctx, c_ctx, ada_w, ada_b, norm_g, final_g, ev_w_in, ev_gate_b, ev_head_g, ev_pool_w,
              ev_pool_scale, ev_w_out, na_w_in, na_rpb, na_w_out, moe_w_router, moe_w_gate, moe_w_up, moe_w_down):
    B, N, D = x.shape
    t = jnp.arange(N, dtype=jnp.int32)
    rows = t // GRID_W
    cols = t % GRID_W
    xl, xc = x, ctx
    for i in range(DEPTH):
        last = i == DEPTH - 1
        j = i // 2
        ml = (jax.nn.silu(c) @ ada_w[i] + ada_b[i])[:, None, :]
        mc = jax.nn.silu(c_ctx) @ ada_w[i] + ada_b[i]
        sh_l, sc_l, ga_l, shf_l, scf_l, gaf_l = jnp.split(ml, 6, axis=-1)
        sh_c, sc_c, ga_c, shf_c, scf_c, gaf_c = jnp.split(mc, 6, axis=-1)
        hl = modulate(rmsnorm(xl, norm_g[i, 0]), sh_l, sc_l)
        hc = modulate(rmsnorm(xc, norm_g[i, 0]), sh_c, sc_c)
        if i % 2 == 0:
            oc, ol = mlstm_pool_mixer(hc, hl, ev_w_in[j], ev_gate_b[j], ev_head_g[j], ev_pool_w[j],
                                      ev_pool_scale[j], ev_w_out[j], rows, cols, not last)
        else:
            oc, ol = na_mixer(hc, hl, na_w_in[j], na_rpb[j], na_w_out[j], not last)
        xl = xl + ga_l * ol
        hl = modulate(rmsnorm(xl, norm_g[i, 1]), shf_l, scf_l)
        xl = xl + gaf_l * expert_choice_ffn(hl, moe_w_router[i], moe_w_gate[i], moe_w_up[i], moe_w_down[i])
        if not last:
            xc = xc + ga_c * oc
            hc = modulate(rmsnorm(xc, norm_g[i, 1]), shf_c, scf_c)
            xc = xc + gaf_c * expert_choice_ffn(hc, moe_w_router[i], moe_w_gate[i], moe_w_up[i], moe_w_down[i])
    return rmsnorm(xl, final_g)
```

```python
import contextlib, time
import numpy as np
import ml_dtypes


import numpy as np
import concourse.bass as bass
import concourse.mybir as mybir
from concourse.bass_utils import run_bass_kernel_spmd

F32 = mybir.dt.float32
BF16 = mybir.dt.bfloat16
I32 = mybir.dt.int32
U32 = mybir.dt.uint32
AF = mybir.ActivationFunctionType
ALU = mybir.AluOpType
AX = mybir.AxisListType

ENGS = ("pe", "act", "dve", "pool", "sp")
DMA_Q = {"sp": 16, "pool": 8, "act": 4}
DMA_SEMS = [(q, i) for q, n in DMA_Q.items() for i in range(n)]


class Prog:
    def __init__(self, nc, same_engine_sync=True):
        self.nc = nc
        self.ops = {e: [] for e in ENGS}
        self.cnt = {e: 0 for e in ENGS}
        self.last_write = {}
        self.reads = {}
        self.waited = {e: {} for e in ENGS}
        self.dma_uses = {k: 0 for k in DMA_SEMS}
        self.dma_rr = {q: 0 for q in DMA_Q}
        self.same_engine_sync = same_engine_sync
        self.final_tokens = []
        self.n_ops = 0
        self.psum_names = set()
        import contextlib
        self._st = contextlib.ExitStack()
        self.sems = {}
        for e in ENGS:
            self.sems[("eng", e)] = self._st.enter_context(nc.semaphore("sem_" + e))
        for i in DMA_SEMS:
            self.sems[("dma", i)] = self._st.enter_context(nc.semaphore("sem_dma_%s%d" % i))

    def psum(self, st, name, shape, dt):
        self.psum_names.add(name)
        return st.enter_context(self.nc.psum_tensor(name, shape, dt))

    @staticmethod
    def _key(x):
        if isinstance(x, tuple):
            return (Prog._key(x[0]), x[1])
        if isinstance(x, str):
            return x
        if hasattr(x, 'tensor'):
            return x.tensor.name
        return x.name

    def _deps(self, eng, reads, writes):
        toks = []
        for k in list(reads) + list(writes):
            t = self.last_write.get(k)
            if t is not None:
                toks.append(t)
        for k in writes:
            toks.extend(self.reads.get(k, ()))
        for k in reads:
            kn = k[0] if isinstance(k, tuple) else k
            if isinstance(kn, str) and (kn.startswith("ps") or kn in self.psum_names):
                for t in self.reads.get(k, ()):
                    if t[0] != ("eng", eng):
                        toks.append(t)
        best = {}
        for (s, v) in toks:
            if best.get(s, -1) < v:
                best[s] = v
        out = []
        w = self.waited[eng]
        for s, v in best.items():
            if (not self.same_engine_sync) and s == ("eng", eng) and eng != "sp":
                continue
            if eng == "pe" and s == ("eng", "pe"):
                continue
            if w.get(s, -1) >= v:
                continue
            w[s] = v
            out.append((s, v))
        return out

    def _commit(self, tok, reads, writes):
        for k in writes:
            self.last_write[k] = tok
            self.reads[k] = []
        for k in reads:
            self.reads.setdefault(k, []).append(tok)

    def op(self, eng, fn, reads=(), writes=()):
        reads = [self._key(r) for r in reads]
        writes = [self._key(r) for r in writes]
        waits = self._deps(eng, reads, writes)
        self.cnt[eng] += 1
        tok = (("eng", eng), self.cnt[eng])
        self.ops[eng].append((waits, fn, tok))
        self._commit(tok, reads, writes)
        self.n_ops += 1
        return tok

    def dma(self, fn, reads=(), writes=(), eng="sp", final=False):
        reads = [self._key(r) for r in reads]
        writes = [self._key(r) for r in writes]
        i = (eng, self.dma_rr[eng])
        self.dma_rr[eng] = (self.dma_rr[eng] + 1) % DMA_Q[eng]
        s = ("dma", i)
        waits = self._deps(eng, reads, writes)
        prev = self.dma_uses[i] * 16
        if prev > 0 and self.waited[eng].get(s, -1) < prev:
            self.waited[eng][s] = prev
            waits.append((s, prev))
        self.dma_uses[i] += 1
        tok = (s, self.dma_uses[i] * 16)
        self.ops[eng].append((waits, fn, tok))
        self._commit(tok, reads, writes)
        if final:
            self.final_tokens.append(tok)
        self.n_ops += 1
        return tok

    def getreg(self, engh, val):
        if not hasattr(self, "_regs"):
            self._regs = {}
        k = (type(engh).__name__, val)
        if k not in self._regs:
            self._regs[k] = engh.to_reg(val)
        return self._regs[k]

    def barrier(self):
        toks = []
        for e in ENGS:
            if e != "sp" and self.cnt[e] > 0:
                toks.append((("eng", e), self.cnt[e]))
        for i in DMA_SEMS:
            if self.dma_uses[i] > 0:
                toks.append((("dma", i), self.dma_uses[i] * 16))
        for e in ENGS:
            waits = []
            for (s, v) in toks:
                if s == ("eng", e):
                    continue
                if self.waited[e].get(s, -1) < v:
                    self.waited[e][s] = v
                    waits.append((s, v))
            if waits:
                self.ops[e].append((waits, None, None))

    def flush(self, last=False):
        nc = self.nc
        sems = self.sems
        handles = {"pe": "tensor", "act": "scalar", "dve": "vector", "pool": "gpsimd", "sp": "sync"}
        with nc.Block() as block:
            def make(e):
                def body(engh):
                    for (waits, fn, tok) in self.ops[e]:
                        for (s, v) in waits:
                            engh.wait_ge(sems[s], v)
                        if fn is None:
                            continue
                        inst = fn(engh)
                        inst.then_inc(sems[tok[0]], 16 if tok[0][0] == "dma" else 1)
                    if e == "sp" and last:
                        for (s, v) in self.final_tokens:
                            engh.wait_ge(sems[s], v)
                return body
            for e in ENGS:
                getattr(block, handles[e])(make(e))
        self.ops = {e: [] for e in ENGS}

    def end_phase(self, last=False):
        self.barrier()
        self.flush(last)

    def emit(self):
        self.flush(True)


D = 2048; EPS = 1e-6
CAP_L = 1024; CAP_C = 32

def emit_g1(nc, P, pre, io, combine, final):
    xl = io["xl"]; xc = io["xc"]; gv = io["g"]; ident_d = io["ident"]
    if not final:
        modl = io["modl"]; modc = io["modc"]; hTl = io["hTl"]; hTc = io["hTc"]
    else:
        outl = io["outl"]
    if combine:
        yl = io["yl"]; yc = io["yc"]; slotl = io["slotl"]; slotc = io["slotc"]; affl = io["affl"]; affc = io["affc"]
        gafl = io["gafl"]; gafc = io["gafc"]; xlo = io["xlo"]; xco = io["xco"]
    with contextlib.ExitStack() as st:
        T = lambda name, shape, dt=F32: st.enter_context(nc.sbuf_tensor(pre + name, shape, dt))
        ident = T("ident_s", [128, 128], BF16)
        P.dma(lambda e: e.dma_start(out=ident[:], in_=ident_d), writes=[ident])
        g_bc = T("g_bc", [128, D])
        P.dma(lambda e: e.dma_start(out=g_bc[:], in_=gv.to_broadcast([128, D])), writes=[g_bc])
        AB = {}
        if not final:
            tmp = T("tmpbc", [128, D])
            for nm, mod in (("l", modl), ("c", modc)):
                A = T("A_" + nm, [128, D]); Bt = T("B_" + nm, [128, D])
                P.dma(lambda e, mod=mod: e.dma_start(out=tmp[:], in_=mod[1:2, :].to_broadcast([128, D])), writes=[tmp])
                P.dma(lambda e, mod=mod, Bt=Bt: e.dma_start(out=Bt[:], in_=mod[0:1, :].to_broadcast([128, D])), writes=[Bt])
                P.op("dve", lambda e, A=A: e.scalar_tensor_tensor(out=A[:], in0=tmp[:], scalar=1.0, in1=g_bc[:], op0=ALU.add, op1=ALU.mult),
                     reads=[tmp, g_bc], writes=[A])
                AB[nm] = (A, Bt)
        if combine:
            gaf = {}
            for nm, src in (("l", gafl), ("c", gafc)):
                t = T("gaf_" + nm, [128, D])
                P.dma(lambda e, t=t, src=src: e.dma_start(out=t[:], in_=src.to_broadcast([128, D])), writes=[t])
                gaf[nm] = t
            yg = [T("yg%d" % i, [128, D]) for i in range(2)]
            for t in yg:
                P.op("pool", lambda e, t=t: e.memset(t[:], 0.0), writes=[t])
            acc = T("acc", [128, D])
            slot_t = [T("slot%d" % i, [128, 16], I32) for i in range(2)]
            slot_f = T("slotf", [128, 16]); aff_t = [T("afft%d" % i, [128, 16]) for i in range(2)]
            ge = T("ge", [128, 16])
        xt = [T("xt%d" % i, [128, D]) for i in range(2)]
        h1 = T("h1", [128, D])
        ss = T("ss", [128, 1]); rstd = T("rstd", [128, 1])
        if not final:
            hb = [T("hb%d" % i, [128, D], BF16) for i in range(2)]
            stg = [T("stg%d" % i, [128, D], BF16) for i in range(2)]
            pst = [P.psum(st, pre + "pst%d" % i, [128, 1024], BF16) for i in range(4)]
        else:
            ob = [T("ob%d" % i, [128, D]) for i in range(2)]

        for it in range(17):
            isl = it < 16
            rows = 128 if isl else 64
            nm = "l" if isl else "c"
            x = xt[it % 2]
            src = xl[it * 128:(it + 1) * 128, :] if isl else xc
            P.dma(lambda e, x=x, src=src, rows=rows: e.dma_start(out=x[:rows, :], in_=src), writes=[x])
            if combine:
                stt = slot_t[it % 2]; aft = aff_t[it % 2]
                if isl:
                    P.dma(lambda e, stt=stt, s=slotl[it]: e.dma_start(out=stt[:], in_=s), writes=[stt])
                    P.dma(lambda e, aft=aft, s=affl[it]: e.dma_start(out=aft[:], in_=s), writes=[aft])
                else:
                    P.op("pool", lambda e, stt=stt: e.memset(stt[:], 32768), writes=[stt])
                    P.op("pool", lambda e, aft=aft: e.memset(aft[:], 0.0), writes=[aft])
                    P.dma(lambda e, stt=stt: e.dma_start(out=stt[:64, :], in_=slotc), writes=[stt])
                    P.dma(lambda e, aft=aft: e.dma_start(out=aft[:64, :], in_=affc), writes=[aft])
                cap = CAP_L if isl else CAP_C
                ysrc = yl if isl else yc
                P.op("dve", lambda e, stt=stt: e.tensor_copy(out=slot_f[:], in_=stt[:]), reads=[stt], writes=[slot_f])
                P.op("dve", lambda e, cap=cap: e.tensor_scalar(out=ge[:], in0=slot_f[:], scalar1=float(cap) - 0.5, scalar2=None, op0=ALU.is_lt),
                     reads=[slot_f], writes=[ge])
                P.op("dve", lambda e, aft=aft: e.tensor_tensor(out=ge[:], in0=ge[:], in1=aft[:], op=ALU.mult), reads=[ge, aft], writes=[ge])
                for ex in range(16):
                    y = yg[ex % 2]
                    P.dma(lambda e, y=y, ex=ex, stt=stt, ysrc=ysrc, cap=cap: e.indirect_dma_start(
                        out=y[:, :], out_offset=None, in_=ysrc[ex][:, :],
                        in_offset=bass.IndirectOffsetOnAxis(ap=stt[:, ex:ex + 1], axis=0),
                        bounds_check=P.getreg(e, cap - 1), oob_is_err=False), eng="pool", reads=[stt], writes=[y])
                    if ex == 0:
                        P.op("dve", lambda e, y=y, ex=ex, rows=rows: e.tensor_scalar(out=acc[:rows, :], in0=y[:rows, :], scalar1=ge[:rows, ex:ex + 1], scalar2=None, op0=ALU.mult),
                             reads=[y, ge], writes=[acc])
                    else:
                        P.op("dve", lambda e, y=y, ex=ex, rows=rows: e.scalar_tensor_tensor(out=acc[:rows, :], in0=y[:rows, :], scalar=ge[:rows, ex:ex + 1], in1=acc[:rows, :], op0=ALU.mult, op1=ALU.add),
                             reads=[y, ge, acc], writes=[acc])
                gt = gaf[nm]
                P.op("dve", lambda e, rows=rows, gt=gt: e.tensor_tensor(out=acc[:rows, :], in0=acc[:rows, :], in1=gt[:rows, :], op=ALU.mult), reads=[acc, gt], writes=[acc])
                P.op("dve", lambda e, rows=rows, x=x: e.tensor_tensor(out=x[:rows, :], in0=x[:rows, :], in1=acc[:rows, :], op=ALU.add), reads=[acc, x], writes=[x])
                dst = xlo[it * 128:(it + 1) * 128, :] if isl else xco
                P.dma(lambda e, x=x, dst=dst, rows=rows: e.dma_start(out=dst, in_=x[:rows, :]), reads=[x], writes=["xo"], final=True)
            if final and not isl:
                continue
            P.op("act", lambda e, x=x, rows=rows: e.activation(out=h1[:rows, :], in_=x[:rows, :], func=AF.Square, accum_out=ss[:rows, :]),
                 reads=[x], writes=[h1, ss])
            P.op("act", lambda e, rows=rows: e.activation(out=rstd[:rows, :], in_=ss[:rows, :], func=AF.Sqrt, scale=1.0 / D, bias=EPS),
                 reads=[ss], writes=[rstd])
            P.op("dve", lambda e, rows=rows: e.reciprocal(out=rstd[:rows, :], in_=rstd[:rows, :]),
                 reads=[rstd], writes=[rstd])
            if final:
                o = ob[it % 2]
                P.op("dve", lambda e, x=x, o=o: e.scalar_tensor_tensor(out=o[:], in0=x[:], scalar=rstd[:, 0:1], in1=g_bc[:], op0=ALU.mult, op1=ALU.mult),
                     reads=[x, rstd, g_bc], writes=[o])
                P.dma(lambda e, o=o, it=it: e.dma_start(out=outl[it * 128:(it + 1) * 128, :], in_=o[:]), reads=[o], writes=["outl"], final=True)
                continue
            A, Bt = AB[nm]
            P.op("dve", lambda e, x=x, rows=rows, A=A: e.scalar_tensor_tensor(out=h1[:rows, :], in0=x[:rows, :], scalar=rstd[:rows, 0:1], in1=A[:rows, :], op0=ALU.mult, op1=ALU.mult),
                 reads=[x, rstd, A], writes=[h1])
            h = hb[it % 2]
            P.op("pool", lambda e, h=h, rows=rows, Bt=Bt: e.tensor_tensor(out=h[:rows, :], in0=h1[:rows, :], in1=Bt[:rows, :], op=ALU.add),
                 reads=[h1, Bt], writes=[h])
            sg = stg[it % 2]
            if isl:
                for half in range(2):
                    pt = pst[(it * 2 + half) % 4]
                    for k in range(8):
                        kc = half * 8 + k
                        P.op("pe", lambda e, pt=pt, h=h, k=k, kc=kc: e.transpose(out=pt[:, k * 128:(k + 1) * 128], in_=h[:, kc * 128:(kc + 1) * 128], identity=ident[:]),
                             reads=[h, ident], writes=[pt])
                    eng = "act" if half == 0 else "dve"
                    if eng == "act":
                        P.op("act", lambda e, pt=pt, sg=sg, half=half: e.copy(out=sg[:, half * 1024:(half + 1) * 1024], in_=pt[:]), reads=[pt], writes=[(sg, half)])
                    else:
                        P.op("dve", lambda e, pt=pt, sg=sg, half=half: e.tensor_copy(out=sg[:, half * 1024:(half + 1) * 1024], in_=pt[:]), reads=[pt], writes=[(sg, half)])
                P.dma(lambda e, sg=sg, it=it: e.dma_start(out=hTl[it], in_=sg[:]), reads=[(sg, 0), (sg, 1)], writes=["hTl"], final=True)
            else:
                pt = pst[0]
                for kc in range(16):
                    P.op("pe", lambda e, pt=pt, h=h, kc=kc: e.transpose(out=pt[:, kc * 64:(kc + 1) * 64], in_=h[:64, kc * 128:(kc + 1) * 128], identity=ident[:64, :64]),
                         reads=[h, ident], writes=[pt])
                P.op("act", lambda e, pt=pt, sg=sg: e.copy(out=sg[:, 0:1024], in_=pt[:]), reads=[pt], writes=[(sg, 0)])
                P.dma(lambda e, sg=sg: e.dma_start(out=hTc, in_=sg[:, 0:1024].rearrange("p (k t) -> p k t", k=16)), reads=[(sg, 0)], writes=["hTc"], final=True)
        P.end_phase()


D = 2048; EPS = 1e-6
NCH = 66
POOL_W = (2, 4, 8, 16)

def emit_mixe(nc, P, pre, io, hp):
    stop = 9; do_pool = True; nheads = 2
    q = hp
    hT = io["HT"]; MIXT = io["MIXT"]; w_in = io["w_in"]; gate_b = io["gate_b"]; head_g = io["head_g"]
    tab = io["tab"]; identb_d = io["identb"]; identf_d = io["identf"]; tri_d = io["tri"]
    pw = io["pool_w"]; pool_scale = io["pool_scale"]
    icl = io["icl"][:, q * 2048:(q + 1) * 2048]; icc = io["icc"][:, q * 64:(q + 1) * 64]
    wu = w_in[:, 4128:5152]
    with contextlib.ExitStack() as st0:
        psf = [P.psum(st0, pre + "psf%d" % i, [128, 512], F32) for i in range(6)]
        psb = [P.psum(st0, pre + "psb%d" % i, [128, 1024], BF16) for i in range(2)]
        T0 = lambda name, shape, dt=F32: st0.enter_context(nc.sbuf_tensor(pre + name, shape, dt))
        identb = T0("identb_s", [128, 128], BF16); identf = T0("identf_s", [128, 128])
        tri = [T0("tri%d" % i, [128, 128]) for i in range(2)]
        ones = T0("ones", [128, 128]); zeros = T0("zeros", [128, 128])
        P.dma(lambda e: e.dma_start(out=identb[:], in_=identb_d), writes=[identb])
        P.dma(lambda e: e.dma_start(out=identf[:], in_=identf_d), writes=[identf])
        for i in range(2):
            P.dma(lambda e, i=i: e.dma_start(out=tri[i][:], in_=tri_d[i]), writes=[tri[i]])
        P.op("pool", lambda e: e.memset(ones[:], 1.0), writes=[ones])
        P.op("pool", lambda e: e.memset(zeros[:], 0.0), writes=[zeros])

        with contextlib.ExitStack() as st:
            T = lambda name, shape, dt=F32: st.enter_context(nc.sbuf_tensor(pre + name, shape, dt))
            Wh = T("Wh", [128, 16 * 512], BF16); Wg = T("Wg", [128, 16 * 4], BF16)
            gbt = T("gbt", [128, 4]); gn = T("gn", [128, 128]); gball = T("gball", [128, 32])
            QKT = T("QKT", [128, NCH * 256], BF16)
            Ktok = T("Ktok", [128, NCH * 128], BF16)
            VE = T("VE", [128, NCH * 129], BF16)
            SO = T("SO", [128, NCH * 128], BF16)
            HF = T("HF", [128, NCH * 128])
            OUT = T("OUTs", [128, NCH * 128], BF16)
            G = T("G", [128, NCH * 4])
            hch = [T("hch%d" % i, [128, 16 * 128], BF16) for i in range(2)]
            tb = [T("tb%d" % i, [128, 256]) for i in range(2)]
            qkt = [T("qkt%d" % i, [128, 256], BF16) for i in range(2)]
            rt = [T("rt%d" % i, [128, 128]) for i in range(4)]
            LF = T("LF", [128, NCH]); Bm = T("Bm", [128, NCH]); U = T("U", [128, NCH]); GS = T("GS", [128, NCH])
            UM = T("UM", [128, NCH]); MS = T("MS", [128, NCH + 1]); ME = T("ME", [128, NCH]); tA = T("tA", [128, NCH])
            um = T("um", [128, 1]); umb = T("umb", [128, 128])
            SPt = [T("SP%d" % i, [128, NCH]) for i in range(2)]
            WAt = [T("WA%d" % i, [128, NCH]) for i in range(2)]
            FLt = [T("FL%d" % i, [128, NCH]) for i in range(2)]
            CTs = [T("CT%d" % i, [128, 129]) for i in range(2)]; CTbs = [T("CTb%d" % i, [128, 129], BF16) for i in range(2)]
            PT = [T("PT%d" % i, [128, 128], BF16) for i in range(2)]
            vw = [T("vw%d" % i, [128, 129], BF16) for i in range(2)]
            rr = [T("rr%d" % i, [128, 1]) for i in range(2)]
            ssq = T("ssq", [128, NCH]); rs = T("rs", [128, NCH])
            hn1 = [T("hn1_%d" % i, [128, 128]) for i in range(2)]
            hnb = [T("hnb%d" % i, [128, 128], BF16) for i in range(2)]

            QKT3 = QKT[:].rearrange("p (c x) -> p c x", x=256)
            K3 = Ktok[:].rearrange("p (c x) -> p c x", x=128)
            VE3 = VE[:].rearrange("p (c x) -> p c x", x=129)
            SO3 = SO[:].rearrange("p (c x) -> p c x", x=128)
            HF3 = HF[:].rearrange("p (c x) -> p c x", x=128)
            G3 = G[:].rearrange("p (c x) -> p c x", x=4)
            Wh3 = Wh[:].rearrange("p (k n) -> p k n", k=16)
            Wg3 = Wg[:].rearrange("p (k n) -> p k n", k=16)

            P.op("pool", lambda e: e.memset(VE[:], 1.0), writes=[VE])
            for hd in range(nheads):
                h = 2 * hp + hd
                for s_ in range(4):
                    c0 = s_ * 1024 + h * 128
                    P.dma(lambda e, s_=s_, c0=c0: e.dma_start(out=Wh3[:, :, s_ * 128:(s_ + 1) * 128], in_=w_in[:, c0:c0 + 128].rearrange("(k p) n -> p k n", p=128)), eng="pool", writes=[Wh])
                for gt in range(4):
                    c0 = 4096 + gt * 8 + h
                    P.dma(lambda e, gt=gt, c0=c0: e.dma_start(out=Wg3[:, :, gt:gt + 1], in_=w_in[:, c0:c0 + 1].rearrange("(k p) n -> p k n", p=128), allow_slow_non_contiguous=True), eng="pool", writes=[Wg])
                P.dma(lambda e: e.dma_start(out=gball[:], in_=gate_b.rearrange("g h -> (g h)").unsqueeze(0).to_broadcast([128, 32])), writes=[gball])
                P.op("dve", lambda e, h=h: e.tensor_copy(out=gbt[:], in_=gball[:].rearrange("p (g h) -> p g h", g=4)[:, :, h]), reads=[gball], writes=[gbt])
                P.dma(lambda e, h=h: e.dma_start(out=gn[:], in_=head_g[:, h * 128:(h + 1) * 128].to_broadcast([128, 128])), writes=[gn])
                for c in range(NCH):
                    hc_ = hch[c % 2]; tbc = tb[c % 2]; qk = qkt[c % 2]
                    pp = psf[c % 2]; pg = psf[2 + c % 2]; pb = psb[c % 2]
                    P.dma(lambda e, hc_=hc_, c=c: e.dma_start(out=hc_[:], in_=hT[c]), writes=[hc_])
                    P.dma(lambda e, tbc=tbc, c=c: e.dma_start(out=tbc[:], in_=tab[c]), writes=[tbc])
                    h3 = hc_[:].rearrange("p (k t) -> p k t", k=16)
                    for kc in range(16):
                        P.op("pe", lambda e, pp=pp, h3=h3, kc=kc: e.matmul(pp[:, :], lhsT=h3[:, kc, :], rhs=Wh3[:, kc, :], start=(kc == 0), stop=(kc == 15)),
                             reads=[hc_, Wh], writes=[pp])
                    for kc in range(16):
                        P.op("pe", lambda e, pg=pg, h3=h3, kc=kc: e.matmul(pg[:, 0:4], lhsT=h3[:, kc, :], rhs=Wg3[:, kc, :], start=(kc == 0), stop=(kc == 15)),
                             reads=[hc_, Wg], writes=[pg])
                    xv = pp[:, 0:256].rearrange("p (a b h d) -> p a b h d", a=2, b=2, h=2)
                    x1 = xv[:, :, :, 0, :]; x2 = xv[:, :, :, 1, :]
                    tv = tbc[:].rearrange("p (s a b d) -> p s a b d", s=2, a=2, b=2)
                    Cv = tv[:, 0]; Sv = tv[:, 1]
                    ov = qk[:].rearrange("p (a b h d) -> p a b h d", a=2, b=2, h=2)
                    r4 = [t[:].rearrange("p (a b d) -> p a b d", a=2, b=2) for t in rt]
                    P.op("dve", lambda e, x1=x1, Cv=Cv, o=r4[0]: e.tensor_tensor(out=o, in0=x1, in1=Cv, op=ALU.mult), reads=[pp, tbc], writes=[rt[0]])
                    P.op("dve", lambda e, x2=x2, Sv=Sv, o=r4[1]: e.tensor_tensor(out=o, in0=x2, in1=Sv, op=ALU.mult), reads=[pp, tbc], writes=[rt[1]])
                    P.op("dve", lambda e, x1=x1, Sv=Sv, o=r4[2]: e.tensor_tensor(out=o, in0=x1, in1=Sv, op=ALU.mult), reads=[pp, tbc], writes=[rt[2]])
                    P.op("dve", lambda e, x2=x2, Cv=Cv, o=r4[3]: e.tensor_tensor(out=o, in0=x2, in1=Cv, op=ALU.mult), reads=[pp, tbc], writes=[rt[3]])
                    P.op("pool", lambda e, o=ov[:, :, :, 0, :], a=r4[0], b=r4[1]: e.tensor_tensor(out=o, in0=a, in1=b, op=ALU.subtract), reads=[rt[0], rt[1]], writes=[(qk, 0)])
                    P.op("pool", lambda e, o=ov[:, :, :, 1, :], a=r4[2], b=r4[3]: e.tensor_tensor(out=o, in0=a, in1=b, op=ALU.add), reads=[rt[2], rt[3]], writes=[(qk, 1)])
                    P.op("act", lambda e, pp=pp, c=c: e.copy(out=VE3[:, c, 0:128], in_=pp[:, 256:384]), reads=[pp], writes=[(VE, c)])
                    P.op("act", lambda e, pp=pp, c=c: e.activation(out=SO3[:, c, :], in_=pp[:, 384:512], func=AF.Sigmoid), reads=[pp], writes=[(SO, c)])
                    P.op("dve", lambda e, pg=pg, c=c: e.tensor_tensor(out=G3[:, c, :], in0=pg[:, 0:4], in1=gbt[:], op=ALU.add), reads=[pg, gbt], writes=[(G, c)])
                    P.op("pool", lambda e, qk=qk, c=c: e.tensor_copy(out=K3[:, c, :], in_=qk[:, 128:256]), reads=[(qk, 0), (qk, 1)], writes=[(Ktok, c)])
                    for a in range(2):
                        P.op("pe", lambda e, pb=pb, qk=qk, a=a: e.transpose(out=pb[:, a * 128:(a + 1) * 128], in_=qk[:, a * 128:(a + 1) * 128], identity=identb[:]),
                             reads=[(qk, 0), (qk, 1), identb], writes=[pb])
                    P.op("act", lambda e, pb=pb, c=c: e.copy(out=QKT3[:, c, :], in_=pb[:, 0:256]), reads=[pb], writes=[(QKT, c)])
                ordrs = []
                for dr in range(2 if stop >= 2 else 0):
                    ordr = list(range(NCH)) if dr == 0 else [1, 0] + list(range(NCH - 1, 1, -1))
                    li = G3[:, :, 2 * dr]; ff = G3[:, :, 2 * dr + 1]
                    Gk = [(G, c) for c in range(NCH)]
                    P.op("act", lambda e, ff=ff: e.activation(out=tA[:], in_=ff, func=AF.Exp, scale=-1.0), reads=Gk, writes=[tA])
                    P.op("act", lambda e: e.activation(out=tA[:], in_=tA[:], func=AF.Ln, bias=1.0), reads=[tA], writes=[tA])
                    P.op("dve", lambda e: e.tensor_scalar(out=LF[:], in0=tA[:], scalar1=-1.0, scalar2=None, op0=ALU.mult), reads=[tA], writes=[LF])
                    pa = psf[4]
                    P.op("pe", lambda e, dr=dr: e.matmul(pa[:, 0:NCH], lhsT=tri[dr][:], rhs=LF[:], start=True, stop=True), reads=[tri[dr], LF], writes=[pa])
                    P.op("pe", lambda e: e.matmul(pa[:, 128:128 + NCH], lhsT=ones[:], rhs=LF[:], start=True, stop=True), reads=[ones, LF], writes=[pa])
                    P.op("act", lambda e: e.copy(out=Bm[:], in_=pa[:, 0:NCH]), reads=[pa], writes=[Bm])
                    P.op("act", lambda e: e.copy(out=GS[:], in_=pa[:, 128:128 + NCH]), reads=[pa], writes=[GS])
                    P.op("dve", lambda e, li=li: e.tensor_tensor(out=U[:], in0=li, in1=Bm[:], op=ALU.subtract), reads=Gk + [Bm], writes=[U])
                    P.op("pe", lambda e: e.transpose(out=pa[:NCH, 256:384], in_=U[:], identity=identf[:]), reads=[U, identf], writes=[pa])
                    P.op("dve", lambda e: e.reduce_max(out=um[:NCH, :], in_=pa[:NCH, 256:384], axis=AX.X), reads=[pa], writes=[um])
                    P.op("dve", lambda e: e.tensor_scalar(out=umb[:NCH, :], in0=zeros[:NCH, :], scalar1=um[:NCH, 0:1], scalar2=None, op0=ALU.add), reads=[um, zeros], writes=[umb])
                    P.op("pe", lambda e: e.matmul(pa[:, 384:384 + NCH], lhsT=umb[:NCH, :], rhs=identf[:NCH, :NCH], start=True, stop=True), reads=[umb, identf], writes=[pa])
                    P.op("act", lambda e: e.copy(out=UM[:], in_=pa[:, 384:384 + NCH]), reads=[pa], writes=[UM])
                    P.op("pool", lambda e: e.memset(MS[:], 0.0), writes=[MS])
                    for n, c in enumerate(ordr):
                        P.op("dve", lambda e, c=c: e.tensor_tensor(out=ME[:, c:c + 1], in0=MS[:, c:c + 1], in1=UM[:, c:c + 1], op=ALU.max), reads=[MS, UM], writes=[ME])
                        if n + 1 < NCH:
                            cn = ordr[n + 1]
                            P.op("dve", lambda e, c=c, cn=cn: e.tensor_tensor(out=MS[:, cn:cn + 1], in0=ME[:, c:c + 1], in1=GS[:, c:c + 1], op=ALU.add), reads=[ME, GS], writes=[MS])
                    SPd, WAd, FLd = SPt[dr], WAt[dr], FLt[dr]
                    P.op("dve", lambda e: e.tensor_tensor(out=tA[:], in0=MS[:, 0:NCH], in1=ME[:], op=ALU.subtract), reads=[MS, ME], writes=[tA])
                    P.op("act", lambda e, SPd=SPd: e.activation(out=SPd[:], in_=tA[:], func=AF.Exp), reads=[tA], writes=[SPd])
                    P.op("dve", lambda e: e.tensor_tensor(out=tA[:], in0=U[:], in1=ME[:], op=ALU.subtract), reads=[U, ME], writes=[tA])
                    P.op("act", lambda e, WAd=WAd: e.activation(out=WAd[:], in_=tA[:], func=AF.Exp), reads=[tA], writes=[WAd])
                    P.op("dve", lambda e: e.tensor_tensor(out=tA[:], in0=Bm[:], in1=ME[:], op=ALU.add), reads=[Bm, ME], writes=[tA])
                    P.op("act", lambda e, FLd=FLd: e.activation(out=FLd[:], in_=tA[:], func=AF.Exp, scale=-1.0), reads=[tA], writes=[FLd])
                    ordrs.append(ordr)
                P.op("pool", lambda e: e.memset(HF[:], 0.0), writes=[(HF, c) for c in range(NCH)])
                for dr in range(2):
                    P.op("pool", lambda e, dr=dr: e.memset(CTs[dr][:], 0.0), writes=[CTs[dr]])
                for n in range(NCH):
                    for dr in range(2):
                        c = ordrs[dr][n]
                        SPd, WAd, FLd = SPt[dr], WAt[dr], FLt[dr]
                        CT = CTs[dr]; CTb = CTbs[dr]
                        pS = psf[dr]; pO = psf[2 + dr]; pC = psf[4 + dr]
                        PTn = PT[dr]; vwn = vw[dr]; rrn = rr[dr]
                        P.op("pe", lambda e, pS=pS, c=c: e.matmul(pS[:, 0:128], lhsT=QKT3[:, c, 128:256], rhs=QKT3[:, c, 0:128], start=True, stop=True),
                             reads=[(QKT, c)], writes=[pS])
                        P.op("dve", lambda e, pS=pS, PTn=PTn, dr=dr: e.tensor_tensor(out=PTn[:], in0=pS[:, 0:128], in1=tri[dr][:], op=ALU.mult), reads=[pS, tri[dr]], writes=[PTn])
                        P.op("act", lambda e, vwn=vwn, c=c, WAd=WAd: e.activation(out=vwn[:], in_=VE3[:, c, :], func=AF.Copy, scale=WAd[:, c:c + 1]), reads=[(VE, c), WAd], writes=[vwn])
                        P.op("dve", lambda e, c=c, SPd=SPd, CT=CT: e.tensor_scalar(out=CT[:], in0=CT[:], scalar1=SPd[:, c:c + 1], scalar2=None, op0=ALU.mult), reads=[CT, SPd], writes=[CT])
                        P.op("act", lambda e, CT=CT, CTb=CTb: e.copy(out=CTb[:], in_=CT[:]), reads=[CT], writes=[CTb])
                        P.op("pe", lambda e, pO=pO, PTn=PTn, vwn=vwn: e.matmul(pO[:, 0:129], lhsT=PTn[:], rhs=vwn[:], start=True, stop=False), reads=[PTn, vwn], writes=[pO])
                        P.op("pe", lambda e, pO=pO, c=c, CTb=CTb: e.matmul(pO[:, 0:129], lhsT=QKT3[:, c, 0:128], rhs=CTb[:], start=False, stop=True), reads=[(QKT, c), CTb], writes=[pO])
                        P.op("pe", lambda e, pC=pC, c=c, vwn=vwn: e.matmul(pC[:, 0:129], lhsT=K3[:, c, :], rhs=vwn[:], start=True, stop=True), reads=[(Ktok, c), vwn], writes=[pC])
                        P.op("dve", lambda e, pC=pC, CT=CT: e.tensor_tensor(out=CT[:], in0=CT[:], in1=pC[:, 0:129], op=ALU.add), reads=[CT, pC], writes=[CT])
                        P.op("act", lambda e, pO=pO, rrn=rrn: e.activation(out=rrn[:], in_=pO[:, 128:129], func=AF.Abs),
                             reads=[pO], writes=[rrn])
                        P.op("dve", lambda e, rrn=rrn, c=c, FLd=FLd: e.tensor_scalar(out=rrn[:], in0=rrn[:], scalar1=FLd[:, c:c + 1], scalar2=None, op0=ALU.max),
                             reads=[rrn, FLd], writes=[rrn])
                        P.op("dve", lambda e, rrn=rrn: e.reciprocal(out=rrn[:], in_=rrn[:]), reads=[rrn], writes=[rrn])
                        P.op("dve", lambda e, pO=pO, rrn=rrn, c=c: e.scalar_tensor_tensor(out=HF3[:, c, :], in0=pO[:, 0:128], scalar=rrn[:, 0:1], in1=HF3[:, c, :], op0=ALU.mult, op1=ALU.add),
                             reads=[pO, rrn, (HF, c)], writes=[(HF, c)])
                if stop < 4:
                    continue
                for c in range(NCH):
                    h1 = hn1[c % 2]
                    P.op("act", lambda e, c=c, h1=h1: e.activation(out=h1[:], in_=HF3[:, c, :], func=AF.Square, accum_out=ssq[:, c:c + 1]), reads=[(HF, c)], writes=[h1, (ssq, c)])
                sk = [(ssq, c) for c in range(NCH)]
                P.op("act", lambda e: e.activation(out=rs[:], in_=ssq[:], func=AF.Sqrt, scale=1.0 / 128, bias=EPS), reads=sk, writes=[rs])
                P.op("dve", lambda e: e.reciprocal(out=rs[:], in_=rs[:]), reads=[rs], writes=[rs])
                OUT3 = OUT[:].rearrange("p (c x) -> p c x", x=128)
                for c in range(NCH):
                    h1 = hn1[c % 2]; hb_ = hnb[c % 2]
                    grp, k = divmod(c, 8)
                    pb = psb[grp % 2]
                    P.op("dve", lambda e, c=c, h1=h1: e.scalar_tensor_tensor(out=h1[:], in0=HF3[:, c, :], scalar=rs[:, c:c + 1], in1=gn[:], op0=ALU.mult, op1=ALU.mult),
                         reads=[(HF, c), rs, gn], writes=[h1])
                    P.op("pool", lambda e, c=c, h1=h1, hb_=hb_: e.tensor_tensor(out=hb_[:], in0=h1[:], in1=SO3[:, c, :], op=ALU.mult), reads=[h1, (SO, c)], writes=[hb_])
                    P.op("pe", lambda e, pb=pb, hb_=hb_, k=k: e.transpose(out=pb[:, k * 128:(k + 1) * 128], in_=hb_[:], identity=identb[:]), reads=[hb_, identb], writes=[pb])
                    if k == 7 or c == NCH - 1:
                        n = k + 1
                        P.op("act", lambda e, pb=pb, grp=grp, n=n: e.copy(out=OUT[:, grp * 1024:grp * 1024 + n * 128], in_=pb[:, 0:n * 128]), reads=[pb], writes=[OUT])
                P.dma(lambda e, h=h: e.dma_start(out=MIXT[:, :, h, :].rearrange("t p x -> p t x"), in_=OUT[:].rearrange("p (t x) -> p t x", x=128)), reads=[OUT], writes=["mixh"])
            P.barrier()
        with contextlib.ExitStack() as st:
            T = lambda name, shape, dt=F32: st.enter_context(nc.sbuf_tensor(pre + name, shape, dt))
            Wu = T("Wu", [128, 16 * 1024], BF16)
            Wu3 = Wu[:].rearrange("p (k n) -> p k n", k=16)
            for kc_ in range(16):
                P.dma(lambda e, kc_=kc_: e.dma_start(out=Wu3[:, kc_, :], in_=wu[kc_ * 128:(kc_ + 1) * 128, :]), eng="pool", writes=[Wu])
            PW = T("PW", [128, 4 * 2 * 256], BF16)
            PW4 = PW[:].rearrange("p (g c n) -> p g c n", g=4, c=2)
            for g in range(4):
                P.dma(lambda e, g=g: e.dma_start(out=PW4[:, g], in_=pw[g].rearrange("(c p) n -> p c n", p=128)), eng="pool", writes=[PW])
            pst = T("pst", [128, 8])
            for et_ in range(8):
                P.dma(lambda e, et_=et_: e.dma_start(out=pst[:, et_:et_ + 1], in_=pool_scale[et_ * 128:(et_ + 1) * 128].unsqueeze(1)), writes=[pst])
            def pool_part(nm, hp, ic, NT, odst):
                NH = NT + 16
                hs = T("hs_" + nm, [128, 16 * NH], BF16)
                hs3 = hs[:].rearrange("p (k t) -> p k t", k=16)
                if nm == "l":
                    t0 = 2 + q * 16
                    for kc_ in range(16):
                        P.dma(lambda e, kc_=kc_: e.dma_start(out=hs3[:, kc_, 8:8 + 2048].rearrange("p (t x) -> p t x", x=128),
                                                              in_=hT[t0:t0 + 16].rearrange("t p (k x) -> p k t x", k=16)[:, kc_]), writes=[hs])
                    if q > 0:
                        P.dma(lambda e: e.dma_start(out=hs3[:, :, 0:8], in_=hT[t0 - 1].rearrange("p (k x) -> p k x", k=16)[:, :, 120:128]), writes=[hs])
                    else:
                        P.op("pool", lambda e: e.memset(hs3[:, :, 0:8], 0.0), writes=[hs])
                    if q < 3:
                        P.dma(lambda e: e.dma_start(out=hs3[:, :, 2056:2064], in_=hT[t0 + 16].rearrange("p (k x) -> p k x", k=16)[:, :, 0:8]), writes=[hs])
                    else:
                        P.op("pool", lambda e: e.memset(hs3[:, :, 2056:2064], 0.0), writes=[hs])
                else:
                    lo = q * 64 - 8
                    pos = 0
                    if lo < 0:
                        P.op("pool", lambda e: e.memset(hs3[:, :, 0:8], 0.0), writes=[hs]); pos = 8; lo = 0
                    hi = min(q * 64 + 72, 256)
                    while lo < hi:
                        t_ = lo // 128; a_ = lo % 128; n_ = min(hi - lo, 128 - a_)
                        P.dma(lambda e, t_=t_, a_=a_, n_=n_, pos=pos: e.dma_start(out=hs3[:, :, pos:pos + n_], in_=hT[t_].rearrange("p (k x) -> p k x", k=16)[:, :, a_:a_ + n_]), writes=[hs])
                        pos += n_; lo += n_
                    if pos < 80:
                        P.op("pool", lambda e, pos=pos: e.memset(hs3[:, :, pos:80], 0.0), writes=[hs])
                Ub = [T("U%s%d" % (nm, i), [128, NH]) for i in range(2)]
                A1 = T("A1" + nm, [128, NH]); A2 = T("A2" + nm, [128, NH])
                icb = T("icb" + nm, [128, NT])
                dT = [T("dT%s%d" % (nm, i), [128, NT], BF16) for i in range(2)]
                ob = [T("ob%s%d" % (nm, i), [128, NT], BF16) for i in range(2)]
                blocks = [(s, min(512, NH - s)) for s in range(0, NH, 512)]
                oblocks = [(s, min(512, NT - s)) for s in range(0, NT, 512)]
                for g in range(4):
                    w = POOL_W[g]
                    P.dma(lambda e, g=g, ic=ic, icb=icb, NT=NT: e.dma_start(out=icb[:], in_=ic[g:g + 1, :].to_broadcast([128, NT])), writes=[icb])
                    for ci in range(2):
                        ct = g * 2 + ci
                        Ut = Ub[ci]
                        for bi, (s0, n) in enumerate(blocks):
                            pp = psf[bi % 4]
                            for kc in range(16):
                                P.op("pe", lambda e, pp=pp, kc=kc, ct=ct, s0=s0, n=n: e.matmul(pp[:, 0:n], lhsT=Wu3[:, kc, ct * 128:(ct + 1) * 128], rhs=hs3[:, kc, s0:s0 + n], start=(kc == 0), stop=(kc == 15)),
                                     reads=[Wu, hs], writes=[pp])
                            P.op("act", lambda e, pp=pp, Ut=Ut, s0=s0, n=n: e.copy(out=Ut[:, s0:s0 + n], in_=pp[:, 0:n]), reads=[pp], writes=[Ut])
                        cur = Ut; L = NH; step = 1; k = 1
                        bufs = [A1, A2]; bi = 0
                        while k < w:
                            nxt = bufs[bi % 2]; bi += 1
                            L2 = L - k
                            P.op("dve", lambda e, cur=cur, nxt=nxt, L2=L2, k=k: e.tensor_tensor(out=nxt[:, 0:L2], in0=cur[:, 0:L2], in1=cur[:, k:k + L2], op=ALU.add), reads=[cur], writes=[nxt])
                            cur = nxt; L = L2; k *= 2
                        off = 8 - w // 2
                        nxt = bufs[bi % 2]
                        P.op("dve", lambda e, cur=cur, nxt=nxt, off=off, NT=NT: e.tensor_tensor(out=nxt[:, 0:NT], in0=cur[:, off:off + NT], in1=icb[:], op=ALU.mult), reads=[cur, icb], writes=[nxt])
                        P.op("dve", lambda e, nxt=nxt, Ut=Ut, d=dT[ci], NT=NT: e.tensor_tensor(out=d[:], in0=nxt[:, 0:NT], in1=Ut[:, 8:8 + NT], op=ALU.subtract), reads=[nxt, Ut], writes=[dT[ci]])
                    for ei in range(2):
                        et = g * 2 + ei
                        o = ob[ei]
                        for bi, (s0, n) in enumerate(oblocks):
                            pp = psf[4 + bi % 2]
                            for ci in range(2):
                                P.op("pe", lambda e, pp=pp, ci=ci, g=g, ei=ei, s0=s0, n=n: e.matmul(pp[:, 0:n], lhsT=PW4[:, g, ci, ei * 128:(ei + 1) * 128], rhs=dT[ci][:, s0:s0 + n], start=(ci == 0), stop=(ci == 1)),
                                     reads=[PW, dT[ci]], writes=[pp])
                            P.op("act", lambda e, pp=pp, o=o, et=et, s0=s0, n=n: e.activation(out=o[:, s0:s0 + n], in_=pp[:, 0:n], func=AF.Copy, scale=pst[:, et:et + 1]), reads=[pp, pst], writes=[o])
                        if nm == "l":
                            P.dma(lambda e, o=o, et=et: e.dma_start(out=MIXT[2 + q * 16:2 + (q + 1) * 16, :, 8 + et, :].rearrange("t p x -> p t x"), in_=o[:].rearrange("p (t x) -> p t x", x=128)), reads=[o], writes=["po"])
                        else:
                            P.dma(lambda e, o=o, et=et: e.dma_start(out=MIXT[q // 2, :, 8 + et, (q % 2) * 64:(q % 2 + 1) * 64], in_=o[:]), reads=[o], writes=["po"])

            pool_part("l", None, icl, 2048, None)
            pool_part("c", None, icc, 64, None)
            P.end_phase()


D = 2048
NCH = 66
NPAT = 5

def na_patterns():
    R, W, WR, WC = 128, 64, 8, 16
    pats = {}; plist = []; per_m = []
    for m in range(64):
        idx = -np.ones((64, 128, 128), np.int32)
        used = set()
        for rl in range(2):
            r = 2 * m + rl
            rs = min(max(r - WR // 2, 0), R - WR)
            for c in range(W):
                cs = min(max(c - WC // 2, 0), W - WC)
                q = rl * 64 + c
                for kr in range(rs, rs + WR):
                    t = kr // 2
                    used.add(t)
                    kk = (kr - 2 * t) * 64 + np.arange(cs, cs + WC)
                    idx[t, kk, q] = (kr - r + 7) * 31 + (np.arange(cs, cs + WC) - c + 15)
        tiles = sorted(used)
        assert len(tiles) <= 5
        im = -np.ones((5, 128, 128), np.int32)
        for j, t in enumerate(tiles):
            im[j] = idx[t]
        while len(tiles) < 5:
            tiles.append(tiles[-1])
        key = im.tobytes()
        if key not in pats:
            pats[key] = len(plist); plist.append(im)
        per_m.append(([t + 2 for t in tiles], pats[key]))
    assert len(plist) == NPAT, len(plist)
    return per_m, np.stack(plist)

def emit_mixo(nc, P, pre, io, hq, ctx_out):
    nheads = 4; phase = 2; skip = ()
    hT = io["HT"]; MIXT = io["MIXT"]; w_in = io["w_in"]; bias = io["bias"]; identb_d = io["identb"]
    per_m, _ = na_patterns()
    scale = 128.0 ** -0.5
    with contextlib.ExitStack() as st:
        psf = [P.psum(st, pre + "psf%d" % i, [128, 512], F32) for i in range(6)]
        psb = [P.psum(st, pre + "psb%d" % i, [128, 1024], BF16) for i in range(2)]
        T = lambda name, shape, dt=F32: st.enter_context(nc.sbuf_tensor(pre + name, shape, dt))
        identb = T("identb_s", [128, 128], BF16); onesb = T("onesb", [128, 128], BF16)
        P.dma(lambda e: e.dma_start(out=identb[:], in_=identb_d), writes=[identb])
        P.op("pool", lambda e: e.memset(onesb[:], 1.0), writes=[onesb])
        W = T("W", [128, 16 * 384], BF16); W3 = W[:].rearrange("p (k n) -> p k n", k=16)
        QT = T("QT", [128, NCH * 128], BF16); KT = T("KT", [128, NCH * 128], BF16); VT = T("VT", [128, NCH * 128], BF16)
        OUT = T("OUTs", [128, NCH * 128], BF16)
        Bs = T("Bs", [128, NPAT * 640])
        hch = [T("hch%d" % i, [128, 16 * 128], BF16) for i in range(2)]
        qk = [T("qk%d" % i, [128, 256], BF16) for i in range(2)]
        tmp = [T("tmp%d" % i, [128, 640]) for i in range(2)]
        PT = [T("PT%d" % i, [128, 896], BF16) for i in range(2)]
        rc = [T("rc%d" % i, [128, 128]) for i in range(2)]
        Q3 = QT[:].rearrange("p (c x) -> p c x", x=128); K3 = KT[:].rearrange("p (c x) -> p c x", x=128)
        V3 = VT[:].rearrange("p (c x) -> p c x", x=128); O3 = OUT[:].rearrange("p (c x) -> p c x", x=128)
        B3 = Bs[:].rearrange("p (a x) -> p a x", a=NPAT)
        for hd in range(nheads):
            h = 4 * hq + hd
            for s_ in range(3):
                c0 = s_ * 2048 + h * 128
                P.dma(lambda e, s_=s_, c0=c0: e.dma_start(out=W3[:, :, s_ * 128:(s_ + 1) * 128], in_=w_in[:, c0:c0 + 128].rearrange("(k p) n -> p k n", p=128)), eng="pool", writes=[W])
            P.dma(lambda e, h=h: e.dma_start(out=Bs[:], in_=bias[h]), writes=[Bs])
            for c in range(NCH):
                hc_ = hch[c % 2]; qk_ = qk[c % 2]; pp = psf[c % 2]; pb = psb[c % 2]
                P.dma(lambda e, hc_=hc_, c=c: e.dma_start(out=hc_[:], in_=hT[c]), writes=[hc_])
                h3 = hc_[:].rearrange("p (k t) -> p k t", k=16)
                for kc in range(16):
                    P.op("pe", lambda e, pp=pp, h3=h3, kc=kc: e.matmul(pp[:, 0:384], lhsT=h3[:, kc, :], rhs=W3[:, kc, :], start=(kc == 0), stop=(kc == 15)),
                         reads=[hc_, W], writes=[pp])
                if "qk" in skip:
                    P.op("act", lambda e, pp=pp, c=c: e.copy(out=V3[:, c, :], in_=pp[:, 256:384]), reads=[pp], writes=[(VT, c)])
                    continue
                P.op("dve", lambda e, pp=pp, qk_=qk_: e.tensor_scalar(out=qk_[:, 0:128], in0=pp[:, 0:128], scalar1=scale, scalar2=None, op0=ALU.mult), reads=[pp], writes=[(qk_, 0)])
                P.op("act", lambda e, pp=pp, qk_=qk_: e.copy(out=qk_[:, 128:256], in_=pp[:, 128:256]), reads=[pp], writes=[(qk_, 1)])
                P.op("act", lambda e, pp=pp, c=c: e.copy(out=V3[:, c, :], in_=pp[:, 256:384]), reads=[pp], writes=[(VT, c)])
                for a in range(2):
                    P.op("pe", lambda e, pb=pb, qk_=qk_, a=a: e.transpose(out=pb[:, a * 128:(a + 1) * 128], in_=qk_[:, a * 128:(a + 1) * 128], identity=identb[:]),
                         reads=[(qk_, a), identb], writes=[pb])
                P.op("dve", lambda e, pb=pb, c=c: e.tensor_copy(out=Q3[:, c, :], in_=pb[:, 0:128]), reads=[pb], writes=[(QT, c)])
                P.op("act", lambda e, pb=pb, c=c: e.copy(out=K3[:, c, :], in_=pb[:, 128:256]), reads=[pb], writes=[(KT, c)])
            qtiles = [(m + 2, per_m[m][0], per_m[m][1]) for m in range(64 if phase >= 2 else 0)]
            if ctx_out:
                qtiles = [(0, None, None), (1, None, None)] + qtiles
            for n, (qc, kts, pat) in enumerate(qtiles):
                pA = psf[2 * (n % 2)]; pB = psf[2 * (n % 2) + 1]; pN = psf[4]; pD = psf[5]
                tm = tmp[n % 2]; pt = PT[n % 2]; r_ = rc[n % 2]
                keys = []
                if kts is not None:
                    for j, kt in enumerate(kts):
                        dst = pA[:, j * 128:(j + 1) * 128] if j < 4 else pB[:, 0:128]
                        P.op("pe", lambda e, dst=dst, kt=kt, qc=qc: e.matmul(dst, lhsT=K3[:, kt, :], rhs=Q3[:, qc, :], start=True, stop=True),
                             reads=[(KT, kt), (QT, qc)], writes=[pA if j < 4 else pB])
                        keys.append((kt, j * 128))
                for j in range(2):
                    P.op("pe", lambda e, pB=pB, j=j, qc=qc: e.matmul(pB[:, 128 + j * 128:256 + j * 128], lhsT=K3[:, j, :], rhs=Q3[:, qc, :], start=True, stop=True),
                         reads=[(KT, j), (QT, qc)], writes=[pB])
                    keys.append((j, 640 + j * 128))
                if kts is not None:
                    P.op("dve", lambda e, pA=pA, tm=tm, pat=pat: e.tensor_tensor(out=tm[:, 0:512], in0=pA[:, :], in1=B3[:, pat, 0:512], op=ALU.add), reads=[pA, Bs], writes=[tm])
                    P.op("dve", lambda e, pB=pB, tm=tm, pat=pat: e.tensor_tensor(out=tm[:, 512:640], in0=pB[:, 0:128], in1=B3[:, pat, 512:640], op=ALU.add), reads=[pB, Bs], writes=[tm])
                    P.op("act", lambda e, tm=tm, pt=pt: e.activation(out=pt[:, 0:640], in_=tm[:, :], func=AF.Exp), reads=[tm], writes=[pt])
                P.op("act", lambda e, pB=pB, pt=pt: e.activation(out=pt[:, 640:896], in_=pB[:, 128:384], func=AF.Exp), reads=[pB], writes=[pt])
                for i, (kt, off) in enumerate(keys):
                    P.op("pe", lambda e, kt=kt, off=off, pt=pt, i=i, nk=len(keys): e.matmul(pN[:, 0:128], lhsT=V3[:, kt, :], rhs=pt[:, off:off + 128], start=(i == 0), stop=(i == nk - 1)),
                         reads=[(VT, kt), pt], writes=[pN])
                for i, (kt, off) in enumerate(keys):
                    P.op("pe", lambda e, off=off, pt=pt, i=i, nk=len(keys): e.matmul(pD[:, 0:128], lhsT=onesb[:], rhs=pt[:, off:off + 128], start=(i == 0), stop=(i == nk - 1)),
                         reads=[onesb, pt], writes=[pD])
                P.op("act", lambda e, r_=r_: e.copy(out=r_[:], in_=pD[:, 0:128]), reads=[pD], writes=[r_])
                P.op("dve", lambda e, r_=r_: e.reciprocal(out=r_[:], in_=r_[:]), reads=[r_], writes=[r_])
                P.op("dve", lambda e, r_=r_, qc=qc: e.tensor_tensor(out=O3[:, qc, :], in0=pN[:, 0:128], in1=r_[:], op=ALU.mult), reads=[pN, r_], writes=[OUT])
            if not ctx_out:
                P.op("pool", lambda e: e.memset(OUT[:, 0:256], 0.0), writes=[OUT])
            P.dma(lambda e, h=h: e.dma_start(out=MIXT[:, :, h, :].rearrange("t p x -> p t x"), in_=OUT[:].rearrange("p (t x) -> p t x", x=128)), reads=[OUT], writes=["mixh"])
        P.end_phase()


D = 2048; EPS = 1e-6

def emit_g2(nc, P, pre, io):
    xl = io["xl"]; xc = io["xc"]; mixl = io["mixl"]; mixc = io["mixc"]; wout = io["wout"]; wr = io["wr"]
    modl = io["modl"]; modc = io["modc"]; gv = io["g"]; identf_d = io["identf"]
    xmo_l = io["xmo_l"]; xmo_c = io["xmo_c"]; h2_l = io["h2_l"]; h2_c = io["h2_c"]; aff_l = io["aff_l"]; aff_c = io["aff_c"]
    with contextlib.ExitStack() as st:
        T = lambda name, shape, dt=F32: st.enter_context(nc.sbuf_tensor(pre + name, shape, dt))
        psf = [P.psum(st, pre + "psf%d" % i, [128, 512], F32) for i in range(8)]
        identf = T("identf_s", [128, 128])
        P.dma(lambda e: e.dma_start(out=identf[:], in_=identf_d), writes=[identf])
        W = T("W", [128, 16 * D], BF16); W3 = W[:].rearrange("p (k n) -> p k n", k=16)
        for kc in range(16):
            P.dma(lambda e, kc=kc: e.dma_start(out=W3[:, kc, :], in_=wout[kc * 128:(kc + 1) * 128, :]), eng="pool", writes=[W])
        WR = T("WR", [128, 16 * 16]); WR3 = WR[:].rearrange("p (k n) -> p k n", k=16)
        P.dma(lambda e: e.dma_start(out=WR3, in_=wr.rearrange("(k p) n -> p k n", p=128)), writes=[WR])
        g_bc = T("g_bc", [128, D]); tmpb = T("tmpb", [128, D])
        P.dma(lambda e: e.dma_start(out=g_bc[:], in_=gv.to_broadcast([128, D])), writes=[g_bc])
        GA = T("GA", [128, D]); A = T("A", [128, D]); Bt = T("Bt", [128, D])
        xt = [T("xt%d" % i, [128, D]) for i in range(2)]
        mt = [T("mt%d" % i, [128, D], BF16) for i in range(2)]
        xm = [T("xm%d" % i, [128, D]) for i in range(2)]
        h1 = T("h1", [128, D])
        h2f = T("h2f", [128, D]); h2b = [T("h2b%d" % i, [128, D], BF16) for i in range(2)]
        h2T = T("h2T", [128, D])
        ss = T("ss", [128, 1]); rstd = T("rstd", [128, 1])
        mx = T("mx", [128, 1]); se = T("se", [128, 1]); ex = T("ex", [128, 16]); af = [T("af%d" % i, [128, 16]) for i in range(2)]

        def load_mod(mod):
            P.dma(lambda e: e.dma_start(out=GA[:], in_=mod[2:3, :].to_broadcast([128, D])), writes=[GA])
            P.dma(lambda e: e.dma_start(out=Bt[:], in_=mod[3:4, :].to_broadcast([128, D])), writes=[Bt])
            P.dma(lambda e: e.dma_start(out=tmpb[:], in_=mod[4:5, :].to_broadcast([128, D])), writes=[tmpb])
            P.op("dve", lambda e: e.scalar_tensor_tensor(out=A[:], in0=tmpb[:], scalar=1.0, in1=g_bc[:], op0=ALU.add, op1=ALU.mult),
                 reads=[tmpb, g_bc], writes=[A])

        def tile(it, rows, xsrc, msrc, xdst, hdst, adst):
            x = xt[it % 2]; m = mt[it % 2]; xo = xm[it % 2]; hb = h2b[it % 2]; a_ = af[it % 2]
            P.dma(lambda e: e.dma_start(out=x[:rows, :], in_=xsrc), writes=[x])
            if rows == 128:
                P.dma(lambda e: e.dma_start(out=m[:], in_=msrc), writes=[m])
            else:
                P.dma(lambda e: e.dma_start(out=m[:, 0:16 * rows].rearrange("p (k t) -> p k t", k=16), in_=msrc), writes=[m])
            m3 = m[:, 0:16 * rows].rearrange("p (k t) -> p k t", k=16)
            for nb in range(4):
                pp = psf[nb]
                for kc in range(16):
                    P.op("pe", lambda e, pp=pp, kc=kc, nb=nb: e.matmul(pp[:rows, :], lhsT=m3[:, kc, :], rhs=W3[:, kc, nb * 512:(nb + 1) * 512], start=(kc == 0), stop=(kc == 15)),
                         reads=[m, W], writes=[pp])
                sl = slice(nb * 512, (nb + 1) * 512)
                P.op("dve", lambda e, pp=pp, sl=sl: e.tensor_tensor(out=h1[:rows, sl], in0=pp[:rows, :], in1=GA[:rows, sl], op=ALU.mult), reads=[pp, GA], writes=[(h1, nb)])
                P.op("pool", lambda e, sl=sl: e.tensor_tensor(out=xo[:rows, sl], in0=x[:rows, sl], in1=h1[:rows, sl], op=ALU.add), reads=[x, (h1, nb)], writes=[(xo, nb)])
            xok = [(xo, nb) for nb in range(4)]; h1k = [(h1, nb) for nb in range(4)]
            P.dma(lambda e: e.dma_start(out=xdst, in_=xo[:rows, :]), reads=xok, writes=["xmo"], final=True)
            P.op("act", lambda e: e.activation(out=h1[:rows, :], in_=xo[:rows, :], func=AF.Square, accum_out=ss[:rows, :]), reads=xok, writes=h1k + [ss])
            P.op("act", lambda e: e.activation(out=rstd[:rows, :], in_=ss[:rows, :], func=AF.Sqrt, scale=1.0 / D, bias=EPS), reads=[ss], writes=[rstd])
            P.op("dve", lambda e: e.reciprocal(out=rstd[:rows, :], in_=rstd[:rows, :]), reads=[rstd], writes=[rstd])
            P.op("dve", lambda e: e.scalar_tensor_tensor(out=h1[:rows, :], in0=xo[:rows, :], scalar=rstd[:rows, 0:1], in1=A[:rows, :], op0=ALU.mult, op1=ALU.mult),
                 reads=xok + [rstd, A], writes=h1k)
            P.op("pool", lambda e: e.tensor_tensor(out=h2f[:rows, :], in0=h1[:rows, :], in1=Bt[:rows, :], op=ALU.add), reads=h1k + [Bt], writes=[h2f])
            P.op("act", lambda e: e.copy(out=hb[:rows, :], in_=h2f[:rows, :]), reads=[h2f], writes=[hb])
            P.dma(lambda e: e.dma_start(out=hdst, in_=hb[:rows, :]), reads=[hb], writes=["h2o"], final=True)
            h2T3 = h2T[:].rearrange("p (k t) -> p k t", k=16)
            for grp in range(4):
                pp = psf[4 + grp]
                for k in range(4):
                    kc = grp * 4 + k
                    P.op("pe", lambda e, pp=pp, k=k, kc=kc: e.transpose(out=pp[:, k * 128:k * 128 + rows], in_=h2f[:rows, kc * 128:(kc + 1) * 128], identity=identf[:rows, :rows]),
                         reads=[h2f, identf], writes=[pp])
                eng = "act" if grp % 2 == 0 else "dve"
                if rows == 128:
                    if eng == "act":
                        P.op("act", lambda e, pp=pp, grp=grp: e.copy(out=h2T[:, grp * 512:(grp + 1) * 512], in_=pp[:, :]), reads=[pp], writes=[(h2T, grp)])
                    else:
                        P.op("dve", lambda e, pp=pp, grp=grp: e.tensor_copy(out=h2T[:, grp * 512:(grp + 1) * 512], in_=pp[:, :]), reads=[pp], writes=[(h2T, grp)])
                else:
                    src = pp[:, :].rearrange("p (k t) -> p k t", k=4)[:, :, 0:rows]
                    dst = h2T3[:, grp * 4:(grp + 1) * 4, 0:rows]
                    P.op("dve", lambda e, src=src, dst=dst: e.tensor_copy(out=dst, in_=src), reads=[pp], writes=[(h2T, grp)])
            pl = psf[0]
            for kc in range(16):
                P.op("pe", lambda e, kc=kc: e.matmul(pl[:rows, 0:16], lhsT=h2T3[:, kc, 0:rows], rhs=WR3[:, kc, :], start=(kc == 0), stop=(kc == 15)),
                     reads=[(h2T, kc // 4), WR], writes=[pl])
            P.op("dve", lambda e: e.reduce_max(out=mx[:rows, :], in_=pl[:rows, 0:16], axis=AX.X), reads=[pl], writes=[mx])
            P.op("dve", lambda e: e.tensor_scalar(out=mx[:rows, :], in0=mx[:rows, :], scalar1=-1.0, scalar2=None, op0=ALU.mult), reads=[mx], writes=[mx])
            P.op("act", lambda e: e.activation(out=ex[:rows, :], in_=pl[:rows, 0:16], func=AF.Exp, bias=mx[:rows, 0:1], accum_out=se[:rows, :]), reads=[pl, mx], writes=[ex, se])
            P.op("dve", lambda e: e.reciprocal(out=se[:rows, :], in_=se[:rows, :]), reads=[se], writes=[se])
            P.op("dve", lambda e: e.tensor_scalar(out=a_[:rows, :], in0=ex[:rows, :], scalar1=se[:rows, 0:1], scalar2=None, op0=ALU.mult), reads=[ex, se], writes=[a_])
            P.dma(lambda e: e.dma_start(out=adst, in_=a_[:rows, :]), reads=[a_], writes=["affo"], final=True)

        load_mod(modl)
        for it in range(16):
            r = slice(it * 128, (it + 1) * 128)
            tile(it, 128, xl[r, :], mixl[it], xmo_l[r, :], h2_l[r, :], aff_l[it])
        load_mod(modc)
        tile(16, 64, xc, mixc, xmo_c, h2_c, aff_c)
        P.end_phase()


D = 2048; FF = 1024
CAPL = 1024; CAPC = 32
BIG = float(1 << 15)
NIT = 28

def emit_moe(nc, P, pre, io, g):
    only_slots = False
    AFF8 = io["AFF8"]; SLOT8 = io["SLOT8"]; capv = io["capv"]; h2l = io["H2L"]; h2c = io["H2C"]
    wg = io["wg"]; wu = io["wu"]; wd = io["wd"]; stri_d = io["stri"]; identb_d = io["identb"]
    xin_l = io["XINL"]; xin_c = io["XINC"]
    with contextlib.ExitStack() as st0:
        psf = [P.psum(st0, pre + "psf%d" % i, [128, 512], F32) for i in range(6)]
        psb = [P.psum(st0, pre + "psb%d" % i, [128, 1024], BF16) for i in range(2)]
        T0 = lambda name, shape, dt=F32: st0.enter_context(nc.sbuf_tensor(pre + name, shape, dt))
        identb = T0("identb_s", [128, 128], BF16)
        P.dma(lambda e: e.dma_start(out=identb[:], in_=identb_d), writes=[identb])
        if True:
            st = st0
            T = lambda name, shape, dt=F32: st.enter_context(nc.sbuf_tensor(pre + name, shape, dt))
            stri = T("stri_s", [128, 128]); ones = T("ones", [128, 128]); zeros = T("zeros", [128, 64])
            A8 = T("A8", [128, 512]); cap = T("cap", [128, 8])
            P.dma(lambda e: e.dma_start(out=stri[:], in_=stri_d), writes=[stri])
            A8raw = T("A8raw", [128, 512])
            Ar3 = A8raw[:].rearrange("p (i l) -> p i l", l=8)
            P.dma(lambda e: e.dma_start(out=Ar3[:, :, 0:4], in_=AFF8[:, :, 4 * g:4 * g + 4]), writes=[A8raw])
            P.dma(lambda e: e.dma_start(out=Ar3[:, :, 4:8], in_=AFF8[:, :, 16 + 4 * g:16 + 4 * g + 4]), writes=[A8raw])
            P.op("dve", lambda e: e.tensor_copy(out=A8[:].rearrange("p (l i) -> p l i", l=8), in_=A8raw[:].rearrange("p (i l) -> p l i", l=8)), reads=[A8raw], writes=[A8])
            P.dma(lambda e: e.dma_start(out=cap[:], in_=capv), writes=[cap])
            P.op("pool", lambda e: e.memset(ones[:], 1.0), writes=[ones])
            P.op("pool", lambda e: e.memset(zeros[:], 0.0), writes=[zeros])
            lo = T("lo", [128, 8]); th = T("th", [128, 8]); cntp = T("cntp", [128, 8]); tt = T("tt", [128, 8])
            cmp_ = T("cmp", [128, 512]); mask = T("mask", [128, 512]); m0 = T("m0", [128, 512])
            cum = T("cum", [128, 512]); s2 = T("s2", [128, 512]); totp = T("totp", [128, 8]); offs = T("offs", [128, 8])
            sloti = T("sloti", [128, 512], I32)
            A3 = A8[:].rearrange("p (l i) -> p l i", l=8)
            c3 = cmp_[:].rearrange("p (l i) -> p l i", l=8)
            pc = psf[0]
            P.op("pool", lambda e: e.memset(lo[:], 0.0), writes=[lo])
            for n in range(NIT):
                dl = 2.0 ** -(n + 1)
                P.op("dve", lambda e, dl=dl: e.tensor_scalar(out=th[:], in0=lo[:], scalar1=dl, scalar2=None, op0=ALU.add), reads=[lo], writes=[th])
                P.op("dve", lambda e: e.tensor_tensor(out=c3, in0=A3, in1=th[:].unsqueeze(2).to_broadcast([128, 8, 64]), op=ALU.is_ge), reads=[A8, th], writes=[cmp_])
                P.op("dve", lambda e: e.reduce_sum(out=cntp[:], in_=c3, axis=AX.X), reads=[cmp_], writes=[cntp])
                P.op("pe", lambda e: e.matmul(pc[:, 0:8], lhsT=ones[:], rhs=cntp[:], start=True, stop=True), reads=[ones, cntp], writes=[pc])
                P.op("dve", lambda e: e.tensor_tensor(out=tt[:], in0=pc[:, 0:8], in1=cap[:], op=ALU.is_ge), reads=[pc, cap], writes=[tt])
                P.op("dve", lambda e, dl=dl: e.scalar_tensor_tensor(out=lo[:], in0=tt[:], scalar=dl, in1=lo[:], op0=ALU.mult, op1=ALU.add), reads=[tt, lo], writes=[lo])
            m3 = mask[:].rearrange("p (l i) -> p l i", l=8)
            P.op("dve", lambda e: e.tensor_tensor(out=m3, in0=A3, in1=lo[:].unsqueeze(2).to_broadcast([128, 8, 64]), op=ALU.is_ge), reads=[A8, lo], writes=[mask])
            P.op("dve", lambda e: e.tensor_scalar(out=m0[:], in0=A8[:], scalar1=0.0, scalar2=None, op0=ALU.is_gt), reads=[A8], writes=[m0])
            P.op("dve", lambda e: e.tensor_tensor(out=mask[:], in0=mask[:], in1=m0[:], op=ALU.mult), reads=[mask, m0], writes=[mask])
            cu3 = cum[:].rearrange("p (l i) -> p l i", l=8)
            for l in range(8):
                P.op("dve", lambda e, l=l: e.tensor_tensor_scan(out=cum[:, l * 64:(l + 1) * 64], data0=mask[:, l * 64:(l + 1) * 64], data1=zeros[:, 0:64], initial=0.0, op0=ALU.add, op1=ALU.add),
                     reads=[mask, zeros], writes=[cum])
            P.op("dve", lambda e: e.tensor_copy(out=totp[:], in_=cu3[:, :, 63]), reads=[cum], writes=[totp])
            P.op("pe", lambda e: e.matmul(pc[:, 0:8], lhsT=stri[:], rhs=totp[:], start=True, stop=True), reads=[stri, totp], writes=[pc])
            P.op("act", lambda e: e.copy(out=offs[:], in_=pc[:, 0:8]), reads=[pc], writes=[offs])
            P.op("dve", lambda e: e.tensor_scalar(out=s2[:], in0=mask[:], scalar1=-BIG, scalar2=BIG - 1.0, op0=ALU.mult, op1=ALU.add), reads=[mask], writes=[s2])
            P.op("dve", lambda e: e.tensor_tensor(out=cu3, in0=cu3, in1=offs[:].unsqueeze(2).to_broadcast([128, 8, 64]), op=ALU.add), reads=[cum, offs], writes=[cum])
            P.op("dve", lambda e: e.tensor_tensor(out=cum[:], in0=cum[:], in1=s2[:], op=ALU.add), reads=[cum, s2], writes=[cum])
            P.op("dve", lambda e: e.tensor_copy(out=sloti[:], in_=cum[:]), reads=[cum], writes=[sloti])
            Sraw = T("Sraw", [128, 512], I32)
            P.op("dve", lambda e: e.tensor_copy(out=Sraw[:].rearrange("p (i l) -> p l i", l=8), in_=sloti[:].rearrange("p (l i) -> p l i", l=8)), reads=[sloti], writes=[Sraw])
            Sr3 = Sraw[:].rearrange("p (i l) -> p i l", l=8)
            P.dma(lambda e: e.dma_start(out=SLOT8[:, :, 4 * g:4 * g + 4], in_=Sr3[:, :, 0:4]), reads=[Sraw], writes=["slot8"])
            P.dma(lambda e: e.dma_start(out=SLOT8[:, :, 16 + 4 * g:16 + 4 * g + 4], in_=Sr3[:, :, 4:8]), reads=[Sraw], writes=["slot8"])
            s3 = sloti[:].rearrange("p (l i) -> p l i", l=8)
            R = [T("R%d" % i, [128, D], BF16) for i in range(3)]
            nR = 0
            hv = h2l.rearrange("(i p) d -> p i d", p=128)
            for i in range(64):
                r = R[nR % 3]; nR += 1
                P.dma(lambda e, r=r, i=i: e.dma_start(out=r[:], in_=hv[:, i, :]), writes=[r])
                for lane in range(4):
                    P.dma(lambda e, r=r, lane=lane, i=i: e.indirect_dma_start(out=xin_l[lane][:, :], out_offset=bass.IndirectOffsetOnAxis(ap=s3[:, lane, i:i + 1], axis=0),
                                                                             in_=r[:, :], in_offset=None, bounds_check=P.getreg(e, CAPL - 1), oob_is_err=False),
                          eng="pool", reads=[r, sloti], writes=["xin_l"])
            hv2 = h2c.rearrange("(i p) d -> p i d", p=128)
            for i in range(2):
                r = R[nR % 3]; nR += 1
                P.dma(lambda e, r=r, i=i: e.dma_start(out=r[:], in_=hv2[:, i, :]), writes=[r])
                for lane in range(4, 8):
                    P.dma(lambda e, r=r, lane=lane, i=i: e.indirect_dma_start(out=xin_c[lane - 4][:, :], out_offset=bass.IndirectOffsetOnAxis(ap=s3[:, lane, i:i + 1], axis=0),
                                                                             in_=r[:, :], in_offset=None, bounds_check=P.getreg(e, CAPC - 1), oob_is_err=False),
                          eng="pool", reads=[r, sloti], writes=["xin_c"])
            P.barrier()
        if True:
            st = st0
            T = lambda name, shape, dt=F32: st.enter_context(nc.sbuf_tensor(pre + name, shape, dt))
            Wg = T("Wg", [128, 16 * FF], BF16); Wu = T("Wu", [128, 16 * FF], BF16); Wd = T("Wd", [128, 8 * D], BF16)
            Wg3 = Wg[:].rearrange("p (k n) -> p k n", k=16); Wu3 = Wu[:].rearrange("p (k n) -> p k n", k=16); Wd3 = Wd[:].rearrange("p (k n) -> p k n", k=8)
            XT = T("XT", [128, 16 * 512], BF16); XT3 = XT[:].rearrange("p (k s) -> p k s", k=16)
            AT = T("AT", [128, 8 * 512], BF16); AT3 = AT[:].rearrange("p (k s) -> p k s", k=8)
            xs = [T("xs%d" % i, [128, D], BF16) for i in range(2)]
            ysb = [T("ysb%d" % i, [128, D]) for i in range(2)]
            sa = [T("sa%d" % i, [128, 512]) for i in range(2)]
            cnt = {"x": 0, "y": 0, "s": 0}

            def ffn_block(xsrc, ydst, ns):
                tiles = [(s, min(128, ns - s)) for s in range(0, ns, 128)]
                for (s0, rows) in tiles:
                    x = xs[cnt["x"] % 2]; cnt["x"] += 1
                    P.dma(lambda e, x=x, s0=s0, rows=rows: e.dma_start(out=x[:rows, :], in_=xsrc[s0:s0 + rows, :]), reads=["xin_l", "xin_c"], writes=[x])
                    for half in range(2):
                        pb = psb[half]
                        for k in range(8):
                            kc = half * 8 + k
                            P.op("pe", lambda e, pb=pb, x=x, k=k, kc=kc, rows=rows: e.transpose(out=pb[:, k * 128:k * 128 + rows], in_=x[:rows, kc * 128:(kc + 1) * 128], identity=identb[:rows, :rows]),
                                 reads=[x, identb], writes=[pb])
                        src = pb[:, :].rearrange("p (k t) -> p k t", k=8)[:, :, 0:rows]
                        dst = XT3[:, half * 8:(half + 1) * 8, s0:s0 + rows]
                        if half == 0:
                            P.op("act", lambda e, src=src, dst=dst: e.copy(out=dst, in_=src), reads=[pb], writes=[XT])
                        else:
                            P.op("dve", lambda e, src=src, dst=dst: e.tensor_copy(out=dst, in_=src), reads=[pb], writes=[XT])
                for ft in range(8):
                    pa = psf[ft % 2]; pu = psf[2 + ft % 2]; s_ = sa[ft % 2]
                    for kc in range(16):
                        P.op("pe", lambda e, pa=pa, kc=kc, ft=ft: e.matmul(pa[:, 0:ns], lhsT=Wg3[:, kc, ft * 128:(ft + 1) * 128], rhs=XT3[:, kc, 0:ns], start=(kc == 0), stop=(kc == 15)),
                             reads=[Wg, XT], writes=[pa])
                    for kc in range(16):
                        P.op("pe", lambda e, pu=pu, kc=kc, ft=ft: e.matmul(pu[:, 0:ns], lhsT=Wu3[:, kc, ft * 128:(ft + 1) * 128], rhs=XT3[:, kc, 0:ns], start=(kc == 0), stop=(kc == 15)),
                             reads=[Wu, XT], writes=[pu])
                    P.op("act", lambda e, pa=pa, s_=s_: e.activation(out=s_[:, 0:ns], in_=pa[:, 0:ns], func=AF.Silu), reads=[pa], writes=[s_])
                    P.op("dve", lambda e, pu=pu, s_=s_, ft=ft: e.tensor_tensor(out=AT3[:, ft, 0:ns], in0=s_[:, 0:ns], in1=pu[:, 0:ns], op=ALU.mult), reads=[s_, pu], writes=[(AT, ft)])
                ATk = [(AT, ft) for ft in range(8)]
                for (s0, rows) in tiles:
                    y = ysb[cnt["y"] % 2]; cnt["y"] += 1
                    for nb in range(4):
                        pd = psf[4 + nb % 2]
                        for fc in range(8):
                            P.op("pe", lambda e, pd=pd, fc=fc, nb=nb, s0=s0, rows=rows: e.matmul(pd[:rows, :], lhsT=AT3[:, fc, s0:s0 + rows], rhs=Wd3[:, fc, nb * 512:(nb + 1) * 512], start=(fc == 0), stop=(fc == 7)),
                                 reads=ATk + [Wd], writes=[pd])
                        if nb % 2 == 0:
                            P.op("act", lambda e, pd=pd, y=y, nb=nb, rows=rows: e.copy(out=y[:rows, nb * 512:(nb + 1) * 512], in_=pd[:rows, :]), reads=[pd], writes=[(y, nb)])
                        else:
                            P.op("dve", lambda e, pd=pd, y=y, nb=nb, rows=rows: e.tensor_copy(out=y[:rows, nb * 512:(nb + 1) * 512], in_=pd[:rows, :]), reads=[pd], writes=[(y, nb)])
                    P.dma(lambda e, y=y, s0=s0, rows=rows: e.dma_start(out=ydst[s0:s0 + rows, :], in_=y[:rows, :]), reads=[(y, nb) for nb in range(4)], writes=["yout"], final=True)

            for el in range(4):
                ex = 4 * g + el
                for kc in range(16):
                    P.dma(lambda e, kc=kc, ex=ex: e.dma_start(out=Wg3[:, kc, :], in_=wg[ex, kc * 128:(kc + 1) * 128, :]), eng="pool", writes=[Wg])
                    P.dma(lambda e, kc=kc, ex=ex: e.dma_start(out=Wu3[:, kc, :], in_=wu[ex, kc * 128:(kc + 1) * 128, :]), eng="pool", writes=[Wu])
                for kc in range(8):
                    P.dma(lambda e, kc=kc, ex=ex: e.dma_start(out=Wd3[:, kc, :], in_=wd[ex, kc * 128:(kc + 1) * 128, :]), eng="pool", writes=[Wd])
                for blk in range(2):
                    ffn_block(xin_l[el][blk * 512:(blk + 1) * 512, :], io["YL"][ex][blk * 512:(blk + 1) * 512, :], 512)
                ffn_block(xin_c[el], io["YC"][ex], CAPC)
            P.end_phase()


BF = ml_dtypes.bfloat16
D = 2048; FF = 1024; N = 8192; NCTX = 256

def emit_k0(nc, P, pre, io):
    cT = io["cT"]; w = io["ada_w"]; b = io["ada_b"]; MOD = io["MOD"]
    with contextlib.ExitStack() as st:
        T = lambda name, shape, dt=F32: st.enter_context(nc.sbuf_tensor(pre + name, shape, dt))
        ps = [P.psum(st, pre + "ps%d" % i, [128, 512], F32) for i in range(2)]
        cs = T("cs", [128, 32]); ss = T("ss", [128, 32])
        wb = [T("wb%d" % i, [128, 16 * 512]) for i in range(3)]
        bt = [T("bt%d" % i, [2, 512]) for i in range(2)]; ot = [T("ot%d" % i, [2, 512]) for i in range(2)]
        zt = T("zt", [128, 2048])
        P.op("pool", lambda e: e.memset(zt[:], 0.0), writes=[zt])
        P.dma(lambda e: e.dma_start(out=io["AFF8"].rearrange("p i l -> p (i l)"), in_=zt[:]), reads=[zt], writes=["aff8"])
        P.dma(lambda e: e.dma_start(out=cs[:], in_=cT), writes=[cs])
        P.op("act", lambda e: e.activation(out=ss[:], in_=cs[:], func=AF.Silu), reads=[cs], writes=[ss])
        blk = 0
        for i in range(4):
            wv = w[i].rearrange("(kc p) n -> p kc n", p=128)
            for j in range(24):
                wt = wb[blk % 3]; pt = ps[blk % 2]; btj = bt[blk % 2]; otj = ot[blk % 2]
                c0 = j * 512
                P.dma(lambda e, wt=wt, wv=wv, c0=c0: e.dma_start(out=wt[:].rearrange("p (kc n) -> p kc n", kc=16), in_=wv[:, :, c0:c0 + 512]), writes=[wt])
                P.dma(lambda e, btj=btj, i=i, c0=c0: e.dma_start(out=btj[:], in_=b[i:i + 1, c0:c0 + 512].to_broadcast([2, 512])), writes=[btj])
                for kc in range(16):
                    P.op("pe", lambda e, pt=pt, wt=wt, kc=kc: e.matmul(pt[0:2, :], lhsT=ss[:, kc * 2:(kc + 1) * 2], rhs=wt[:, kc * 512:(kc + 1) * 512], start=(kc == 0), stop=(kc == 15)),
                         reads=[ss, wt], writes=[pt])
                P.op("dve", lambda e, pt=pt, btj=btj, otj=otj: e.tensor_tensor(out=otj[:], in0=pt[0:2, :], in1=btj[:], op=ALU.add), reads=[pt, btj], writes=[otj])
                P.dma(lambda e, otj=otj, i=i, c0=c0: e.dma_start(out=MOD[i, :, c0:c0 + 512], in_=otj[:]), reads=[otj], writes=["mod"])
                blk += 1
        P.end_phase()

def build_fused(nlayers=4):
    nc = bass.Bass("TRN2", target_bir_lowering=False)
    DI = lambda n, s, dt=F32: nc.dram_tensor(n, s, dt, kind="ExternalInput").ap()
    SC = lambda n, s, dt=F32: nc.dram_tensor(n, s, dt, kind="Internal").ap()
    x = DI("x", [N, D]); ctx = DI("ctx", [NCTX, D]); cT = DI("cT", [128, 32])
    ada_w = DI("ada_w", [4, D, 12288]); ada_b = DI("ada_b", [4, 12288]); norm_g = DI("norm_g", [4, 2, D]); final_g = DI("final_g", [1, D])
    ev_w_in = DI("ev_w_in", [2, D, 5152]); ev_gate_b = DI("ev_gate_b", [2, 4, 8]); ev_head_g = DI("ev_head_g", [2, 1024])
    ev_pool_w = DI("ev_pool_w", [2, 4, 256, 256]); ev_pool_scale = DI("ev_pool_scale", [2, 1024]); ev_w_out = DI("ev_w_out", [2, D, D])
    na_w_in = DI("na_w_in", [2, D, 6144]); na_bias = DI("na_bias", [2, 16, 128, 3200]); na_w_out = DI("na_w_out", [2, D, D])
    w_router = DI("moe_w_router", [4, D, 16]); w_gate = DI("moe_w_gate", [4, 16, D, FF]); w_up = DI("moe_w_up", [4, 16, D, FF]); w_down = DI("moe_w_down", [4, 16, FF, D])
    identb = DI("identb", [128, 128], BF16); identf = DI("identf", [128, 128]); tri = DI("tri", [2, 128, 128]); stri = DI("stri", [128, 128])
    tab = DI("tab", [66, 128, 256]); icl = DI("icl", [4, N]); icc = DI("icc", [4, NCTX]); capv = DI("capv", [128, 8])
    out = nc.dram_tensor("out", [N, D], F32, kind="ExternalOutput").ap()
    MOD = SC("MOD", [4, 2, 12288]); XL = SC("XL", [N, D]); XC = SC("XC", [NCTX, D])
    HT = SC("HT", [66, 128, 2048], BF16); MIXT = SC("MIXT", [66, 128, 16, 128], BF16)
    H2L = SC("H2L", [N, D], BF16); H2C = SC("H2C", [NCTX, D], BF16)
    AFF8 = SC("AFF8", [128, 64, 32]); SLOT8 = SC("SLOT8", [128, 64, 32], I32)
    YL = [SC("YL%d" % i, [1024, D]) for i in range(16)]; YC = [SC("YC%d" % i, [32, D]) for i in range(16)]
    XINL = [SC("XINL%d" % i, [1024, D], BF16) for i in range(4)]; XINC = [SC("XINC%d" % i, [32, D], BF16) for i in range(4)]
    P = Prog(nc)
    emit_k0(nc, P, "k0_", {"cT": cT, "ada_w": ada_w, "ada_b": ada_b, "MOD": MOD, "AFF8": AFF8})
    mrows = lambda i, r: MOD[i, r].rearrange("(s d) -> s d", s=6)

    def g1_io(i, q, comb, final):
        src_l, src_c = (x, ctx) if (i == 0 and not comb) else (XL, XC)
        io = {"xl": src_l[q * 2048:(q + 1) * 2048, :], "xc": src_c[q * 64:(q + 1) * 64, :], "ident": identb}
        if final:
            io["g"] = final_g; io["outl"] = out[q * 2048:(q + 1) * 2048, :]
        else:
            io["g"] = norm_g[i, 0:1, :]; io["modl"] = mrows(i, 0); io["modc"] = mrows(i, 1)
            io["hTl"] = [HT[2 + q * 16 + it] for it in range(16)]
            io["hTc"] = HT[q // 2].rearrange("p (k x) -> p k x", k=16)[:, :, (q % 2) * 64:(q % 2 + 1) * 64]
        if comb:
            ip = i - 1
            io.update({"yl": YL, "yc": YC, "slotl": [SLOT8[:, q * 16 + it, 0:16] for it in range(16)], "affl": [AFF8[:, q * 16 + it, 0:16] for it in range(16)],
                       "slotc": SLOT8[(q % 2) * 64:(q % 2 + 1) * 64, q // 2, 16:32], "affc": AFF8[(q % 2) * 64:(q % 2 + 1) * 64, q // 2, 16:32],
                       "gafl": mrows(ip, 0)[5:6, :], "gafc": mrows(ip, 1)[5:6, :], "xlo": XL[q * 2048:(q + 1) * 2048, :], "xco": XC[q * 64:(q + 1) * 64, :]})
        return io

    for i in range(nlayers):
        j = i // 2
        for q in range(4):
            emit_g1(nc, P, "L%dg1q%d_" % (i, q), g1_io(i, q, i > 0, False), i > 0, False)
        if i % 2 == 0:
            for hp in range(4):
                emit_mixe(nc, P, "L%dmxh%d_" % (i, hp), {"HT": HT, "MIXT": MIXT, "w_in": ev_w_in[j], "gate_b": ev_gate_b[j], "head_g": ev_head_g[j:j + 1, :],
                                                         "tab": tab, "identb": identb, "identf": identf, "tri": tri, "pool_w": ev_pool_w[j],
                                                         "pool_scale": ev_pool_scale[j], "icl": icl, "icc": icc}, hp)
            wout = ev_w_out[j]
        else:
            for hq in range(4):
                emit_mixo(nc, P, "L%dmxh%d_" % (i, hq), {"HT": HT, "MIXT": MIXT, "w_in": na_w_in[j], "bias": na_bias[j], "identb": identb}, hq, i != 3)
            wout = na_w_out[j]
        for q in range(4):
            src_l, src_c = (x, ctx) if i == 0 else (XL, XC)
            emit_g2(nc, P, "L%dg2q%d_" % (i, q), {
                "xl": src_l[q * 2048:(q + 1) * 2048, :], "xc": src_c[q * 64:(q + 1) * 64, :],
                "mixl": [MIXT[2 + q * 16 + it].rearrange("p k x -> p (k x)") for it in range(16)],
                "mixc": MIXT[q // 2][:, :, (q % 2) * 64:(q % 2 + 1) * 64], "wout": wout, "wr": w_router[i],
                "modl": mrows(i, 0), "modc": mrows(i, 1), "g": norm_g[i, 1:2, :], "identf": identf,
                "xmo_l": XL[q * 2048:(q + 1) * 2048, :], "xmo_c": XC[q * 64:(q + 1) * 64, :],
                "h2_l": H2L[q * 2048:(q + 1) * 2048, :], "h2_c": H2C[q * 64:(q + 1) * 64, :],
                "aff_l": [AFF8[:, q * 16 + it, 0:16] for it in range(16)], "aff_c": AFF8[(q % 2) * 64:(q % 2 + 1) * 64, q // 2, 16:32]})
        for g in range(4):
            emit_moe(nc, P, "L%dmoe%d_" % (i, g), {"AFF8": AFF8, "SLOT8": SLOT8, "capv": capv, "H2L": H2L, "H2C": H2C, "wg": w_gate[i], "wu": w_up[i], "wd": w_down[i],
                                                    "stri": stri, "identb": identb, "YL": YL, "YC": YC, "XINL": XINL, "XINC": XINC}, g)
    for q in range(4):
        emit_g1(nc, P, "Fg1q%d_" % q, g1_io(nlayers, q, True, True), True, True)
    P.flush(True)
    return nc

def host_consts(na_rpb):
    c = {}
    c["identb"] = np.eye(128, dtype=BF); c["identf"] = np.eye(128, dtype=np.float32)
    l = np.arange(128)
    c["tri"] = np.stack([(l[:, None] <= l[None, :]), (l[:, None] >= l[None, :])]).astype(np.float32)
    c["stri"] = (l[:, None] < l[None, :]).astype(np.float32)
    capv = np.zeros((128, 8), np.float32); capv[:, :4] = 1024; capv[:, 4:] = 32
    c["capv"] = capv
    tab = np.zeros((66, 128, 2, 2, 2, 32), np.float32); sc = 128.0 ** -0.5
    tab[0:2, :, 0, 0] = 1.0; tab[0:2, :, 0, 1] = sc
    inv = (10000.0 ** (-np.arange(32, dtype=np.float32) / 32)).astype(np.float32)
    t = np.arange(N); rows = (t // 64).astype(np.float32); cols = (t % 64).astype(np.float32)
    for blk, pos in ((0, rows), (1, cols)):
        ang = (pos[:, None] * inv[None, :]).astype(np.float32)
        cs = np.cos(ang).astype(np.float32).reshape(64, 128, 32); sn = np.sin(ang).astype(np.float32).reshape(64, 128, 32)
        tab[2:, :, 0, 0, blk] = cs; tab[2:, :, 1, 0, blk] = sn; tab[2:, :, 0, 1, blk] = cs * sc; tab[2:, :, 1, 1, blk] = sn * sc
    c["tab"] = tab.reshape(66, 128, 256)
    def inv_cnt(n):
        t = np.arange(n); o = np.zeros((4, n), np.float32)
        for g, w in enumerate((2, 4, 8, 16)):
            lo = np.clip(t - w // 2, 0, n); hi = np.clip(t + w // 2, 0, n); o[g] = 1.0 / (hi - lo)
        return o
    c["icl"] = inv_cnt(N); c["icc"] = inv_cnt(NCTX)
    per_m, idxmaps = na_patterns()
    nb = np.zeros((2, 16, 128, 5, 5, 128), np.float32)
    for j in range(2):
        rf = na_rpb[j].reshape(16, 15 * 31)
        for h in range(16):
            g = np.where(idxmaps >= 0, rf[h][np.maximum(idxmaps, 0)], np.float32(-30000.0))
            nb[j, h] = g.transpose(2, 0, 1, 3)
    c["na_bias"] = nb.reshape(2, 16, 128, 3200)
    return c


def kernel(x, c, ctx, c_ctx, ada_w, ada_b, norm_g, final_g, ev_w_in, ev_gate_b, ev_head_g, ev_pool_w,
           ev_pool_scale, ev_w_out, na_w_in, na_rpb, na_w_out, moe_w_router, moe_w_gate, moe_w_up, moe_w_down):
    A = lambda a: np.ascontiguousarray(np.asarray(a, dtype=np.float32))
    x = A(x); ctx = A(ctx); c = A(c); c_ctx = A(c_ctx)
    shared = {"ada_w": A(ada_w), "ada_b": A(ada_b), "norm_g": A(norm_g), "final_g": A(final_g)[None, :],
              "ev_w_in": A(ev_w_in), "ev_gate_b": A(ev_gate_b), "ev_head_g": A(ev_head_g), "ev_pool_w": A(ev_pool_w),
              "ev_pool_scale": A(ev_pool_scale), "ev_w_out": A(ev_w_out), "na_w_in": A(na_w_in), "na_w_out": A(na_w_out),
              "moe_w_router": A(moe_w_router), "moe_w_gate": A(moe_w_gate), "moe_w_up": A(moe_w_up), "moe_w_down": A(moe_w_down)}
    shared.update(host_consts(A(na_rpb)))
    nc = build_fused(4)
    in_maps = []
    for b in range(2):
        cv = np.stack([c[b], c_ctx])
        m = {"x": x[b], "ctx": ctx[b], "cT": np.ascontiguousarray(cv.reshape(2, 16, 128).transpose(2, 1, 0)).reshape(128, 32)}
        m.update(shared)
        in_maps.append(m)
    res = run_bass_kernel_spmd(nc, in_maps, core_ids=[0, 1])
    return np.stack([res.results[0]["out"], res.results[1]["out"]]).astype(np.float32)
```

```python
import contextlib, time
import numpy as np
import ml_dtypes


import numpy as np
import concourse.bass as bass
import concourse.mybir as mybir
from concourse.bass_utils import run_bass_kernel_spmd

F32 = mybir.dt.float32
BF16 = mybir.dt.bfloat16
I32 = mybir.dt.int32
U32 = mybir.dt.uint32
AF = mybir.ActivationFunctionType
ALU = mybir.AluOpType
AX = mybir.AxisListType

ENGS = ("pe", "act", "dve", "pool", "sp")
DMA_Q = {"sp": 16, "pool": 8, "act": 4}
DMA_SEMS = [(q, i) for q, n in DMA_Q.items() for i in range(n)]


class Prog:
    def __init__(self, nc, same_engine_sync=True):
        self.nc = nc
        self.ops = {e: [] for e in ENGS}
        self.cnt = {e: 0 for e in ENGS}
        self.last_write = {}
        self.reads = {}
        self.waited = {e: {} for e in ENGS}
        self.dma_uses = {k: 0 for k in DMA_SEMS}
        self.dma_rr = {q: 0 for q in DMA_Q}
        self.same_engine_sync = same_engine_sync
        self.final_tokens = []
        self.n_ops = 0
        self.psum_names = set()
        import contextlib
        self._st = contextlib.ExitStack()
        self.sems = {}
        for e in ENGS:
            self.sems[("eng", e)] = self._st.enter_context(nc.semaphore("sem_" + e))
        for i in DMA_SEMS:
            self.sems[("dma", i)] = self._st.enter_context(nc.semaphore("sem_dma_%s%d" % i))

    def psum(self, st, name, shape, dt):
        self.psum_names.add(name)
        return st.enter_context(self.nc.psum_tensor(name, shape, dt))

    @staticmethod
    def _key(x):
        if isinstance(x, tuple):
            return (Prog._key(x[0]), x[1])
        if isinstance(x, str):
            return x
        if hasattr(x, 'tensor'):
            return x.tensor.name
        return x.name

    def _deps(self, eng, reads, writes):
        toks = []
        for k in list(reads) + list(writes):
            t = self.last_write.get(k)
            if t is not None:
                toks.append(t)
        for k in writes:
            toks.extend(self.reads.get(k, ()))
        for k in reads:
            kn = k[0] if isinstance(k, tuple) else k
            if isinstance(kn, str) and (kn.startswith("ps") or kn in self.psum_names):
                for t in self.reads.get(k, ()):
                    if t[0] != ("eng", eng):
                        toks.append(t)
        best = {}
        for (s, v) in toks:
            if best.get(s, -1) < v:
                best[s] = v
        out = []
        w = self.waited[eng]
        for s, v in best.items():
            if (not self.same_engine_sync) and s == ("eng", eng) and eng != "sp":
                continue
            if eng == "pe" and s == ("eng", "pe"):
                continue
            if w.get(s, -1) >= v:
                continue
            w[s] = v
            out.append((s, v))
        return out

    def _commit(self, tok, reads, writes):
        for k in writes:
            self.last_write[k] = tok
            self.reads[k] = []
        for k in reads:
            self.reads.setdefault(k, []).append(tok)

    def op(self, eng, fn, reads=(), writes=()):
        reads = [self._key(r) for r in reads]
        writes = [self._key(r) for r in writes]
        waits = self._deps(eng, reads, writes)
        self.cnt[eng] += 1
        tok = (("eng", eng), self.cnt[eng])
        self.ops[eng].append((waits, fn, tok))
        self._commit(tok, reads, writes)
        self.n_ops += 1
        return tok

    def dma(self, fn, reads=(), writes=(), eng="sp", final=False):
        reads = [self._key(r) for r in reads]
        writes = [self._key(r) for r in writes]
        i = (eng, self.dma_rr[eng])
        self.dma_rr[eng] = (self.dma_rr[eng] + 1) % DMA_Q[eng]
        s = ("dma", i)
        waits = self._deps(eng, reads, writes)
        prev = self.dma_uses[i] * 16
        if prev > 0 and self.waited[eng].get(s, -1) < prev:
            self.waited[eng][s] = prev
            waits.append((s, prev))
        self.dma_uses[i] += 1
        tok = (s, self.dma_uses[i] * 16)
        self.ops[eng].append((waits, fn, tok))
        self._commit(tok, reads, writes)
        if final:
            self.final_tokens.append(tok)
        self.n_ops += 1
        return tok

    def getreg(self, engh, val):
        if not hasattr(self, "_regs"):
            self._regs = {}
        k = (type(engh).__name__, val)
        if k not in self._regs:
            self._regs[k] = engh.to_reg(val)
        return self._regs[k]

    def barrier(self):
        toks = []
        for e in ENGS:
            if e != "sp" and self.cnt[e] > 0:
                toks.append((("eng", e), self.cnt[e]))
        for i in DMA_SEMS:
            if self.dma_uses[i] > 0:
                toks.append((("dma", i), self.dma_uses[i] * 16))
        for e in ENGS:
            waits = []
            for (s, v) in toks:
                if s == ("eng", e):
                    continue
                if self.waited[e].get(s, -1) < v:
                    self.waited[e][s] = v
                    waits.append((s, v))
            if waits:
                self.ops[e].append((waits, None, None))

    def flush(self, last=False):
        nc = self.nc
        sems = self.sems
        handles = {"pe": "tensor", "act": "scalar", "dve": "vector", "pool": "gpsimd", "sp": "sync"}
        with nc.Block() as block:
            def make(e):
                def body(engh):
                    for (waits, fn, tok) in self.ops[e]:
                        for (s, v) in waits:
                            engh.wait_ge(sems[s], v)
                        if fn is None:
                            continue
                        inst = fn(engh)
                        inst.then_inc(sems[tok[0]], 16 if tok[0][0] == "dma" else 1)
                    if e == "sp" and last:
                        for (s, v) in self.final_tokens:
                            engh.wait_ge(sems[s], v)
                return body
            for e in ENGS:
                getattr(block, handles[e])(make(e))
        self.ops = {e: [] for e in ENGS}

    def end_phase(self, last=False):
        self.barrier()
        self.flush(last)

    def emit(self):
        self.flush(True)


D = 2048; EPS = 1e-6
CAP_L = 1024; CAP_C = 32

def emit_g1(nc, P, pre, io, combine, final):
    xl = io["xl"]; xc = io["xc"]; gv = io["g"]; ident_d = io["ident"]
    if not final:
        modl = io["modl"]; modc = io["modc"]; hTl = io["hTl"]; hTc = io["hTc"]
    else:
        outl = io["outl"]
    if combine:
        yl = io["yl"]; yc = io["yc"]; slotl = io["slotl"]; slotc = io["slotc"]; affl = io["affl"]; affc = io["affc"]
        gafl = io["gafl"]; gafc = io["gafc"]; xlo = io["xlo"]; xco = io["xco"]
    with contextlib.ExitStack() as st:
        T = lambda name, shape, dt=F32: st.enter_context(nc.sbuf_tensor(pre + name, shape, dt))
        ident = T("ident_s", [128, 128], BF16)
        P.dma(lambda e: e.dma_start(out=ident[:], in_=ident_d), writes=[ident])
        g_bc = T("g_bc", [128, D])
        P.dma(lambda e: e.dma_start(out=g_bc[:], in_=gv.to_broadcast([128, D])), writes=[g_bc])
        AB = {}
        if not final:
            tmp = T("tmpbc", [128, D])
            for nm, mod in (("l", modl), ("c", modc)):
                A = T("A_" + nm, [128, D]); Bt = T("B_" + nm, [128, D])
                P.dma(lambda e, mod=mod: e.dma_start(out=tmp[:], in_=mod[1:2, :].to_broadcast([128, D])), writes=[tmp])
                P.dma(lambda e, mod=mod, Bt=Bt: e.dma_start(out=Bt[:], in_=mod[0:1, :].to_broadcast([128, D])), writes=[Bt])
                P.op("dve", lambda e, A=A: e.scalar_tensor_tensor(out=A[:], in0=tmp[:], scalar=1.0, in1=g_bc[:], op0=ALU.add, op1=ALU.mult),
                     reads=[tmp, g_bc], writes=[A])
                AB[nm] = (A, Bt)
        if combine:
            gaf = {}
            for nm, src in (("l", gafl), ("c", gafc)):
                t = T("gaf_" + nm, [128, D])
                P.dma(lambda e, t=t, src=src: e.dma_start(out=t[:], in_=src.to_broadcast([128, D])), writes=[t])
                gaf[nm] = t
            yg = [T("yg%d" % i, [128, D]) for i in range(2)]
            for t in yg:
                P.op("pool", lambda e, t=t: e.memset(t[:], 0.0), writes=[t])
            acc = T("acc", [128, D])
            slot_t = [T("slot%d" % i, [128, 16], I32) for i in range(2)]
            slot_f = T("slotf", [128, 16]); aff_t = [T("afft%d" % i, [128, 16]) for i in range(2)]
            ge = T("ge", [128, 16])
        xt = [T("xt%d" % i, [128, D]) for i in range(2)]
        h1 = T("h1", [128, D])
        ss = T("ss", [128, 1]); rstd = T("rstd", [128, 1])
        if not final:
            hb = [T("hb%d" % i, [128, D], BF16) for i in range(2)]
            stg = [T("stg%d" % i, [128, D], BF16) for i in range(2)]
            pst = [P.psum(st, pre + "pst%d" % i, [128, 1024], BF16) for i in range(4)]
        else:
            ob = [T("ob%d" % i, [128, D]) for i in range(2)]

        for it in range(17):
            isl = it < 16
            rows = 128 if isl else 64
            nm = "l" if isl else "c"
            x = xt[it % 2]
            src = xl[it * 128:(it + 1) * 128, :] if isl else xc
            P.dma(lambda e, x=x, src=src, rows=rows: e.dma_start(out=x[:rows, :], in_=src), writes=[x])
            if combine:
                stt = slot_t[it % 2]; aft = aff_t[it % 2]
                if isl:
                    P.dma(lambda e, stt=stt, s=slotl[it]: e.dma_start(out=stt[:], in_=s), writes=[stt])
                    P.dma(lambda e, aft=aft, s=affl[it]: e.dma_start(out=aft[:], in_=s), writes=[aft])
                else:
                    P.op("pool", lambda e, stt=stt: e.memset(stt[:], 32768), writes=[stt])
                    P.op("pool", lambda e, aft=aft: e.memset(aft[:], 0.0), writes=[aft])
                    P.dma(lambda e, stt=stt: e.dma_start(out=stt[:64, :], in_=slotc), writes=[stt])
                    P.dma(lambda e, aft=aft: e.dma_start(out=aft[:64, :], in_=affc), writes=[aft])
                cap = CAP_L if isl else CAP_C
                ysrc = yl if isl else yc
                P.op("dve", lambda e, stt=stt: e.tensor_copy(out=slot_f[:], in_=stt[:]), reads=[stt], writes=[slot_f])
                P.op("dve", lambda e, cap=cap: e.tensor_scalar(out=ge[:], in0=slot_f[:], scalar1=float(cap) - 0.5, scalar2=None, op0=ALU.is_lt),
                     reads=[slot_f], writes=[ge])
                P.op("dve", lambda e, aft=aft: e.tensor_tensor(out=ge[:], in0=ge[:], in1=aft[:], op=ALU.mult), reads=[ge, aft], writes=[ge])
                for ex in range(16):
                    y = yg[ex % 2]
                    P.dma(lambda e, y=y, ex=ex, stt=stt, ysrc=ysrc, cap=cap: e.indirect_dma_start(
                        out=y[:, :], out_offset=None, in_=ysrc[ex][:, :],
                        in_offset=bass.IndirectOffsetOnAxis(ap=stt[:, ex:ex + 1], axis=0),
                        bounds_check=P.getreg(e, cap - 1), oob_is_err=False), eng="pool", reads=[stt], writes=[y])
                    if ex == 0:
                        P.op("dve", lambda e, y=y, ex=ex, rows=rows: e.tensor_scalar(out=acc[:rows, :], in0=y[:rows, :], scalar1=ge[:rows, ex:ex + 1], scalar2=None, op0=ALU.mult),
                             reads=[y, ge], writes=[acc])
                    else:
                        P.op("dve", lambda e, y=y, ex=ex, rows=rows: e.scalar_tensor_tensor(out=acc[:rows, :], in0=y[:rows, :], scalar=ge[:rows, ex:ex + 1], in1=acc[:rows, :], op0=ALU.mult, op1=ALU.add),
                             reads=[y, ge, acc], writes=[acc])
                gt = gaf[nm]
                P.op("dve", lambda e, rows=rows, gt=gt: e.tensor_tensor(out=acc[:rows, :], in0=acc[:rows, :], in1=gt[:rows, :], op=ALU.mult), reads=[acc, gt], writes=[acc])
                P.op("dve", lambda e, rows=rows, x=x: e.tensor_tensor(out=x[:rows, :], in0=x[:rows, :], in1=acc[:rows, :], op=ALU.add), reads=[acc, x], writes=[x])
                dst = xlo[it * 128:(it + 1) * 128, :] if isl else xco
                P.dma(lambda e, x=x, dst=dst, rows=rows: e.dma_start(out=dst, in_=x[:rows, :]), reads=[x], writes=["xo"], final=True)
            if final and not isl:
                continue
            P.op("act", lambda e, x=x, rows=rows: e.activation(out=h1[:rows, :], in_=x[:rows, :], func=AF.Square, accum_out=ss[:rows, :]),
                 reads=[x], writes=[h1, ss])
            P.op("act", lambda e, rows=rows: e.activation(out=rstd[:rows, :], in_=ss[:rows, :], func=AF.Sqrt, scale=1.0 / D, bias=EPS),
                 reads=[ss], writes=[rstd])
            P.op("dve", lambda e, rows=rows: e.reciprocal(out=rstd[:rows, :], in_=rstd[:rows, :]),
                 reads=[rstd], writes=[rstd])
            if final:
                o = ob[it % 2]
                P.op("dve", lambda e, x=x, o=o: e.scalar_tensor_tensor(out=o[:], in0=x[:], scalar=rstd[:, 0:1], in1=g_bc[:], op0=ALU.mult, op1=ALU.mult),
                     reads=[x, rstd, g_bc], writes=[o])
                P.dma(lambda e, o=o, it=it: e.dma_start(out=outl[it * 128:(it + 1) * 128, :], in_=o[:]), reads=[o], writes=["outl"], final=True)
                continue
            A, Bt = AB[nm]
            P.op("dve", lambda e, x=x, rows=rows, A=A: e.scalar_tensor_tensor(out=h1[:rows, :], in0=x[:rows, :], scalar=rstd[:rows, 0:1], in1=A[:rows, :], op0=ALU.mult, op1=ALU.mult),
                 reads=[x, rstd, A], writes=[h1])
            h = hb[it % 2]
            P.op("pool", lambda e, h=h, rows=rows, Bt=Bt: e.tensor_tensor(out=h[:rows, :], in0=h1[:rows, :], in1=Bt[:rows, :], op=ALU.add),
                 reads=[h1, Bt], writes=[h])
            sg = stg[it % 2]
            if isl:
                for half in range(2):
                    pt = pst[(it * 2 + half) % 4]
                    for k in range(8):
                        kc = half * 8 + k
                        P.op("pe", lambda e, pt=pt, h=h, k=k, kc=kc: e.transpose(out=pt[:, k * 128:(k + 1) * 128], in_=h[:, kc * 128:(kc + 1) * 128], identity=ident[:]),
                             reads=[h, ident], writes=[pt])
                    eng = "act" if half == 0 else "dve"
                    if eng == "act":
                        P.op("act", lambda e, pt=pt, sg=sg, half=half: e.copy(out=sg[:, half * 1024:(half + 1) * 1024], in_=pt[:]), reads=[pt], writes=[(sg, half)])
                    else:
                        P.op("dve", lambda e, pt=pt, sg=sg, half=half: e.tensor_copy(out=sg[:, half * 1024:(half + 1) * 1024], in_=pt[:]), reads=[pt], writes=[(sg, half)])
                P.dma(lambda e, sg=sg, it=it: e.dma_start(out=hTl[it], in_=sg[:]), reads=[(sg, 0), (sg, 1)], writes=["hTl"], final=True)
            else:
                pt = pst[0]
                for kc in range(16):
                    P.op("pe", lambda e, pt=pt, h=h, kc=kc: e.transpose(out=pt[:, kc * 64:(kc + 1) * 64], in_=h[:64, kc * 128:(kc + 1) * 128], identity=ident[:64, :64]),
                         reads=[h, ident], writes=[pt])
                P.op("act", lambda e, pt=pt, sg=sg: e.copy(out=sg[:, 0:1024], in_=pt[:]), reads=[pt], writes=[(sg, 0)])
                P.dma(lambda e, sg=sg: e.dma_start(out=hTc, in_=sg[:, 0:1024].rearrange("p (k t) -> p k t", k=16)), reads=[(sg, 0)], writes=["hTc"], final=True)
        P.end_phase()


D = 2048; EPS = 1e-6
NCH = 66
POOL_W = (2, 4, 8, 16)

def emit_mixe(nc, P, pre, io, hp):
    stop = 9; do_pool = True; nheads = 2
    q = hp
    hT = io["HT"]; MIXT = io["MIXT"]; w_in = io["w_in"]; gate_b = io["gate_b"]; head_g = io["head_g"]
    tab = io["tab"]; identb_d = io["identb"]; identf_d = io["identf"]; tri_d = io["tri"]
    pw = io["pool_w"]; pool_scale = io["pool_scale"]
    icl = io["icl"][:, q * 2048:(q + 1) * 2048]; icc = io["icc"][:, q * 64:(q + 1) * 64]
    wu = w_in[:, 4128:5152]
    with contextlib.ExitStack() as st0:
        psf = [P.psum(st0, pre + "psf%d" % i, [128, 512], F32) for i in range(6)]
        psb = [P.psum(st0, pre + "psb%d" % i, [128, 1024], BF16) for i in range(2)]
        T0 = lambda name, shape, dt=F32: st0.enter_context(nc.sbuf_tensor(pre + name, shape, dt))
        identb = T0("identb_s", [128, 128], BF16); identf = T0("identf_s", [128, 128])
        tri = [T0("tri%d" % i, [128, 128]) for i in range(2)]
        ones = T0("ones", [128, 128]); zeros = T0("zeros", [128, 128])
        P.dma(lambda e: e.dma_start(out=identb[:], in_=identb_d), writes=[identb])
        P.dma(lambda e: e.dma_start(out=identf[:], in_=identf_d), writes=[identf])
        for i in range(2):
            P.dma(lambda e, i=i: e.dma_start(out=tri[i][:], in_=tri_d[i]), writes=[tri[i]])
        P.op("pool", lambda e: e.memset(ones[:], 1.0), writes=[ones])
        P.op("pool", lambda e: e.memset(zeros[:], 0.0), writes=[zeros])

        with contextlib.ExitStack() as st:
            T = lambda name, shape, dt=F32: st.enter_context(nc.sbuf_tensor(pre + name, shape, dt))
            Wh = T("Wh", [128, 16 * 512], BF16); Wg = T("Wg", [128, 16 * 4], BF16)
            gbt = T("gbt", [128, 4]); gn = T("gn", [128, 128]); gball = T("gball", [128, 32])
            QKT = T("QKT", [128, NCH * 256], BF16)
            Ktok = T("Ktok", [128, NCH * 128], BF16)
            VE = T("VE", [128, NCH * 129], BF16)
            SO = T("SO", [128, NCH * 128], BF16)
            HF = T("HF", [128, NCH * 128])
            OUT = T("OUTs", [128, NCH * 128], BF16)
            G = T("G", [128, NCH * 4])
            hch = [T("hch%d" % i, [128, 16 * 128], BF16) for i in range(2)]
            tb = [T("tb%d" % i, [128, 256]) for i in range(2)]
            qkt = [T("qkt%d" % i, [128, 256], BF16) for i in range(2)]
            rt = [T("rt%d" % i, [128, 128]) for i in range(4)]
            LF = T("LF", [128, NCH]); Bm = T("Bm", [128, NCH]); U = T("U", [128, NCH]); GS = T("GS", [128, NCH])
            UM = T("UM", [128, NCH]); MS = T("MS", [128, NCH + 1]); ME = T("ME", [128, NCH]); tA = T("tA", [128, NCH])
            um = T("um", [128, 1]); umb = T("umb", [128, 128])
            SPt = [T("SP%d" % i, [128, NCH]) for i in range(2)]
            WAt = [T("WA%d" % i, [128, NCH]) for i in range(2)]
            FLt = [T("FL%d" % i, [128, NCH]) for i in range(2)]
            CTs = [T("CT%d" % i, [128, 129]) for i in range(2)]; CTbs = [T("CTb%d" % i, [128, 129], BF16) for i in range(2)]
            PT = [T("PT%d" % i, [128, 128], BF16) for i in range(2)]
            vw = [T("vw%d" % i, [128, 129], BF16) for i in range(2)]
            rr = [T("rr%d" % i, [128, 1]) for i in range(2)]
            ssq = T("ssq", [128, NCH]); rs = T("rs", [128, NCH])
            hn1 = [T("hn1_%d" % i, [128, 128]) for i in range(2)]
            hnb = [T("hnb%d" % i, [128, 128], BF16) for i in range(2)]

            QKT3 = QKT[:].rearrange("p (c x) -> p c x", x=256)
            K3 = Ktok[:].rearrange("p (c x) -> p c x", x=128)
            VE3 = VE[:].rearrange("p (c x) -> p c x", x=129)
            SO3 = SO[:].rearrange("p (c x) -> p c x", x=128)
            HF3 = HF[:].rearrange("p (c x) -> p c x", x=128)
            G3 = G[:].rearrange("p (c x) -> p c x", x=4)
            Wh3 = Wh[:].rearrange("p (k n) -> p k n", k=16)
            Wg3 = Wg[:].rearrange("p (k n) -> p k n", k=16)

            P.op("pool", lambda e: e.memset(VE[:], 1.0), writes=[VE])
            for hd in range(nheads):
                h = 2 * hp + hd
                for s_ in range(4):
                    c0 = s_ * 1024 + h * 128
                    P.dma(lambda e, s_=s_, c0=c0: e.dma_start(out=Wh3[:, :, s_ * 128:(s_ + 1) * 128], in_=w_in[:, c0:c0 + 128].rearrange("(k p) n -> p k n", p=128)), eng="pool", writes=[Wh])
                for gt in range(4):
                    c0 = 4096 + gt * 8 + h
                    P.dma(lambda e, gt=gt, c0=c0: e.dma_start(out=Wg3[:, :, gt:gt + 1], in_=w_in[:, c0:c0 + 1].rearrange("(k p) n -> p k n", p=128), allow_slow_non_contiguous=True), eng="pool", writes=[Wg])
                P.dma(lambda e: e.dma_start(out=gball[:], in_=gate_b.rearrange("g h -> (g h)").unsqueeze(0).to_broadcast([128, 32])), writes=[gball])
                P.op("dve", lambda e, h=h: e.tensor_copy(out=gbt[:], in_=gball[:].rearrange("p (g h) -> p g h", g=4)[:, :, h]), reads=[gball], writes=[gbt])
                P.dma(lambda e, h=h: e.dma_start(out=gn[:], in_=head_g[:, h * 128:(h + 1) * 128].to_broadcast([128, 128])), writes=[gn])
                def p1_proj(c):
                    hc_ = hch[c % 2]; tbc = tb[c % 2]; qk = qkt[c % 2]
                    pp = psf[c % 2]; pg = psf[2 + c % 2]; pb = psb[c % 2]
                    P.dma(lambda e, hc_=hc_, c=c: e.dma_start(out=hc_[:], in_=hT[c]), writes=[hc_])
                    P.dma(lambda e, tbc=tbc, c=c: e.dma_start(out=tbc[:], in_=tab[c]), writes=[tbc])
                    h3 = hc_[:].rearrange("p (k t) -> p k t", k=16)
                    for kc in range(16):
                        P.op("pe", lambda e, pp=pp, h3=h3, kc=kc: e.matmul(pp[:, :], lhsT=h3[:, kc, :], rhs=Wh3[:, kc, :], start=(kc == 0), stop=(kc == 15)),
                             reads=[hc_, Wh], writes=[pp])
                    for kc in range(16):
                        P.op("pe", lambda e, pg=pg, h3=h3, kc=kc: e.matmul(pg[:, 0:4], lhsT=h3[:, kc, :], rhs=Wg3[:, kc, :], start=(kc == 0), stop=(kc == 15)),
                             reads=[hc_, Wg], writes=[pg])
                def p1_post(c):
                    hc_ = hch[c % 2]; tbc = tb[c % 2]; qk = qkt[c % 2]
                    pp = psf[c % 2]; pg = psf[2 + c % 2]; pb = psb[c % 2]
                    xv = pp[:, 0:256].rearrange("p (a b h d) -> p a b h d", a=2, b=2, h=2)
                    x1 = xv[:, :, :, 0, :]; x2 = xv[:, :, :, 1, :]
                    tv = tbc[:].rearrange("p (s a b d) -> p s a b d", s=2, a=2, b=2)
                    Cv = tv[:, 0]; Sv = tv[:, 1]
                    ov = qk[:].rearrange("p (a b h d) -> p a b h d", a=2, b=2, h=2)
                    r4 = [t[:].rearrange("p (a b d) -> p a b d", a=2, b=2) for t in rt]
                    P.op("dve", lambda e, x1=x1, Cv=Cv, o=r4[0]: e.tensor_tensor(out=o, in0=x1, in1=Cv, op=ALU.mult), reads=[pp, tbc], writes=[rt[0]])
                    P.op("dve", lambda e, x2=x2, Sv=Sv, o=r4[1]: e.tensor_tensor(out=o, in0=x2, in1=Sv, op=ALU.mult), reads=[pp, tbc], writes=[rt[1]])
                    P.op("dve", lambda e, x1=x1, Sv=Sv, o=r4[2]: e.tensor_tensor(out=o, in0=x1, in1=Sv, op=ALU.mult), reads=[pp, tbc], writes=[rt[2]])
                    P.op("dve", lambda e, x2=x2, Cv=Cv, o=r4[3]: e.tensor_tensor(out=o, in0=x2, in1=Cv, op=ALU.mult), reads=[pp, tbc], writes=[rt[3]])
                    P.op("pool", lambda e, o=ov[:, :, :, 0, :], a=r4[0], b=r4[1]: e.tensor_tensor(out=o, in0=a, in1=b, op=ALU.subtract), reads=[rt[0], rt[1]], writes=[(qk, 0)])
                    P.op("pool", lambda e, o=ov[:, :, :, 1, :], a=r4[2], b=r4[3]: e.tensor_tensor(out=o, in0=a, in1=b, op=ALU.add), reads=[rt[2], rt[3]], writes=[(qk, 1)])
                    P.op("act", lambda e, pp=pp, c=c: e.copy(out=VE3[:, c, 0:128], in_=pp[:, 256:384]), reads=[pp], writes=[(VE, c)])
                    P.op("act", lambda e, pp=pp, c=c: e.activation(out=SO3[:, c, :], in_=pp[:, 384:512], func=AF.Sigmoid), reads=[pp], writes=[(SO, c)])
                    P.op("dve", lambda e, pg=pg, c=c: e.tensor_tensor(out=G3[:, c, :], in0=pg[:, 0:4], in1=gbt[:], op=ALU.add), reads=[pg, gbt], writes=[(G, c)])
                    P.op("pool", lambda e, qk=qk, c=c: e.tensor_copy(out=K3[:, c, :], in_=qk[:, 128:256]), reads=[(qk, 0), (qk, 1)], writes=[(Ktok, c)])
                    for a in range(2):
                        P.op("pe", lambda e, pb=pb, qk=qk, a=a: e.transpose(out=pb[:, a * 128:(a + 1) * 128], in_=qk[:, a * 128:(a + 1) * 128], identity=identb[:]),
                             reads=[(qk, 0), (qk, 1), identb], writes=[pb])
                    P.op("act", lambda e, pb=pb, c=c: e.copy(out=QKT3[:, c, :], in_=pb[:, 0:256]), reads=[pb], writes=[(QKT, c)])

                p1_proj(0)
                for c in range(NCH):
                    if c + 1 < NCH:
                        p1_proj(c + 1)
                    p1_post(c)
                ordrs = []
                for dr in range(2 if stop >= 2 else 0):
                    ordr = list(range(NCH)) if dr == 0 else [1, 0] + list(range(NCH - 1, 1, -1))
                    li = G3[:, :, 2 * dr]; ff = G3[:, :, 2 * dr + 1]
                    Gk = [(G, c) for c in range(NCH)]
                    P.op("act", lambda e, ff=ff: e.activation(out=tA[:], in_=ff, func=AF.Exp, scale=-1.0), reads=Gk, writes=[tA])
                    P.op("act", lambda e: e.activation(out=tA[:], in_=tA[:], func=AF.Ln, bias=1.0), reads=[tA], writes=[tA])
                    P.op("dve", lambda e: e.tensor_scalar(out=LF[:], in0=tA[:], scalar1=-1.0, scalar2=None, op0=ALU.mult), reads=[tA], writes=[LF])
                    pa = psf[4]
                    P.op("pe", lambda e, dr=dr: e.matmul(pa[:, 0:NCH], lhsT=tri[dr][:], rhs=LF[:], start=True, stop=True), reads=[tri[dr], LF], writes=[pa])
                    P.op("pe", lambda e: e.matmul(pa[:, 128:128 + NCH], lhsT=ones[:], rhs=LF[:], start=True, stop=True), reads=[ones, LF], writes=[pa])
                    P.op("act", lambda e: e.copy(out=Bm[:], in_=pa[:, 0:NCH]), reads=[pa], writes=[Bm])
                    P.op("act", lambda e: e.copy(out=GS[:], in_=pa[:, 128:128 + NCH]), reads=[pa], writes=[GS])
                    P.op("dve", lambda e, li=li: e.tensor_tensor(out=U[:], in0=li, in1=Bm[:], op=ALU.subtract), reads=Gk + [Bm], writes=[U])
                    P.op("pe", lambda e: e.transpose(out=pa[:NCH, 256:384], in_=U[:], identity=identf[:]), reads=[U, identf], writes=[pa])
                    P.op("dve", lambda e: e.reduce_max(out=um[:NCH, :], in_=pa[:NCH, 256:384], axis=AX.X), reads=[pa], writes=[um])
                    P.op("dve", lambda e: e.tensor_scalar(out=umb[:NCH, :], in0=zeros[:NCH, :], scalar1=um[:NCH, 0:1], scalar2=None, op0=ALU.add), reads=[um, zeros], writes=[umb])
                    P.op("pe", lambda e: e.matmul(pa[:, 384:384 + NCH], lhsT=umb[:NCH, :], rhs=identf[:NCH, :NCH], start=True, stop=True), reads=[umb, identf], writes=[pa])
                    P.op("act", lambda e: e.copy(out=UM[:], in_=pa[:, 384:384 + NCH]), reads=[pa], writes=[UM])
                    P.op("pool", lambda e: e.memset(MS[:], 0.0), writes=[MS])
                    for n, c in enumerate(ordr):
                        P.op("dve", lambda e, c=c: e.tensor_tensor(out=ME[:, c:c + 1], in0=MS[:, c:c + 1], in1=UM[:, c:c + 1], op=ALU.max), reads=[MS, UM], writes=[ME])
                        if n + 1 < NCH:
                            cn = ordr[n + 1]
                            P.op("dve", lambda e, c=c, cn=cn: e.tensor_tensor(out=MS[:, cn:cn + 1], in0=ME[:, c:c + 1], in1=GS[:, c:c + 1], op=ALU.add), reads=[ME, GS], writes=[MS])
                    SPd, WAd, FLd = SPt[dr], WAt[dr], FLt[dr]
                    P.op("dve", lambda e: e.tensor_tensor(out=tA[:], in0=MS[:, 0:NCH], in1=ME[:], op=ALU.subtract), reads=[MS, ME], writes=[tA])
                    P.op("act", lambda e, SPd=SPd: e.activation(out=SPd[:], in_=tA[:], func=AF.Exp), reads=[tA], writes=[SPd])
                    P.op("dve", lambda e: e.tensor_tensor(out=tA[:], in0=U[:], in1=ME[:], op=ALU.subtract), reads=[U, ME], writes=[tA])
                    P.op("act", lambda e, WAd=WAd: e.activation(out=WAd[:], in_=tA[:], func=AF.Exp), reads=[tA], writes=[WAd])
                    P.op("dve", lambda e: e.tensor_tensor(out=tA[:], in0=Bm[:], in1=ME[:], op=ALU.add), reads=[Bm, ME], writes=[tA])
                    P.op("act", lambda e, FLd=FLd: e.activation(out=FLd[:], in_=tA[:], func=AF.Exp, scale=-1.0), reads=[tA], writes=[FLd])
                    ordrs.append(ordr)
                P.op("pool", lambda e: e.memset(HF[:], 0.0), writes=[(HF, c) for c in range(NCH)])
                for dr in range(2):
                    P.op("pool", lambda e, dr=dr: e.memset(CTs[dr][:], 0.0), writes=[CTs[dr]])
                for n in range(NCH):
                    for dr in range(2):
                        c = ordrs[dr][n]
                        SPd, WAd, FLd = SPt[dr], WAt[dr], FLt[dr]
                        CT = CTs[dr]; CTb = CTbs[dr]
                        pS = psf[dr]; pO = psf[2 + dr]; pC = psf[4 + dr]
                        PTn = PT[dr]; vwn = vw[dr]; rrn = rr[dr]
                        P.op("pe", lambda e, pS=pS, c=c: e.matmul(pS[:, 0:128], lhsT=QKT3[:, c, 128:256], rhs=QKT3[:, c, 0:128], start=True, stop=True),
                             reads=[(QKT, c)], writes=[pS])
                        P.op("dve", lambda e, pS=pS, PTn=PTn, dr=dr: e.tensor_tensor(out=PTn[:], in0=pS[:, 0:128], in1=tri[dr][:], op=ALU.mult), reads=[pS, tri[dr]], writes=[PTn])
                        P.op("act", lambda e, vwn=vwn, c=c, WAd=WAd: e.activation(out=vwn[:], in_=VE3[:, c, :], func=AF.Copy, scale=WAd[:, c:c + 1]), reads=[(VE, c), WAd], writes=[vwn])
                        P.op("dve", lambda e, c=c, SPd=SPd, CT=CT: e.tensor_scalar(out=CT[:], in0=CT[:], scalar1=SPd[:, c:c + 1], scalar2=None, op0=ALU.mult), reads=[CT, SPd], writes=[CT])
                        P.op("act", lambda e, CT=CT, CTb=CTb: e.copy(out=CTb[:], in_=CT[:]), reads=[CT], writes=[CTb])
                        P.op("pe", lambda e, pO=pO, PTn=PTn, vwn=vwn: e.matmul(pO[:, 0:129], lhsT=PTn[:], rhs=vwn[:], start=True, stop=False), reads=[PTn, vwn], writes=[pO])
                        P.op("pe", lambda e, pO=pO, c=c, CTb=CTb: e.matmul(pO[:, 0:129], lhsT=QKT3[:, c, 0:128], rhs=CTb[:], start=False, stop=True), reads=[(QKT, c), CTb], writes=[pO])
                        P.op("pe", lambda e, pC=pC, c=c, vwn=vwn: e.matmul(pC[:, 0:129], lhsT=K3[:, c, :], rhs=vwn[:], start=True, stop=True), reads=[(Ktok, c), vwn], writes=[pC])
                        P.op("dve", lambda e, pC=pC, CT=CT: e.tensor_tensor(out=CT[:], in0=CT[:], in1=pC[:, 0:129], op=ALU.add), reads=[CT, pC], writes=[CT])
                        P.op("act", lambda e, pO=pO, rrn=rrn: e.activation(out=rrn[:], in_=pO[:, 128:129], func=AF.Abs),
                             reads=[pO], writes=[rrn])
                        P.op("dve", lambda e, rrn=rrn, c=c, FLd=FLd: e.tensor_scalar(out=rrn[:], in0=rrn[:], scalar1=FLd[:, c:c + 1], scalar2=None, op0=ALU.max),
                             reads=[rrn, FLd], writes=[rrn])
                        P.op("dve", lambda e, rrn=rrn: e.reciprocal(out=rrn[:], in_=rrn[:]), reads=[rrn], writes=[rrn])
                        P.op("dve", lambda e, pO=pO, rrn=rrn, c=c: e.scalar_tensor_tensor(out=HF3[:, c, :], in0=pO[:, 0:128], scalar=rrn[:, 0:1], in1=HF3[:, c, :], op0=ALU.mult, op1=ALU.add),
                             reads=[pO, rrn, (HF, c)], writes=[(HF, c)])
                if stop < 4:
                    continue
                for c in range(NCH):
                    h1 = hn1[c % 2]
                    P.op("act", lambda e, c=c, h1=h1: e.activation(out=h1[:], in_=HF3[:, c, :], func=AF.Square, accum_out=ssq[:, c:c + 1]), reads=[(HF, c)], writes=[h1, (ssq, c)])
                sk = [(ssq, c) for c in range(NCH)]
                P.op("act", lambda e: e.activation(out=rs[:], in_=ssq[:], func=AF.Sqrt, scale=1.0 / 128, bias=EPS), reads=sk, writes=[rs])
                P.op("dve", lambda e: e.reciprocal(out=rs[:], in_=rs[:]), reads=[rs], writes=[rs])
                OUT3 = OUT[:].rearrange("p (c x) -> p c x", x=128)
                for c in range(NCH):
                    h1 = hn1[c % 2]; hb_ = hnb[c % 2]
                    grp, k = divmod(c, 8)
                    pb = psb[grp % 2]
                    P.op("dve", lambda e, c=c, h1=h1: e.scalar_tensor_tensor(out=h1[:], in0=HF3[:, c, :], scalar=rs[:, c:c + 1], in1=gn[:], op0=ALU.mult, op1=ALU.mult),
                         reads=[(HF, c), rs, gn], writes=[h1])
                    P.op("pool", lambda e, c=c, h1=h1, hb_=hb_: e.tensor_tensor(out=hb_[:], in0=h1[:], in1=SO3[:, c, :], op=ALU.mult), reads=[h1, (SO, c)], writes=[hb_])
                    P.op("pe", lambda e, pb=pb, hb_=hb_, k=k: e.transpose(out=pb[:, k * 128:(k + 1) * 128], in_=hb_[:], identity=identb[:]), reads=[hb_, identb], writes=[pb])
                    if k == 7 or c == NCH - 1:
                        n = k + 1
                        P.op("act", lambda e, pb=pb, grp=grp, n=n: e.copy(out=OUT[:, grp * 1024:grp * 1024 + n * 128], in_=pb[:, 0:n * 128]), reads=[pb], writes=[OUT])
                P.dma(lambda e, h=h: e.dma_start(out=MIXT[:, :, h, :].rearrange("t p x -> p t x"), in_=OUT[:].rearrange("p (t x) -> p t x", x=128)), reads=[OUT], writes=["mixh"])
            P.barrier()
        with contextlib.ExitStack() as st:
            T = lambda name, shape, dt=F32: st.enter_context(nc.sbuf_tensor(pre + name, shape, dt))
            Wu = T("Wu", [128, 16 * 1024], BF16)
            Wu3 = Wu[:].rearrange("p (k n) -> p k n", k=16)
            for kc_ in range(16):
                P.dma(lambda e, kc_=kc_: e.dma_start(out=Wu3[:, kc_, :], in_=wu[kc_ * 128:(kc_ + 1) * 128, :]), eng="pool", writes=[Wu])
            PW = T("PW", [128, 4 * 2 * 256], BF16)
            PW4 = PW[:].rearrange("p (g c n) -> p g c n", g=4, c=2)
            for g in range(4):
                P.dma(lambda e, g=g: e.dma_start(out=PW4[:, g], in_=pw[g].rearrange("(c p) n -> p c n", p=128)), eng="pool", writes=[PW])
            pst = T("pst", [128, 8])
            for et_ in range(8):
                P.dma(lambda e, et_=et_: e.dma_start(out=pst[:, et_:et_ + 1], in_=pool_scale[et_ * 128:(et_ + 1) * 128].unsqueeze(1)), writes=[pst])
            def pool_part(nm, hp, ic, NT, odst):
                NH = NT + 16
                hs = T("hs_" + nm, [128, 16 * NH], BF16)
                hs3 = hs[:].rearrange("p (k t) -> p k t", k=16)
                if nm == "l":
                    t0 = 2 + q * 16
                    for kc_ in range(16):
                        P.dma(lambda e, kc_=kc_: e.dma_start(out=hs3[:, kc_, 8:8 + 2048].rearrange("p (t x) -> p t x", x=128),
                                                              in_=hT[t0:t0 + 16].rearrange("t p (k x) -> p k t x", k=16)[:, kc_]), writes=[hs])
                    if q > 0:
                        P.dma(lambda e: e.dma_start(out=hs3[:, :, 0:8], in_=hT[t0 - 1].rearrange("p (k x) -> p k x", k=16)[:, :, 120:128]), writes=[hs])
                    else:
                        P.op("pool", lambda e: e.memset(hs3[:, :, 0:8], 0.0), writes=[hs])
                    if q < 3:
                        P.dma(lambda e: e.dma_start(out=hs3[:, :, 2056:2064], in_=hT[t0 + 16].rearrange("p (k x) -> p k x", k=16)[:, :, 0:8]), writes=[hs])
                    else:
                        P.op("pool", lambda e: e.memset(hs3[:, :, 2056:2064], 0.0), writes=[hs])
                else:
                    lo = q * 64 - 8
                    pos = 0
                    if lo < 0:
                        P.op("pool", lambda e: e.memset(hs3[:, :, 0:8], 0.0), writes=[hs]); pos = 8; lo = 0
                    hi = min(q * 64 + 72, 256)
                    while lo < hi:
                        t_ = lo // 128; a_ = lo % 128; n_ = min(hi - lo, 128 - a_)
                        P.dma(lambda e, t_=t_, a_=a_, n_=n_, pos=pos: e.dma_start(out=hs3[:, :, pos:pos + n_], in_=hT[t_].rearrange("p (k x) -> p k x", k=16)[:, :, a_:a_ + n_]), writes=[hs])
                        pos += n_; lo += n_
                    if pos < 80:
                        P.op("pool", lambda e, pos=pos: e.memset(hs3[:, :, pos:80], 0.0), writes=[hs])
                Ub = [T("U%s%d" % (nm, i), [128, NH]) for i in range(2)]
                A1 = T("A1" + nm, [128, NH]); A2 = T("A2" + nm, [128, NH])
                icb = T("icb" + nm, [128, NT])
                dT = [T("dT%s%d" % (nm, i), [128, NT], BF16) for i in range(2)]
                ob = [T("ob%s%d" % (nm, i), [128, NT], BF16) for i in range(2)]
                blocks = [(s, min(512, NH - s)) for s in range(0, NH, 512)]
                oblocks = [(s, min(512, NT - s)) for s in range(0, NT, 512)]
                for g in range(4):
                    w = POOL_W[g]
                    P.dma(lambda e, g=g, ic=ic, icb=icb, NT=NT: e.dma_start(out=icb[:], in_=ic[g:g + 1, :].to_broadcast([128, NT])), writes=[icb])
                    for ci in range(2):
                        ct = g * 2 + ci
                        Ut = Ub[ci]
                        for bi, (s0, n) in enumerate(blocks):
                            pp = psf[bi % 4]
                            for kc in range(16):
                                P.op("pe", lambda e, pp=pp, kc=kc, ct=ct, s0=s0, n=n: e.matmul(pp[:, 0:n], lhsT=Wu3[:, kc, ct * 128:(ct + 1) * 128], rhs=hs3[:, kc, s0:s0 + n], start=(kc == 0), stop=(kc == 15)),
                                     reads=[Wu, hs], writes=[pp])
                            P.op("act", lambda e, pp=pp, Ut=Ut, s0=s0, n=n: e.copy(out=Ut[:, s0:s0 + n], in_=pp[:, 0:n]), reads=[pp], writes=[Ut])
                        cur = Ut; L = NH; step = 1; k = 1
                        bufs = [A1, A2]; bi = 0
                        while k < w:
                            nxt = bufs[bi % 2]; bi += 1
                            L2 = L - k
                            P.op("dve", lambda e, cur=cur, nxt=nxt, L2=L2, k=k: e.tensor_tensor(out=nxt[:, 0:L2], in0=cur[:, 0:L2], in1=cur[:, k:k + L2], op=ALU.add), reads=[cur], writes=[nxt])
                            cur = nxt; L = L2; k *= 2
                        off = 8 - w // 2
                        nxt = bufs[bi % 2]
                        P.op("dve", lambda e, cur=cur, nxt=nxt, off=off, NT=NT: e.tensor_tensor(out=nxt[:, 0:NT], in0=cur[:, off:off + NT], in1=icb[:], op=ALU.mult), reads=[cur, icb], writes=[nxt])
                        P.op("dve", lambda e, nxt=nxt, Ut=Ut, d=dT[ci], NT=NT: e.tensor_tensor(out=d[:], in0=nxt[:, 0:NT], in1=Ut[:, 8:8 + NT], op=ALU.subtract), reads=[nxt, Ut], writes=[dT[ci]])
                    for ei in range(2):
                        et = g * 2 + ei
                        o = ob[ei]
                        for bi, (s0, n) in enumerate(oblocks):
                            pp = psf[4 + bi % 2]
                            for ci in range(2):
                                P.op("pe", lambda e, pp=pp, ci=ci, g=g, ei=ei, s0=s0, n=n: e.matmul(pp[:, 0:n], lhsT=PW4[:, g, ci, ei * 128:(ei + 1) * 128], rhs=dT[ci][:, s0:s0 + n], start=(ci == 0), stop=(ci == 1)),
                                     reads=[PW, dT[ci]], writes=[pp])
                            P.op("act", lambda e, pp=pp, o=o, et=et, s0=s0, n=n: e.activation(out=o[:, s0:s0 + n], in_=pp[:, 0:n], func=AF.Copy, scale=pst[:, et:et + 1]), reads=[pp, pst], writes=[o])
                        if nm == "l":
                            P.dma(lambda e, o=o, et=et: e.dma_start(out=MIXT[2 + q * 16:2 + (q + 1) * 16, :, 8 + et, :].rearrange("t p x -> p t x"), in_=o[:].rearrange("p (t x) -> p t x", x=128)), reads=[o], writes=["po"])
                        else:
                            P.dma(lambda e, o=o, et=et: e.dma_start(out=MIXT[q // 2, :, 8 + et, (q % 2) * 64:(q % 2 + 1) * 64], in_=o[:]), reads=[o], writes=["po"])

            pool_part("l", None, icl, 2048, None)
            pool_part("c", None, icc, 64, None)
            P.end_phase()


D = 2048
NCH = 66
NPAT = 5

def na_patterns():
    R, W, WR, WC = 128, 64, 8, 16
    pats = {}; plist = []; per_m = []
    for m in range(64):
        idx = -np.ones((64, 128, 128), np.int32)
        used = set()
        for rl in range(2):
            r = 2 * m + rl
            rs = min(max(r - WR // 2, 0), R - WR)
            for c in range(W):
                cs = min(max(c - WC // 2, 0), W - WC)
                q = rl * 64 + c
                for kr in range(rs, rs + WR):
                    t = kr // 2
                    used.add(t)
                    kk = (kr - 2 * t) * 64 + np.arange(cs, cs + WC)
                    idx[t, kk, q] = (kr - r + 7) * 31 + (np.arange(cs, cs + WC) - c + 15)
        tiles = sorted(used)
        assert len(tiles) <= 5
        im = -np.ones((5, 128, 128), np.int32)
        for j, t in enumerate(tiles):
            im[j] = idx[t]
        while len(tiles) < 5:
            tiles.append(tiles[-1])
        key = im.tobytes()
        if key not in pats:
            pats[key] = len(plist); plist.append(im)
        per_m.append(([t + 2 for t in tiles], pats[key]))
    assert len(plist) == NPAT, len(plist)
    return per_m, np.stack(plist)

def emit_mixo(nc, P, pre, io, hq, ctx_out):
    nheads = 4; phase = 2; skip = ()
    hT = io["HT"]; MIXT = io["MIXT"]; w_in = io["w_in"]; bias = io["bias"]; identb_d = io["identb"]
    per_m, _ = na_patterns()
    scale = 128.0 ** -0.5
    with contextlib.ExitStack() as st:
        psf = [P.psum(st, pre + "psf%d" % i, [128, 512], F32) for i in range(6)]
        psb = [P.psum(st, pre + "psb%d" % i, [128, 1024], BF16) for i in range(2)]
        T = lambda name, shape, dt=F32: st.enter_context(nc.sbuf_tensor(pre + name, shape, dt))
        identb = T("identb_s", [128, 128], BF16); onesb = T("onesb", [128, 128], BF16)
        P.dma(lambda e: e.dma_start(out=identb[:], in_=identb_d), writes=[identb])
        P.op("pool", lambda e: e.memset(onesb[:], 1.0), writes=[onesb])
        W = T("W", [128, 16 * 384], BF16); W3 = W[:].rearrange("p (k n) -> p k n", k=16)
        QT = T("QT", [128, NCH * 128], BF16); KT = T("KT", [128, NCH * 128], BF16); VT = T("VT", [128, NCH * 128], BF16)
        OUT = T("OUTs", [128, NCH * 128], BF16)
        Bs = T("Bs", [128, NPAT * 640])
        hch = [T("hch%d" % i, [128, 16 * 128], BF16) for i in range(2)]
        qk = [T("qk%d" % i, [128, 256], BF16) for i in range(2)]
        tmp = [T("tmp%d" % i, [128, 640]) for i in range(2)]
        PT = [T("PT%d" % i, [128, 896], BF16) for i in range(2)]
        rc = [T("rc%d" % i, [128, 128]) for i in range(2)]
        Q3 = QT[:].rearrange("p (c x) -> p c x", x=128); K3 = KT[:].rearrange("p (c x) -> p c x", x=128)
        V3 = VT[:].rearrange("p (c x) -> p c x", x=128); O3 = OUT[:].rearrange("p (c x) -> p c x", x=128)
        B3 = Bs[:].rearrange("p (a x) -> p a x", a=NPAT)
        for hd in range(nheads):
            h = 4 * hq + hd
            for s_ in range(3):
                c0 = s_ * 2048 + h * 128
                P.dma(lambda e, s_=s_, c0=c0: e.dma_start(out=W3[:, :, s_ * 128:(s_ + 1) * 128], in_=w_in[:, c0:c0 + 128].rearrange("(k p) n -> p k n", p=128)), eng="pool", writes=[W])
            P.dma(lambda e, h=h: e.dma_start(out=Bs[:], in_=bias[h]), writes=[Bs])
            def p1_proj(c):
                hc_ = hch[c % 2]; pp = psf[c % 2]
                P.dma(lambda e, hc_=hc_, c=c: e.dma_start(out=hc_[:], in_=hT[c]), writes=[hc_])
                h3 = hc_[:].rearrange("p (k t) -> p k t", k=16)
                for kc in range(16):
                    P.op("pe", lambda e, pp=pp, h3=h3, kc=kc: e.matmul(pp[:, 0:384], lhsT=h3[:, kc, :], rhs=W3[:, kc, :], start=(kc == 0), stop=(kc == 15)),
                         reads=[hc_, W], writes=[pp])

            def p1_post(c):
                qk_ = qk[c % 2]; pp = psf[c % 2]; pb = psb[c % 2]
                P.op("dve", lambda e, pp=pp, qk_=qk_: e.tensor_scalar(out=qk_[:, 0:128], in0=pp[:, 0:128], scalar1=scale, scalar2=None, op0=ALU.mult), reads=[pp], writes=[(qk_, 0)])
                P.op("act", lambda e, pp=pp, qk_=qk_: e.copy(out=qk_[:, 128:256], in_=pp[:, 128:256]), reads=[pp], writes=[(qk_, 1)])
                P.op("act", lambda e, pp=pp, c=c: e.copy(out=V3[:, c, :], in_=pp[:, 256:384]), reads=[pp], writes=[(VT, c)])
                for a in range(2):
                    P.op("pe", lambda e, pb=pb, qk_=qk_, a=a: e.transpose(out=pb[:, a * 128:(a + 1) * 128], in_=qk_[:, a * 128:(a + 1) * 128], identity=identb[:]),
                         reads=[(qk_, a), identb], writes=[pb])
                P.op("dve", lambda e, pb=pb, c=c: e.tensor_copy(out=Q3[:, c, :], in_=pb[:, 0:128]), reads=[pb], writes=[(QT, c)])
                P.op("act", lambda e, pb=pb, c=c: e.copy(out=K3[:, c, :], in_=pb[:, 128:256]), reads=[pb], writes=[(KT, c)])

            p1_proj(0)
            for c in range(NCH):
                if c + 1 < NCH:
                    p1_proj(c + 1)
                p1_post(c)
            qtiles = [(m + 2, per_m[m][0], per_m[m][1]) for m in range(64 if phase >= 2 else 0)]
            if ctx_out:
                qtiles = [(0, None, None), (1, None, None)] + qtiles
            def att_S(n):
                qc, kts, pat = qtiles[n]
                pA = psf[2 * (n % 2)]; pB = psf[2 * (n % 2) + 1]
                if kts is not None:
                    for j, kt in enumerate(kts):
                        dst = pA[:, j * 128:(j + 1) * 128] if j < 4 else pB[:, 0:128]
                        P.op("pe", lambda e, dst=dst, kt=kt, qc=qc: e.matmul(dst, lhsT=K3[:, kt, :], rhs=Q3[:, qc, :], start=True, stop=True),
                             reads=[(KT, kt), (QT, qc)], writes=[pA if j < 4 else pB])
                for j in range(2):
                    P.op("pe", lambda e, pB=pB, j=j, qc=qc: e.matmul(pB[:, 128 + j * 128:256 + j * 128], lhsT=K3[:, j, :], rhs=Q3[:, qc, :], start=True, stop=True),
                         reads=[(KT, j), (QT, qc)], writes=[pB])

            def att_rest(n):
                qc, kts, pat = qtiles[n]
                pA = psf[2 * (n % 2)]; pB = psf[2 * (n % 2) + 1]; pN = psf[4]; pD = psf[5]
                tm = tmp[n % 2]; pt = PT[n % 2]; r_ = rc[n % 2]
                keys = []
                if kts is not None:
                    for j, kt in enumerate(kts):
                        keys.append((kt, j * 128))
                for j in range(2):
                    keys.append((j, 640 + j * 128))
                if kts is not None:
                    P.op("dve", lambda e, pA=pA, tm=tm, pat=pat: e.tensor_tensor(out=tm[:, 0:512], in0=pA[:, :], in1=B3[:, pat, 0:512], op=ALU.add), reads=[pA, Bs], writes=[tm])
                    P.op("dve", lambda e, pB=pB, tm=tm, pat=pat: e.tensor_tensor(out=tm[:, 512:640], in0=pB[:, 0:128], in1=B3[:, pat, 512:640], op=ALU.add), reads=[pB, Bs], writes=[tm])
                    P.op("act", lambda e, tm=tm, pt=pt: e.activation(out=pt[:, 0:640], in_=tm[:, :], func=AF.Exp), reads=[tm], writes=[pt])
                P.op("act", lambda e, pB=pB, pt=pt: e.activation(out=pt[:, 640:896], in_=pB[:, 128:384], func=AF.Exp), reads=[pB], writes=[pt])
                for i, (kt, off) in enumerate(keys):
                    P.op("pe", lambda e, kt=kt, off=off, pt=pt, i=i, nk=len(keys): e.matmul(pN[:, 0:128], lhsT=V3[:, kt, :], rhs=pt[:, off:off + 128], start=(i == 0), stop=(i == nk - 1)),
                         reads=[(VT, kt), pt], writes=[pN])
                for i, (kt, off) in enumerate(keys):
                    P.op("pe", lambda e, off=off, pt=pt, i=i, nk=len(keys): e.matmul(pD[:, 0:128], lhsT=onesb[:], rhs=pt[:, off:off + 128], start=(i == 0), stop=(i == nk - 1)),
                         reads=[onesb, pt], writes=[pD])
                P.op("act", lambda e, r_=r_: e.copy(out=r_[:], in_=pD[:, 0:128]), reads=[pD], writes=[r_])
                P.op("dve", lambda e, r_=r_: e.reciprocal(out=r_[:], in_=r_[:]), reads=[r_], writes=[r_])
                P.op("dve", lambda e, r_=r_, qc=qc: e.tensor_tensor(out=O3[:, qc, :], in0=pN[:, 0:128], in1=r_[:], op=ALU.mult), reads=[pN, r_], writes=[OUT])

            if qtiles:
                att_S(0)
            for n in range(len(qtiles)):
                if n + 1 < len(qtiles):
                    att_S(n + 1)
                att_rest(n)
            if not ctx_out:
                P.op("pool", lambda e: e.memset(OUT[:, 0:256], 0.0), writes=[OUT])
            P.dma(lambda e, h=h: e.dma_start(out=MIXT[:, :, h, :].rearrange("t p x -> p t x"), in_=OUT[:].rearrange("p (t x) -> p t x", x=128)), reads=[OUT], writes=["mixh"])
        P.end_phase()


D = 2048; EPS = 1e-6

def emit_g2(nc, P, pre, io):
    xl = io["xl"]; xc = io["xc"]; mixl = io["mixl"]; mixc = io["mixc"]; wout = io["wout"]; wr = io["wr"]
    modl = io["modl"]; modc = io["modc"]; gv = io["g"]; identf_d = io["identf"]
    xmo_l = io["xmo_l"]; xmo_c = io["xmo_c"]; h2_l = io["h2_l"]; h2_c = io["h2_c"]; aff_l = io["aff_l"]; aff_c = io["aff_c"]
    with contextlib.ExitStack() as st:
        T = lambda name, shape, dt=F32: st.enter_context(nc.sbuf_tensor(pre + name, shape, dt))
        psf = [P.psum(st, pre + "psf%d" % i, [128, 512], F32) for i in range(8)]
        identf = T("identf_s", [128, 128])
        P.dma(lambda e: e.dma_start(out=identf[:], in_=identf_d), writes=[identf])
        W = T("W", [128, 16 * D], BF16); W3 = W[:].rearrange("p (k n) -> p k n", k=16)
        for kc in range(16):
            P.dma(lambda e, kc=kc: e.dma_start(out=W3[:, kc, :], in_=wout[kc * 128:(kc + 1) * 128, :]), eng="pool", writes=[W])
        WR = T("WR", [128, 16 * 16]); WR3 = WR[:].rearrange("p (k n) -> p k n", k=16)
        P.dma(lambda e: e.dma_start(out=WR3, in_=wr.rearrange("(k p) n -> p k n", p=128)), writes=[WR])
        g_bc = T("g_bc", [128, D]); tmpb = T("tmpb", [128, D])
        P.dma(lambda e: e.dma_start(out=g_bc[:], in_=gv.to_broadcast([128, D])), writes=[g_bc])
        GA = T("GA", [128, D]); A = T("A", [128, D]); Bt = T("Bt", [128, D])
        xt = [T("xt%d" % i, [128, D]) for i in range(2)]
        mt = [T("mt%d" % i, [128, D], BF16) for i in range(2)]
        xm = [T("xm%d" % i, [128, D]) for i in range(2)]
        h1 = T("h1", [128, D])
        h2f = T("h2f", [128, D]); h2b = [T("h2b%d" % i, [128, D], BF16) for i in range(2)]
        h2T = T("h2T", [128, D])
        ss = T("ss", [128, 1]); rstd = T("rstd", [128, 1])
        mx = T("mx", [128, 1]); se = T("se", [128, 1]); ex = T("ex", [128, 16]); af = [T("af%d" % i, [128, 16]) for i in range(2)]

        def load_mod(mod):
            P.dma(lambda e: e.dma_start(out=GA[:], in_=mod[2:3, :].to_broadcast([128, D])), writes=[GA])
            P.dma(lambda e: e.dma_start(out=Bt[:], in_=mod[3:4, :].to_broadcast([128, D])), writes=[Bt])
            P.dma(lambda e: e.dma_start(out=tmpb[:], in_=mod[4:5, :].to_broadcast([128, D])), writes=[tmpb])
            P.op("dve", lambda e: e.scalar_tensor_tensor(out=A[:], in0=tmpb[:], scalar=1.0, in1=g_bc[:], op0=ALU.add, op1=ALU.mult),
                 reads=[tmpb, g_bc], writes=[A])

        def tile(it, rows, xsrc, msrc, xdst, hdst, adst):
            x = xt[it % 2]; m = mt[it % 2]; xo = xm[it % 2]; hb = h2b[it % 2]; a_ = af[it % 2]
            P.dma(lambda e: e.dma_start(out=x[:rows, :], in_=xsrc), writes=[x])
            if rows == 128:
                P.dma(lambda e: e.dma_start(out=m[:], in_=msrc), writes=[m])
            else:
                P.dma(lambda e: e.dma_start(out=m[:, 0:16 * rows].rearrange("p (k t) -> p k t", k=16), in_=msrc), writes=[m])
            m3 = m[:, 0:16 * rows].rearrange("p (k t) -> p k t", k=16)
            for nb in range(4):
                pp = psf[nb]
                for kc in range(16):
                    P.op("pe", lambda e, pp=pp, kc=kc, nb=nb: e.matmul(pp[:rows, :], lhsT=m3[:, kc, :], rhs=W3[:, kc, nb * 512:(nb + 1) * 512], start=(kc == 0), stop=(kc == 15)),
                         reads=[m, W], writes=[pp])
            yield "A"
            for nb in range(4):
                pp = psf[nb]
                sl = slice(nb * 512, (nb + 1) * 512)
                P.op("dve", lambda e, pp=pp, sl=sl: e.tensor_tensor(out=h1[:rows, sl], in0=pp[:rows, :], in1=GA[:rows, sl], op=ALU.mult), reads=[pp, GA], writes=[(h1, nb)])
                P.op("pool", lambda e, sl=sl: e.tensor_tensor(out=xo[:rows, sl], in0=x[:rows, sl], in1=h1[:rows, sl], op=ALU.add), reads=[x, (h1, nb)], writes=[(xo, nb)])
            xok = [(xo, nb) for nb in range(4)]; h1k = [(h1, nb) for nb in range(4)]
            P.dma(lambda e: e.dma_start(out=xdst, in_=xo[:rows, :]), reads=xok, writes=["xmo"], final=True)
            P.op("act", lambda e: e.activation(out=h1[:rows, :], in_=xo[:rows, :], func=AF.Square, accum_out=ss[:rows, :]), reads=xok, writes=h1k + [ss])
            P.op("act", lambda e: e.activation(out=rstd[:rows, :], in_=ss[:rows, :], func=AF.Sqrt, scale=1.0 / D, bias=EPS), reads=[ss], writes=[rstd])
            P.op("dve", lambda e: e.reciprocal(out=rstd[:rows, :], in_=rstd[:rows, :]), reads=[rstd], writes=[rstd])
            P.op("dve", lambda e: e.scalar_tensor_tensor(out=h1[:rows, :], in0=xo[:rows, :], scalar=rstd[:rows, 0:1], in1=A[:rows, :], op0=ALU.mult, op1=ALU.mult),
                 reads=xok + [rstd, A], writes=h1k)
            P.op("pool", lambda e: e.tensor_tensor(out=h2f[:rows, :], in0=h1[:rows, :], in1=Bt[:rows, :], op=ALU.add), reads=h1k + [Bt], writes=[h2f])
            P.op("act", lambda e: e.copy(out=hb[:rows, :], in_=h2f[:rows, :]), reads=[h2f], writes=[hb])
            P.dma(lambda e: e.dma_start(out=hdst, in_=hb[:rows, :]), reads=[hb], writes=["h2o"], final=True)
            yield "M"
            h2T3 = h2T[:].rearrange("p (k t) -> p k t", k=16)
            for grp in range(4):
                pp = psf[4 + grp]
                for k in range(4):
                    kc = grp * 4 + k
                    P.op("pe", lambda e, pp=pp, k=k, kc=kc: e.transpose(out=pp[:, k * 128:k * 128 + rows], in_=h2f[:rows, kc * 128:(kc + 1) * 128], identity=identf[:rows, :rows]),
                         reads=[h2f, identf], writes=[pp])
                eng = "act" if grp % 2 == 0 else "dve"
                if rows == 128:
                    if eng == "act":
                        P.op("act", lambda e, pp=pp, grp=grp: e.copy(out=h2T[:, grp * 512:(grp + 1) * 512], in_=pp[:, :]), reads=[pp], writes=[(h2T, grp)])
                    else:
                        P.op("dve", lambda e, pp=pp, grp=grp: e.tensor_copy(out=h2T[:, grp * 512:(grp + 1) * 512], in_=pp[:, :]), reads=[pp], writes=[(h2T, grp)])
                else:
                    src = pp[:, :].rearrange("p (k t) -> p k t", k=4)[:, :, 0:rows]
                    dst = h2T3[:, grp * 4:(grp + 1) * 4, 0:rows]
                    P.op("dve", lambda e, src=src, dst=dst: e.tensor_copy(out=dst, in_=src), reads=[pp], writes=[(h2T, grp)])
            pl = psf[7]
            for kc in range(16):
                P.op("pe", lambda e, kc=kc: e.matmul(pl[:rows, 0:16], lhsT=h2T3[:, kc, 0:rows], rhs=WR3[:, kc, :], start=(kc == 0), stop=(kc == 15)),
                     reads=[(h2T, kc // 4), WR], writes=[pl])
            P.op("dve", lambda e: e.reduce_max(out=mx[:rows, :], in_=pl[:rows, 0:16], axis=AX.X), reads=[pl], writes=[mx])
            P.op("dve", lambda e: e.tensor_scalar(out=mx[:rows, :], in0=mx[:rows, :], scalar1=-1.0, scalar2=None, op0=ALU.mult), reads=[mx], writes=[mx])
            P.op("act", lambda e: e.activation(out=ex[:rows, :], in_=pl[:rows, 0:16], func=AF.Exp, bias=mx[:rows, 0:1], accum_out=se[:rows, :]), reads=[pl, mx], writes=[ex, se])
            P.op("dve", lambda e: e.reciprocal(out=se[:rows, :], in_=se[:rows, :]), reads=[se], writes=[se])
            P.op("dve", lambda e: e.tensor_scalar(out=a_[:rows, :], in0=ex[:rows, :], scalar1=se[:rows, 0:1], scalar2=None, op0=ALU.mult), reads=[ex, se], writes=[a_])
            P.dma(lambda e: e.dma_start(out=adst, in_=a_[:rows, :]), reads=[a_], writes=["affo"], final=True)

        load_mod(modl)
        gens = []
        for it in range(16):
            r = slice(it * 128, (it + 1) * 128)
            gens.append(tile(it, 128, xl[r, :], mixl[it], xmo_l[r, :], h2_l[r, :], aff_l[it]))
        gens.append(tile(16, 64, xc, mixc, xmo_c, h2_c, aff_c))
        next(gens[0])
        for t in range(17):
            next(gens[t])
            if t == 15:
                load_mod(modc)
            if t + 1 < 17:
                next(gens[t + 1])
            for _ in gens[t]:
                pass
        P.end_phase()


D = 2048; FF = 1024
CAPL = 1024; CAPC = 32
BIG = float(1 << 15)
NIT = 28

def emit_moe(nc, P, pre, io, g):
    only_slots = False
    AFF8 = io["AFF8"]; SLOT8 = io["SLOT8"]; capv = io["capv"]; h2l = io["H2L"]; h2c = io["H2C"]
    wg = io["wg"]; wu = io["wu"]; wd = io["wd"]; stri_d = io["stri"]; identb_d = io["identb"]
    xin_l = io["XINL"]; xin_c = io["XINC"]
    with contextlib.ExitStack() as st0:
        psf = [P.psum(st0, pre + "psf%d" % i, [128, 512], F32) for i in range(6)]
        psb = [P.psum(st0, pre + "psb%d" % i, [128, 1024], BF16) for i in range(2)]
        T0 = lambda name, shape, dt=F32: st0.enter_context(nc.sbuf_tensor(pre + name, shape, dt))
        identb = T0("identb_s", [128, 128], BF16)
        P.dma(lambda e: e.dma_start(out=identb[:], in_=identb_d), writes=[identb])
        if True:
            st = st0
            T = lambda name, shape, dt=F32: st.enter_context(nc.sbuf_tensor(pre + name, shape, dt))
            stri = T("stri_s", [128, 128]); ones = T("ones", [128, 128]); zeros = T("zeros", [128, 64])
            A8 = T("A8", [128, 512]); cap = T("cap", [128, 8])
            P.dma(lambda e: e.dma_start(out=stri[:], in_=stri_d), writes=[stri])
            A8raw = T("A8raw", [128, 512])
            Ar3 = A8raw[:].rearrange("p (i l) -> p i l", l=8)
            P.dma(lambda e: e.dma_start(out=Ar3[:, :, 0:4], in_=AFF8[:, :, 4 * g:4 * g + 4]), writes=[A8raw])
            P.dma(lambda e: e.dma_start(out=Ar3[:, :, 4:8], in_=AFF8[:, :, 16 + 4 * g:16 + 4 * g + 4]), writes=[A8raw])
            P.op("dve", lambda e: e.tensor_copy(out=A8[:].rearrange("p (l i) -> p l i", l=8), in_=A8raw[:].rearrange("p (i l) -> p l i", l=8)), reads=[A8raw], writes=[A8])
            P.dma(lambda e: e.dma_start(out=cap[:], in_=capv), writes=[cap])
            P.op("pool", lambda e: e.memset(ones[:], 1.0), writes=[ones])
            P.op("pool", lambda e: e.memset(zeros[:], 0.0), writes=[zeros])
            lo = T("lo", [128, 8]); th = T("th", [128, 8]); cntp = T("cntp", [128, 8]); tt = T("tt", [128, 8])
            cmp_ = T("cmp", [128, 512]); mask = T("mask", [128, 512]); m0 = T("m0", [128, 512])
            cum = T("cum", [128, 512]); s2 = T("s2", [128, 512]); totp = T("totp", [128, 8]); offs = T("offs", [128, 8])
            sloti = T("sloti", [128, 512], I32)
            A3 = A8[:].rearrange("p (l i) -> p l i", l=8)
            c3 = cmp_[:].rearrange("p (l i) -> p l i", l=8)
            pc = psf[0]
            P.op("pool", lambda e: e.memset(lo[:], 0.0), writes=[lo])
            for n in range(NIT):
                dl = 2.0 ** -(n + 1)
                P.op("dve", lambda e, dl=dl: e.tensor_scalar(out=th[:], in0=lo[:], scalar1=dl, scalar2=None, op0=ALU.add), reads=[lo], writes=[th])
                P.op("dve", lambda e: e.tensor_tensor(out=c3, in0=A3, in1=th[:].unsqueeze(2).to_broadcast([128, 8, 64]), op=ALU.is_ge), reads=[A8, th], writes=[cmp_])
                P.op("dve", lambda e: e.reduce_sum(out=cntp[:], in_=c3, axis=AX.X), reads=[cmp_], writes=[cntp])
                P.op("pe", lambda e: e.matmul(pc[:, 0:8], lhsT=ones[:], rhs=cntp[:], start=True, stop=True), reads=[ones, cntp], writes=[pc])
                P.op("dve", lambda e: e.tensor_tensor(out=tt[:], in0=pc[:, 0:8], in1=cap[:], op=ALU.is_ge), reads=[pc, cap], writes=[tt])
                P.op("dve", lambda e, dl=dl: e.scalar_tensor_tensor(out=lo[:], in0=tt[:], scalar=dl, in1=lo[:], op0=ALU.mult, op1=ALU.add), reads=[tt, lo], writes=[lo])
            m3 = mask[:].rearrange("p (l i) -> p l i", l=8)
            P.op("dve", lambda e: e.tensor_tensor(out=m3, in0=A3, in1=lo[:].unsqueeze(2).to_broadcast([128, 8, 64]), op=ALU.is_ge), reads=[A8, lo], writes=[mask])
            P.op("dve", lambda e: e.tensor_scalar(out=m0[:], in0=A8[:], scalar1=0.0, scalar2=None, op0=ALU.is_gt), reads=[A8], writes=[m0])
            P.op("dve", lambda e: e.tensor_tensor(out=mask[:], in0=mask[:], in1=m0[:], op=ALU.mult), reads=[mask, m0], writes=[mask])
            cu3 = cum[:].rearrange("p (l i) -> p l i", l=8)
            for l in range(8):
                P.op("dve", lambda e, l=l: e.tensor_tensor_scan(out=cum[:, l * 64:(l + 1) * 64], data0=mask[:, l * 64:(l + 1) * 64], data1=zeros[:, 0:64], initial=0.0, op0=ALU.add, op1=ALU.add),
                     reads=[mask, zeros], writes=[cum])
            P.op("dve", lambda e: e.tensor_copy(out=totp[:], in_=cu3[:, :, 63]), reads=[cum], writes=[totp])
            P.op("pe", lambda e: e.matmul(pc[:, 0:8], lhsT=stri[:], rhs=totp[:], start=True, stop=True), reads=[stri, totp], writes=[pc])
            P.op("act", lambda e: e.copy(out=offs[:], in_=pc[:, 0:8]), reads=[pc], writes=[offs])
            P.op("dve", lambda e: e.tensor_scalar(out=s2[:], in0=mask[:], scalar1=-BIG, scalar2=BIG - 1.0, op0=ALU.mult, op1=ALU.add), reads=[mask], writes=[s2])
            P.op("dve", lambda e: e.tensor_tensor(out=cu3, in0=cu3, in1=offs[:].unsqueeze(2).to_broadcast([128, 8, 64]), op=ALU.add), reads=[cum, offs], writes=[cum])
            P.op("dve", lambda e: e.tensor_tensor(out=cum[:], in0=cum[:], in1=s2[:], op=ALU.add), reads=[cum, s2], writes=[cum])
            P.op("dve", lambda e: e.tensor_copy(out=sloti[:], in_=cum[:]), reads=[cum], writes=[sloti])
            Sraw = T("Sraw", [128, 512], I32)
            P.op("dve", lambda e: e.tensor_copy(out=Sraw[:].rearrange("p (i l) -> p l i", l=8), in_=sloti[:].rearrange("p (l i) -> p l i", l=8)), reads=[sloti], writes=[Sraw])
            Sr3 = Sraw[:].rearrange("p (i l) -> p i l", l=8)
            P.dma(lambda e: e.dma_start(out=SLOT8[:, :, 4 * g:4 * g + 4], in_=Sr3[:, :, 0:4]), reads=[Sraw], writes=["slot8"])
            P.dma(lambda e: e.dma_start(out=SLOT8[:, :, 16 + 4 * g:16 + 4 * g + 4], in_=Sr3[:, :, 4:8]), reads=[Sraw], writes=["slot8"])
            s3 = sloti[:].rearrange("p (l i) -> p l i", l=8)
            R = [T("R%d" % i, [128, D], BF16) for i in range(3)]
            nR = 0
            hv = h2l.rearrange("(i p) d -> p i d", p=128)
            for i in range(64):
                r = R[nR % 3]; nR += 1
                P.dma(lambda e, r=r, i=i: e.dma_start(out=r[:], in_=hv[:, i, :]), writes=[r])
                for lane in range(4):
                    P.dma(lambda e, r=r, lane=lane, i=i: e.indirect_dma_start(out=xin_l[lane][:, :], out_offset=bass.IndirectOffsetOnAxis(ap=s3[:, lane, i:i + 1], axis=0),
                                                                             in_=r[:, :], in_offset=None, bounds_check=P.getreg(e, CAPL - 1), oob_is_err=False),
                          eng="pool", reads=[r, sloti], writes=["xin_l"])
            hv2 = h2c.rearrange("(i p) d -> p i d", p=128)
            for i in range(2):
                r = R[nR % 3]; nR += 1
                P.dma(lambda e, r=r, i=i: e.dma_start(out=r[:], in_=hv2[:, i, :]), writes=[r])
                for lane in range(4, 8):
                    P.dma(lambda e, r=r, lane=lane, i=i: e.indirect_dma_start(out=xin_c[lane - 4][:, :], out_offset=bass.IndirectOffsetOnAxis(ap=s3[:, lane, i:i + 1], axis=0),
                                                                             in_=r[:, :], in_offset=None, bounds_check=P.getreg(e, CAPC - 1), oob_is_err=False),
                          eng="pool", reads=[r, sloti], writes=["xin_c"])
            P.barrier()
        if True:
            st = st0
            T = lambda name, shape, dt=F32: st.enter_context(nc.sbuf_tensor(pre + name, shape, dt))
            Wg = T("Wg", [128, 16 * FF], BF16); Wu = T("Wu", [128, 16 * FF], BF16); Wd = T("Wd", [128, 8 * D], BF16)
            Wg3 = Wg[:].rearrange("p (k n) -> p k n", k=16); Wu3 = Wu[:].rearrange("p (k n) -> p k n", k=16); Wd3 = Wd[:].rearrange("p (k n) -> p k n", k=8)
            XT = T("XT", [128, 16 * 512], BF16); XT3 = XT[:].rearrange("p (k s) -> p k s", k=16)
            ATa = T("ATa", [128, 8 * 512], BF16); ATb = T("ATb", [128, 8 * 512], BF16); ATc = T("ATc", [128, 8 * 32], BF16)
            xs = [T("xs%d" % i, [128, D], BF16) for i in range(2)]
            ysb = [T("ysb%d" % i, [128, D]) for i in range(2)]
            sa = [T("sa%d" % i, [128, 512]) for i in range(2)]
            cnt = {"x": 0, "y": 0, "s": 0}

            def ffn_gu(xsrc, ns, AT):
                AT3 = AT[:].rearrange("p (k s) -> p k s", k=8)
                tiles = [(s, min(128, ns - s)) for s in range(0, ns, 128)]
                for (s0, rows) in tiles:
                    x = xs[cnt["x"] % 2]; cnt["x"] += 1
                    P.dma(lambda e, x=x, s0=s0, rows=rows: e.dma_start(out=x[:rows, :], in_=xsrc[s0:s0 + rows, :]), reads=["xin_l", "xin_c"], writes=[x])
                    for half in range(2):
                        pb = psb[half]
                        for k in range(8):
                            kc = half * 8 + k
                            P.op("pe", lambda e, pb=pb, x=x, k=k, kc=kc, rows=rows: e.transpose(out=pb[:, k * 128:k * 128 + rows], in_=x[:rows, kc * 128:(kc + 1) * 128], identity=identb[:rows, :rows]),
                                 reads=[x, identb], writes=[pb])
                        src = pb[:, :].rearrange("p (k t) -> p k t", k=8)[:, :, 0:rows]
                        dst = XT3[:, half * 8:(half + 1) * 8, s0:s0 + rows]
                        if half == 0:
                            P.op("act", lambda e, src=src, dst=dst: e.copy(out=dst, in_=src), reads=[pb], writes=[XT])
                        else:
                            P.op("dve", lambda e, src=src, dst=dst: e.tensor_copy(out=dst, in_=src), reads=[pb], writes=[XT])
                for ft in range(8):
                    pa = psf[ft % 2]; pu = psf[2 + ft % 2]; s_ = sa[ft % 2]
                    for kc in range(16):
                        P.op("pe", lambda e, pa=pa, kc=kc, ft=ft: e.matmul(pa[:, 0:ns], lhsT=Wg3[:, kc, ft * 128:(ft + 1) * 128], rhs=XT3[:, kc, 0:ns], start=(kc == 0), stop=(kc == 15)),
                             reads=[Wg, XT], writes=[pa])
                    for kc in range(16):
                        P.op("pe", lambda e, pu=pu, kc=kc, ft=ft: e.matmul(pu[:, 0:ns], lhsT=Wu3[:, kc, ft * 128:(ft + 1) * 128], rhs=XT3[:, kc, 0:ns], start=(kc == 0), stop=(kc == 15)),
                             reads=[Wu, XT], writes=[pu])
                    P.op("act", lambda e, pa=pa, s_=s_: e.activation(out=s_[:, 0:ns], in_=pa[:, 0:ns], func=AF.Silu), reads=[pa], writes=[s_])
                    P.op("dve", lambda e, pu=pu, s_=s_, ft=ft: e.tensor_tensor(out=AT3[:, ft, 0:ns], in0=s_[:, 0:ns], in1=pu[:, 0:ns], op=ALU.mult), reads=[s_, pu], writes=[(AT, ft)])

            def ffn_down(ydst, ns, AT):
                AT3 = AT[:].rearrange("p (k s) -> p k s", k=8)
                tiles = [(s, min(128, ns - s)) for s in range(0, ns, 128)]
                ATk = [(AT, ft) for ft in range(8)]
                for (s0, rows) in tiles:
                    y = ysb[cnt["y"] % 2]; cnt["y"] += 1
                    for nb in range(4):
                        pd = psf[4 + nb % 2]
                        for fc in range(8):
                            P.op("pe", lambda e, pd=pd, fc=fc, nb=nb, s0=s0, rows=rows: e.matmul(pd[:rows, :], lhsT=AT3[:, fc, s0:s0 + rows], rhs=Wd3[:, fc, nb * 512:(nb + 1) * 512], start=(fc == 0), stop=(fc == 7)),
                                 reads=ATk + [Wd], writes=[pd])
                        if nb % 2 == 0:
                            P.op("act", lambda e, pd=pd, y=y, nb=nb, rows=rows: e.copy(out=y[:rows, nb * 512:(nb + 1) * 512], in_=pd[:rows, :]), reads=[pd], writes=[(y, nb)])
                        else:
                            P.op("dve", lambda e, pd=pd, y=y, nb=nb, rows=rows: e.tensor_copy(out=y[:rows, nb * 512:(nb + 1) * 512], in_=pd[:rows, :]), reads=[pd], writes=[(y, nb)])
                    P.dma(lambda e, y=y, s0=s0, rows=rows: e.dma_start(out=ydst[s0:s0 + rows, :], in_=y[:rows, :]), reads=[(y, nb) for nb in range(4)], writes=["yout"], final=True)

            for el in range(4):
                ex = 4 * g + el
                for kc in range(16):
                    P.dma(lambda e, kc=kc, ex=ex: e.dma_start(out=Wg3[:, kc, :], in_=wg[ex, kc * 128:(kc + 1) * 128, :]), eng="pool", writes=[Wg])
                    P.dma(lambda e, kc=kc, ex=ex: e.dma_start(out=Wu3[:, kc, :], in_=wu[ex, kc * 128:(kc + 1) * 128, :]), eng="pool", writes=[Wu])
                for kc in range(8):
                    P.dma(lambda e, kc=kc, ex=ex: e.dma_start(out=Wd3[:, kc, :], in_=wd[ex, kc * 128:(kc + 1) * 128, :]), eng="pool", writes=[Wd])
                ffn_gu(xin_l[el][0:512, :], 512, ATa)
                ffn_gu(xin_l[el][512:1024, :], 512, ATb)
                ffn_gu(xin_c[el], CAPC, ATc)
                ffn_down(io["YL"][ex][0:512, :], 512, ATa)
                ffn_down(io["YL"][ex][512:1024, :], 512, ATb)
                ffn_down(io["YC"][ex], CAPC, ATc)
            P.end_phase()


BF = ml_dtypes.bfloat16
D = 2048; FF = 1024; N = 8192; NCTX = 256

def emit_k0(nc, P, pre, io):
    cT = io["cT"]; w = io["ada_w"]; b = io["ada_b"]; MOD = io["MOD"]
    with contextlib.ExitStack() as st:
        T = lambda name, shape, dt=F32: st.enter_context(nc.sbuf_tensor(pre + name, shape, dt))
        ps = [P.psum(st, pre + "ps%d" % i, [128, 512], F32) for i in range(2)]
        cs = T("cs", [128, 32]); ss = T("ss", [128, 32])
        wb = [T("wb%d" % i, [128, 16 * 512]) for i in range(3)]
        bt = [T("bt%d" % i, [2, 512]) for i in range(2)]; ot = [T("ot%d" % i, [2, 512]) for i in range(2)]
        zt = T("zt", [128, 2048])
        P.op("pool", lambda e: e.memset(zt[:], 0.0), writes=[zt])
        P.dma(lambda e: e.dma_start(out=io["AFF8"].rearrange("p i l -> p (i l)"), in_=zt[:]), reads=[zt], writes=["aff8"])
        P.dma(lambda e: e.dma_start(out=cs[:], in_=cT), writes=[cs])
        P.op("act", lambda e: e.activation(out=ss[:], in_=cs[:], func=AF.Silu), reads=[cs], writes=[ss])
        blk = 0
        for i in range(4):
            wv = w[i].rearrange("(kc p) n -> p kc n", p=128)
            for j in range(24):
                wt = wb[blk % 3]; pt = ps[blk % 2]; btj = bt[blk % 2]; otj = ot[blk % 2]
                c0 = j * 512
                P.dma(lambda e, wt=wt, wv=wv, c0=c0: e.dma_start(out=wt[:].rearrange("p (kc n) -> p kc n", kc=16), in_=wv[:, :, c0:c0 + 512]), writes=[wt])
                P.dma(lambda e, btj=btj, i=i, c0=c0: e.dma_start(out=btj[:], in_=b[i:i + 1, c0:c0 + 512].to_broadcast([2, 512])), writes=[btj])
                for kc in range(16):
                    P.op("pe", lambda e, pt=pt, wt=wt, kc=kc: e.matmul(pt[0:2, :], lhsT=ss[:, kc * 2:(kc + 1) * 2], rhs=wt[:, kc * 512:(kc + 1) * 512], start=(kc == 0), stop=(kc == 15)),
                         reads=[ss, wt], writes=[pt])
                P.op("dve", lambda e, pt=pt, btj=btj, otj=otj: e.tensor_tensor(out=otj[:], in0=pt[0:2, :], in1=btj[:], op=ALU.add), reads=[pt, btj], writes=[otj])
                P.dma(lambda e, otj=otj, i=i, c0=c0: e.dma_start(out=MOD[i, :, c0:c0 + 512], in_=otj[:]), reads=[otj], writes=["mod"])
                blk += 1
        P.end_phase()

def build_fused(nlayers=4):
    nc = bass.Bass("TRN2", target_bir_lowering=False)
    DI = lambda n, s, dt=F32: nc.dram_tensor(n, s, dt, kind="ExternalInput").ap()
    SC = lambda n, s, dt=F32: nc.dram_tensor(n, s, dt, kind="Internal").ap()
    x = DI("x", [N, D]); ctx = DI("ctx", [NCTX, D]); cT = DI("cT", [128, 32])
    ada_w = DI("ada_w", [4, D, 12288]); ada_b = DI("ada_b", [4, 12288]); norm_g = DI("norm_g", [4, 2, D]); final_g = DI("final_g", [1, D])
    ev_w_in = DI("ev_w_in", [2, D, 5152]); ev_gate_b = DI("ev_gate_b", [2, 4, 8]); ev_head_g = DI("ev_head_g", [2, 1024])
    ev_pool_w = DI("ev_pool_w", [2, 4, 256, 256]); ev_pool_scale = DI("ev_pool_scale", [2, 1024]); ev_w_out = DI("ev_w_out", [2, D, D])
    na_w_in = DI("na_w_in", [2, D, 6144]); na_bias = DI("na_bias", [2, 16, 128, 3200]); na_w_out = DI("na_w_out", [2, D, D])
    w_router = DI("moe_w_router", [4, D, 16]); w_gate = DI("moe_w_gate", [4, 16, D, FF]); w_up = DI("moe_w_up", [4, 16, D, FF]); w_down = DI("moe_w_down", [4, 16, FF, D])
    identb = DI("identb", [128, 128], BF16); identf = DI("identf", [128, 128]); tri = DI("tri", [2, 128, 128]); stri = DI("stri", [128, 128])
    tab = DI("tab", [66, 128, 256]); icl = DI("icl", [4, N]); icc = DI("icc", [4, NCTX]); capv = DI("capv", [128, 8])
    out = nc.dram_tensor("out", [N, D], F32, kind="ExternalOutput").ap()
    MOD = SC("MOD", [4, 2, 12288]); XL = SC("XL", [N, D]); XC = SC("XC", [NCTX, D])
    HT = SC("HT", [66, 128, 2048], BF16); MIXT = SC("MIXT", [66, 128, 16, 128], BF16)
    H2L = SC("H2L", [N, D], BF16); H2C = SC("H2C", [NCTX, D], BF16)
    AFF8 = SC("AFF8", [128, 64, 32]); SLOT8 = SC("SLOT8", [128, 64, 32], I32)
    YL = [SC("YL%d" % i, [1024, D]) for i in range(16)]; YC = [SC("YC%d" % i, [32, D]) for i in range(16)]
    XINL = [SC("XINL%d" % i, [1024, D], BF16) for i in range(4)]; XINC = [SC("XINC%d" % i, [32, D], BF16) for i in range(4)]
    P = Prog(nc)
    emit_k0(nc, P, "k0_", {"cT": cT, "ada_w": ada_w, "ada_b": ada_b, "MOD": MOD, "AFF8": AFF8})
    mrows = lambda i, r: MOD[i, r].rearrange("(s d) -> s d", s=6)

    def g1_io(i, q, comb, final):
        src_l, src_c = (x, ctx) if (i == 0 and not comb) else (XL, XC)
        io = {"xl": src_l[q * 2048:(q + 1) * 2048, :], "xc": src_c[q * 64:(q + 1) * 64, :], "ident": identb}
        if final:
            io["g"] = final_g; io["outl"] = out[q * 2048:(q + 1) * 2048, :]
        else:
            io["g"] = norm_g[i, 0:1, :]; io["modl"] = mrows(i, 0); io["modc"] = mrows(i, 1)
            io["hTl"] = [HT[2 + q * 16 + it] for it in range(16)]
            io["hTc"] = HT[q // 2].rearrange("p (k x) -> p k x", k=16)[:, :, (q % 2) * 64:(q % 2 + 1) * 64]
        if comb:
            ip = i - 1
            io.update({"yl": YL, "yc": YC, "slotl": [SLOT8[:, q * 16 + it, 0:16] for it in range(16)], "affl": [AFF8[:, q * 16 + it, 0:16] for it in range(16)],
                       "slotc": SLOT8[(q % 2) * 64:(q % 2 + 1) * 64, q // 2, 16:32], "affc": AFF8[(q % 2) * 64:(q % 2 + 1) * 64, q // 2, 16:32],
                       "gafl": mrows(ip, 0)[5:6, :], "gafc": mrows(ip, 1)[5:6, :], "xlo": XL[q * 2048:(q + 1) * 2048, :], "xco": XC[q * 64:(q + 1) * 64, :]})
        return io

    for i in range(nlayers):
        j = i // 2
        for q in range(4):
            emit_g1(nc, P, "L%dg1q%d_" % (i, q), g1_io(i, q, i > 0, False), i > 0, False)
        if i % 2 == 0:
            for hp in range(4):
                emit_mixe(nc, P, "L%dmxh%d_" % (i, hp), {"HT": HT, "MIXT": MIXT, "w_in": ev_w_in[j], "gate_b": ev_gate_b[j], "head_g": ev_head_g[j:j + 1, :],
                                                         "tab": tab, "identb": identb, "identf": identf, "tri": tri, "pool_w": ev_pool_w[j],
                                                         "pool_scale": ev_pool_scale[j], "icl": icl, "icc": icc}, hp)
            wout = ev_w_out[j]
        else:
            for hq in range(4):
                emit_mixo(nc, P, "L%dmxh%d_" % (i, hq), {"HT": HT, "MIXT": MIXT, "w_in": na_w_in[j], "bias": na_bias[j], "identb": identb}, hq, i != 3)
            wout = na_w_out[j]
        for q in range(4):
            src_l, src_c = (x, ctx) if i == 0 else (XL, XC)
            emit_g2(nc, P, "L%dg2q%d_" % (i, q), {
                "xl": src_l[q * 2048:(q + 1) * 2048, :], "xc": src_c[q * 64:(q + 1) * 64, :],
                "mixl": [MIXT[2 + q * 16 + it].rearrange("p k x -> p (k x)") for it in range(16)],
                "mixc": MIXT[q // 2][:, :, (q % 2) * 64:(q % 2 + 1) * 64], "wout": wout, "wr": w_router[i],
                "modl": mrows(i, 0), "modc": mrows(i, 1), "g": norm_g[i, 1:2, :], "identf": identf,
                "xmo_l": XL[q * 2048:(q + 1) * 2048, :], "xmo_c": XC[q * 64:(q + 1) * 64, :],
                "h2_l": H2L[q * 2048:(q + 1) * 2048, :], "h2_c": H2C[q * 64:(q + 1) * 64, :],
                "aff_l": [AFF8[:, q * 16 + it, 0:16] for it in range(16)], "aff_c": AFF8[(q % 2) * 64:(q % 2 + 1) * 64, q // 2, 16:32]})
        for g in range(4):
            emit_moe(nc, P, "L%dmoe%d_" % (i, g), {"AFF8": AFF8, "SLOT8": SLOT8, "capv": capv, "H2L": H2L, "H2C": H2C, "wg": w_gate[i], "wu": w_up[i], "wd": w_down[i],
                                                    "stri": stri, "identb": identb, "YL": YL, "YC": YC, "XINL": XINL, "XINC": XINC}, g)
    for q in range(4):
        emit_g1(nc, P, "Fg1q%d_" % q, g1_io(nlayers, q, True, True), True, True)
    P.flush(True)
    return nc

def host_consts(na_rpb):
    c = {}
    c["identb"] = np.eye(128, dtype=BF); c["identf"] = np.eye(128, dtype=np.float32)
    l = np.arange(128)
    c["tri"] = np.stack([(l[:, None] <= l[None, :]), (l[:, None] >= l[None, :])]).astype(np.float32)
    c["stri"] = (l[:, None] < l[None, :]).astype(np.float32)
    capv = np.zeros((128, 8), np.float32); capv[:, :4] = 1024; capv[:, 4:] = 32
    c["capv"] = capv
    tab = np.zeros((66, 128, 2, 2, 2, 32), np.float32); sc = 128.0 ** -0.5
    tab[0:2, :, 0, 0] = 1.0; tab[0:2, :, 0, 1] = sc
    inv = (10000.0 ** (-np.arange(32, dtype=np.float32) / 32)).astype(np.float32)
    t = np.arange(N); rows = (t // 64).astype(np.float32); cols = (t % 64).astype(np.float32)
    for blk, pos in ((0, rows), (1, cols)):
        ang = (pos[:, None] * inv[None, :]).astype(np.float32)
        cs = np.cos(ang).astype(np.float32).reshape(64, 128, 32); sn = np.sin(ang).astype(np.float32).reshape(64, 128, 32)
        tab[2:, :, 0, 0, blk] = cs; tab[2:, :, 1, 0, blk] = sn; tab[2:, :, 0, 1, blk] = cs * sc; tab[2:, :, 1, 1, blk] = sn * sc
    c["tab"] = tab.reshape(66, 128, 256)
    def inv_cnt(n):
        t = np.arange(n); o = np.zeros((4, n), np.float32)
        for g, w in enumerate((2, 4, 8, 16)):
            lo = np.clip(t - w // 2, 0, n); hi = np.clip(t + w // 2, 0, n); o[g] = 1.0 / (hi - lo)
        return o
    c["icl"] = inv_cnt(N); c["icc"] = inv_cnt(NCTX)
    per_m, idxmaps = na_patterns()
    nb = np.zeros((2, 16, 128, 5, 5, 128), np.float32)
    for j in range(2):
        rf = na_rpb[j].reshape(16, 15 * 31)
        for h in range(16):
            g = np.where(idxmaps >= 0, rf[h][np.maximum(idxmaps, 0)], np.float32(-30000.0))
            nb[j, h] = g.transpose(2, 0, 1, 3)
    c["na_bias"] = nb.reshape(2, 16, 128, 3200)
    return c


def kernel(x, c, ctx, c_ctx, ada_w, ada_b, norm_g, final_g, ev_w_in, ev_gate_b, ev_head_g, ev_pool_w,
           ev_pool_scale, ev_w_out, na_w_in, na_rpb, na_w_out, moe_w_router, moe_w_gate, moe_w_up, moe_w_down):
    A = lambda a: np.ascontiguousarray(np.asarray(a, dtype=np.float32))
    x = A(x); ctx = A(ctx); c = A(c); c_ctx = A(c_ctx)
    shared = {"ada_w": A(ada_w), "ada_b": A(ada_b), "norm_g": A(norm_g), "final_g": A(final_g)[None, :],
              "ev_w_in": A(ev_w_in), "ev_gate_b": A(ev_gate_b), "ev_head_g": A(ev_head_g), "ev_pool_w": A(ev_pool_w),
              "ev_pool_scale": A(ev_pool_scale), "ev_w_out": A(ev_w_out), "na_w_in": A(na_w_in), "na_w_out": A(na_w_out),
              "moe_w_router": A(moe_w_router), "moe_w_gate": A(moe_w_gate), "moe_w_up": A(moe_w_up), "moe_w_down": A(moe_w_down)}
    shared.update(host_consts(A(na_rpb)))
    nc = build_fused(4)
    in_maps = []
    for b in range(2):
        cv = np.stack([c[b], c_ctx])
        m = {"x": x[b], "ctx": ctx[b], "cT": np.ascontiguousarray(cv.reshape(2, 16, 128).transpose(2, 1, 0)).reshape(128, 32)}
        m.update(shared)
        in_maps.append(m)
    res = run_bass_kernel_spmd(nc, in_maps, core_ids=[0, 1])
    return np.stack([res.results[0]["out"], res.results[1]["out"]]).astype(np.float32)
```

```python
import contextlib, time
import numpy as np
import ml_dtypes


import numpy as np
import concourse.bass as bass
import concourse.mybir as mybir
from concourse.bass_utils import run_bass_kernel_spmd

F32 = mybir.dt.float32
BF16 = mybir.dt.bfloat16
I32 = mybir.dt.int32
U32 = mybir.dt.uint32
AF = mybir.ActivationFunctionType
ALU = mybir.AluOpType
AX = mybir.AxisListType

ENGS = ("pe", "act", "dve", "pool", "sp")
DMA_Q = {"sp": 16, "pool": 8, "act": 4}
DMA_SEMS = [(q, i) for q, n in DMA_Q.items() for i in range(n)]


class Prog:
    def __init__(self, nc, same_engine_sync=True):
        self.nc = nc
        self.ops = {e: [] for e in ENGS}
        self.cnt = {e: 0 for e in ENGS}
        self.last_write = {}
        self.reads = {}
        self.waited = {e: {} for e in ENGS}
        self.dma_uses = {k: 0 for k in DMA_SEMS}
        self.dma_rr = {q: 0 for q in DMA_Q}
        self.same_engine_sync = same_engine_sync
        self.final_tokens = []
        self.n_ops = 0
        self.psum_names = set()
        import contextlib
        self._st = contextlib.ExitStack()
        self.sems = {}
        for e in ENGS:
            self.sems[("eng", e)] = self._st.enter_context(nc.semaphore("sem_" + e))
        for i in DMA_SEMS:
            self.sems[("dma", i)] = self._st.enter_context(nc.semaphore("sem_dma_%s%d" % i))

    def psum(self, st, name, shape, dt):
        self.psum_names.add(name)
        return st.enter_context(self.nc.psum_tensor(name, shape, dt))

    @staticmethod
    def _key(x):
        if isinstance(x, tuple):
            return (Prog._key(x[0]), x[1])
        if isinstance(x, str):
            return x
        if hasattr(x, 'tensor'):
            return x.tensor.name
        return x.name

    def _deps(self, eng, reads, writes):
        toks = []
        for k in list(reads) + list(writes):
            t = self.last_write.get(k)
            if t is not None:
                toks.append(t)
        for k in writes:
            toks.extend(self.reads.get(k, ()))
        for k in reads:
            kn = k[0] if isinstance(k, tuple) else k
            if isinstance(kn, str) and (kn.startswith("ps") or kn in self.psum_names):
                for t in self.reads.get(k, ()):
                    if t[0] != ("eng", eng):
                        toks.append(t)
        best = {}
        for (s, v) in toks:
            if best.get(s, -1) < v:
                best[s] = v
        out = []
        w = self.waited[eng]
        for s, v in best.items():
            if (not self.same_engine_sync) and s == ("eng", eng) and eng != "sp":
                continue
            if eng == "pe" and s == ("eng", "pe"):
                continue
            if w.get(s, -1) >= v:
                continue
            w[s] = v
            out.append((s, v))
        return out

    def _commit(self, tok, reads, writes):
        for k in writes:
            self.last_write[k] = tok
            self.reads[k] = []
        for k in reads:
            self.reads.setdefault(k, []).append(tok)

    def op(self, eng, fn, reads=(), writes=()):
        reads = [self._key(r) for r in reads]
        writes = [self._key(r) for r in writes]
        waits = self._deps(eng, reads, writes)
        self.cnt[eng] += 1
        tok = (("eng", eng), self.cnt[eng])
        self.ops[eng].append((waits, fn, tok))
        self._commit(tok, reads, writes)
        self.n_ops += 1
        return tok

    def dma(self, fn, reads=(), writes=(), eng="sp", final=False):
        reads = [self._key(r) for r in reads]
        writes = [self._key(r) for r in writes]
        i = (eng, self.dma_rr[eng])
        self.dma_rr[eng] = (self.dma_rr[eng] + 1) % DMA_Q[eng]
        s = ("dma", i)
        waits = self._deps(eng, reads, writes)
        prev = self.dma_uses[i] * 16
        if prev > 0 and self.waited[eng].get(s, -1) < prev:
            self.waited[eng][s] = prev
            waits.append((s, prev))
        self.dma_uses[i] += 1
        tok = (s, self.dma_uses[i] * 16)
        self.ops[eng].append((waits, fn, tok))
        self._commit(tok, reads, writes)
        if final:
            self.final_tokens.append(tok)
        self.n_ops += 1
        return tok

    def getreg(self, engh, val):
        if not hasattr(self, "_regs"):
            self._regs = {}
        k = (type(engh).__name__, val)
        if k not in self._regs:
            self._regs[k] = engh.to_reg(val)
        return self._regs[k]

    def barrier(self):
        toks = []
        for e in ENGS:
            if e != "sp" and self.cnt[e] > 0:
                toks.append((("eng", e), self.cnt[e]))
        for i in DMA_SEMS:
            if self.dma_uses[i] > 0:
                toks.append((("dma", i), self.dma_uses[i] * 16))
        for e in ENGS:
            waits = []
            for (s, v) in toks:
                if s == ("eng", e):
                    continue
                if self.waited[e].get(s, -1) < v:
                    self.waited[e][s] = v
                    waits.append((s, v))
            if waits:
                self.ops[e].append((waits, None, None))

    def flush(self, last=False):
        nc = self.nc
        sems = self.sems
        handles = {"pe": "tensor", "act": "scalar", "dve": "vector", "pool": "gpsimd", "sp": "sync"}
        with nc.Block() as block:
            def make(e):
                def body(engh):
                    for (waits, fn, tok) in self.ops[e]:
                        for (s, v) in waits:
                            engh.wait_ge(sems[s], v)
                        if fn is None:
                            continue
                        inst = fn(engh)
                        inst.then_inc(sems[tok[0]], 16 if tok[0][0] == "dma" else 1)
                    if e == "sp" and last:
                        for (s, v) in self.final_tokens:
                            engh.wait_ge(sems[s], v)
                return body
            for e in ENGS:
                getattr(block, handles[e])(make(e))
        self.ops = {e: [] for e in ENGS}

    def end_phase(self, last=False):
        self.barrier()
        self.flush(last)

    def emit(self):
        self.flush(True)


D = 2048; EPS = 1e-6
CAP_L = 1024; CAP_C = 32

def emit_g1(nc, P, pre, io, combine, final):
    xl = io["xl"]; xc = io["xc"]; gv = io["g"]; ident_d = io["ident"]
    if not final:
        modl = io["modl"]; modc = io["modc"]; hTl = io["hTl"]; hTc = io["hTc"]
    else:
        outl = io["outl"]
    if combine:
        yl = io["yl"]; yc = io["yc"]; slotl = io["slotl"]; slotc = io["slotc"]; affl = io["affl"]; affc = io["affc"]
        gafl = io["gafl"]; gafc = io["gafc"]; xlo = io["xlo"]; xco = io["xco"]
    with contextlib.ExitStack() as st:
        T = lambda name, shape, dt=F32: st.enter_context(nc.sbuf_tensor(pre + name, shape, dt))
        ident = T("ident_s", [128, 128], BF16)
        P.dma(lambda e: e.dma_start(out=ident[:], in_=ident_d), writes=[ident])
        g_bc = T("g_bc", [128, D])
        P.dma(lambda e: e.dma_start(out=g_bc[:], in_=gv.to_broadcast([128, D])), writes=[g_bc])
        AB = {}
        if not final:
            tmp = T("tmpbc", [128, D])
            for nm, mod in (("l", modl), ("c", modc)):
                A = T("A_" + nm, [128, D]); Bt = T("B_" + nm, [128, D])
                P.dma(lambda e, mod=mod: e.dma_start(out=tmp[:], in_=mod[1:2, :].to_broadcast([128, D])), writes=[tmp])
                P.dma(lambda e, mod=mod, Bt=Bt: e.dma_start(out=Bt[:], in_=mod[0:1, :].to_broadcast([128, D])), writes=[Bt])
                P.op("dve", lambda e, A=A: e.scalar_tensor_tensor(out=A[:], in0=tmp[:], scalar=1.0, in1=g_bc[:], op0=ALU.add, op1=ALU.mult),
                     reads=[tmp, g_bc], writes=[A])
                AB[nm] = (A, Bt)
        if combine:
            gaf = {}
            for nm, src in (("l", gafl), ("c", gafc)):
                t = T("gaf_" + nm, [128, D])
                P.dma(lambda e, t=t, src=src: e.dma_start(out=t[:], in_=src.to_broadcast([128, D])), writes=[t])
                gaf[nm] = t
            yg = [T("yg%d" % i, [128, D]) for i in range(2)]
            for t in yg:
                P.op("pool", lambda e, t=t: e.memset(t[:], 0.0), writes=[t])
            acc = T("acc", [128, D])
            slot_t = [T("slot%d" % i, [128, 16], I32) for i in range(2)]
            slot_f = T("slotf", [128, 16]); aff_t = [T("afft%d" % i, [128, 16]) for i in range(2)]
            ge = T("ge", [128, 16])
        xt = [T("xt%d" % i, [128, D]) for i in range(2)]
        h1 = T("h1", [128, D])
        ss = T("ss", [128, 1]); rstd = T("rstd", [128, 1])
        if not final:
            hb = [T("hb%d" % i, [128, D], BF16) for i in range(2)]
            stg = [T("stg%d" % i, [128, D], BF16) for i in range(2)]
            pst = [P.psum(st, pre + "pst%d" % i, [128, 1024], BF16) for i in range(4)]
        else:
            ob = [T("ob%d" % i, [128, D]) for i in range(2)]

        for it in range(17):
            isl = it < 16
            rows = 128 if isl else 64
            nm = "l" if isl else "c"
            x = xt[it % 2]
            src = xl[it * 128:(it + 1) * 128, :] if isl else xc
            P.dma(lambda e, x=x, src=src, rows=rows: e.dma_start(out=x[:rows, :], in_=src), writes=[x])
            if combine:
                stt = slot_t[it % 2]; aft = aff_t[it % 2]
                if isl:
                    P.dma(lambda e, stt=stt, s=slotl[it]: e.dma_start(out=stt[:], in_=s), writes=[stt])
                    P.dma(lambda e, aft=aft, s=affl[it]: e.dma_start(out=aft[:], in_=s), writes=[aft])
                else:
                    P.op("pool", lambda e, stt=stt: e.memset(stt[:], 32768), writes=[stt])
                    P.op("pool", lambda e, aft=aft: e.memset(aft[:], 0.0), writes=[aft])
                    P.dma(lambda e, stt=stt: e.dma_start(out=stt[:64, :], in_=slotc), writes=[stt])
                    P.dma(lambda e, aft=aft: e.dma_start(out=aft[:64, :], in_=affc), writes=[aft])
                cap = CAP_L if isl else CAP_C
                ysrc = yl if isl else yc
                P.op("dve", lambda e, stt=stt: e.tensor_copy(out=slot_f[:], in_=stt[:]), reads=[stt], writes=[slot_f])
                P.op("dve", lambda e, cap=cap: e.tensor_scalar(out=ge[:], in0=slot_f[:], scalar1=float(cap) - 0.5, scalar2=None, op0=ALU.is_lt),
                     reads=[slot_f], writes=[ge])
                P.op("dve", lambda e, aft=aft: e.tensor_tensor(out=ge[:], in0=ge[:], in1=aft[:], op=ALU.mult), reads=[ge, aft], writes=[ge])
                for ex in range(16):
                    y = yg[ex % 2]
                    P.dma(lambda e, y=y, ex=ex, stt=stt, ysrc=ysrc, cap=cap: e.indirect_dma_start(
                        out=y[:, :], out_offset=None, in_=ysrc[ex][:, :],
                        in_offset=bass.IndirectOffsetOnAxis(ap=stt[:, ex:ex + 1], axis=0),
                        bounds_check=P.getreg(e, cap - 1), oob_is_err=False), eng="pool", reads=[stt], writes=[y])
                    if ex == 0:
                        P.op("dve", lambda e, y=y, ex=ex, rows=rows: e.tensor_scalar(out=acc[:rows, :], in0=y[:rows, :], scalar1=ge[:rows, ex:ex + 1], scalar2=None, op0=ALU.mult),
                             reads=[y, ge], writes=[acc])
                    else:
                        P.op("dve", lambda e, y=y, ex=ex, rows=rows: e.scalar_tensor_tensor(out=acc[:rows, :], in0=y[:rows, :], scalar=ge[:rows, ex:ex + 1], in1=acc[:rows, :], op0=ALU.mult, op1=ALU.add),
                             reads=[y, ge, acc], writes=[acc])
                gt = gaf[nm]
                P.op("dve", lambda e, rows=rows, gt=gt: e.tensor_tensor(out=acc[:rows, :], in0=acc[:rows, :], in1=gt[:rows, :], op=ALU.mult), reads=[acc, gt], writes=[acc])
                P.op("dve", lambda e, rows=rows, x=x: e.tensor_tensor(out=x[:rows, :], in0=x[:rows, :], in1=acc[:rows, :], op=ALU.add), reads=[acc, x], writes=[x])
                dst = xlo[it * 128:(it + 1) * 128, :] if isl else xco
                P.dma(lambda e, x=x, dst=dst, rows=rows: e.dma_start(out=dst, in_=x[:rows, :]), reads=[x], writes=["xo"], final=True)
            if final and not isl:
                continue
            P.op("act", lambda e, x=x, rows=rows: e.activation(out=h1[:rows, :], in_=x[:rows, :], func=AF.Square, accum_out=ss[:rows, :]),
                 reads=[x], writes=[h1, ss])
            P.op("act", lambda e, rows=rows: e.activation(out=rstd[:rows, :], in_=ss[:rows, :], func=AF.Sqrt, scale=1.0 / D, bias=EPS),
                 reads=[ss], writes=[rstd])
            P.op("dve", lambda e, rows=rows: e.reciprocal(out=rstd[:rows, :], in_=rstd[:rows, :]),
                 reads=[rstd], writes=[rstd])
            if final:
                o = ob[it % 2]
                P.op("dve", lambda e, x=x, o=o: e.scalar_tensor_tensor(out=o[:], in0=x[:], scalar=rstd[:, 0:1], in1=g_bc[:], op0=ALU.mult, op1=ALU.mult),
                     reads=[x, rstd, g_bc], writes=[o])
                P.dma(lambda e, o=o, it=it: e.dma_start(out=outl[it * 128:(it + 1) * 128, :], in_=o[:]), reads=[o], writes=["outl"], final=True)
                continue
            A, Bt = AB[nm]
            P.op("dve", lambda e, x=x, rows=rows, A=A: e.scalar_tensor_tensor(out=h1[:rows, :], in0=x[:rows, :], scalar=rstd[:rows, 0:1], in1=A[:rows, :], op0=ALU.mult, op1=ALU.mult),
                 reads=[x, rstd, A], writes=[h1])
            h = hb[it % 2]
            P.op("pool", lambda e, h=h, rows=rows, Bt=Bt: e.tensor_tensor(out=h[:rows, :], in0=h1[:rows, :], in1=Bt[:rows, :], op=ALU.add),
                 reads=[h1, Bt], writes=[h])
            sg = stg[it % 2]
            if isl:
                for half in range(2):
                    pt = pst[(it * 2 + half) % 4]
                    for k in range(8):
                        kc = half * 8 + k
                        P.op("pe", lambda e, pt=pt, h=h, k=k, kc=kc: e.transpose(out=pt[:, k * 128:(k + 1) * 128], in_=h[:, kc * 128:(kc + 1) * 128], identity=ident[:]),
                             reads=[h, ident], writes=[pt])
                    eng = "act" if half == 0 else "dve"
                    if eng == "act":
                        P.op("act", lambda e, pt=pt, sg=sg, half=half: e.copy(out=sg[:, half * 1024:(half + 1) * 1024], in_=pt[:]), reads=[pt], writes=[(sg, half)])
                    else:
                        P.op("dve", lambda e, pt=pt, sg=sg, half=half: e.tensor_copy(out=sg[:, half * 1024:(half + 1) * 1024], in_=pt[:]), reads=[pt], writes=[(sg, half)])
                P.dma(lambda e, sg=sg, it=it: e.dma_start(out=hTl[it], in_=sg[:]), reads=[(sg, 0), (sg, 1)], writes=["hTl"], final=True)
            else:
                pt = pst[0]
                for kc in range(16):
                    P.op("pe", lambda e, pt=pt, h=h, kc=kc: e.transpose(out=pt[:, kc * 64:(kc + 1) * 64], in_=h[:64, kc * 128:(kc + 1) * 128], identity=ident[:64, :64]),
                         reads=[h, ident], writes=[pt])
                P.op("act", lambda e, pt=pt, sg=sg: e.copy(out=sg[:, 0:1024], in_=pt[:]), reads=[pt], writes=[(sg, 0)])
                P.dma(lambda e, sg=sg: e.dma_start(out=hTc, in_=sg[:, 0:1024].rearrange("p (k t) -> p k t", k=16)), reads=[(sg, 0)], writes=["hTc"], final=True)
        P.end_phase()


D = 2048; EPS = 1e-6
NCH = 66
POOL_W = (2, 4, 8, 16)

def emit_mixe(nc, P, pre, io, hp):
    stop = 9; do_pool = True; nheads = 2
    q = hp
    hT = io["HT"]; MIXT = io["MIXT"]; w_in = io["w_in"]; gate_b = io["gate_b"]; head_g = io["head_g"]
    tab = io["tab"]; identb_d = io["identb"]; identf_d = io["identf"]; tri_d = io["tri"]
    pw = io["pool_w"]; pool_scale = io["pool_scale"]
    icl = io["icl"][:, q * 2048:(q + 1) * 2048]; icc = io["icc"][:, q * 64:(q + 1) * 64]
    wu = w_in[:, 4128:5152]
    with contextlib.ExitStack() as st0:
        psf = [P.psum(st0, pre + "psf%d" % i, [128, 512], F32) for i in range(6)]
        psb = [P.psum(st0, pre + "psb%d" % i, [128, 1024], BF16) for i in range(2)]
        T0 = lambda name, shape, dt=F32: st0.enter_context(nc.sbuf_tensor(pre + name, shape, dt))
        identb = T0("identb_s", [128, 128], BF16); identf = T0("identf_s", [128, 128])
        tri = [T0("tri%d" % i, [128, 128]) for i in range(2)]
        ones = T0("ones", [128, 128]); zeros = T0("zeros", [128, 128])
        P.dma(lambda e: e.dma_start(out=identb[:], in_=identb_d), writes=[identb])
        P.dma(lambda e: e.dma_start(out=identf[:], in_=identf_d), writes=[identf])
        for i in range(2):
            P.dma(lambda e, i=i: e.dma_start(out=tri[i][:], in_=tri_d[i]), writes=[tri[i]])
        P.op("pool", lambda e: e.memset(ones[:], 1.0), writes=[ones])
        P.op("pool", lambda e: e.memset(zeros[:], 0.0), writes=[zeros])

        with contextlib.ExitStack() as st:
            T = lambda name, shape, dt=F32: st.enter_context(nc.sbuf_tensor(pre + name, shape, dt))
            Wh = T("Wh", [128, 16 * 512], BF16); Wg = T("Wg", [128, 16 * 4], BF16)
            gbt = T("gbt", [128, 4]); gn = T("gn", [128, 128]); gball = T("gball", [128, 32])
            QKT = T("QKT", [128, NCH * 256], BF16)
            Ktok = T("Ktok", [128, NCH * 128], BF16)
            VE = T("VE", [128, NCH * 129], BF16)
            SO = T("SO", [128, NCH * 128], BF16)
            HF = T("HF", [128, NCH * 128])
            OUT = T("OUTs", [128, NCH * 128], BF16)
            G = T("G", [128, NCH * 4])
            hch = [T("hch%d" % i, [128, 16 * 128], BF16) for i in range(2)]
            tb = [T("tb%d" % i, [128, 256]) for i in range(2)]
            qkt = [T("qkt%d" % i, [128, 256], BF16) for i in range(2)]
            rt = [T("rt%d" % i, [128, 128]) for i in range(4)]
            LF = T("LF", [128, NCH]); Bm = T("Bm", [128, NCH]); U = T("U", [128, NCH]); GS = T("GS", [128, NCH])
            UM = T("UM", [128, NCH]); MS = T("MS", [128, NCH + 1]); ME = T("ME", [128, NCH]); tA = T("tA", [128, NCH])
            um = T("um", [128, 1]); umb = T("umb", [128, 128])
            SPt = [T("SP%d" % i, [128, NCH]) for i in range(2)]
            WAt = [T("WA%d" % i, [128, NCH]) for i in range(2)]
            FLt = [T("FL%d" % i, [128, NCH]) for i in range(2)]
            CTs = [T("CT%d" % i, [128, 129]) for i in range(2)]; CTbs = [T("CTb%d" % i, [128, 129], BF16) for i in range(2)]
            PT = [T("PT%d" % i, [128, 128], BF16) for i in range(2)]
            vw = [T("vw%d" % i, [128, 129], BF16) for i in range(2)]
            rr = [T("rr%d" % i, [128, 1]) for i in range(2)]
            ssq = T("ssq", [128, NCH]); rs = T("rs", [128, NCH])
            hn1 = [T("hn1_%d" % i, [128, 128]) for i in range(2)]
            hnb = [T("hnb%d" % i, [128, 128], BF16) for i in range(2)]

            QKT3 = QKT[:].rearrange("p (c x) -> p c x", x=256)
            K3 = Ktok[:].rearrange("p (c x) -> p c x", x=128)
            VE3 = VE[:].rearrange("p (c x) -> p c x", x=129)
            SO3 = SO[:].rearrange("p (c x) -> p c x", x=128)
            HF3 = HF[:].rearrange("p (c x) -> p c x", x=128)
            G3 = G[:].rearrange("p (c x) -> p c x", x=4)
            Wh3 = Wh[:].rearrange("p (k n) -> p k n", k=16)
            Wg3 = Wg[:].rearrange("p (k n) -> p k n", k=16)

            P.op("pool", lambda e: e.memset(VE[:], 1.0), writes=[VE])
            for hd in range(nheads):
                h = 2 * hp + hd
                for s_ in range(4):
                    c0 = s_ * 1024 + h * 128
                    P.dma(lambda e, s_=s_, c0=c0: e.dma_start(out=Wh3[:, :, s_ * 128:(s_ + 1) * 128], in_=w_in[:, c0:c0 + 128].rearrange("(k p) n -> p k n", p=128)), eng="pool", writes=[Wh])
                for gt in range(4):
                    c0 = 4096 + gt * 8 + h
                    P.dma(lambda e, gt=gt, c0=c0: e.dma_start(out=Wg3[:, :, gt:gt + 1], in_=w_in[:, c0:c0 + 1].rearrange("(k p) n -> p k n", p=128), allow_slow_non_contiguous=True), eng="pool", writes=[Wg])
                P.dma(lambda e: e.dma_start(out=gball[:], in_=gate_b.rearrange("g h -> (g h)").unsqueeze(0).to_broadcast([128, 32])), writes=[gball])
                P.op("dve", lambda e, h=h: e.tensor_copy(out=gbt[:], in_=gball[:].rearrange("p (g h) -> p g h", g=4)[:, :, h]), reads=[gball], writes=[gbt])
                P.dma(lambda e, h=h: e.dma_start(out=gn[:], in_=head_g[:, h * 128:(h + 1) * 128].to_broadcast([128, 128])), writes=[gn])
                def p1_proj(c):
                    hc_ = hch[c % 2]; tbc = tb[c % 2]; qk = qkt[c % 2]
                    pp = psf[c % 2]; pg = psf[2 + c % 2]; pb = psb[c % 2]
                    P.dma(lambda e, hc_=hc_, c=c: e.dma_start(out=hc_[:], in_=hT[c]), writes=[hc_])
                    P.dma(lambda e, tbc=tbc, c=c: e.dma_start(out=tbc[:], in_=tab[c]), writes=[tbc])
                    h3 = hc_[:].rearrange("p (k t) -> p k t", k=16)
                    for kc in range(16):
                        P.op("pe", lambda e, pp=pp, h3=h3, kc=kc: e.matmul(pp[:, :], lhsT=h3[:, kc, :], rhs=Wh3[:, kc, :], start=(kc == 0), stop=(kc == 15)),
                             reads=[hc_, Wh], writes=[pp])
                    for kc in range(16):
                        P.op("pe", lambda e, pg=pg, h3=h3, kc=kc: e.matmul(pg[:, 0:4], lhsT=h3[:, kc, :], rhs=Wg3[:, kc, :], start=(kc == 0), stop=(kc == 15)),
                             reads=[hc_, Wg], writes=[pg])
                def p1_post(c):
                    hc_ = hch[c % 2]; tbc = tb[c % 2]; qk = qkt[c % 2]
                    pp = psf[c % 2]; pg = psf[2 + c % 2]; pb = psb[c % 2]
                    xv = pp[:, 0:256].rearrange("p (a b h d) -> p a b h d", a=2, b=2, h=2)
                    x1 = xv[:, :, :, 0, :]; x2 = xv[:, :, :, 1, :]
                    tv = tbc[:].rearrange("p (s a b d) -> p s a b d", s=2, a=2, b=2)
                    Cv = tv[:, 0]; Sv = tv[:, 1]
                    ov = qk[:].rearrange("p (a b h d) -> p a b h d", a=2, b=2, h=2)
                    r4 = [t[:].rearrange("p (a b d) -> p a b d", a=2, b=2) for t in rt]
                    P.op("dve", lambda e, x1=x1, Cv=Cv, o=r4[0]: e.tensor_tensor(out=o, in0=x1, in1=Cv, op=ALU.mult), reads=[pp, tbc], writes=[rt[0]])
                    P.op("dve", lambda e, x2=x2, Sv=Sv, o=r4[1]: e.tensor_tensor(out=o, in0=x2, in1=Sv, op=ALU.mult), reads=[pp, tbc], writes=[rt[1]])
                    P.op("dve", lambda e, x1=x1, Sv=Sv, o=r4[2]: e.tensor_tensor(out=o, in0=x1, in1=Sv, op=ALU.mult), reads=[pp, tbc], writes=[rt[2]])
                    P.op("dve", lambda e, x2=x2, Cv=Cv, o=r4[3]: e.tensor_tensor(out=o, in0=x2, in1=Cv, op=ALU.mult), reads=[pp, tbc], writes=[rt[3]])
                    P.op("pool", lambda e, o=ov[:, :, :, 0, :], a=r4[0], b=r4[1]: e.tensor_tensor(out=o, in0=a, in1=b, op=ALU.subtract), reads=[rt[0], rt[1]], writes=[(qk, 0)])
                    P.op("pool", lambda e, o=ov[:, :, :, 1, :], a=r4[2], b=r4[3]: e.tensor_tensor(out=o, in0=a, in1=b, op=ALU.add), reads=[rt[2], rt[3]], writes=[(qk, 1)])
                    P.op("act", lambda e, pp=pp, c=c: e.copy(out=VE3[:, c, 0:128], in_=pp[:, 256:384]), reads=[pp], writes=[(VE, c)])
                    P.op("act", lambda e, pp=pp, c=c: e.activation(out=SO3[:, c, :], in_=pp[:, 384:512], func=AF.Sigmoid), reads=[pp], writes=[(SO, c)])
                    P.op("dve", lambda e, pg=pg, c=c: e.tensor_tensor(out=G3[:, c, :], in0=pg[:, 0:4], in1=gbt[:], op=ALU.add), reads=[pg, gbt], writes=[(G, c)])
                    P.op("pool", lambda e, qk=qk, c=c: e.tensor_copy(out=K3[:, c, :], in_=qk[:, 128:256]), reads=[(qk, 0), (qk, 1)], writes=[(Ktok, c)])
                    for a in range(2):
                        P.op("pe", lambda e, pb=pb, qk=qk, a=a: e.transpose(out=pb[:, a * 128:(a + 1) * 128], in_=qk[:, a * 128:(a + 1) * 128], identity=identb[:]),
                             reads=[(qk, 0), (qk, 1), identb], writes=[pb])
                    P.op("act", lambda e, pb=pb, c=c: e.copy(out=QKT3[:, c, :], in_=pb[:, 0:256]), reads=[pb], writes=[(QKT, c)])

                p1_proj(0)
                for c in range(NCH):
                    if c + 1 < NCH:
                        p1_proj(c + 1)
                    p1_post(c)
                ordrs = []
                for dr in range(2 if stop >= 2 else 0):
                    ordr = list(range(NCH)) if dr == 0 else [1, 0] + list(range(NCH - 1, 1, -1))
                    li = G3[:, :, 2 * dr]; ff = G3[:, :, 2 * dr + 1]
                    Gk = [(G, c) for c in range(NCH)]
                    P.op("act", lambda e, ff=ff: e.activation(out=tA[:], in_=ff, func=AF.Exp, scale=-1.0), reads=Gk, writes=[tA])
                    P.op("act", lambda e: e.activation(out=tA[:], in_=tA[:], func=AF.Ln, bias=1.0), reads=[tA], writes=[tA])
                    P.op("dve", lambda e: e.tensor_scalar(out=LF[:], in0=tA[:], scalar1=-1.0, scalar2=None, op0=ALU.mult), reads=[tA], writes=[LF])
                    pa = psf[4]
                    P.op("pe", lambda e, dr=dr: e.matmul(pa[:, 0:NCH], lhsT=tri[dr][:], rhs=LF[:], start=True, stop=True), reads=[tri[dr], LF], writes=[pa])
                    P.op("pe", lambda e: e.matmul(pa[:, 128:128 + NCH], lhsT=ones[:], rhs=LF[:], start=True, stop=True), reads=[ones, LF], writes=[pa])
                    P.op("act", lambda e: e.copy(out=Bm[:], in_=pa[:, 0:NCH]), reads=[pa], writes=[Bm])
                    P.op("act", lambda e: e.copy(out=GS[:], in_=pa[:, 128:128 + NCH]), reads=[pa], writes=[GS])
                    P.op("dve", lambda e, li=li: e.tensor_tensor(out=U[:], in0=li, in1=Bm[:], op=ALU.subtract), reads=Gk + [Bm], writes=[U])
                    P.op("pe", lambda e: e.transpose(out=pa[:NCH, 256:384], in_=U[:], identity=identf[:]), reads=[U, identf], writes=[pa])
                    P.op("dve", lambda e: e.reduce_max(out=um[:NCH, :], in_=pa[:NCH, 256:384], axis=AX.X), reads=[pa], writes=[um])
                    P.op("dve", lambda e: e.tensor_scalar(out=umb[:NCH, :], in0=zeros[:NCH, :], scalar1=um[:NCH, 0:1], scalar2=None, op0=ALU.add), reads=[um, zeros], writes=[umb])
                    P.op("pe", lambda e: e.matmul(pa[:, 384:384 + NCH], lhsT=umb[:NCH, :], rhs=identf[:NCH, :NCH], start=True, stop=True), reads=[umb, identf], writes=[pa])
                    P.op("act", lambda e: e.copy(out=UM[:], in_=pa[:, 384:384 + NCH]), reads=[pa], writes=[UM])
                    P.op("pool", lambda e: e.memset(MS[:], 0.0), writes=[MS])
                    for n, c in enumerate(ordr):
                        P.op("dve", lambda e, c=c: e.tensor_tensor(out=ME[:, c:c + 1], in0=MS[:, c:c + 1], in1=UM[:, c:c + 1], op=ALU.max), reads=[MS, UM], writes=[ME])
                        if n + 1 < NCH:
                            cn = ordr[n + 1]
                            P.op("dve", lambda e, c=c, cn=cn: e.tensor_tensor(out=MS[:, cn:cn + 1], in0=ME[:, c:c + 1], in1=GS[:, c:c + 1], op=ALU.add), reads=[ME, GS], writes=[MS])
                    SPd, WAd, FLd = SPt[dr], WAt[dr], FLt[dr]
                    P.op("dve", lambda e: e.tensor_tensor(out=tA[:], in0=MS[:, 0:NCH], in1=ME[:], op=ALU.subtract), reads=[MS, ME], writes=[tA])
                    P.op("act", lambda e, SPd=SPd: e.activation(out=SPd[:], in_=tA[:], func=AF.Exp), reads=[tA], writes=[SPd])
                    P.op("dve", lambda e: e.tensor_tensor(out=tA[:], in0=U[:], in1=ME[:], op=ALU.subtract), reads=[U, ME], writes=[tA])
                    P.op("act", lambda e, WAd=WAd: e.activation(out=WAd[:], in_=tA[:], func=AF.Exp), reads=[tA], writes=[WAd])
                    P.op("dve", lambda e: e.tensor_tensor(out=tA[:], in0=Bm[:], in1=ME[:], op=ALU.add), reads=[Bm, ME], writes=[tA])
                    P.op("act", lambda e, FLd=FLd: e.activation(out=FLd[:], in_=tA[:], func=AF.Exp, scale=-1.0), reads=[tA], writes=[FLd])
                    ordrs.append(ordr)
                P.op("pool", lambda e: e.memset(HF[:], 0.0), writes=[(HF, c) for c in range(NCH)])
                for dr in range(2):
                    P.op("pool", lambda e, dr=dr: e.memset(CTs[dr][:], 0.0), writes=[CTs[dr]])
                for n in range(NCH):
                    for dr in range(2):
                        c = ordrs[dr][n]
                        SPd, WAd, FLd = SPt[dr], WAt[dr], FLt[dr]
                        CT = CTs[dr]; CTb = CTbs[dr]
                        pS = psf[dr]; pO = psf[2 + dr]; pC = psf[4 + dr]
                        PTn = PT[dr]; vwn = vw[dr]; rrn = rr[dr]
                        P.op("pe", lambda e, pS=pS, c=c: e.matmul(pS[:, 0:128], lhsT=QKT3[:, c, 128:256], rhs=QKT3[:, c, 0:128], start=True, stop=True),
                             reads=[(QKT, c)], writes=[pS])
                        P.op("dve", lambda e, pS=pS, PTn=PTn, dr=dr: e.tensor_tensor(out=PTn[:], in0=pS[:, 0:128], in1=tri[dr][:], op=ALU.mult), reads=[pS, tri[dr]], writes=[PTn])
                        P.op("act", lambda e, vwn=vwn, c=c, WAd=WAd: e.activation(out=vwn[:], in_=VE3[:, c, :], func=AF.Copy, scale=WAd[:, c:c + 1]), reads=[(VE, c), WAd], writes=[vwn])
                        P.op("dve", lambda e, c=c, SPd=SPd, CT=CT: e.tensor_scalar(out=CT[:], in0=CT[:], scalar1=SPd[:, c:c + 1], scalar2=None, op0=ALU.mult), reads=[CT, SPd], writes=[CT])
                        P.op("act", lambda e, CT=CT, CTb=CTb: e.copy(out=CTb[:], in_=CT[:]), reads=[CT], writes=[CTb])
                        P.op("pe", lambda e, pO=pO, PTn=PTn, vwn=vwn: e.matmul(pO[:, 0:129], lhsT=PTn[:], rhs=vwn[:], start=True, stop=False), reads=[PTn, vwn], writes=[pO])
                        P.op("pe", lambda e, pO=pO, c=c, CTb=CTb: e.matmul(pO[:, 0:129], lhsT=QKT3[:, c, 0:128], rhs=CTb[:], start=False, stop=True), reads=[(QKT, c), CTb], writes=[pO])
                        P.op("pe", lambda e, pC=pC, c=c, vwn=vwn: e.matmul(pC[:, 0:129], lhsT=K3[:, c, :], rhs=vwn[:], start=True, stop=True), reads=[(Ktok, c), vwn], writes=[pC])
                        P.op("dve", lambda e, pC=pC, CT=CT: e.tensor_tensor(out=CT[:], in0=CT[:], in1=pC[:, 0:129], op=ALU.add), reads=[CT, pC], writes=[CT])
                        P.op("act", lambda e, pO=pO, rrn=rrn: e.activation(out=rrn[:], in_=pO[:, 128:129], func=AF.Abs),
                             reads=[pO], writes=[rrn])
                        P.op("dve", lambda e, rrn=rrn, c=c, FLd=FLd: e.tensor_scalar(out=rrn[:], in0=rrn[:], scalar1=FLd[:, c:c + 1], scalar2=None, op0=ALU.max),
                             reads=[rrn, FLd], writes=[rrn])
                        P.op("dve", lambda e, rrn=rrn: e.reciprocal(out=rrn[:], in_=rrn[:]), reads=[rrn], writes=[rrn])
                        P.op("dve", lambda e, pO=pO, rrn=rrn, c=c: e.scalar_tensor_tensor(out=HF3[:, c, :], in0=pO[:, 0:128], scalar=rrn[:, 0:1], in1=HF3[:, c, :], op0=ALU.mult, op1=ALU.add),
                             reads=[pO, rrn, (HF, c)], writes=[(HF, c)])
                if stop < 4:
                    continue
                for c in range(NCH):
                    h1 = hn1[c % 2]
                    P.op("act", lambda e, c=c, h1=h1: e.activation(out=h1[:], in_=HF3[:, c, :], func=AF.Square, accum_out=ssq[:, c:c + 1]), reads=[(HF, c)], writes=[h1, (ssq, c)])
                sk = [(ssq, c) for c in range(NCH)]
                P.op("act", lambda e: e.activation(out=rs[:], in_=ssq[:], func=AF.Sqrt, scale=1.0 / 128, bias=EPS), reads=sk, writes=[rs])
                P.op("dve", lambda e: e.reciprocal(out=rs[:], in_=rs[:]), reads=[rs], writes=[rs])
                OUT3 = OUT[:].rearrange("p (c x) -> p c x", x=128)
                for c in range(NCH):
                    h1 = hn1[c % 2]; hb_ = hnb[c % 2]
                    grp, k = divmod(c, 8)
                    pb = psb[grp % 2]
                    P.op("dve", lambda e, c=c, h1=h1: e.scalar_tensor_tensor(out=h1[:], in0=HF3[:, c, :], scalar=rs[:, c:c + 1], in1=gn[:], op0=ALU.mult, op1=ALU.mult),
                         reads=[(HF, c), rs, gn], writes=[h1])
                    P.op("pool", lambda e, c=c, h1=h1, hb_=hb_: e.tensor_tensor(out=hb_[:], in0=h1[:], in1=SO3[:, c, :], op=ALU.mult), reads=[h1, (SO, c)], writes=[hb_])
                    P.op("pe", lambda e, pb=pb, hb_=hb_, k=k: e.transpose(out=pb[:, k * 128:(k + 1) * 128], in_=hb_[:], identity=identb[:]), reads=[hb_, identb], writes=[pb])
                    if k == 7 or c == NCH - 1:
                        n = k + 1
                        P.op("act", lambda e, pb=pb, grp=grp, n=n: e.copy(out=OUT[:, grp * 1024:grp * 1024 + n * 128], in_=pb[:, 0:n * 128]), reads=[pb], writes=[OUT])
                P.dma(lambda e, h=h: e.dma_start(out=MIXT[:, :, h, :].rearrange("t p x -> p t x"), in_=OUT[:].rearrange("p (t x) -> p t x", x=128)), reads=[OUT], writes=["mixh"])
            P.barrier()
        with contextlib.ExitStack() as st:
            T = lambda name, shape, dt=F32: st.enter_context(nc.sbuf_tensor(pre + name, shape, dt))
            Wu = T("Wu", [128, 16 * 1024], BF16)
            Wu3 = Wu[:].rearrange("p (k n) -> p k n", k=16)
            for kc_ in range(16):
                P.dma(lambda e, kc_=kc_: e.dma_start(out=Wu3[:, kc_, :], in_=wu[kc_ * 128:(kc_ + 1) * 128, :]), eng="pool", writes=[Wu])
            PW = T("PW", [128, 4 * 2 * 256], BF16)
            PW4 = PW[:].rearrange("p (g c n) -> p g c n", g=4, c=2)
            for g in range(4):
                P.dma(lambda e, g=g: e.dma_start(out=PW4[:, g], in_=pw[g].rearrange("(c p) n -> p c n", p=128)), eng="pool", writes=[PW])
            pst = T("pst", [128, 8])
            for et_ in range(8):
                P.dma(lambda e, et_=et_: e.dma_start(out=pst[:, et_:et_ + 1], in_=pool_scale[et_ * 128:(et_ + 1) * 128].unsqueeze(1)), writes=[pst])
            def pool_part(nm, hp, ic, NT, odst):
                NH = NT + 16
                hs = T("hs_" + nm, [128, 16 * NH], BF16)
                hs3 = hs[:].rearrange("p (k t) -> p k t", k=16)
                if nm == "l":
                    t0 = 2 + q * 16
                    for kc_ in range(16):
                        P.dma(lambda e, kc_=kc_: e.dma_start(out=hs3[:, kc_, 8:8 + 2048].rearrange("p (t x) -> p t x", x=128),
                                                              in_=hT[t0:t0 + 16].rearrange("t p (k x) -> p k t x", k=16)[:, kc_]), writes=[hs])
                    if q > 0:
                        P.dma(lambda e: e.dma_start(out=hs3[:, :, 0:8], in_=hT[t0 - 1].rearrange("p (k x) -> p k x", k=16)[:, :, 120:128]), writes=[hs])
                    else:
                        P.op("pool", lambda e: e.memset(hs3[:, :, 0:8], 0.0), writes=[hs])
                    if q < 3:
                        P.dma(lambda e: e.dma_start(out=hs3[:, :, 2056:2064], in_=hT[t0 + 16].rearrange("p (k x) -> p k x", k=16)[:, :, 0:8]), writes=[hs])
                    else:
                        P.op("pool", lambda e: e.memset(hs3[:, :, 2056:2064], 0.0), writes=[hs])
                else:
                    lo = q * 64 - 8
                    pos = 0
                    if lo < 0:
                        P.op("pool", lambda e: e.memset(hs3[:, :, 0:8], 0.0), writes=[hs]); pos = 8; lo = 0
                    hi = min(q * 64 + 72, 256)
                    while lo < hi:
                        t_ = lo // 128; a_ = lo % 128; n_ = min(hi - lo, 128 - a_)
                        P.dma(lambda e, t_=t_, a_=a_, n_=n_, pos=pos: e.dma_start(out=hs3[:, :, pos:pos + n_], in_=hT[t_].rearrange("p (k x) -> p k x", k=16)[:, :, a_:a_ + n_]), writes=[hs])
                        pos += n_; lo += n_
                    if pos < 80:
                        P.op("pool", lambda e, pos=pos: e.memset(hs3[:, :, pos:80], 0.0), writes=[hs])
                Ub = [T("U%s%d" % (nm, i), [128, NH]) for i in range(2)]
                A1 = T("A1" + nm, [128, NH]); A2 = T("A2" + nm, [128, NH])
                icb = T("icb" + nm, [128, NT])
                dT = [T("dT%s%d" % (nm, i), [128, NT], BF16) for i in range(2)]
                ob = [T("ob%s%d" % (nm, i), [128, NT], BF16) for i in range(2)]
                blocks = [(s, min(512, NH - s)) for s in range(0, NH, 512)]
                oblocks = [(s, min(512, NT - s)) for s in range(0, NT, 512)]
                for g in range(4):
                    w = POOL_W[g]
                    P.dma(lambda e, g=g, ic=ic, icb=icb, NT=NT: e.dma_start(out=icb[:], in_=ic[g:g + 1, :].to_broadcast([128, NT])), writes=[icb])
                    for ci in range(2):
                        ct = g * 2 + ci
                        Ut = Ub[ci]
                        for bi, (s0, n) in enumerate(blocks):
                            pp = psf[bi % 4]
                            for kc in range(16):
                                P.op("pe", lambda e, pp=pp, kc=kc, ct=ct, s0=s0, n=n: e.matmul(pp[:, 0:n], lhsT=Wu3[:, kc, ct * 128:(ct + 1) * 128], rhs=hs3[:, kc, s0:s0 + n], start=(kc == 0), stop=(kc == 15)),
                                     reads=[Wu, hs], writes=[pp])
                            P.op("act", lambda e, pp=pp, Ut=Ut, s0=s0, n=n: e.copy(out=Ut[:, s0:s0 + n], in_=pp[:, 0:n]), reads=[pp], writes=[Ut])
                        cur = Ut; L = NH; step = 1; k = 1
                        bufs = [A1, A2]; bi = 0
                        while k < w:
                            nxt = bufs[bi % 2]; bi += 1
                            L2 = L - k
                            P.op("dve", lambda e, cur=cur, nxt=nxt, L2=L2, k=k: e.tensor_tensor(out=nxt[:, 0:L2], in0=cur[:, 0:L2], in1=cur[:, k:k + L2], op=ALU.add), reads=[cur], writes=[nxt])
                            cur = nxt; L = L2; k *= 2
                        off = 8 - w // 2
                        nxt = bufs[bi % 2]
                        P.op("dve", lambda e, cur=cur, nxt=nxt, off=off, NT=NT: e.tensor_tensor(out=nxt[:, 0:NT], in0=cur[:, off:off + NT], in1=icb[:], op=ALU.mult), reads=[cur, icb], writes=[nxt])
                        P.op("dve", lambda e, nxt=nxt, Ut=Ut, d=dT[ci], NT=NT: e.tensor_tensor(out=d[:], in0=nxt[:, 0:NT], in1=Ut[:, 8:8 + NT], op=ALU.subtract), reads=[nxt, Ut], writes=[dT[ci]])
                    for ei in range(2):
                        et = g * 2 + ei
                        o = ob[ei]
                        for bi, (s0, n) in enumerate(oblocks):
                            pp = psf[4 + bi % 2]
                            for ci in range(2):
                                P.op("pe", lambda e, pp=pp, ci=ci, g=g, ei=ei, s0=s0, n=n: e.matmul(pp[:, 0:n], lhsT=PW4[:, g, ci, ei * 128:(ei + 1) * 128], rhs=dT[ci][:, s0:s0 + n], start=(ci == 0), stop=(ci == 1)),
                                     reads=[PW, dT[ci]], writes=[pp])
                            P.op("act", lambda e, pp=pp, o=o, et=et, s0=s0, n=n: e.activation(out=o[:, s0:s0 + n], in_=pp[:, 0:n], func=AF.Copy, scale=pst[:, et:et + 1]), reads=[pp, pst], writes=[o])
                        if nm == "l":
                            P.dma(lambda e, o=o, et=et: e.dma_start(out=MIXT[2 + q * 16:2 + (q + 1) * 16, :, 8 + et, :].rearrange("t p x -> p t x"), in_=o[:].rearrange("p (t x) -> p t x", x=128)), reads=[o], writes=["po"])
                        else:
                            P.dma(lambda e, o=o, et=et: e.dma_start(out=MIXT[q // 2, :, 8 + et, (q % 2) * 64:(q % 2 + 1) * 64], in_=o[:]), reads=[o], writes=["po"])

            pool_part("l", None, icl, 2048, None)
            pool_part("c", None, icc, 64, None)
            P.end_phase()


D = 2048
NCH = 66
NPAT = 5

def na_patterns():
    R, W, WR, WC = 128, 64, 8, 16
    pats = {}; plist = []; per_m = []
    for m in range(64):
        idx = -np.ones((64, 128, 128), np.int32)
        used = set()
        for rl in range(2):
            r = 2 * m + rl
            rs = min(max(r - WR // 2, 0), R - WR)
            for c in range(W):
                cs = min(max(c - WC // 2, 0), W - WC)
                q = rl * 64 + c
                for kr in range(rs, rs + WR):
                    t = kr // 2
                    used.add(t)
                    kk = (kr - 2 * t) * 64 + np.arange(cs, cs + WC)
                    idx[t, kk, q] = (kr - r + 7) * 31 + (np.arange(cs, cs + WC) - c + 15)
        tiles = sorted(used)
        assert len(tiles) <= 5
        im = -np.ones((5, 128, 128), np.int32)
        for j, t in enumerate(tiles):
            im[j] = idx[t]
        while len(tiles) < 5:
            tiles.append(tiles[-1])
        key = im.tobytes()
        if key not in pats:
            pats[key] = len(plist); plist.append(im)
        per_m.append(([t + 2 for t in tiles], pats[key]))
    assert len(plist) == NPAT, len(plist)
    return per_m, np.stack(plist)

def emit_mixo(nc, P, pre, io, hq, ctx_out):
    nheads = 4; phase = 2; skip = ()
    hT = io["HT"]; MIXT = io["MIXT"]; w_in = io["w_in"]; bias = io["bias"]; identb_d = io["identb"]
    per_m, _ = na_patterns()
    scale = 128.0 ** -0.5
    with contextlib.ExitStack() as st:
        psf = [P.psum(st, pre + "psf%d" % i, [128, 512], F32) for i in range(6)]
        psb = [P.psum(st, pre + "psb%d" % i, [128, 1024], BF16) for i in range(2)]
        T = lambda name, shape, dt=F32: st.enter_context(nc.sbuf_tensor(pre + name, shape, dt))
        identb = T("identb_s", [128, 128], BF16); onesb = T("onesb", [128, 128], BF16)
        P.dma(lambda e: e.dma_start(out=identb[:], in_=identb_d), writes=[identb])
        P.op("pool", lambda e: e.memset(onesb[:], 1.0), writes=[onesb])
        W = T("W", [128, 16 * 384], BF16); W3 = W[:].rearrange("p (k n) -> p k n", k=16)
        QT = T("QT", [128, NCH * 128], BF16); KT = T("KT", [128, NCH * 128], BF16); VT = T("VT", [128, NCH * 129], BF16)
        OUT = T("OUTs", [128, NCH * 128], BF16)
        Bs = T("Bs", [128, NPAT * 640])
        hch = [T("hch%d" % i, [128, 16 * 128], BF16) for i in range(2)]
        qk = [T("qk%d" % i, [128, 256], BF16) for i in range(2)]
        tmp = [T("tmp%d" % i, [128, 640]) for i in range(2)]
        PT = [T("PT%d" % i, [128, 896], BF16) for i in range(2)]
        rc = [T("rc%d" % i, [128, 128]) for i in range(2)]
        onbs = [T("onb%d" % i, [128, 128], BF16) for i in range(2)]; rrs = [T("rrs%d" % i, [128, 1]) for i in range(2)]
        Q3 = QT[:].rearrange("p (c x) -> p c x", x=128); K3 = KT[:].rearrange("p (c x) -> p c x", x=128)
        V3 = VT[:].rearrange("p (c x) -> p c x", x=129); O3 = OUT[:].rearrange("p (c x) -> p c x", x=128)
        B3 = Bs[:].rearrange("p (a x) -> p a x", a=NPAT)
        P.op("pool", lambda e: e.memset(VT[:], 1.0), writes=[(VT, c) for c in range(NCH)])
        for hd in range(nheads):
            h = 4 * hq + hd
            for s_ in range(3):
                c0 = s_ * 2048 + h * 128
                P.dma(lambda e, s_=s_, c0=c0: e.dma_start(out=W3[:, :, s_ * 128:(s_ + 1) * 128], in_=w_in[:, c0:c0 + 128].rearrange("(k p) n -> p k n", p=128)), eng="pool", writes=[W])
            P.dma(lambda e, h=h: e.dma_start(out=Bs[:], in_=bias[h]), writes=[Bs])
            def p1_proj(c):
                hc_ = hch[c % 2]; pp = psf[c % 2]
                P.dma(lambda e, hc_=hc_, c=c: e.dma_start(out=hc_[:], in_=hT[c]), writes=[hc_])
                h3 = hc_[:].rearrange("p (k t) -> p k t", k=16)
                for kc in range(16):
                    P.op("pe", lambda e, pp=pp, h3=h3, kc=kc: e.matmul(pp[:, 0:384], lhsT=h3[:, kc, :], rhs=W3[:, kc, :], start=(kc == 0), stop=(kc == 15)),
                         reads=[hc_, W], writes=[pp])

            def p1_post(c):
                qk_ = qk[c % 2]; pp = psf[c % 2]; pb = psb[c % 2]
                P.op("dve", lambda e, pp=pp, qk_=qk_: e.tensor_scalar(out=qk_[:, 0:128], in0=pp[:, 0:128], scalar1=scale, scalar2=None, op0=ALU.mult), reads=[pp], writes=[(qk_, 0)])
                P.op("act", lambda e, pp=pp, qk_=qk_: e.copy(out=qk_[:, 128:256], in_=pp[:, 128:256]), reads=[pp], writes=[(qk_, 1)])
                P.op("act", lambda e, pp=pp, c=c: e.copy(out=V3[:, c, 0:128], in_=pp[:, 256:384]), reads=[pp], writes=[(VT, c)])
                for a in range(2):
                    P.op("pe", lambda e, pb=pb, qk_=qk_, a=a: e.transpose(out=pb[:, a * 128:(a + 1) * 128], in_=qk_[:, a * 128:(a + 1) * 128], identity=identb[:]),
                         reads=[(qk_, a), identb], writes=[pb])
                P.op("dve", lambda e, pb=pb, c=c: e.tensor_copy(out=Q3[:, c, :], in_=pb[:, 0:128]), reads=[pb], writes=[(QT, c)])
                P.op("act", lambda e, pb=pb, c=c: e.copy(out=K3[:, c, :], in_=pb[:, 128:256]), reads=[pb], writes=[(KT, c)])

            p1_proj(0)
            for c in range(NCH):
                if c + 1 < NCH:
                    p1_proj(c + 1)
                p1_post(c)
            qtiles = [(m + 2, per_m[m][0], per_m[m][1]) for m in range(64 if phase >= 2 else 0)]
            if ctx_out:
                qtiles = [(0, None, None), (1, None, None)] + qtiles
            def att_S(n):
                qc, kts, pat = qtiles[n]
                pA = psf[2 * (n % 2)]; pB = psf[2 * (n % 2) + 1]
                if kts is not None:
                    for j, kt in enumerate(kts):
                        dst = pA[:, j * 128:(j + 1) * 128] if j < 4 else pB[:, 0:128]
                        P.op("pe", lambda e, dst=dst, kt=kt, qc=qc: e.matmul(dst, lhsT=K3[:, kt, :], rhs=Q3[:, qc, :], start=True, stop=True),
                             reads=[(KT, kt), (QT, qc)], writes=[pA if j < 4 else pB])
                for j in range(2):
                    P.op("pe", lambda e, pB=pB, j=j, qc=qc: e.matmul(pB[:, 128 + j * 128:256 + j * 128], lhsT=K3[:, j, :], rhs=Q3[:, qc, :], start=True, stop=True),
                         reads=[(KT, j), (QT, qc)], writes=[pB])

            def att_rest(n):
                qc, kts, pat = qtiles[n]
                pA = psf[2 * (n % 2)]; pB = psf[2 * (n % 2) + 1]; pN = psf[4]; pD = psf[5]
                tm = tmp[n % 2]; pt = PT[n % 2]; r_ = rc[n % 2]
                keys = []
                if kts is not None:
                    for j, kt in enumerate(kts):
                        keys.append((kt, j * 128))
                for j in range(2):
                    keys.append((j, 640 + j * 128))
                if kts is not None:
                    P.op("dve", lambda e, pA=pA, tm=tm, pat=pat: e.tensor_tensor(out=tm[:, 0:512], in0=pA[:, :], in1=B3[:, pat, 0:512], op=ALU.add), reads=[pA, Bs], writes=[tm])
                    P.op("dve", lambda e, pB=pB, tm=tm, pat=pat: e.tensor_tensor(out=tm[:, 512:640], in0=pB[:, 0:128], in1=B3[:, pat, 512:640], op=ALU.add), reads=[pB, Bs], writes=[tm])
                    P.op("act", lambda e, tm=tm, pt=pt: e.activation(out=pt[:, 0:640], in_=tm[:, :], func=AF.Exp), reads=[tm], writes=[pt])
                P.op("act", lambda e, pB=pB, pt=pt: e.activation(out=pt[:, 640:896], in_=pB[:, 128:384], func=AF.Exp), reads=[pB], writes=[pt])
                pN = psf[4 + n % 2]; pb = psb[n % 2]; onb = onbs[n % 2]; rr_ = rrs[n % 2]
                for i, (kt, off) in enumerate(keys):
                    P.op("pe", lambda e, kt=kt, off=off, pt=pt, i=i, nk=len(keys), pN=pN: e.matmul(pN[:, 0:129], lhsT=pt[:, off:off + 128], rhs=V3[:, kt, :], start=(i == 0), stop=(i == nk - 1)),
                         reads=[(VT, kt), pt], writes=[pN])
                P.op("act", lambda e, rr_=rr_, pN=pN: e.copy(out=rr_[:], in_=pN[:, 128:129]), reads=[pN], writes=[rr_])
                P.op("dve", lambda e, rr_=rr_: e.reciprocal(out=rr_[:], in_=rr_[:]), reads=[rr_], writes=[rr_])
                P.op("act", lambda e, rr_=rr_, pN=pN, onb=onb: e.activation(out=onb[:], in_=pN[:, 0:128], func=AF.Copy, scale=rr_[:, 0:1]), reads=[pN, rr_], writes=[onb])

            def att_fin(n):
                qc, kts, pat = qtiles[n]
                pb = psb[n % 2]; onb = onbs[n % 2]
                P.op("pe", lambda e, pb=pb, onb=onb: e.transpose(out=pb[:, 0:128], in_=onb[:], identity=identb[:]), reads=[onb, identb], writes=[pb])
                P.op("dve", lambda e, pb=pb, qc=qc: e.tensor_copy(out=O3[:, qc, :], in_=pb[:, 0:128]), reads=[pb], writes=[OUT])

            if qtiles:
                att_S(0)
            for n in range(len(qtiles)):
                if n + 1 < len(qtiles):
                    att_S(n + 1)
                att_rest(n)
                if n >= 1:
                    att_fin(n - 1)
            if qtiles:
                att_fin(len(qtiles) - 1)
            if not ctx_out:
                P.op("pool", lambda e: e.memset(OUT[:, 0:256], 0.0), writes=[OUT])
            P.dma(lambda e, h=h: e.dma_start(out=MIXT[:, :, h, :].rearrange("t p x -> p t x"), in_=OUT[:].rearrange("p (t x) -> p t x", x=128)), reads=[OUT], writes=["mixh"])
        P.end_phase()


D = 2048; EPS = 1e-6

def emit_g2(nc, P, pre, io):
    xl = io["xl"]; xc = io["xc"]; mixl = io["mixl"]; mixc = io["mixc"]; wout = io["wout"]; wr = io["wr"]
    modl = io["modl"]; modc = io["modc"]; gv = io["g"]; identf_d = io["identf"]
    xmo_l = io["xmo_l"]; xmo_c = io["xmo_c"]; h2_l = io["h2_l"]; h2_c = io["h2_c"]; aff_l = io["aff_l"]; aff_c = io["aff_c"]
    with contextlib.ExitStack() as st:
        T = lambda name, shape, dt=F32: st.enter_context(nc.sbuf_tensor(pre + name, shape, dt))
        psf = [P.psum(st, pre + "psf%d" % i, [128, 512], F32) for i in range(8)]
        identf = T("identf_s", [128, 128])
        P.dma(lambda e: e.dma_start(out=identf[:], in_=identf_d), writes=[identf])
        W = T("W", [128, 16 * D], BF16); W3 = W[:].rearrange("p (k n) -> p k n", k=16)
        for kc in range(16):
            P.dma(lambda e, kc=kc: e.dma_start(out=W3[:, kc, :], in_=wout[kc * 128:(kc + 1) * 128, :]), eng="pool", writes=[W])
        WR = T("WR", [128, 16 * 16]); WR3 = WR[:].rearrange("p (k n) -> p k n", k=16)
        P.dma(lambda e: e.dma_start(out=WR3, in_=wr.rearrange("(k p) n -> p k n", p=128)), writes=[WR])
        g_bc = T("g_bc", [128, D]); tmpb = T("tmpb", [128, D])
        P.dma(lambda e: e.dma_start(out=g_bc[:], in_=gv.to_broadcast([128, D])), writes=[g_bc])
        GA = T("GA", [128, D]); A = T("A", [128, D]); Bt = T("Bt", [128, D])
        xt = [T("xt%d" % i, [128, D]) for i in range(2)]
        mt = [T("mt%d" % i, [128, D], BF16) for i in range(2)]
        xm = [T("xm%d" % i, [128, D]) for i in range(2)]
        h1 = T("h1", [128, D])
        h2f = T("h2f", [128, D]); h2b = [T("h2b%d" % i, [128, D], BF16) for i in range(2)]
        h2T = T("h2T", [128, D])
        ss = T("ss", [128, 1]); rstd = T("rstd", [128, 1])
        mx = T("mx", [128, 1]); se = T("se", [128, 1]); ex = T("ex", [128, 16]); af = [T("af%d" % i, [128, 16]) for i in range(2)]

        def load_mod(mod):
            P.dma(lambda e: e.dma_start(out=GA[:], in_=mod[2:3, :].to_broadcast([128, D])), writes=[GA])
            P.dma(lambda e: e.dma_start(out=Bt[:], in_=mod[3:4, :].to_broadcast([128, D])), writes=[Bt])
            P.dma(lambda e: e.dma_start(out=tmpb[:], in_=mod[4:5, :].to_broadcast([128, D])), writes=[tmpb])
            P.op("dve", lambda e: e.scalar_tensor_tensor(out=A[:], in0=tmpb[:], scalar=1.0, in1=g_bc[:], op0=ALU.add, op1=ALU.mult),
                 reads=[tmpb, g_bc], writes=[A])

        def tile(it, rows, xsrc, msrc, xdst, hdst, adst):
            x = xt[it % 2]; m = mt[it % 2]; xo = xm[it % 2]; hb = h2b[it % 2]; a_ = af[it % 2]
            P.dma(lambda e: e.dma_start(out=x[:rows, :], in_=xsrc), writes=[x])
            if rows == 128:
                P.dma(lambda e: e.dma_start(out=m[:], in_=msrc), writes=[m])
            else:
                P.dma(lambda e: e.dma_start(out=m[:, 0:16 * rows].rearrange("p (k t) -> p k t", k=16), in_=msrc), writes=[m])
            m3 = m[:, 0:16 * rows].rearrange("p (k t) -> p k t", k=16)
            for nb in range(4):
                pp = psf[nb]
                for kc in range(16):
                    P.op("pe", lambda e, pp=pp, kc=kc, nb=nb: e.matmul(pp[:rows, :], lhsT=m3[:, kc, :], rhs=W3[:, kc, nb * 512:(nb + 1) * 512], start=(kc == 0), stop=(kc == 15)),
                         reads=[m, W], writes=[pp])
            yield "A"
            for nb in range(4):
                pp = psf[nb]
                sl = slice(nb * 512, (nb + 1) * 512)
                P.op("dve", lambda e, pp=pp, sl=sl: e.tensor_tensor(out=h1[:rows, sl], in0=pp[:rows, :], in1=GA[:rows, sl], op=ALU.mult), reads=[pp, GA], writes=[(h1, nb)])
                P.op("pool", lambda e, sl=sl: e.tensor_tensor(out=xo[:rows, sl], in0=x[:rows, sl], in1=h1[:rows, sl], op=ALU.add), reads=[x, (h1, nb)], writes=[(xo, nb)])
            xok = [(xo, nb) for nb in range(4)]; h1k = [(h1, nb) for nb in range(4)]
            P.dma(lambda e: e.dma_start(out=xdst, in_=xo[:rows, :]), reads=xok, writes=["xmo"], final=True)
            P.op("act", lambda e: e.activation(out=h1[:rows, :], in_=xo[:rows, :], func=AF.Square, accum_out=ss[:rows, :]), reads=xok, writes=h1k + [ss])
            P.op("act", lambda e: e.activation(out=rstd[:rows, :], in_=ss[:rows, :], func=AF.Sqrt, scale=1.0 / D, bias=EPS), reads=[ss], writes=[rstd])
            P.op("dve", lambda e: e.reciprocal(out=rstd[:rows, :], in_=rstd[:rows, :]), reads=[rstd], writes=[rstd])
            P.op("dve", lambda e: e.scalar_tensor_tensor(out=h1[:rows, :], in0=xo[:rows, :], scalar=rstd[:rows, 0:1], in1=A[:rows, :], op0=ALU.mult, op1=ALU.mult),
                 reads=xok + [rstd, A], writes=h1k)
            P.op("pool", lambda e: e.tensor_tensor(out=h2f[:rows, :], in0=h1[:rows, :], in1=Bt[:rows, :], op=ALU.add), reads=h1k + [Bt], writes=[h2f])
            P.op("act", lambda e: e.copy(out=hb[:rows, :], in_=h2f[:rows, :]), reads=[h2f], writes=[hb])
            P.dma(lambda e: e.dma_start(out=hdst, in_=hb[:rows, :]), reads=[hb], writes=["h2o"], final=True)
            yield "M"
            h2T3 = h2T[:].rearrange("p (k t) -> p k t", k=16)
            for grp in range(4):
                pp = psf[4 + grp]
                for k in range(4):
                    kc = grp * 4 + k
                    P.op("pe", lambda e, pp=pp, k=k, kc=kc: e.transpose(out=pp[:, k * 128:k * 128 + rows], in_=h2f[:rows, kc * 128:(kc + 1) * 128], identity=identf[:rows, :rows]),
                         reads=[h2f, identf], writes=[pp])
                eng = "act" if grp % 2 == 0 else "dve"
                if rows == 128:
                    if eng == "act":
                        P.op("act", lambda e, pp=pp, grp=grp: e.copy(out=h2T[:, grp * 512:(grp + 1) * 512], in_=pp[:, :]), reads=[pp], writes=[(h2T, grp)])
                    else:
                        P.op("dve", lambda e, pp=pp, grp=grp: e.tensor_copy(out=h2T[:, grp * 512:(grp + 1) * 512], in_=pp[:, :]), reads=[pp], writes=[(h2T, grp)])
                else:
                    src = pp[:, :].rearrange("p (k t) -> p k t", k=4)[:, :, 0:rows]
                    dst = h2T3[:, grp * 4:(grp + 1) * 4, 0:rows]
                    P.op("dve", lambda e, src=src, dst=dst: e.tensor_copy(out=dst, in_=src), reads=[pp], writes=[(h2T, grp)])
            pl = psf[7]
            for kc in range(16):
                P.op("pe", lambda e, kc=kc: e.matmul(pl[:rows, 0:16], lhsT=h2T3[:, kc, 0:rows], rhs=WR3[:, kc, :], start=(kc == 0), stop=(kc == 15)),
                     reads=[(h2T, kc // 4), WR], writes=[pl])
            P.op("dve", lambda e: e.reduce_max(out=mx[:rows, :], in_=pl[:rows, 0:16], axis=AX.X), reads=[pl], writes=[mx])
            P.op("dve", lambda e: e.tensor_scalar(out=mx[:rows, :], in0=mx[:rows, :], scalar1=-1.0, scalar2=None, op0=ALU.mult), reads=[mx], writes=[mx])
            P.op("act", lambda e: e.activation(out=ex[:rows, :], in_=pl[:rows, 0:16], func=AF.Exp, bias=mx[:rows, 0:1], accum_out=se[:rows, :]), reads=[pl, mx], writes=[ex, se])
            P.op("dve", lambda e: e.reciprocal(out=se[:rows, :], in_=se[:rows, :]), reads=[se], writes=[se])
            P.op("dve", lambda e: e.tensor_scalar(out=a_[:rows, :], in0=ex[:rows, :], scalar1=se[:rows, 0:1], scalar2=None, op0=ALU.mult), reads=[ex, se], writes=[a_])
            P.dma(lambda e: e.dma_start(out=adst, in_=a_[:rows, :]), reads=[a_], writes=["affo"], final=True)

        load_mod(modl)
        gens = []
        for it in range(16):
            r = slice(it * 128, (it + 1) * 128)
            gens.append(tile(it, 128, xl[r, :], mixl[it], xmo_l[r, :], h2_l[r, :], aff_l[it]))
        gens.append(tile(16, 64, xc, mixc, xmo_c, h2_c, aff_c))
        next(gens[0])
        for t in range(17):
            next(gens[t])
            if t == 15:
                load_mod(modc)
            if t + 1 < 17:
                next(gens[t + 1])
            for _ in gens[t]:
                pass
        P.end_phase()


D = 2048; FF = 1024
CAPL = 1024; CAPC = 32
BIG = float(1 << 15)
NIT = 28

def emit_moe(nc, P, pre, io, g):
    only_slots = False
    AFF8 = io["AFF8"]; SLOT8 = io["SLOT8"]; capv = io["capv"]; h2l = io["H2L"]; h2c = io["H2C"]
    wg = io["wg"]; wu = io["wu"]; wd = io["wd"]; stri_d = io["stri"]; identb_d = io["identb"]
    xin_l = io["XINL"]; xin_c = io["XINC"]
    with contextlib.ExitStack() as st0:
        psf = [P.psum(st0, pre + "psf%d" % i, [128, 512], F32) for i in range(6)]
        psb = [P.psum(st0, pre + "psb%d" % i, [128, 1024], BF16) for i in range(2)]
        T0 = lambda name, shape, dt=F32: st0.enter_context(nc.sbuf_tensor(pre + name, shape, dt))
        identb = T0("identb_s", [128, 128], BF16)
        P.dma(lambda e: e.dma_start(out=identb[:], in_=identb_d), writes=[identb])
        if True:
            st = st0
            T = lambda name, shape, dt=F32: st.enter_context(nc.sbuf_tensor(pre + name, shape, dt))
            stri = T("stri_s", [128, 128]); ones = T("ones", [128, 128]); zeros = T("zeros", [128, 64])
            A8 = T("A8", [128, 512]); cap = T("cap", [128, 8])
            P.dma(lambda e: e.dma_start(out=stri[:], in_=stri_d), writes=[stri])
            A8raw = T("A8raw", [128, 512])
            Ar3 = A8raw[:].rearrange("p (i l) -> p i l", l=8)
            P.dma(lambda e: e.dma_start(out=Ar3[:, :, 0:4], in_=AFF8[:, :, 4 * g:4 * g + 4]), writes=[A8raw])
            P.dma(lambda e: e.dma_start(out=Ar3[:, :, 4:8], in_=AFF8[:, :, 16 + 4 * g:16 + 4 * g + 4]), writes=[A8raw])
            P.op("dve", lambda e: e.tensor_copy(out=A8[:].rearrange("p (l i) -> p l i", l=8), in_=A8raw[:].rearrange("p (i l) -> p l i", l=8)), reads=[A8raw], writes=[A8])
            P.dma(lambda e: e.dma_start(out=cap[:], in_=capv), writes=[cap])
            P.op("pool", lambda e: e.memset(ones[:], 1.0), writes=[ones])
            P.op("pool", lambda e: e.memset(zeros[:], 0.0), writes=[zeros])
            lo = T("lo", [128, 8]); th = T("th", [128, 8]); cntp = T("cntp", [128, 8]); tt = T("tt", [128, 8])
            cmp_ = T("cmp", [128, 512]); mask = T("mask", [128, 512]); m0 = T("m0", [128, 512])
            cum = T("cum", [128, 512]); s2 = T("s2", [128, 512]); totp = T("totp", [128, 8]); offs = T("offs", [128, 8])
            sloti = T("sloti", [128, 512], I32)
            A3 = A8[:].rearrange("p (l i) -> p l i", l=8)
            c3 = cmp_[:].rearrange("p (l i) -> p l i", l=8)
            pc = psf[0]
            P.op("pool", lambda e: e.memset(lo[:], 0.0), writes=[lo])
            for n in range(NIT):
                dl = 2.0 ** -(n + 1)
                P.op("dve", lambda e, dl=dl: e.tensor_scalar(out=th[:], in0=lo[:], scalar1=dl, scalar2=None, op0=ALU.add), reads=[lo], writes=[th])
                P.op("dve", lambda e: e.tensor_tensor(out=c3, in0=A3, in1=th[:].unsqueeze(2).to_broadcast([128, 8, 64]), op=ALU.is_ge), reads=[A8, th], writes=[cmp_])
                P.op("dve", lambda e: e.reduce_sum(out=cntp[:], in_=c3, axis=AX.X), reads=[cmp_], writes=[cntp])
                P.op("pe", lambda e: e.matmul(pc[:, 0:8], lhsT=ones[:], rhs=cntp[:], start=True, stop=True), reads=[ones, cntp], writes=[pc])
                P.op("dve", lambda e: e.tensor_tensor(out=tt[:], in0=pc[:, 0:8], in1=cap[:], op=ALU.is_ge), reads=[pc, cap], writes=[tt])
                P.op("dve", lambda e, dl=dl: e.scalar_tensor_tensor(out=lo[:], in0=tt[:], scalar=dl, in1=lo[:], op0=ALU.mult, op1=ALU.add), reads=[tt, lo], writes=[lo])
            m3 = mask[:].rearrange("p (l i) -> p l i", l=8)
            P.op("dve", lambda e: e.tensor_tensor(out=m3, in0=A3, in1=lo[:].unsqueeze(2).to_broadcast([128, 8, 64]), op=ALU.is_ge), reads=[A8, lo], writes=[mask])
            P.op("dve", lambda e: e.tensor_scalar(out=m0[:], in0=A8[:], scalar1=0.0, scalar2=None, op0=ALU.is_gt), reads=[A8], writes=[m0])
            P.op("dve", lambda e: e.tensor_tensor(out=mask[:], in0=mask[:], in1=m0[:], op=ALU.mult), reads=[mask, m0], writes=[mask])
            cu3 = cum[:].rearrange("p (l i) -> p l i", l=8)
            for l in range(8):
                P.op("dve", lambda e, l=l: e.tensor_tensor_scan(out=cum[:, l * 64:(l + 1) * 64], data0=mask[:, l * 64:(l + 1) * 64], data1=zeros[:, 0:64], initial=0.0, op0=ALU.add, op1=ALU.add),
                     reads=[mask, zeros], writes=[cum])
            P.op("dve", lambda e: e.tensor_copy(out=totp[:], in_=cu3[:, :, 63]), reads=[cum], writes=[totp])
            P.op("pe", lambda e: e.matmul(pc[:, 0:8], lhsT=stri[:], rhs=totp[:], start=True, stop=True), reads=[stri, totp], writes=[pc])
            P.op("act", lambda e: e.copy(out=offs[:], in_=pc[:, 0:8]), reads=[pc], writes=[offs])
            P.op("dve", lambda e: e.tensor_scalar(out=s2[:], in0=mask[:], scalar1=-BIG, scalar2=BIG - 1.0, op0=ALU.mult, op1=ALU.add), reads=[mask], writes=[s2])
            P.op("dve", lambda e: e.tensor_tensor(out=cu3, in0=cu3, in1=offs[:].unsqueeze(2).to_broadcast([128, 8, 64]), op=ALU.add), reads=[cum, offs], writes=[cum])
            P.op("dve", lambda e: e.tensor_tensor(out=cum[:], in0=cum[:], in1=s2[:], op=ALU.add), reads=[cum, s2], writes=[cum])
            P.op("dve", lambda e: e.tensor_copy(out=sloti[:], in_=cum[:]), reads=[cum], writes=[sloti])
            Sraw = T("Sraw", [128, 512], I32)
            P.op("dve", lambda e: e.tensor_copy(out=Sraw[:].rearrange("p (i l) -> p l i", l=8), in_=sloti[:].rearrange("p (l i) -> p l i", l=8)), reads=[sloti], writes=[Sraw])
            Sr3 = Sraw[:].rearrange("p (i l) -> p i l", l=8)
            P.dma(lambda e: e.dma_start(out=SLOT8[:, :, 4 * g:4 * g + 4], in_=Sr3[:, :, 0:4]), reads=[Sraw], writes=["slot8"])
            P.dma(lambda e: e.dma_start(out=SLOT8[:, :, 16 + 4 * g:16 + 4 * g + 4], in_=Sr3[:, :, 4:8]), reads=[Sraw], writes=["slot8"])
            s3 = sloti[:].rearrange("p (l i) -> p l i", l=8)
            R = [T("R%d" % i, [128, D], BF16) for i in range(3)]
            nR = 0
            hv = h2l.rearrange("(i p) d -> p i d", p=128)
            for i in range(64):
                r = R[nR % 3]; nR += 1
                P.dma(lambda e, r=r, i=i: e.dma_start(out=r[:], in_=hv[:, i, :]), writes=[r])
                for lane in range(4):
                    P.dma(lambda e, r=r, lane=lane, i=i: e.indirect_dma_start(out=xin_l[lane][:, :], out_offset=bass.IndirectOffsetOnAxis(ap=s3[:, lane, i:i + 1], axis=0),
                                                                             in_=r[:, :], in_offset=None, bounds_check=P.getreg(e, CAPL - 1), oob_is_err=False),
                          eng="pool", reads=[r, sloti], writes=["xin_l"])
            hv2 = h2c.rearrange("(i p) d -> p i d", p=128)
            for i in range(2):
                r = R[nR % 3]; nR += 1
                P.dma(lambda e, r=r, i=i: e.dma_start(out=r[:], in_=hv2[:, i, :]), writes=[r])
                for lane in range(4, 8):
                    P.dma(lambda e, r=r, lane=lane, i=i: e.indirect_dma_start(out=xin_c[lane - 4][:, :], out_offset=bass.IndirectOffsetOnAxis(ap=s3[:, lane, i:i + 1], axis=0),
                                                                             in_=r[:, :], in_offset=None, bounds_check=P.getreg(e, CAPC - 1), oob_is_err=False),
                          eng="pool", reads=[r, sloti], writes=["xin_c"])
            P.barrier()
        if True:
            st = st0
            T = lambda name, shape, dt=F32: st.enter_context(nc.sbuf_tensor(pre + name, shape, dt))
            Wg = T("Wg", [128, 16 * FF], BF16); Wu = T("Wu", [128, 16 * FF], BF16); Wd = T("Wd", [128, 8 * D], BF16)
            Wg3 = Wg[:].rearrange("p (k n) -> p k n", k=16); Wu3 = Wu[:].rearrange("p (k n) -> p k n", k=16); Wd3 = Wd[:].rearrange("p (k n) -> p k n", k=8)
            XT = T("XT", [128, 16 * 512], BF16); XT3 = XT[:].rearrange("p (k s) -> p k s", k=16)
            ATa = T("ATa", [128, 8 * 512], BF16); ATb = T("ATb", [128, 8 * 512], BF16); ATc = T("ATc", [128, 8 * 32], BF16)
            xs = [T("xs%d" % i, [128, D], BF16) for i in range(2)]
            ysb = [T("ysb%d" % i, [128, D]) for i in range(2)]
            sa = [T("sa%d" % i, [128, 512]) for i in range(2)]
            cnt = {"x": 0, "y": 0, "s": 0}

            def ffn_gu(xsrc, ns, AT):
                AT3 = AT[:].rearrange("p (k s) -> p k s", k=8)
                tiles = [(s, min(128, ns - s)) for s in range(0, ns, 128)]
                for (s0, rows) in tiles:
                    x = xs[cnt["x"] % 2]; cnt["x"] += 1
                    P.dma(lambda e, x=x, s0=s0, rows=rows: e.dma_start(out=x[:rows, :], in_=xsrc[s0:s0 + rows, :]), reads=["xin_l", "xin_c"], writes=[x])
                    for half in range(2):
                        pb = psb[half]
                        for k in range(8):
                            kc = half * 8 + k
                            P.op("pe", lambda e, pb=pb, x=x, k=k, kc=kc, rows=rows: e.transpose(out=pb[:, k * 128:k * 128 + rows], in_=x[:rows, kc * 128:(kc + 1) * 128], identity=identb[:rows, :rows]),
                                 reads=[x, identb], writes=[pb])
                        src = pb[:, :].rearrange("p (k t) -> p k t", k=8)[:, :, 0:rows]
                        dst = XT3[:, half * 8:(half + 1) * 8, s0:s0 + rows]
                        if half == 0:
                            P.op("act", lambda e, src=src, dst=dst: e.copy(out=dst, in_=src), reads=[pb], writes=[XT])
                        else:
                            P.op("dve", lambda e, src=src, dst=dst: e.tensor_copy(out=dst, in_=src), reads=[pb], writes=[XT])
                for ft in range(8):
                    pa = psf[ft % 2]; pu = psf[2 + ft % 2]; s_ = sa[ft % 2]
                    for kc in range(16):
                        P.op("pe", lambda e, pa=pa, kc=kc, ft=ft: e.matmul(pa[:, 0:ns], lhsT=Wg3[:, kc, ft * 128:(ft + 1) * 128], rhs=XT3[:, kc, 0:ns], start=(kc == 0), stop=(kc == 15)),
                             reads=[Wg, XT], writes=[pa])
                    for kc in range(16):
                        P.op("pe", lambda e, pu=pu, kc=kc, ft=ft: e.matmul(pu[:, 0:ns], lhsT=Wu3[:, kc, ft * 128:(ft + 1) * 128], rhs=XT3[:, kc, 0:ns], start=(kc == 0), stop=(kc == 15)),
                             reads=[Wu, XT], writes=[pu])
                    P.op("act", lambda e, pa=pa, s_=s_: e.activation(out=s_[:, 0:ns], in_=pa[:, 0:ns], func=AF.Silu), reads=[pa], writes=[s_])
                    P.op("dve", lambda e, pu=pu, s_=s_, ft=ft: e.tensor_tensor(out=AT3[:, ft, 0:ns], in0=s_[:, 0:ns], in1=pu[:, 0:ns], op=ALU.mult), reads=[s_, pu], writes=[(AT, ft)])

            def ffn_down(ydst, ns, AT):
                AT3 = AT[:].rearrange("p (k s) -> p k s", k=8)
                tiles = [(s, min(128, ns - s)) for s in range(0, ns, 128)]
                ATk = [(AT, ft) for ft in range(8)]
                for (s0, rows) in tiles:
                    y = ysb[cnt["y"] % 2]; cnt["y"] += 1
                    for nb in range(4):
                        pd = psf[4 + nb % 2]
                        for fc in range(8):
                            P.op("pe", lambda e, pd=pd, fc=fc, nb=nb, s0=s0, rows=rows: e.matmul(pd[:rows, :], lhsT=AT3[:, fc, s0:s0 + rows], rhs=Wd3[:, fc, nb * 512:(nb + 1) * 512], start=(fc == 0), stop=(fc == 7)),
                                 reads=ATk + [Wd], writes=[pd])
                        if nb % 2 == 0:
                            P.op("act", lambda e, pd=pd, y=y, nb=nb, rows=rows: e.copy(out=y[:rows, nb * 512:(nb + 1) * 512], in_=pd[:rows, :]), reads=[pd], writes=[(y, nb)])
                        else:
                            P.op("dve", lambda e, pd=pd, y=y, nb=nb, rows=rows: e.tensor_copy(out=y[:rows, nb * 512:(nb + 1) * 512], in_=pd[:rows, :]), reads=[pd], writes=[(y, nb)])
                    P.dma(lambda e, y=y, s0=s0, rows=rows: e.dma_start(out=ydst[s0:s0 + rows, :], in_=y[:rows, :]), reads=[(y, nb) for nb in range(4)], writes=["yout"], final=True)

            for el in range(4):
                ex = 4 * g + el
                for kc in range(16):
                    P.dma(lambda e, kc=kc, ex=ex: e.dma_start(out=Wg3[:, kc, :], in_=wg[ex, kc * 128:(kc + 1) * 128, :]), eng="pool", writes=[Wg])
                    P.dma(lambda e, kc=kc, ex=ex: e.dma_start(out=Wu3[:, kc, :], in_=wu[ex, kc * 128:(kc + 1) * 128, :]), eng="pool", writes=[Wu])
                for kc in range(8):
                    P.dma(lambda e, kc=kc, ex=ex: e.dma_start(out=Wd3[:, kc, :], in_=wd[ex, kc * 128:(kc + 1) * 128, :]), eng="pool", writes=[Wd])
                ffn_gu(xin_l[el][0:512, :], 512, ATa)
                ffn_gu(xin_l[el][512:1024, :], 512, ATb)
                ffn_gu(xin_c[el], CAPC, ATc)
                ffn_down(io["YL"][ex][0:512, :], 512, ATa)
                ffn_down(io["YL"][ex][512:1024, :], 512, ATb)
                ffn_down(io["YC"][ex], CAPC, ATc)
            P.end_phase()


BF = ml_dtypes.bfloat16
D = 2048; FF = 1024; N = 8192; NCTX = 256

def emit_k0(nc, P, pre, io):
    cT = io["cT"]; w = io["ada_w"]; b = io["ada_b"]; MOD = io["MOD"]
    with contextlib.ExitStack() as st:
        T = lambda name, shape, dt=F32: st.enter_context(nc.sbuf_tensor(pre + name, shape, dt))
        ps = [P.psum(st, pre + "ps%d" % i, [128, 512], F32) for i in range(2)]
        cs = T("cs", [128, 32]); ss = T("ss", [128, 32])
        wb = [T("wb%d" % i, [128, 16 * 512]) for i in range(3)]
        bt = [T("bt%d" % i, [2, 512]) for i in range(2)]; ot = [T("ot%d" % i, [2, 512]) for i in range(2)]
        zt = T("zt", [128, 2048])
        P.op("pool", lambda e: e.memset(zt[:], 0.0), writes=[zt])
        P.dma(lambda e: e.dma_start(out=io["AFF8"].rearrange("p i l -> p (i l)"), in_=zt[:]), reads=[zt], writes=["aff8"])
        P.dma(lambda e: e.dma_start(out=cs[:], in_=cT), writes=[cs])
        P.op("act", lambda e: e.activation(out=ss[:], in_=cs[:], func=AF.Silu), reads=[cs], writes=[ss])
        blk = 0
        for i in range(4):
            wv = w[i].rearrange("(kc p) n -> p kc n", p=128)
            for j in range(24):
                wt = wb[blk % 3]; pt = ps[blk % 2]; btj = bt[blk % 2]; otj = ot[blk % 2]
                c0 = j * 512
                P.dma(lambda e, wt=wt, wv=wv, c0=c0: e.dma_start(out=wt[:].rearrange("p (kc n) -> p kc n", kc=16), in_=wv[:, :, c0:c0 + 512]), writes=[wt])
                P.dma(lambda e, btj=btj, i=i, c0=c0: e.dma_start(out=btj[:], in_=b[i:i + 1, c0:c0 + 512].to_broadcast([2, 512])), writes=[btj])
                for kc in range(16):
                    P.op("pe", lambda e, pt=pt, wt=wt, kc=kc: e.matmul(pt[0:2, :], lhsT=ss[:, kc * 2:(kc + 1) * 2], rhs=wt[:, kc * 512:(kc + 1) * 512], start=(kc == 0), stop=(kc == 15)),
                         reads=[ss, wt], writes=[pt])
                P.op("dve", lambda e, pt=pt, btj=btj, otj=otj: e.tensor_tensor(out=otj[:], in0=pt[0:2, :], in1=btj[:], op=ALU.add), reads=[pt, btj], writes=[otj])
                P.dma(lambda e, otj=otj, i=i, c0=c0: e.dma_start(out=MOD[i, :, c0:c0 + 512], in_=otj[:]), reads=[otj], writes=["mod"])
                blk += 1
        P.end_phase()

def build_fused(nlayers=4):
    nc = bass.Bass("TRN2", target_bir_lowering=False)
    DI = lambda n, s, dt=F32: nc.dram_tensor(n, s, dt, kind="ExternalInput").ap()
    SC = lambda n, s, dt=F32: nc.dram_tensor(n, s, dt, kind="Internal").ap()
    x = DI("x", [N, D]); ctx = DI("ctx", [NCTX, D]); cT = DI("cT", [128, 32])
    ada_w = DI("ada_w", [4, D, 12288]); ada_b = DI("ada_b", [4, 12288]); norm_g = DI("norm_g", [4, 2, D]); final_g = DI("final_g", [1, D])
    ev_w_in = DI("ev_w_in", [2, D, 5152]); ev_gate_b = DI("ev_gate_b", [2, 4, 8]); ev_head_g = DI("ev_head_g", [2, 1024])
    ev_pool_w = DI("ev_pool_w", [2, 4, 256, 256]); ev_pool_scale = DI("ev_pool_scale", [2, 1024]); ev_w_out = DI("ev_w_out", [2, D, D])
    na_w_in = DI("na_w_in", [2, D, 6144]); na_bias = DI("na_bias", [2, 16, 128, 3200]); na_w_out = DI("na_w_out", [2, D, D])
    w_router = DI("moe_w_router", [4, D, 16]); w_gate = DI("moe_w_gate", [4, 16, D, FF]); w_up = DI("moe_w_up", [4, 16, D, FF]); w_down = DI("moe_w_down", [4, 16, FF, D])
    identb = DI("identb", [128, 128], BF16); identf = DI("identf", [128, 128]); tri = DI("tri", [2, 128, 128]); stri = DI("stri", [128, 128])
    tab = DI("tab", [66, 128, 256]); icl = DI("icl", [4, N]); icc = DI("icc", [4, NCTX]); capv = DI("capv", [128, 8])
    out = nc.dram_tensor("out", [N, D], F32, kind="ExternalOutput").ap()
    MOD = SC("MOD", [4, 2, 12288]); XL = SC("XL", [N, D]); XC = SC("XC", [NCTX, D])
    HT = SC("HT", [66, 128, 2048], BF16); MIXT = SC("MIXT", [66, 128, 16, 128], BF16)
    H2L = SC("H2L", [N, D], BF16); H2C = SC("H2C", [NCTX, D], BF16)
    AFF8 = SC("AFF8", [128, 64, 32]); SLOT8 = SC("SLOT8", [128, 64, 32], I32)
    YL = [SC("YL%d" % i, [1024, D]) for i in range(16)]; YC = [SC("YC%d" % i, [32, D]) for i in range(16)]
    XINL = [SC("XINL%d" % i, [1024, D], BF16) for i in range(4)]; XINC = [SC("XINC%d" % i, [32, D], BF16) for i in range(4)]
    P = Prog(nc)
    emit_k0(nc, P, "k0_", {"cT": cT, "ada_w": ada_w, "ada_b": ada_b, "MOD": MOD, "AFF8": AFF8})
    mrows = lambda i, r: MOD[i, r].rearrange("(s d) -> s d", s=6)

    def g1_io(i, q, comb, final):
        src_l, src_c = (x, ctx) if (i == 0 and not comb) else (XL, XC)
        io = {"xl": src_l[q * 2048:(q + 1) * 2048, :], "xc": src_c[q * 64:(q + 1) * 64, :], "ident": identb}
        if final:
            io["g"] = final_g; io["outl"] = out[q * 2048:(q + 1) * 2048, :]
        else:
            io["g"] = norm_g[i, 0:1, :]; io["modl"] = mrows(i, 0); io["modc"] = mrows(i, 1)
            io["hTl"] = [HT[2 + q * 16 + it] for it in range(16)]
            io["hTc"] = HT[q // 2].rearrange("p (k x) -> p k x", k=16)[:, :, (q % 2) * 64:(q % 2 + 1) * 64]
        if comb:
            ip = i - 1
            io.update({"yl": YL, "yc": YC, "slotl": [SLOT8[:, q * 16 + it, 0:16] for it in range(16)], "affl": [AFF8[:, q * 16 + it, 0:16] for it in range(16)],
                       "slotc": SLOT8[(q % 2) * 64:(q % 2 + 1) * 64, q // 2, 16:32], "affc": AFF8[(q % 2) * 64:(q % 2 + 1) * 64, q // 2, 16:32],
                       "gafl": mrows(ip, 0)[5:6, :], "gafc": mrows(ip, 1)[5:6, :], "xlo": XL[q * 2048:(q + 1) * 2048, :], "xco": XC[q * 64:(q + 1) * 64, :]})
        return io

    for i in range(nlayers):
        j = i // 2
        for q in range(4):
            emit_g1(nc, P, "L%dg1q%d_" % (i, q), g1_io(i, q, i > 0, False), i > 0, False)
        if i % 2 == 0:
            for hp in range(4):
                emit_mixe(nc, P, "L%dmxh%d_" % (i, hp), {"HT": HT, "MIXT": MIXT, "w_in": ev_w_in[j], "gate_b": ev_gate_b[j], "head_g": ev_head_g[j:j + 1, :],
                                                         "tab": tab, "identb": identb, "identf": identf, "tri": tri, "pool_w": ev_pool_w[j],
                                                         "pool_scale": ev_pool_scale[j], "icl": icl, "icc": icc}, hp)
            wout = ev_w_out[j]
        else:
            for hq in range(4):
                emit_mixo(nc, P, "L%dmxh%d_" % (i, hq), {"HT": HT, "MIXT": MIXT, "w_in": na_w_in[j], "bias": na_bias[j], "identb": identb}, hq, i != 3)
            wout = na_w_out[j]
        for q in range(4):
            src_l, src_c = (x, ctx) if i == 0 else (XL, XC)
            emit_g2(nc, P, "L%dg2q%d_" % (i, q), {
                "xl": src_l[q * 2048:(q + 1) * 2048, :], "xc": src_c[q * 64:(q + 1) * 64, :],
                "mixl": [MIXT[2 + q * 16 + it].rearrange("p k x -> p (k x)") for it in range(16)],
                "mixc": MIXT[q // 2][:, :, (q % 2) * 64:(q % 2 + 1) * 64], "wout": wout, "wr": w_router[i],
                "modl": mrows(i, 0), "modc": mrows(i, 1), "g": norm_g[i, 1:2, :], "identf": identf,
                "xmo_l": XL[q * 2048:(q + 1) * 2048, :], "xmo_c": XC[q * 64:(q + 1) * 64, :],
                "h2_l": H2L[q * 2048:(q + 1) * 2048, :], "h2_c": H2C[q * 64:(q + 1) * 64, :],
                "aff_l": [AFF8[:, q * 16 + it, 0:16] for it in range(16)], "aff_c": AFF8[(q % 2) * 64:(q % 2 + 1) * 64, q // 2, 16:32]})
        for g in range(4):
            emit_moe(nc, P, "L%dmoe%d_" % (i, g), {"AFF8": AFF8, "SLOT8": SLOT8, "capv": capv, "H2L": H2L, "H2C": H2C, "wg": w_gate[i], "wu": w_up[i], "wd": w_down[i],
                                                    "stri": stri, "identb": identb, "YL": YL, "YC": YC, "XINL": XINL, "XINC": XINC}, g)
    for q in range(4):
        emit_g1(nc, P, "Fg1q%d_" % q, g1_io(nlayers, q, True, True), True, True)
    P.flush(True)
    return nc

def host_consts(na_rpb):
    c = {}
    c["identb"] = np.eye(128, dtype=BF); c["identf"] = np.eye(128, dtype=np.float32)
    l = np.arange(128)
    c["tri"] = np.stack([(l[:, None] <= l[None, :]), (l[:, None] >= l[None, :])]).astype(np.float32)
    c["stri"] = (l[:, None] < l[None, :]).astype(np.float32)
    capv = np.zeros((128, 8), np.float32); capv[:, :4] = 1024; capv[:, 4:] = 32
    c["capv"] = capv
    tab = np.zeros((66, 128, 2, 2, 2, 32), np.float32); sc = 128.0 ** -0.5
    tab[0:2, :, 0, 0] = 1.0; tab[0:2, :, 0, 1] = sc
    inv = (10000.0 ** (-np.arange(32, dtype=np.float32) / 32)).astype(np.float32)
    t = np.arange(N); rows = (t // 64).astype(np.float32); cols = (t % 64).astype(np.float32)
    for blk, pos in ((0, rows), (1, cols)):
        ang = (pos[:, None] * inv[None, :]).astype(np.float32)
        cs = np.cos(ang).astype(np.float32).reshape(64, 128, 32); sn = np.sin(ang).astype(np.float32).reshape(64, 128, 32)
        tab[2:, :, 0, 0, blk] = cs; tab[2:, :, 1, 0, blk] = sn; tab[2:, :, 0, 1, blk] = cs * sc; tab[2:, :, 1, 1, blk] = sn * sc
    c["tab"] = tab.reshape(66, 128, 256)
    def inv_cnt(n):
        t = np.arange(n); o = np.zeros((4, n), np.float32)
        for g, w in enumerate((2, 4, 8, 16)):
            lo = np.clip(t - w // 2, 0, n); hi = np.clip(t + w // 2, 0, n); o[g] = 1.0 / (hi - lo)
        return o
    c["icl"] = inv_cnt(N); c["icc"] = inv_cnt(NCTX)
    per_m, idxmaps = na_patterns()
    nb = np.zeros((2, 16, 128, 5, 5, 128), np.float32)
    for j in range(2):
        rf = na_rpb[j].reshape(16, 15 * 31)
        for h in range(16):
            g = np.where(idxmaps >= 0, rf[h][np.maximum(idxmaps, 0)], np.float32(-30000.0))
            nb[j, h] = g.transpose(2, 0, 1, 3)
    c["na_bias"] = nb.reshape(2, 16, 128, 3200)
    return c


def kernel(x, c, ctx, c_ctx, ada_w, ada_b, norm_g, final_g, ev_w_in, ev_gate_b, ev_head_g, ev_pool_w,
           ev_pool_scale, ev_w_out, na_w_in, na_rpb, na_w_out, moe_w_router, moe_w_gate, moe_w_up, moe_w_down):
    A = lambda a: np.ascontiguousarray(np.asarray(a, dtype=np.float32))
    x = A(x); ctx = A(ctx); c = A(c); c_ctx = A(c_ctx)
    shared = {"ada_w": A(ada_w), "ada_b": A(ada_b), "norm_g": A(norm_g), "final_g": A(final_g)[None, :],
              "ev_w_in": A(ev_w_in), "ev_gate_b": A(ev_gate_b), "ev_head_g": A(ev_head_g), "ev_pool_w": A(ev_pool_w),
              "ev_pool_scale": A(ev_pool_scale), "ev_w_out": A(ev_w_out), "na_w_in": A(na_w_in), "na_w_out": A(na_w_out),
              "moe_w_router": A(moe_w_router), "moe_w_gate": A(moe_w_gate), "moe_w_up": A(moe_w_up), "moe_w_down": A(moe_w_down)}
    shared.update(host_consts(A(na_rpb)))
    nc = build_fused(4)
    in_maps = []
    for b in range(2):
        cv = np.stack([c[b], c_ctx])
        m = {"x": x[b], "ctx": ctx[b], "cT": np.ascontiguousarray(cv.reshape(2, 16, 128).transpose(2, 1, 0)).reshape(128, 32)}
        m.update(shared)
        in_maps.append(m)
    res = run_bass_kernel_spmd(nc, in_maps, core_ids=[0, 1])
    return np.stack([res.results[0]["out"], res.results[1]["out"]]).astype(np.float32)
```

```python
import contextlib, time
import numpy as np
import ml_dtypes


import numpy as np
import concourse.bass as bass
import concourse.mybir as mybir
from concourse.bass_utils import run_bass_kernel_spmd

F32 = mybir.dt.float32
BF16 = mybir.dt.bfloat16
I32 = mybir.dt.int32
U32 = mybir.dt.uint32
AF = mybir.ActivationFunctionType
ALU = mybir.AluOpType
AX = mybir.AxisListType

ENGS = ("pe", "act", "dve", "pool", "sp")
DMA_Q = {"sp": 16, "pool": 8, "act": 4}
DMA_SEMS = [(q, i) for q, n in DMA_Q.items() for i in range(n)]


class Prog:
    def __init__(self, nc, same_engine_sync=True):
        self.nc = nc
        self.ops = {e: [] for e in ENGS}
        self.cnt = {e: 0 for e in ENGS}
        self.last_write = {}
        self.reads = {}
        self.waited = {e: {} for e in ENGS}
        self.dma_uses = {k: 0 for k in DMA_SEMS}
        self.dma_rr = {q: 0 for q in DMA_Q}
        self.same_engine_sync = same_engine_sync
        self.final_tokens = []
        self.n_ops = 0
        self.psum_names = set()
        import contextlib
        self._st = contextlib.ExitStack()
        self.sems = {}
        for e in ENGS:
            self.sems[("eng", e)] = self._st.enter_context(nc.semaphore("sem_" + e))
        for i in DMA_SEMS:
            self.sems[("dma", i)] = self._st.enter_context(nc.semaphore("sem_dma_%s%d" % i))

    def psum(self, st, name, shape, dt):
        self.psum_names.add(name)
        return st.enter_context(self.nc.psum_tensor(name, shape, dt))

    @staticmethod
    def _key(x):
        if isinstance(x, tuple):
            return (Prog._key(x[0]), x[1])
        if isinstance(x, str):
            return x
        if hasattr(x, 'tensor'):
            return x.tensor.name
        return x.name

    def _deps(self, eng, reads, writes):
        toks = []
        for k in list(reads) + list(writes):
            t = self.last_write.get(k)
            if t is not None:
                toks.append(t)
        for k in writes:
            toks.extend(self.reads.get(k, ()))
        for k in reads:
            kn = k[0] if isinstance(k, tuple) else k
            if isinstance(kn, str) and (kn.startswith("ps") or kn in self.psum_names):
                for t in self.reads.get(k, ()):
                    if t[0] != ("eng", eng):
                        toks.append(t)
        best = {}
        for (s, v) in toks:
            if best.get(s, -1) < v:
                best[s] = v
        out = []
        w = self.waited[eng]
        for s, v in best.items():
            if (not self.same_engine_sync) and s == ("eng", eng) and eng != "sp":
                continue
            if eng == "pe" and s == ("eng", "pe"):
                continue
            if w.get(s, -1) >= v:
                continue
            w[s] = v
            out.append((s, v))
        return out

    def _commit(self, tok, reads, writes):
        for k in writes:
            self.last_write[k] = tok
            self.reads[k] = []
        for k in reads:
            self.reads.setdefault(k, []).append(tok)

    def op(self, eng, fn, reads=(), writes=()):
        reads = [self._key(r) for r in reads]
        writes = [self._key(r) for r in writes]
        waits = self._deps(eng, reads, writes)
        self.cnt[eng] += 1
        tok = (("eng", eng), self.cnt[eng])
        self.ops[eng].append((waits, fn, tok))
        self._commit(tok, reads, writes)
        self.n_ops += 1
        return tok

    def dma(self, fn, reads=(), writes=(), eng="sp", final=False):
        reads = [self._key(r) for r in reads]
        writes = [self._key(r) for r in writes]
        i = (eng, self.dma_rr[eng])
        self.dma_rr[eng] = (self.dma_rr[eng] + 1) % DMA_Q[eng]
        s = ("dma", i)
        waits = self._deps(eng, reads, writes)
        prev = self.dma_uses[i] * 16
        if prev > 0 and self.waited[eng].get(s, -1) < prev:
            self.waited[eng][s] = prev
            waits.append((s, prev))
        self.dma_uses[i] += 1
        tok = (s, self.dma_uses[i] * 16)
        self.ops[eng].append((waits, fn, tok))
        self._commit(tok, reads, writes)
        if final:
            self.final_tokens.append(tok)
        self.n_ops += 1
        return tok

    def getreg(self, engh, val):
        if not hasattr(self, "_regs"):
            self._regs = {}
        k = (type(engh).__name__, val)
        if k not in self._regs:
            self._regs[k] = engh.to_reg(val)
        return self._regs[k]

    def barrier(self):
        toks = []
        for e in ENGS:
            if e != "sp" and self.cnt[e] > 0:
                toks.append((("eng", e), self.cnt[e]))
        for i in DMA_SEMS:
            if self.dma_uses[i] > 0:
                toks.append((("dma", i), self.dma_uses[i] * 16))
        for e in ENGS:
            waits = []
            for (s, v) in toks:
                if s == ("eng", e):
                    continue
                if self.waited[e].get(s, -1) < v:
                    self.waited[e][s] = v
                    waits.append((s, v))
            if waits:
                self.ops[e].append((waits, None, None))

    def flush(self, last=False):
        nc = self.nc
        sems = self.sems
        handles = {"pe": "tensor", "act": "scalar", "dve": "vector", "pool": "gpsimd", "sp": "sync"}
        with nc.Block() as block:
            def make(e):
                def body(engh):
                    for (waits, fn, tok) in self.ops[e]:
                        for (s, v) in waits:
                            engh.wait_ge(sems[s], v)
                        if fn is None:
                            continue
                        inst = fn(engh)
                        inst.then_inc(sems[tok[0]], 16 if tok[0][0] == "dma" else 1)
                    if e == "sp" and last:
                        for (s, v) in self.final_tokens:
                            engh.wait_ge(sems[s], v)
                return body
            for e in ENGS:
                getattr(block, handles[e])(make(e))
        self.ops = {e: [] for e in ENGS}

    def end_phase(self, last=False):
        self.barrier()
        self.flush(last)

    def emit(self):
        self.flush(True)


D = 2048; EPS = 1e-6
CAP_L = 1024; CAP_C = 32

def emit_g1(nc, P, pre, io, combine, final):
    xl = io["xl"]; xc = io["xc"]; gv = io["g"]; ident_d = io["ident"]
    if not final:
        modl = io["modl"]; modc = io["modc"]; hTl = io["hTl"]; hTc = io["hTc"]
    else:
        outl = io["outl"]
    if combine:
        yl = io["yl"]; yc = io["yc"]; slotl = io["slotl"]; slotc = io["slotc"]; affl = io["affl"]; affc = io["affc"]
        gafl = io["gafl"]; gafc = io["gafc"]; xlo = io["xlo"]; xco = io["xco"]
    with contextlib.ExitStack() as st:
        T = lambda name, shape, dt=F32: st.enter_context(nc.sbuf_tensor(pre + name, shape, dt))
        ident = T("ident_s", [128, 128], BF16)
        P.dma(lambda e: e.dma_start(out=ident[:], in_=ident_d), writes=[ident])
        g_bc = T("g_bc", [128, D])
        P.dma(lambda e: e.dma_start(out=g_bc[:], in_=gv.to_broadcast([128, D])), writes=[g_bc])
        AB = {}
        if not final:
            tmp = T("tmpbc", [128, D])
            for nm, mod in (("l", modl), ("c", modc)):
                A = T("A_" + nm, [128, D]); Bt = T("B_" + nm, [128, D])
                P.dma(lambda e, mod=mod: e.dma_start(out=tmp[:], in_=mod[1:2, :].to_broadcast([128, D])), writes=[tmp])
                P.dma(lambda e, mod=mod, Bt=Bt: e.dma_start(out=Bt[:], in_=mod[0:1, :].to_broadcast([128, D])), writes=[Bt])
                P.op("dve", lambda e, A=A: e.scalar_tensor_tensor(out=A[:], in0=tmp[:], scalar=1.0, in1=g_bc[:], op0=ALU.add, op1=ALU.mult),
                     reads=[tmp, g_bc], writes=[A])
                AB[nm] = (A, Bt)
        if combine:
            gaf = {}
            for nm, src in (("l", gafl), ("c", gafc)):
                t = T("gaf_" + nm, [128, D])
                P.dma(lambda e, t=t, src=src: e.dma_start(out=t[:], in_=src.to_broadcast([128, D])), writes=[t])
                gaf[nm] = t
            yg = [T("yg%d" % i, [128, D]) for i in range(2)]
            for t in yg:
                P.op("pool", lambda e, t=t: e.memset(t[:], 0.0), writes=[t])
            acc = T("acc", [128, D])
            slot_t = [T("slot%d" % i, [128, 16], I32) for i in range(2)]
            slot_f = T("slotf", [128, 16]); aff_t = [T("afft%d" % i, [128, 16]) for i in range(2)]
            ge = T("ge", [128, 16])
        xt = [T("xt%d" % i, [128, D]) for i in range(2)]
        h1 = T("h1", [128, D])
        ss = T("ss", [128, 1]); rstd = T("rstd", [128, 1])
        if not final:
            hb = [T("hb%d" % i, [128, D], BF16) for i in range(2)]
            stg = [T("stg%d" % i, [128, D], BF16) for i in range(2)]
            pst = [P.psum(st, pre + "pst%d" % i, [128, 1024], BF16) for i in range(4)]
        else:
            ob = [T("ob%d" % i, [128, D]) for i in range(2)]

        for it in range(17):
            isl = it < 16
            rows = 128 if isl else 64
            nm = "l" if isl else "c"
            x = xt[it % 2]
            src = xl[it * 128:(it + 1) * 128, :] if isl else xc
            P.dma(lambda e, x=x, src=src, rows=rows: e.dma_start(out=x[:rows, :], in_=src), writes=[x])
            if combine:
                stt = slot_t[it % 2]; aft = aff_t[it % 2]
                if isl:
                    P.dma(lambda e, stt=stt, s=slotl[it]: e.dma_start(out=stt[:], in_=s), writes=[stt])
                    P.dma(lambda e, aft=aft, s=affl[it]: e.dma_start(out=aft[:], in_=s), writes=[aft])
                else:
                    P.op("pool", lambda e, stt=stt: e.memset(stt[:], 32768), writes=[stt])
                    P.op("pool", lambda e, aft=aft: e.memset(aft[:], 0.0), writes=[aft])
                    P.dma(lambda e, stt=stt: e.dma_start(out=stt[:64, :], in_=slotc), writes=[stt])
                    P.dma(lambda e, aft=aft: e.dma_start(out=aft[:64, :], in_=affc), writes=[aft])
                cap = CAP_L if isl else CAP_C
                ysrc = yl if isl else yc
                P.op("dve", lambda e, stt=stt: e.tensor_copy(out=slot_f[:], in_=stt[:]), reads=[stt], writes=[slot_f])
                P.op("dve", lambda e, cap=cap: e.tensor_scalar(out=ge[:], in0=slot_f[:], scalar1=float(cap) - 0.5, scalar2=None, op0=ALU.is_lt),
                     reads=[slot_f], writes=[ge])
                P.op("dve", lambda e, aft=aft: e.tensor_tensor(out=ge[:], in0=ge[:], in1=aft[:], op=ALU.mult), reads=[ge, aft], writes=[ge])
                for ex in range(16):
                    y = yg[ex % 2]
                    P.dma(lambda e, y=y, ex=ex, stt=stt, ysrc=ysrc, cap=cap: e.indirect_dma_start(
                        out=y[:, :], out_offset=None, in_=ysrc[ex][:, :],
                        in_offset=bass.IndirectOffsetOnAxis(ap=stt[:, ex:ex + 1], axis=0),
                        bounds_check=P.getreg(e, cap - 1), oob_is_err=False), eng="pool", reads=[stt], writes=[y])
                    if ex == 0:
                        P.op("dve", lambda e, y=y, ex=ex, rows=rows: e.tensor_scalar(out=acc[:rows, :], in0=y[:rows, :], scalar1=ge[:rows, ex:ex + 1], scalar2=None, op0=ALU.mult),
                             reads=[y, ge], writes=[acc])
                    else:
                        P.op("dve", lambda e, y=y, ex=ex, rows=rows: e.scalar_tensor_tensor(out=acc[:rows, :], in0=y[:rows, :], scalar=ge[:rows, ex:ex + 1], in1=acc[:rows, :], op0=ALU.mult, op1=ALU.add),
                             reads=[y, ge, acc], writes=[acc])
                gt = gaf[nm]
                P.op("dve", lambda e, rows=rows, gt=gt: e.tensor_tensor(out=acc[:rows, :], in0=acc[:rows, :], in1=gt[:rows, :], op=ALU.mult), reads=[acc, gt], writes=[acc])
                P.op("dve", lambda e, rows=rows, x=x: e.tensor_tensor(out=x[:rows, :], in0=x[:rows, :], in1=acc[:rows, :], op=ALU.add), reads=[acc, x], writes=[x])
                dst = xlo[it * 128:(it + 1) * 128, :] if isl else xco
                P.dma(lambda e, x=x, dst=dst, rows=rows: e.dma_start(out=dst, in_=x[:rows, :]), reads=[x], writes=["xo"], final=True)
            if final and not isl:
                continue
            P.op("act", lambda e, x=x, rows=rows: e.activation(out=h1[:rows, :], in_=x[:rows, :], func=AF.Square, accum_out=ss[:rows, :]),
                 reads=[x], writes=[h1, ss])
            P.op("act", lambda e, rows=rows: e.activation(out=rstd[:rows, :], in_=ss[:rows, :], func=AF.Sqrt, scale=1.0 / D, bias=EPS),
                 reads=[ss], writes=[rstd])
            P.op("dve", lambda e, rows=rows: e.reciprocal(out=rstd[:rows, :], in_=rstd[:rows, :]),
                 reads=[rstd], writes=[rstd])
            if final:
                o = ob[it % 2]
                P.op("dve", lambda e, x=x, o=o: e.scalar_tensor_tensor(out=o[:], in0=x[:], scalar=rstd[:, 0:1], in1=g_bc[:], op0=ALU.mult, op1=ALU.mult),
                     reads=[x, rstd, g_bc], writes=[o])
                P.dma(lambda e, o=o, it=it: e.dma_start(out=outl[it * 128:(it + 1) * 128, :], in_=o[:]), reads=[o], writes=["outl"], final=True)
                continue
            A, Bt = AB[nm]
            P.op("dve", lambda e, x=x, rows=rows, A=A: e.scalar_tensor_tensor(out=h1[:rows, :], in0=x[:rows, :], scalar=rstd[:rows, 0:1], in1=A[:rows, :], op0=ALU.mult, op1=ALU.mult),
                 reads=[x, rstd, A], writes=[h1])
            h = hb[it % 2]
            P.op("pool", lambda e, h=h, rows=rows, Bt=Bt: e.tensor_tensor(out=h[:rows, :], in0=h1[:rows, :], in1=Bt[:rows, :], op=ALU.add),
                 reads=[h1, Bt], writes=[h])
            sg = stg[it % 2]
            if isl:
                for half in range(2):
                    pt = pst[(it * 2 + half) % 4]
                    for k in range(8):
                        kc = half * 8 + k
                        P.op("pe", lambda e, pt=pt, h=h, k=k, kc=kc: e.transpose(out=pt[:, k * 128:(k + 1) * 128], in_=h[:, kc * 128:(kc + 1) * 128], identity=ident[:]),
                             reads=[h, ident], writes=[pt])
                    eng = "act" if half == 0 else "dve"
                    if eng == "act":
                        P.op("act", lambda e, pt=pt, sg=sg, half=half: e.copy(out=sg[:, half * 1024:(half + 1) * 1024], in_=pt[:]), reads=[pt], writes=[(sg, half)])
                    else:
                        P.op("dve", lambda e, pt=pt, sg=sg, half=half: e.tensor_copy(out=sg[:, half * 1024:(half + 1) * 1024], in_=pt[:]), reads=[pt], writes=[(sg, half)])
                P.dma(lambda e, sg=sg, it=it: e.dma_start(out=hTl[it], in_=sg[:]), reads=[(sg, 0), (sg, 1)], writes=["hTl"], final=True)
            else:
                pt = pst[0]
                for kc in range(16):
                    P.op("pe", lambda e, pt=pt, h=h, kc=kc: e.transpose(out=pt[:, kc * 64:(kc + 1) * 64], in_=h[:64, kc * 128:(kc + 1) * 128], identity=ident[:64, :64]),
                         reads=[h, ident], writes=[pt])
                P.op("act", lambda e, pt=pt, sg=sg: e.copy(out=sg[:, 0:1024], in_=pt[:]), reads=[pt], writes=[(sg, 0)])
                P.dma(lambda e, sg=sg: e.dma_start(out=hTc, in_=sg[:, 0:1024].rearrange("p (k t) -> p k t", k=16)), reads=[(sg, 0)], writes=["hTc"], final=True)
        P.end_phase()


D = 2048; EPS = 1e-6
NCH = 66
POOL_W = (2, 4, 8, 16)

def emit_mixe(nc, P, pre, io, hp):
    stop = 9; do_pool = True; nheads = 2
    q = hp
    hT = io["HT"]; MIXT = io["MIXT"]; w_in = io["w_in"]; gate_b = io["gate_b"]; head_g = io["head_g"]
    tab = io["tab"]; identb_d = io["identb"]; identf_d = io["identf"]; tri_d = io["tri"]
    pw = io["pool_w"]; pool_scale = io["pool_scale"]
    icl = io["icl"][:, q * 2048:(q + 1) * 2048]; icc = io["icc"][:, q * 64:(q + 1) * 64]
    wu = w_in[:, 4128:5152]
    with contextlib.ExitStack() as st0:
        psf = [P.psum(st0, pre + "psf%d" % i, [128, 512], F32) for i in range(6)]
        psb = [P.psum(st0, pre + "psb%d" % i, [128, 1024], BF16) for i in range(2)]
        T0 = lambda name, shape, dt=F32: st0.enter_context(nc.sbuf_tensor(pre + name, shape, dt))
        identb = T0("identb_s", [128, 128], BF16); identf = T0("identf_s", [128, 128])
        tri = [T0("tri%d" % i, [128, 128]) for i in range(2)]
        ones = T0("ones", [128, 128]); zeros = T0("zeros", [128, 128])
        P.dma(lambda e: e.dma_start(out=identb[:], in_=identb_d), writes=[identb])
        P.dma(lambda e: e.dma_start(out=identf[:], in_=identf_d), writes=[identf])
        for i in range(2):
            P.dma(lambda e, i=i: e.dma_start(out=tri[i][:], in_=tri_d[i]), writes=[tri[i]])
        P.op("pool", lambda e: e.memset(ones[:], 1.0), writes=[ones])
        P.op("pool", lambda e: e.memset(zeros[:], 0.0), writes=[zeros])

        with contextlib.ExitStack() as st:
            T = lambda name, shape, dt=F32: st.enter_context(nc.sbuf_tensor(pre + name, shape, dt))
            Wh = T("Wh", [128, 16 * 512], BF16); Wg = T("Wg", [128, 16 * 4], BF16)
            gbt = T("gbt", [128, 4]); gn = T("gn", [128, 128]); gball = T("gball", [128, 32])
            QKT = T("QKT", [128, NCH * 256], BF16)
            Ktok = T("Ktok", [128, NCH * 128], BF16)
            VE = T("VE", [128, NCH * 129], BF16)
            SO = T("SO", [128, NCH * 128], BF16)
            HF = T("HF", [128, NCH * 128])
            OUT = T("OUTs", [128, NCH * 128], BF16)
            G = T("G", [128, NCH * 4])
            hch = [T("hch%d" % i, [128, 16 * 128], BF16) for i in range(2)]
            tb = [T("tb%d" % i, [128, 256]) for i in range(2)]
            qkt = [T("qkt%d" % i, [128, 256], BF16) for i in range(2)]
            rt = [T("rt%d" % i, [128, 128]) for i in range(4)]
            LF = T("LF", [128, NCH]); Bm = T("Bm", [128, NCH]); U = T("U", [128, NCH]); GS = T("GS", [128, NCH])
            UM = T("UM", [128, NCH]); MS = T("MS", [128, NCH + 1]); ME = T("ME", [128, NCH]); tA = T("tA", [128, NCH])
            um = T("um", [128, 1]); umb = T("umb", [128, 128])
            SPt = [T("SP%d" % i, [128, NCH]) for i in range(2)]
            WAt = [T("WA%d" % i, [128, NCH]) for i in range(2)]
            FLt = [T("FL%d" % i, [128, NCH]) for i in range(2)]
            CTs = [T("CT%d" % i, [128, 129]) for i in range(2)]; CTbs = [T("CTb%d" % i, [128, 129], BF16) for i in range(2)]
            PT = [T("PT%d" % i, [128, 128], BF16) for i in range(2)]
            vw = [T("vw%d" % i, [128, 129], BF16) for i in range(2)]
            rr = [T("rr%d" % i, [128, 1]) for i in range(2)]
            ssq = T("ssq", [128, NCH]); rs = T("rs", [128, NCH])
            hn1 = [T("hn1_%d" % i, [128, 128]) for i in range(2)]
            hnb = [T("hnb%d" % i, [128, 128], BF16) for i in range(2)]

            QKT3 = QKT[:].rearrange("p (c x) -> p c x", x=256)
            K3 = Ktok[:].rearrange("p (c x) -> p c x", x=128)
            VE3 = VE[:].rearrange("p (c x) -> p c x", x=129)
            SO3 = SO[:].rearrange("p (c x) -> p c x", x=128)
            HF3 = HF[:].rearrange("p (c x) -> p c x", x=128)
            G3 = G[:].rearrange("p (c x) -> p c x", x=4)
            Wh3 = Wh[:].rearrange("p (k n) -> p k n", k=16)
            Wg3 = Wg[:].rearrange("p (k n) -> p k n", k=16)

            P.op("pool", lambda e: e.memset(VE[:], 1.0), writes=[VE])
            for hd in range(nheads):
                h = 2 * hp + hd
                for s_ in range(4):
                    c0 = s_ * 1024 + h * 128
                    P.dma(lambda e, s_=s_, c0=c0: e.dma_start(out=Wh3[:, :, s_ * 128:(s_ + 1) * 128], in_=w_in[:, c0:c0 + 128].rearrange("(k p) n -> p k n", p=128)), eng="pool", writes=[Wh])
                for gt in range(4):
                    c0 = 4096 + gt * 8 + h
                    P.dma(lambda e, gt=gt, c0=c0: e.dma_start(out=Wg3[:, :, gt:gt + 1], in_=w_in[:, c0:c0 + 1].rearrange("(k p) n -> p k n", p=128), allow_slow_non_contiguous=True), eng="pool", writes=[Wg])
                P.dma(lambda e: e.dma_start(out=gball[:], in_=gate_b.rearrange("g h -> (g h)").unsqueeze(0).to_broadcast([128, 32])), writes=[gball])
                P.op("dve", lambda e, h=h: e.tensor_copy(out=gbt[:], in_=gball[:].rearrange("p (g h) -> p g h", g=4)[:, :, h]), reads=[gball], writes=[gbt])
                P.dma(lambda e, h=h: e.dma_start(out=gn[:], in_=head_g[:, h * 128:(h + 1) * 128].to_broadcast([128, 128])), writes=[gn])
                def p1_proj(c):
                    hc_ = hch[c % 2]; tbc = tb[c % 2]; qk = qkt[c % 2]
                    pp = psf[c % 2]; pg = psf[2 + c % 2]; pb = psb[c % 2]
                    P.dma(lambda e, hc_=hc_, c=c: e.dma_start(out=hc_[:], in_=hT[c]), writes=[hc_])
                    P.dma(lambda e, tbc=tbc, c=c: e.dma_start(out=tbc[:], in_=tab[c]), writes=[tbc])
                    h3 = hc_[:].rearrange("p (k t) -> p k t", k=16)
                    for kc in range(16):
                        P.op("pe", lambda e, pp=pp, h3=h3, kc=kc: e.matmul(pp[:, :], lhsT=h3[:, kc, :], rhs=Wh3[:, kc, :], start=(kc == 0), stop=(kc == 15)),
                             reads=[hc_, Wh], writes=[pp])
                    for kc in range(16):
                        P.op("pe", lambda e, pg=pg, h3=h3, kc=kc: e.matmul(pg[:, 0:4], lhsT=h3[:, kc, :], rhs=Wg3[:, kc, :], start=(kc == 0), stop=(kc == 15)),
                             reads=[hc_, Wg], writes=[pg])
                def p1_post(c):
                    hc_ = hch[c % 2]; tbc = tb[c % 2]; qk = qkt[c % 2]
                    pp = psf[c % 2]; pg = psf[2 + c % 2]; pb = psb[c % 2]
                    xv = pp[:, 0:256].rearrange("p (a b h d) -> p a b h d", a=2, b=2, h=2)
                    x1 = xv[:, :, :, 0, :]; x2 = xv[:, :, :, 1, :]
                    tv = tbc[:].rearrange("p (s a b d) -> p s a b d", s=2, a=2, b=2)
                    Cv = tv[:, 0]; Sv = tv[:, 1]
                    ov = qk[:].rearrange("p (a b h d) -> p a b h d", a=2, b=2, h=2)
                    r4 = [t[:].rearrange("p (a b d) -> p a b d", a=2, b=2) for t in rt]
                    P.op("dve", lambda e, x1=x1, Cv=Cv, o=r4[0]: e.tensor_tensor(out=o, in0=x1, in1=Cv, op=ALU.mult), reads=[pp, tbc], writes=[rt[0]])
                    P.op("dve", lambda e, x2=x2, Sv=Sv, o=r4[1]: e.tensor_tensor(out=o, in0=x2, in1=Sv, op=ALU.mult), reads=[pp, tbc], writes=[rt[1]])
                    P.op("dve", lambda e, x1=x1, Sv=Sv, o=r4[2]: e.tensor_tensor(out=o, in0=x1, in1=Sv, op=ALU.mult), reads=[pp, tbc], writes=[rt[2]])
                    P.op("dve", lambda e, x2=x2, Cv=Cv, o=r4[3]: e.tensor_tensor(out=o, in0=x2, in1=Cv, op=ALU.mult), reads=[pp, tbc], writes=[rt[3]])
                    P.op("pool", lambda e, o=ov[:, :, :, 0, :], a=r4[0], b=r4[1]: e.tensor_tensor(out=o, in0=a, in1=b, op=ALU.subtract), reads=[rt[0], rt[1]], writes=[(qk, 0)])
                    P.op("pool", lambda e, o=ov[:, :, :, 1, :], a=r4[2], b=r4[3]: e.tensor_tensor(out=o, in0=a, in1=b, op=ALU.add), reads=[rt[2], rt[3]], writes=[(qk, 1)])
                    P.op("act", lambda e, pp=pp, c=c: e.copy(out=VE3[:, c, 0:128], in_=pp[:, 256:384]), reads=[pp], writes=[(VE, c)])
                    P.op("act", lambda e, pp=pp, c=c: e.activation(out=SO3[:, c, :], in_=pp[:, 384:512], func=AF.Sigmoid), reads=[pp], writes=[(SO, c)])
                    P.op("dve", lambda e, pg=pg, c=c: e.tensor_tensor(out=G3[:, c, :], in0=pg[:, 0:4], in1=gbt[:], op=ALU.add), reads=[pg, gbt], writes=[(G, c)])
                    P.op("pool", lambda e, qk=qk, c=c: e.tensor_copy(out=K3[:, c, :], in_=qk[:, 128:256]), reads=[(qk, 0), (qk, 1)], writes=[(Ktok, c)])
                    for a in range(2):
                        P.op("pe", lambda e, pb=pb, qk=qk, a=a: e.transpose(out=pb[:, a * 128:(a + 1) * 128], in_=qk[:, a * 128:(a + 1) * 128], identity=identb[:]),
                             reads=[(qk, 0), (qk, 1), identb], writes=[pb])
                    P.op("act", lambda e, pb=pb, c=c: e.copy(out=QKT3[:, c, :], in_=pb[:, 0:256]), reads=[pb], writes=[(QKT, c)])

                p1_proj(0)
                for c in range(NCH):
                    if c + 1 < NCH:
                        p1_proj(c + 1)
                    p1_post(c)
                ordrs = []
                for dr in range(2 if stop >= 2 else 0):
                    ordr = list(range(NCH)) if dr == 0 else [1, 0] + list(range(NCH - 1, 1, -1))
                    li = G3[:, :, 2 * dr]; ff = G3[:, :, 2 * dr + 1]
                    Gk = [(G, c) for c in range(NCH)]
                    P.op("act", lambda e, ff=ff: e.activation(out=tA[:], in_=ff, func=AF.Exp, scale=-1.0), reads=Gk, writes=[tA])
                    P.op("act", lambda e: e.activation(out=tA[:], in_=tA[:], func=AF.Ln, bias=1.0), reads=[tA], writes=[tA])
                    P.op("dve", lambda e: e.tensor_scalar(out=LF[:], in0=tA[:], scalar1=-1.0, scalar2=None, op0=ALU.mult), reads=[tA], writes=[LF])
                    pa = psf[4]
                    P.op("pe", lambda e, dr=dr: e.matmul(pa[:, 0:NCH], lhsT=tri[dr][:], rhs=LF[:], start=True, stop=True), reads=[tri[dr], LF], writes=[pa])
                    P.op("pe", lambda e: e.matmul(pa[:, 128:128 + NCH], lhsT=ones[:], rhs=LF[:], start=True, stop=True), reads=[ones, LF], writes=[pa])
                    P.op("act", lambda e: e.copy(out=Bm[:], in_=pa[:, 0:NCH]), reads=[pa], writes=[Bm])
                    P.op("act", lambda e: e.copy(out=GS[:], in_=pa[:, 128:128 + NCH]), reads=[pa], writes=[GS])
                    P.op("dve", lambda e, li=li: e.tensor_tensor(out=U[:], in0=li, in1=Bm[:], op=ALU.subtract), reads=Gk + [Bm], writes=[U])
                    P.op("pe", lambda e: e.transpose(out=pa[:NCH, 256:384], in_=U[:], identity=identf[:]), reads=[U, identf], writes=[pa])
                    P.op("dve", lambda e: e.reduce_max(out=um[:NCH, :], in_=pa[:NCH, 256:384], axis=AX.X), reads=[pa], writes=[um])
                    P.op("dve", lambda e: e.tensor_scalar(out=umb[:NCH, :], in0=zeros[:NCH, :], scalar1=um[:NCH, 0:1], scalar2=None, op0=ALU.add), reads=[um, zeros], writes=[umb])
                    P.op("pe", lambda e: e.matmul(pa[:, 384:384 + NCH], lhsT=umb[:NCH, :], rhs=identf[:NCH, :NCH], start=True, stop=True), reads=[umb, identf], writes=[pa])
                    P.op("act", lambda e: e.copy(out=UM[:], in_=pa[:, 384:384 + NCH]), reads=[pa], writes=[UM])
                    P.op("pool", lambda e: e.memset(MS[:], 0.0), writes=[MS])
                    for n, c in enumerate(ordr):
                        P.op("dve", lambda e, c=c: e.tensor_tensor(out=ME[:, c:c + 1], in0=MS[:, c:c + 1], in1=UM[:, c:c + 1], op=ALU.max), reads=[MS, UM], writes=[ME])
                        if n + 1 < NCH:
                            cn = ordr[n + 1]
                            P.op("dve", lambda e, c=c, cn=cn: e.tensor_tensor(out=MS[:, cn:cn + 1], in0=ME[:, c:c + 1], in1=GS[:, c:c + 1], op=ALU.add), reads=[ME, GS], writes=[MS])
                    SPd, WAd, FLd = SPt[dr], WAt[dr], FLt[dr]
                    P.op("dve", lambda e: e.tensor_tensor(out=tA[:], in0=MS[:, 0:NCH], in1=ME[:], op=ALU.subtract), reads=[MS, ME], writes=[tA])
                    P.op("act", lambda e, SPd=SPd: e.activation(out=SPd[:], in_=tA[:], func=AF.Exp), reads=[tA], writes=[SPd])
                    P.op("dve", lambda e: e.tensor_tensor(out=tA[:], in0=U[:], in1=ME[:], op=ALU.subtract), reads=[U, ME], writes=[tA])
                    P.op("act", lambda e, WAd=WAd: e.activation(out=WAd[:], in_=tA[:], func=AF.Exp), reads=[tA], writes=[WAd])
                    P.op("dve", lambda e: e.tensor_tensor(out=tA[:], in0=Bm[:], in1=ME[:], op=ALU.add), reads=[Bm, ME], writes=[tA])
                    P.op("act", lambda e, FLd=FLd: e.activation(out=FLd[:], in_=tA[:], func=AF.Exp, scale=-1.0), reads=[tA], writes=[FLd])
                    ordrs.append(ordr)
                P.op("pool", lambda e: e.memset(HF[:], 0.0), writes=[(HF, c) for c in range(NCH)])
                for dr in range(2):
                    P.op("pool", lambda e, dr=dr: e.memset(CTs[dr][:], 0.0), writes=[CTs[dr]])
                for n in range(NCH):
                    for dr in range(2):
                        c = ordrs[dr][n]
                        SPd, WAd, FLd = SPt[dr], WAt[dr], FLt[dr]
                        CT = CTs[dr]; CTb = CTbs[dr]
                        pS = psf[dr]; pO = psf[2 + dr]; pC = psf[4 + dr]
                        PTn = PT[dr]; vwn = vw[dr]; rrn = rr[dr]
                        P.op("pe", lambda e, pS=pS, c=c: e.matmul(pS[:, 0:128], lhsT=QKT3[:, c, 128:256], rhs=QKT3[:, c, 0:128], start=True, stop=True),
                             reads=[(QKT, c)], writes=[pS])
                        P.op("dve", lambda e, pS=pS, PTn=PTn, dr=dr: e.tensor_tensor(out=PTn[:], in0=pS[:, 0:128], in1=tri[dr][:], op=ALU.mult), reads=[pS, tri[dr]], writes=[PTn])
                        P.op("act", lambda e, vwn=vwn, c=c, WAd=WAd: e.activation(out=vwn[:], in_=VE3[:, c, :], func=AF.Copy, scale=WAd[:, c:c + 1]), reads=[(VE, c), WAd], writes=[vwn])
                        P.op("dve", lambda e, c=c, SPd=SPd, CT=CT: e.tensor_scalar(out=CT[:], in0=CT[:], scalar1=SPd[:, c:c + 1], scalar2=None, op0=ALU.mult), reads=[CT, SPd], writes=[CT])
                        P.op("act", lambda e, CT=CT, CTb=CTb: e.copy(out=CTb[:], in_=CT[:]), reads=[CT], writes=[CTb])
                        P.op("pe", lambda e, pO=pO, PTn=PTn, vwn=vwn: e.matmul(pO[:, 0:129], lhsT=PTn[:], rhs=vwn[:], start=True, stop=False), reads=[PTn, vwn], writes=[pO])
                        P.op("pe", lambda e, pO=pO, c=c, CTb=CTb: e.matmul(pO[:, 0:129], lhsT=QKT3[:, c, 0:128], rhs=CTb[:], start=False, stop=True), reads=[(QKT, c), CTb], writes=[pO])
                        P.op("pe", lambda e, pC=pC, c=c, vwn=vwn: e.matmul(pC[:, 0:129], lhsT=K3[:, c, :], rhs=vwn[:], start=True, stop=True), reads=[(Ktok, c), vwn], writes=[pC])
                        P.op("dve", lambda e, pC=pC, CT=CT: e.tensor_tensor(out=CT[:], in0=CT[:], in1=pC[:, 0:129], op=ALU.add), reads=[CT, pC], writes=[CT])
                        P.op("act", lambda e, pO=pO, rrn=rrn: e.activation(out=rrn[:], in_=pO[:, 128:129], func=AF.Abs),
                             reads=[pO], writes=[rrn])
                        P.op("dve", lambda e, rrn=rrn, c=c, FLd=FLd: e.tensor_scalar(out=rrn[:], in0=rrn[:], scalar1=FLd[:, c:c + 1], scalar2=None, op0=ALU.max),
                             reads=[rrn, FLd], writes=[rrn])
                        P.op("dve", lambda e, rrn=rrn: e.reciprocal(out=rrn[:], in_=rrn[:]), reads=[rrn], writes=[rrn])
                        P.op("dve", lambda e, pO=pO, rrn=rrn, c=c: e.scalar_tensor_tensor(out=HF3[:, c, :], in0=pO[:, 0:128], scalar=rrn[:, 0:1], in1=HF3[:, c, :], op0=ALU.mult, op1=ALU.add),
                             reads=[pO, rrn, (HF, c)], writes=[(HF, c)])
                if stop < 4:
                    continue
                for c in range(NCH):
                    h1 = hn1[c % 2]
                    P.op("act", lambda e, c=c, h1=h1: e.activation(out=h1[:], in_=HF3[:, c, :], func=AF.Square, accum_out=ssq[:, c:c + 1]), reads=[(HF, c)], writes=[h1, (ssq, c)])
                sk = [(ssq, c) for c in range(NCH)]
                P.op("act", lambda e: e.activation(out=rs[:], in_=ssq[:], func=AF.Sqrt, scale=1.0 / 128, bias=EPS), reads=sk, writes=[rs])
                P.op("dve", lambda e: e.reciprocal(out=rs[:], in_=rs[:]), reads=[rs], writes=[rs])
                OUT3 = OUT[:].rearrange("p (c x) -> p c x", x=128)
                for c in range(NCH):
                    h1 = hn1[c % 2]; hb_ = hnb[c % 2]
                    grp, k = divmod(c, 8)
                    pb = psb[grp % 2]
                    P.op("dve", lambda e, c=c, h1=h1: e.scalar_tensor_tensor(out=h1[:], in0=HF3[:, c, :], scalar=rs[:, c:c + 1], in1=gn[:], op0=ALU.mult, op1=ALU.mult),
                         reads=[(HF, c), rs, gn], writes=[h1])
                    P.op("pool", lambda e, c=c, h1=h1, hb_=hb_: e.tensor_tensor(out=hb_[:], in0=h1[:], in1=SO3[:, c, :], op=ALU.mult), reads=[h1, (SO, c)], writes=[hb_])
                    P.op("pe", lambda e, pb=pb, hb_=hb_, k=k: e.transpose(out=pb[:, k * 128:(k + 1) * 128], in_=hb_[:], identity=identb[:]), reads=[hb_, identb], writes=[pb])
                    if k == 7 or c == NCH - 1:
                        n = k + 1
                        P.op("act", lambda e, pb=pb, grp=grp, n=n: e.copy(out=OUT[:, grp * 1024:grp * 1024 + n * 128], in_=pb[:, 0:n * 128]), reads=[pb], writes=[OUT])
                P.dma(lambda e, h=h: e.dma_start(out=MIXT[:, :, h, :].rearrange("t p x -> p t x"), in_=OUT[:].rearrange("p (t x) -> p t x", x=128)), reads=[OUT], writes=["mixh"])
            P.barrier()
        with contextlib.ExitStack() as st:
            T = lambda name, shape, dt=F32: st.enter_context(nc.sbuf_tensor(pre + name, shape, dt))
            Wu = T("Wu", [128, 16 * 1024], BF16)
            Wu3 = Wu[:].rearrange("p (k n) -> p k n", k=16)
            for kc_ in range(16):
                P.dma(lambda e, kc_=kc_: e.dma_start(out=Wu3[:, kc_, :], in_=wu[kc_ * 128:(kc_ + 1) * 128, :]), eng="pool", writes=[Wu])
            PW = T("PW", [128, 4 * 2 * 256], BF16)
            PW4 = PW[:].rearrange("p (g c n) -> p g c n", g=4, c=2)
            for g in range(4):
                P.dma(lambda e, g=g: e.dma_start(out=PW4[:, g], in_=pw[g].rearrange("(c p) n -> p c n", p=128)), eng="pool", writes=[PW])
            pst = T("pst", [128, 8])
            for et_ in range(8):
                P.dma(lambda e, et_=et_: e.dma_start(out=pst[:, et_:et_ + 1], in_=pool_scale[et_ * 128:(et_ + 1) * 128].unsqueeze(1)), writes=[pst])
            def pool_part(nm, hp, ic, NT, odst):
                NH = NT + 16
                hs = T("hs_" + nm, [128, 16 * NH], BF16)
                hs3 = hs[:].rearrange("p (k t) -> p k t", k=16)
                if nm == "l":
                    t0 = 2 + q * 16
                    for kc_ in range(16):
                        P.dma(lambda e, kc_=kc_: e.dma_start(out=hs3[:, kc_, 8:8 + 2048].rearrange("p (t x) -> p t x", x=128),
                                                              in_=hT[t0:t0 + 16].rearrange("t p (k x) -> p k t x", k=16)[:, kc_]), writes=[hs])
                    if q > 0:
                        P.dma(lambda e: e.dma_start(out=hs3[:, :, 0:8], in_=hT[t0 - 1].rearrange("p (k x) -> p k x", k=16)[:, :, 120:128]), writes=[hs])
                    else:
                        P.op("pool", lambda e: e.memset(hs3[:, :, 0:8], 0.0), writes=[hs])
                    if q < 3:
                        P.dma(lambda e: e.dma_start(out=hs3[:, :, 2056:2064], in_=hT[t0 + 16].rearrange("p (k x) -> p k x", k=16)[:, :, 0:8]), writes=[hs])
                    else:
                        P.op("pool", lambda e: e.memset(hs3[:, :, 2056:2064], 0.0), writes=[hs])
                else:
                    lo = q * 64 - 8
                    pos = 0
                    if lo < 0:
                        P.op("pool", lambda e: e.memset(hs3[:, :, 0:8], 0.0), writes=[hs]); pos = 8; lo = 0
                    hi = min(q * 64 + 72, 256)
                    while lo < hi:
                        t_ = lo // 128; a_ = lo % 128; n_ = min(hi - lo, 128 - a_)
                        P.dma(lambda e, t_=t_, a_=a_, n_=n_, pos=pos: e.dma_start(out=hs3[:, :, pos:pos + n_], in_=hT[t_].rearrange("p (k x) -> p k x", k=16)[:, :, a_:a_ + n_]), writes=[hs])
                        pos += n_; lo += n_
                    if pos < 80:
                        P.op("pool", lambda e, pos=pos: e.memset(hs3[:, :, pos:80], 0.0), writes=[hs])
                Ub = [T("U%s%d" % (nm, i), [128, NH]) for i in range(2)]
                A1 = T("A1" + nm, [128, NH]); A2 = T("A2" + nm, [128, NH])
                icb = T("icb" + nm, [128, NT])
                dT = [T("dT%s%d" % (nm, i), [128, NT], BF16) for i in range(2)]
                ob = [T("ob%s%d" % (nm, i), [128, NT], BF16) for i in range(2)]
                blocks = [(s, min(512, NH - s)) for s in range(0, NH, 512)]
                oblocks = [(s, min(512, NT - s)) for s in range(0, NT, 512)]
                for g in range(4):
                    w = POOL_W[g]
                    P.dma(lambda e, g=g, ic=ic, icb=icb, NT=NT: e.dma_start(out=icb[:], in_=ic[g:g + 1, :].to_broadcast([128, NT])), writes=[icb])
                    for ci in range(2):
                        ct = g * 2 + ci
                        Ut = Ub[ci]
                        for bi, (s0, n) in enumerate(blocks):
                            pp = psf[bi % 4]
                            for kc in range(16):
                                P.op("pe", lambda e, pp=pp, kc=kc, ct=ct, s0=s0, n=n: e.matmul(pp[:, 0:n], lhsT=Wu3[:, kc, ct * 128:(ct + 1) * 128], rhs=hs3[:, kc, s0:s0 + n], start=(kc == 0), stop=(kc == 15)),
                                     reads=[Wu, hs], writes=[pp])
                            P.op("act", lambda e, pp=pp, Ut=Ut, s0=s0, n=n: e.copy(out=Ut[:, s0:s0 + n], in_=pp[:, 0:n]), reads=[pp], writes=[Ut])
                        cur = Ut; L = NH; step = 1; k = 1
                        bufs = [A1, A2]; bi = 0
                        while k < w:
                            nxt = bufs[bi % 2]; bi += 1
                            L2 = L - k
                            P.op("dve", lambda e, cur=cur, nxt=nxt, L2=L2, k=k: e.tensor_tensor(out=nxt[:, 0:L2], in0=cur[:, 0:L2], in1=cur[:, k:k + L2], op=ALU.add), reads=[cur], writes=[nxt])
                            cur = nxt; L = L2; k *= 2
                        off = 8 - w // 2
                        nxt = bufs[bi % 2]
                        P.op("dve", lambda e, cur=cur, nxt=nxt, off=off, NT=NT: e.tensor_tensor(out=nxt[:, 0:NT], in0=cur[:, off:off + NT], in1=icb[:], op=ALU.mult), reads=[cur, icb], writes=[nxt])
                        P.op("dve", lambda e, nxt=nxt, Ut=Ut, d=dT[ci], NT=NT: e.tensor_tensor(out=d[:], in0=nxt[:, 0:NT], in1=Ut[:, 8:8 + NT], op=ALU.subtract), reads=[nxt, Ut], writes=[dT[ci]])
                    for ei in range(2):
                        et = g * 2 + ei
                        o = ob[ei]
                        for bi, (s0, n) in enumerate(oblocks):
                            pp = psf[4 + bi % 2]
                            for ci in range(2):
                                P.op("pe", lambda e, pp=pp, ci=ci, g=g, ei=ei, s0=s0, n=n: e.matmul(pp[:, 0:n], lhsT=PW4[:, g, ci, ei * 128:(ei + 1) * 128], rhs=dT[ci][:, s0:s0 + n], start=(ci == 0), stop=(ci == 1)),
                                     reads=[PW, dT[ci]], writes=[pp])
                            P.op("act", lambda e, pp=pp, o=o, et=et, s0=s0, n=n: e.activation(out=o[:, s0:s0 + n], in_=pp[:, 0:n], func=AF.Copy, scale=pst[:, et:et + 1]), reads=[pp, pst], writes=[o])
                        if nm == "l":
                            P.dma(lambda e, o=o, et=et: e.dma_start(out=MIXT[2 + q * 16:2 + (q + 1) * 16, :, 8 + et, :].rearrange("t p x -> p t x"), in_=o[:].rearrange("p (t x) -> p t x", x=128)), reads=[o], writes=["po"])
                        else:
                            P.dma(lambda e, o=o, et=et: e.dma_start(out=MIXT[q // 2, :, 8 + et, (q % 2) * 64:(q % 2 + 1) * 64], in_=o[:]), reads=[o], writes=["po"])

            pool_part("l", None, icl, 2048, None)
            pool_part("c", None, icc, 64, None)
            P.end_phase()


D = 2048
NCH = 66
NPAT = 5

def na_patterns():
    R, W, WR, WC = 128, 64, 8, 16
    pats = {}; plist = []; per_m = []
    for m in range(64):
        idx = -np.ones((64, 128, 128), np.int32)
        used = set()
        for rl in range(2):
            r = 2 * m + rl
            rs = min(max(r - WR // 2, 0), R - WR)
            for c in range(W):
                cs = min(max(c - WC // 2, 0), W - WC)
                q = rl * 64 + c
                for kr in range(rs, rs + WR):
                    t = kr // 2
                    used.add(t)
                    kk = (kr - 2 * t) * 64 + np.arange(cs, cs + WC)
                    idx[t, kk, q] = (kr - r + 7) * 31 + (np.arange(cs, cs + WC) - c + 15)
        tiles = sorted(used)
        assert len(tiles) <= 5
        im = -np.ones((5, 128, 128), np.int32)
        for j, t in enumerate(tiles):
            im[j] = idx[t]
        while len(tiles) < 5:
            tiles.append(tiles[-1])
        key = im.tobytes()
        if key not in pats:
            pats[key] = len(plist); plist.append(im)
        per_m.append(([t + 2 for t in tiles], pats[key]))
    assert len(plist) == NPAT, len(plist)
    return per_m, np.stack(plist)

def emit_mixo(nc, P, pre, io, hq, ctx_out):
    nheads = 4; phase = 2; skip = ()
    hT = io["HT"]; MIXT = io["MIXT"]; w_in = io["w_in"]; bias = io["bias"]; identb_d = io["identb"]
    per_m, _ = na_patterns()
    scale = 128.0 ** -0.5
    with contextlib.ExitStack() as st:
        psf = [P.psum(st, pre + "psf%d" % i, [128, 512], F32) for i in range(6)]
        psb = [P.psum(st, pre + "psb%d" % i, [128, 1024], BF16) for i in range(2)]
        T = lambda name, shape, dt=F32: st.enter_context(nc.sbuf_tensor(pre + name, shape, dt))
        identb = T("identb_s", [128, 128], BF16); onesb = T("onesb", [128, 128], BF16)
        P.dma(lambda e: e.dma_start(out=identb[:], in_=identb_d), writes=[identb])
        P.op("pool", lambda e: e.memset(onesb[:], 1.0), writes=[onesb])
        W = T("W", [128, 16 * 384], BF16); W3 = W[:].rearrange("p (k n) -> p k n", k=16)
        QT = T("QT", [128, NCH * 128], BF16); KT = T("KT", [128, NCH * 128], BF16); VT = T("VT", [128, NCH * 129], BF16)
        OUT = T("OUTs", [128, NCH * 128], BF16)
        Bs = T("Bs", [128, NPAT * 640])
        hch = [T("hch%d" % i, [128, 16 * 128], BF16) for i in range(2)]
        qk = [T("qk%d" % i, [128, 256], BF16) for i in range(2)]
        tmp = [T("tmp%d" % i, [128, 640]) for i in range(2)]
        PT = [T("PT%d" % i, [128, 896], BF16) for i in range(2)]
        rc = [T("rc%d" % i, [128, 128]) for i in range(2)]
        onbs = [T("onb%d" % i, [128, 128], BF16) for i in range(2)]; rrs = [T("rrs%d" % i, [128, 1]) for i in range(2)]
        Q3 = QT[:].rearrange("p (c x) -> p c x", x=128); K3 = KT[:].rearrange("p (c x) -> p c x", x=128)
        V3 = VT[:].rearrange("p (c x) -> p c x", x=129); O3 = OUT[:].rearrange("p (c x) -> p c x", x=128)
        B3 = Bs[:].rearrange("p (a x) -> p a x", a=NPAT)
        P.op("pool", lambda e: e.memset(VT[:], 1.0), writes=[(VT, c) for c in range(NCH)])
        for hd in range(nheads):
            h = 4 * hq + hd
            for s_ in range(3):
                c0 = s_ * 2048 + h * 128
                P.dma(lambda e, s_=s_, c0=c0: e.dma_start(out=W3[:, :, s_ * 128:(s_ + 1) * 128], in_=w_in[:, c0:c0 + 128].rearrange("(k p) n -> p k n", p=128)), eng="pool", writes=[W])
            P.dma(lambda e, h=h: e.dma_start(out=Bs[:], in_=bias[h]), writes=[Bs])
            def p1_proj(c):
                hc_ = hch[c % 2]; pp = psf[c % 2]
                P.dma(lambda e, hc_=hc_, c=c: e.dma_start(out=hc_[:], in_=hT[c]), writes=[hc_])
                h3 = hc_[:].rearrange("p (k t) -> p k t", k=16)
                for kc in range(16):
                    P.op("pe", lambda e, pp=pp, h3=h3, kc=kc: e.matmul(pp[:, 0:384], lhsT=h3[:, kc, :], rhs=W3[:, kc, :], start=(kc == 0), stop=(kc == 15)),
                         reads=[hc_, W], writes=[pp])

            def p1_post(c):
                qk_ = qk[c % 2]; pp = psf[c % 2]; pb = psb[c % 2]
                P.op("dve", lambda e, pp=pp, qk_=qk_: e.tensor_scalar(out=qk_[:, 0:128], in0=pp[:, 0:128], scalar1=scale, scalar2=None, op0=ALU.mult), reads=[pp], writes=[(qk_, 0)])
                P.op("act", lambda e, pp=pp, qk_=qk_: e.copy(out=qk_[:, 128:256], in_=pp[:, 128:256]), reads=[pp], writes=[(qk_, 1)])
                P.op("act", lambda e, pp=pp, c=c: e.copy(out=V3[:, c, 0:128], in_=pp[:, 256:384]), reads=[pp], writes=[(VT, c)])
                for a in range(2):
                    P.op("pe", lambda e, pb=pb, qk_=qk_, a=a: e.transpose(out=pb[:, a * 128:(a + 1) * 128], in_=qk_[:, a * 128:(a + 1) * 128], identity=identb[:]),
                         reads=[(qk_, a), identb], writes=[pb])
                P.op("dve", lambda e, pb=pb, c=c: e.tensor_copy(out=Q3[:, c, :], in_=pb[:, 0:128]), reads=[pb], writes=[(QT, c)])
                P.op("act", lambda e, pb=pb, c=c: e.copy(out=K3[:, c, :], in_=pb[:, 128:256]), reads=[pb], writes=[(KT, c)])

            p1_proj(0)
            for c in range(NCH):
                if c + 1 < NCH:
                    p1_proj(c + 1)
                p1_post(c)
            qtiles = [(m + 2, per_m[m][0], per_m[m][1]) for m in range(64 if phase >= 2 else 0)]
            if ctx_out:
                qtiles = [(0, None, None), (1, None, None)] + qtiles
            def att_S(n):
                qc, kts, pat = qtiles[n]
                pA = psf[2 * (n % 2)]; pB = psf[2 * (n % 2) + 1]
                if kts is not None:
                    for j, kt in enumerate(kts):
                        dst = pA[:, j * 128:(j + 1) * 128] if j < 4 else pB[:, 0:128]
                        P.op("pe", lambda e, dst=dst, kt=kt, qc=qc: e.matmul(dst, lhsT=K3[:, kt, :], rhs=Q3[:, qc, :], start=True, stop=True),
                             reads=[(KT, kt), (QT, qc)], writes=[pA if j < 4 else pB])
                for j in range(2):
                    P.op("pe", lambda e, pB=pB, j=j, qc=qc: e.matmul(pB[:, 128 + j * 128:256 + j * 128], lhsT=K3[:, j, :], rhs=Q3[:, qc, :], start=True, stop=True),
                         reads=[(KT, j), (QT, qc)], writes=[pB])

            def att_rest(n):
                qc, kts, pat = qtiles[n]
                pA = psf[2 * (n % 2)]; pB = psf[2 * (n % 2) + 1]; pN = psf[4]; pD = psf[5]
                tm = tmp[n % 2]; pt = PT[n % 2]; r_ = rc[n % 2]
                keys = []
                if kts is not None:
                    for j, kt in enumerate(kts):
                        keys.append((kt, j * 128))
                for j in range(2):
                    keys.append((j, 640 + j * 128))
                if kts is not None:
                    P.op("dve", lambda e, pA=pA, tm=tm, pat=pat: e.tensor_tensor(out=tm[:, 0:512], in0=pA[:, :], in1=B3[:, pat, 0:512], op=ALU.add), reads=[pA, Bs], writes=[tm])
                    P.op("dve", lambda e, pB=pB, tm=tm, pat=pat: e.tensor_tensor(out=tm[:, 512:640], in0=pB[:, 0:128], in1=B3[:, pat, 512:640], op=ALU.add), reads=[pB, Bs], writes=[tm])
                    P.op("act", lambda e, tm=tm, pt=pt: e.activation(out=pt[:, 0:640], in_=tm[:, :], func=AF.Exp), reads=[tm], writes=[pt])
                P.op("act", lambda e, pB=pB, pt=pt: e.activation(out=pt[:, 640:896], in_=pB[:, 128:384], func=AF.Exp), reads=[pB], writes=[pt])
                pN = psf[4 + n % 2]; pb = psb[n % 2]; onb = onbs[n % 2]; rr_ = rrs[n % 2]
                for i, (kt, off) in enumerate(keys):
                    P.op("pe", lambda e, kt=kt, off=off, pt=pt, i=i, nk=len(keys), pN=pN: e.matmul(pN[:, 0:129], lhsT=pt[:, off:off + 128], rhs=V3[:, kt, :], start=(i == 0), stop=(i == nk - 1)),
                         reads=[(VT, kt), pt], writes=[pN])
                P.op("act", lambda e, rr_=rr_, pN=pN: e.copy(out=rr_[:], in_=pN[:, 128:129]), reads=[pN], writes=[rr_])
                P.op("dve", lambda e, rr_=rr_: e.reciprocal(out=rr_[:], in_=rr_[:]), reads=[rr_], writes=[rr_])
                P.op("act", lambda e, rr_=rr_, pN=pN, onb=onb: e.activation(out=onb[:], in_=pN[:, 0:128], func=AF.Copy, scale=rr_[:, 0:1]), reads=[pN, rr_], writes=[onb])

            def att_fin(n):
                qc, kts, pat = qtiles[n]
                pb = psb[n % 2]; onb = onbs[n % 2]
                P.op("pe", lambda e, pb=pb, onb=onb: e.transpose(out=pb[:, 0:128], in_=onb[:], identity=identb[:]), reads=[onb, identb], writes=[pb])
                P.op("dve", lambda e, pb=pb, qc=qc: e.tensor_copy(out=O3[:, qc, :], in_=pb[:, 0:128]), reads=[pb], writes=[OUT])

            if qtiles:
                att_S(0)
            for n in range(len(qtiles)):
                if n + 1 < len(qtiles):
                    att_S(n + 1)
                att_rest(n)
                if n >= 1:
                    att_fin(n - 1)
            if qtiles:
                att_fin(len(qtiles) - 1)
            if not ctx_out:
                P.op("pool", lambda e: e.memset(OUT[:, 0:256], 0.0), writes=[OUT])
            P.dma(lambda e, h=h: e.dma_start(out=MIXT[:, :, h, :].rearrange("t p x -> p t x"), in_=OUT[:].rearrange("p (t x) -> p t x", x=128)), reads=[OUT], writes=["mixh"])
        P.end_phase()


D = 2048; EPS = 1e-6

def emit_g2(nc, P, pre, io):
    xl = io["xl"]; xc = io["xc"]; mixl = io["mixl"]; mixc = io["mixc"]; wout = io["wout"]; wr = io["wr"]
    modl = io["modl"]; modc = io["modc"]; gv = io["g"]; identf_d = io["identf"]
    xmo_l = io["xmo_l"]; xmo_c = io["xmo_c"]; h2_l = io["h2_l"]; h2_c = io["h2_c"]; aff_l = io["aff_l"]; aff_c = io["aff_c"]
    with contextlib.ExitStack() as st:
        T = lambda name, shape, dt=F32: st.enter_context(nc.sbuf_tensor(pre + name, shape, dt))
        psf = [P.psum(st, pre + "psf%d" % i, [128, 512], F32) for i in range(8)]
        identf = T("identf_s", [128, 128])
        P.dma(lambda e: e.dma_start(out=identf[:], in_=identf_d), writes=[identf])
        W = T("W", [128, 16 * D], BF16); W3 = W[:].rearrange("p (k n) -> p k n", k=16)
        for kc in range(16):
            P.dma(lambda e, kc=kc: e.dma_start(out=W3[:, kc, :], in_=wout[kc * 128:(kc + 1) * 128, :]), eng="pool", writes=[(W, kc)])
        WR = T("WR", [128, 16 * 16]); WR3 = WR[:].rearrange("p (k n) -> p k n", k=16)
        P.dma(lambda e: e.dma_start(out=WR3, in_=wr.rearrange("(k p) n -> p k n", p=128)), writes=[WR])
        g_bc = T("g_bc", [128, D]); tmpb = T("tmpb", [128, D])
        P.dma(lambda e: e.dma_start(out=g_bc[:], in_=gv.to_broadcast([128, D])), writes=[g_bc])
        GA = T("GA", [128, D]); A = T("A", [128, D]); Bt = T("Bt", [128, D])
        xt = [T("xt%d" % i, [128, D]) for i in range(2)]
        mt = [T("mt%d" % i, [128, D], BF16) for i in range(2)]
        xm = [T("xm%d" % i, [128, D]) for i in range(2)]
        h1 = T("h1", [128, D])
        h2f = T("h2f", [128, D]); h2b = [T("h2b%d" % i, [128, D], BF16) for i in range(2)]
        h2T = T("h2T", [128, D])
        ss = T("ss", [128, 1]); rstd = T("rstd", [128, 1])
        mx = T("mx", [128, 1]); se = T("se", [128, 1]); ex = T("ex", [128, 16]); af = [T("af%d" % i, [128, 16]) for i in range(2)]

        def load_mod(mod):
            P.dma(lambda e: e.dma_start(out=GA[:], in_=mod[2:3, :].to_broadcast([128, D])), writes=[GA])
            P.dma(lambda e: e.dma_start(out=Bt[:], in_=mod[3:4, :].to_broadcast([128, D])), writes=[Bt])
            P.dma(lambda e: e.dma_start(out=tmpb[:], in_=mod[4:5, :].to_broadcast([128, D])), writes=[tmpb])
            P.op("dve", lambda e: e.scalar_tensor_tensor(out=A[:], in0=tmpb[:], scalar=1.0, in1=g_bc[:], op0=ALU.add, op1=ALU.mult),
                 reads=[tmpb, g_bc], writes=[A])

        def tile(it, rows, xsrc, msrc, xdst, hdst, adst):
            x = xt[it % 2]; m = mt[it % 2]; xo = xm[it % 2]; hb = h2b[it % 2]; a_ = af[it % 2]
            P.dma(lambda e: e.dma_start(out=x[:rows, :], in_=xsrc), writes=[x])
            if rows == 128:
                P.dma(lambda e: e.dma_start(out=m[:], in_=msrc), writes=[m])
            else:
                P.dma(lambda e: e.dma_start(out=m[:, 0:16 * rows].rearrange("p (k t) -> p k t", k=16), in_=msrc), writes=[m])
            m3 = m[:, 0:16 * rows].rearrange("p (k t) -> p k t", k=16)
            for nb in range(4):
                pp = psf[nb]
                for kc in range(16):
                    P.op("pe", lambda e, pp=pp, kc=kc, nb=nb: e.matmul(pp[:rows, :], lhsT=m3[:, kc, :], rhs=W3[:, kc, nb * 512:(nb + 1) * 512], start=(kc == 0), stop=(kc == 15)),
                         reads=[m, (W, kc)], writes=[pp])
            yield "A"
            for nb in range(4):
                pp = psf[nb]
                sl = slice(nb * 512, (nb + 1) * 512)
                P.op("dve", lambda e, pp=pp, sl=sl: e.tensor_tensor(out=h1[:rows, sl], in0=pp[:rows, :], in1=GA[:rows, sl], op=ALU.mult), reads=[pp, GA], writes=[(h1, nb)])
                P.op("pool", lambda e, sl=sl: e.tensor_tensor(out=xo[:rows, sl], in0=x[:rows, sl], in1=h1[:rows, sl], op=ALU.add), reads=[x, (h1, nb)], writes=[(xo, nb)])
            xok = [(xo, nb) for nb in range(4)]; h1k = [(h1, nb) for nb in range(4)]
            P.dma(lambda e: e.dma_start(out=xdst, in_=xo[:rows, :]), reads=xok, writes=["xmo"], final=True)
            P.op("act", lambda e: e.activation(out=h1[:rows, :], in_=xo[:rows, :], func=AF.Square, accum_out=ss[:rows, :]), reads=xok, writes=h1k + [ss])
            P.op("act", lambda e: e.activation(out=rstd[:rows, :], in_=ss[:rows, :], func=AF.Sqrt, scale=1.0 / D, bias=EPS), reads=[ss], writes=[rstd])
            P.op("dve", lambda e: e.reciprocal(out=rstd[:rows, :], in_=rstd[:rows, :]), reads=[rstd], writes=[rstd])
            P.op("dve", lambda e: e.scalar_tensor_tensor(out=h1[:rows, :], in0=xo[:rows, :], scalar=rstd[:rows, 0:1], in1=A[:rows, :], op0=ALU.mult, op1=ALU.mult),
                 reads=xok + [rstd, A], writes=h1k)
            P.op("pool", lambda e: e.tensor_tensor(out=h2f[:rows, :], in0=h1[:rows, :], in1=Bt[:rows, :], op=ALU.add), reads=h1k + [Bt], writes=[h2f])
            P.op("act", lambda e: e.copy(out=hb[:rows, :], in_=h2f[:rows, :]), reads=[h2f], writes=[hb])
            P.dma(lambda e: e.dma_start(out=hdst, in_=hb[:rows, :]), reads=[hb], writes=["h2o"], final=True)
            yield "M"
            h2T3 = h2T[:].rearrange("p (k t) -> p k t", k=16)
            for grp in range(4):
                pp = psf[4 + grp]
                for k in range(4):
                    kc = grp * 4 + k
                    P.op("pe", lambda e, pp=pp, k=k, kc=kc: e.transpose(out=pp[:, k * 128:k * 128 + rows], in_=h2f[:rows, kc * 128:(kc + 1) * 128], identity=identf[:rows, :rows]),
                         reads=[h2f, identf], writes=[pp])
                eng = "act" if grp % 2 == 0 else "dve"
                if rows == 128:
                    if eng == "act":
                        P.op("act", lambda e, pp=pp, grp=grp: e.copy(out=h2T[:, grp * 512:(grp + 1) * 512], in_=pp[:, :]), reads=[pp], writes=[(h2T, grp)])
                    else:
                        P.op("dve", lambda e, pp=pp, grp=grp: e.tensor_copy(out=h2T[:, grp * 512:(grp + 1) * 512], in_=pp[:, :]), reads=[pp], writes=[(h2T, grp)])
                else:
                    src = pp[:, :].rearrange("p (k t) -> p k t", k=4)[:, :, 0:rows]
                    dst = h2T3[:, grp * 4:(grp + 1) * 4, 0:rows]
                    P.op("dve", lambda e, src=src, dst=dst: e.tensor_copy(out=dst, in_=src), reads=[pp], writes=[(h2T, grp)])
            pl = psf[7]
            for kc in range(16):
                P.op("pe", lambda e, kc=kc: e.matmul(pl[:rows, 0:16], lhsT=h2T3[:, kc, 0:rows], rhs=WR3[:, kc, :], start=(kc == 0), stop=(kc == 15)),
                     reads=[(h2T, kc // 4), WR], writes=[pl])
            P.op("dve", lambda e: e.reduce_max(out=mx[:rows, :], in_=pl[:rows, 0:16], axis=AX.X), reads=[pl], writes=[mx])
            P.op("dve", lambda e: e.tensor_scalar(out=mx[:rows, :], in0=mx[:rows, :], scalar1=-1.0, scalar2=None, op0=ALU.mult), reads=[mx], writes=[mx])
            P.op("act", lambda e: e.activation(out=ex[:rows, :], in_=pl[:rows, 0:16], func=AF.Exp, bias=mx[:rows, 0:1], accum_out=se[:rows, :]), reads=[pl, mx], writes=[ex, se])
            P.op("dve", lambda e: e.reciprocal(out=se[:rows, :], in_=se[:rows, :]), reads=[se], writes=[se])
            P.op("dve", lambda e: e.tensor_scalar(out=a_[:rows, :], in0=ex[:rows, :], scalar1=se[:rows, 0:1], scalar2=None, op0=ALU.mult), reads=[ex, se], writes=[a_])
            P.dma(lambda e: e.dma_start(out=adst, in_=a_[:rows, :]), reads=[a_], writes=["affo"], final=True)

        load_mod(modl)
        gens = []
        for it in range(16):
            r = slice(it * 128, (it + 1) * 128)
            gens.append(tile(it, 128, xl[r, :], mixl[it], xmo_l[r, :], h2_l[r, :], aff_l[it]))
        gens.append(tile(16, 64, xc, mixc, xmo_c, h2_c, aff_c))
        next(gens[0])
        for t in range(17):
            next(gens[t])
            if t == 15:
                load_mod(modc)
            if t + 1 < 17:
                next(gens[t + 1])
            for _ in gens[t]:
                pass
        P.end_phase()


D = 2048; FF = 1024
CAPL = 1024; CAPC = 32
BIG = float(1 << 15)
NIT = 28

def emit_moe(nc, P, pre, io, g):
    only_slots = False
    AFF8 = io["AFF8"]; SLOT8 = io["SLOT8"]; capv = io["capv"]; h2l = io["H2L"]; h2c = io["H2C"]
    wg = io["wg"]; wu = io["wu"]; wd = io["wd"]; stri_d = io["stri"]; identb_d = io["identb"]
    xin_l = io["XINL"]; xin_c = io["XINC"]
    with contextlib.ExitStack() as st0:
        psf = [P.psum(st0, pre + "psf%d" % i, [128, 512], F32) for i in range(6)]
        psb = [P.psum(st0, pre + "psb%d" % i, [128, 1024], BF16) for i in range(2)]
        T0 = lambda name, shape, dt=F32: st0.enter_context(nc.sbuf_tensor(pre + name, shape, dt))
        identb = T0("identb_s", [128, 128], BF16)
        P.dma(lambda e: e.dma_start(out=identb[:], in_=identb_d), writes=[identb])
        if True:
            st = st0
            T = lambda name, shape, dt=F32: st.enter_context(nc.sbuf_tensor(pre + name, shape, dt))
            stri = T("stri_s", [128, 128]); ones = T("ones", [128, 128]); zeros = T("zeros", [128, 64])
            A8 = T("A8", [128, 512]); cap = T("cap", [128, 8])
            P.dma(lambda e: e.dma_start(out=stri[:], in_=stri_d), writes=[stri])
            A8raw = T("A8raw", [128, 512])
            Ar3 = A8raw[:].rearrange("p (i l) -> p i l", l=8)
            P.dma(lambda e: e.dma_start(out=Ar3[:, :, 0:4], in_=AFF8[:, :, 4 * g:4 * g + 4]), writes=[A8raw])
            P.dma(lambda e: e.dma_start(out=Ar3[:, :, 4:8], in_=AFF8[:, :, 16 + 4 * g:16 + 4 * g + 4]), writes=[A8raw])
            P.op("dve", lambda e: e.tensor_copy(out=A8[:].rearrange("p (l i) -> p l i", l=8), in_=A8raw[:].rearrange("p (i l) -> p l i", l=8)), reads=[A8raw], writes=[A8])
            P.dma(lambda e: e.dma_start(out=cap[:], in_=capv), writes=[cap])
            P.op("pool", lambda e: e.memset(ones[:], 1.0), writes=[ones])
            P.op("pool", lambda e: e.memset(zeros[:], 0.0), writes=[zeros])
            lo = T("lo", [128, 8]); th = T("th", [128, 8]); cntp = T("cntp", [128, 8]); tt = T("tt", [128, 8])
            cmp_ = T("cmp", [128, 512]); mask = T("mask", [128, 512]); m0 = T("m0", [128, 512])
            cum = T("cum", [128, 512]); s2 = T("s2", [128, 512]); totp = T("totp", [128, 8]); offs = T("offs", [128, 8])
            sloti = T("sloti", [128, 512], I32)
            A3 = A8[:].rearrange("p (l i) -> p l i", l=8)
            c3 = cmp_[:].rearrange("p (l i) -> p l i", l=8)
            pc = psf[0]
            P.op("pool", lambda e: e.memset(lo[:], 0.0), writes=[lo])
            for n in range(NIT):
                dl = 2.0 ** -(n + 1)
                P.op("dve", lambda e, dl=dl: e.tensor_scalar(out=th[:], in0=lo[:], scalar1=dl, scalar2=None, op0=ALU.add), reads=[lo], writes=[th])
                P.op("dve", lambda e: e.tensor_tensor(out=c3, in0=A3, in1=th[:].unsqueeze(2).to_broadcast([128, 8, 64]), op=ALU.is_ge), reads=[A8, th], writes=[cmp_])
                P.op("dve", lambda e: e.reduce_sum(out=cntp[:], in_=c3, axis=AX.X), reads=[cmp_], writes=[cntp])
                P.op("pe", lambda e: e.matmul(pc[:, 0:8], lhsT=ones[:], rhs=cntp[:], start=True, stop=True), reads=[ones, cntp], writes=[pc])
                P.op("dve", lambda e: e.tensor_tensor(out=tt[:], in0=pc[:, 0:8], in1=cap[:], op=ALU.is_ge), reads=[pc, cap], writes=[tt])
                P.op("dve", lambda e, dl=dl: e.scalar_tensor_tensor(out=lo[:], in0=tt[:], scalar=dl, in1=lo[:], op0=ALU.mult, op1=ALU.add), reads=[tt, lo], writes=[lo])
            m3 = mask[:].rearrange("p (l i) -> p l i", l=8)
            P.op("dve", lambda e: e.tensor_tensor(out=m3, in0=A3, in1=lo[:].unsqueeze(2).to_broadcast([128, 8, 64]), op=ALU.is_ge), reads=[A8, lo], writes=[mask])
            P.op("dve", lambda e: e.tensor_scalar(out=m0[:], in0=A8[:], scalar1=0.0, scalar2=None, op0=ALU.is_gt), reads=[A8], writes=[m0])
            P.op("dve", lambda e: e.tensor_tensor(out=mask[:], in0=mask[:], in1=m0[:], op=ALU.mult), reads=[mask, m0], writes=[mask])
            cu3 = cum[:].rearrange("p (l i) -> p l i", l=8)
            for l in range(8):
                P.op("dve", lambda e, l=l: e.tensor_tensor_scan(out=cum[:, l * 64:(l + 1) * 64], data0=mask[:, l * 64:(l + 1) * 64], data1=zeros[:, 0:64], initial=0.0, op0=ALU.add, op1=ALU.add),
                     reads=[mask, zeros], writes=[cum])
            P.op("dve", lambda e: e.tensor_copy(out=totp[:], in_=cu3[:, :, 63]), reads=[cum], writes=[totp])
            P.op("pe", lambda e: e.matmul(pc[:, 0:8], lhsT=stri[:], rhs=totp[:], start=True, stop=True), reads=[stri, totp], writes=[pc])
            P.op("act", lambda e: e.copy(out=offs[:], in_=pc[:, 0:8]), reads=[pc], writes=[offs])
            P.op("dve", lambda e: e.tensor_scalar(out=s2[:], in0=mask[:], scalar1=-BIG, scalar2=BIG - 1.0, op0=ALU.mult, op1=ALU.add), reads=[mask], writes=[s2])
            P.op("dve", lambda e: e.tensor_tensor(out=cu3, in0=cu3, in1=offs[:].unsqueeze(2).to_broadcast([128, 8, 64]), op=ALU.add), reads=[cum, offs], writes=[cum])
            P.op("dve", lambda e: e.tensor_tensor(out=cum[:], in0=cum[:], in1=s2[:], op=ALU.add), reads=[cum, s2], writes=[cum])
            P.op("dve", lambda e: e.tensor_copy(out=sloti[:], in_=cum[:]), reads=[cum], writes=[sloti])
            Sraw = T("Sraw", [128, 512], I32)
            P.op("dve", lambda e: e.tensor_copy(out=Sraw[:].rearrange("p (i l) -> p l i", l=8), in_=sloti[:].rearrange("p (l i) -> p l i", l=8)), reads=[sloti], writes=[Sraw])
            Sr3 = Sraw[:].rearrange("p (i l) -> p i l", l=8)
            P.dma(lambda e: e.dma_start(out=SLOT8[:, :, 4 * g:4 * g + 4], in_=Sr3[:, :, 0:4]), reads=[Sraw], writes=["slot8"])
            P.dma(lambda e: e.dma_start(out=SLOT8[:, :, 16 + 4 * g:16 + 4 * g + 4], in_=Sr3[:, :, 4:8]), reads=[Sraw], writes=["slot8"])
            s3 = sloti[:].rearrange("p (l i) -> p l i", l=8)
            R = [T("R%d" % i, [128, D], BF16) for i in range(3)]
            nR = 0
            hv = h2l.rearrange("(i p) d -> p i d", p=128)
            for i in range(64):
                r = R[nR % 3]; nR += 1
                P.dma(lambda e, r=r, i=i: e.dma_start(out=r[:], in_=hv[:, i, :]), writes=[r])
                for lane in range(4):
                    P.dma(lambda e, r=r, lane=lane, i=i: e.indirect_dma_start(out=xin_l[lane][:, :], out_offset=bass.IndirectOffsetOnAxis(ap=s3[:, lane, i:i + 1], axis=0),
                                                                             in_=r[:, :], in_offset=None, bounds_check=P.getreg(e, CAPL - 1), oob_is_err=False),
                          eng="pool", reads=[r, sloti], writes=["xin_l"])
            hv2 = h2c.rearrange("(i p) d -> p i d", p=128)
            for i in range(2):
                r = R[nR % 3]; nR += 1
                P.dma(lambda e, r=r, i=i: e.dma_start(out=r[:], in_=hv2[:, i, :]), writes=[r])
                for lane in range(4, 8):
                    P.dma(lambda e, r=r, lane=lane, i=i: e.indirect_dma_start(out=xin_c[lane - 4][:, :], out_offset=bass.IndirectOffsetOnAxis(ap=s3[:, lane, i:i + 1], axis=0),
                                                                             in_=r[:, :], in_offset=None, bounds_check=P.getreg(e, CAPC - 1), oob_is_err=False),
                          eng="pool", reads=[r, sloti], writes=["xin_c"])
            P.barrier()
        if True:
            st = st0
            T = lambda name, shape, dt=F32: st.enter_context(nc.sbuf_tensor(pre + name, shape, dt))
            Wg = T("Wg", [128, 16 * FF], BF16); Wu = T("Wu", [128, 16 * FF], BF16); Wd = T("Wd", [128, 8 * D], BF16)
            Wg3 = Wg[:].rearrange("p (k n) -> p k n", k=16); Wu3 = Wu[:].rearrange("p (k n) -> p k n", k=16); Wd3 = Wd[:].rearrange("p (k n) -> p k n", k=8)
            XT = T("XT", [128, 16 * 512], BF16); XT3 = XT[:].rearrange("p (k s) -> p k s", k=16)
            ATa = T("ATa", [128, 8 * 512], BF16); ATb = T("ATb", [128, 8 * 512], BF16); ATc = T("ATc", [128, 8 * 32], BF16)
            xs = [T("xs%d" % i, [128, D], BF16) for i in range(2)]
            ysb = [T("ysb%d" % i, [128, D]) for i in range(2)]
            sa = [T("sa%d" % i, [128, 512]) for i in range(2)]
            cnt = {"x": 0, "y": 0, "s": 0}

            def ffn_gu(xsrc, ns, AT):
                AT3 = AT[:].rearrange("p (k s) -> p k s", k=8)
                tiles = [(s, min(128, ns - s)) for s in range(0, ns, 128)]
                for (s0, rows) in tiles:
                    x = xs[cnt["x"] % 2]; cnt["x"] += 1
                    P.dma(lambda e, x=x, s0=s0, rows=rows: e.dma_start(out=x[:rows, :], in_=xsrc[s0:s0 + rows, :]), reads=["xin_l", "xin_c"], writes=[x])
                    for half in range(2):
                        pb = psb[half]
                        for k in range(8):
                            kc = half * 8 + k
                            P.op("pe", lambda e, pb=pb, x=x, k=k, kc=kc, rows=rows: e.transpose(out=pb[:, k * 128:k * 128 + rows], in_=x[:rows, kc * 128:(kc + 1) * 128], identity=identb[:rows, :rows]),
                                 reads=[x, identb], writes=[pb])
                        src = pb[:, :].rearrange("p (k t) -> p k t", k=8)[:, :, 0:rows]
                        dst = XT3[:, half * 8:(half + 1) * 8, s0:s0 + rows]
                        if half == 0:
                            P.op("act", lambda e, src=src, dst=dst: e.copy(out=dst, in_=src), reads=[pb], writes=[XT])
                        else:
                            P.op("dve", lambda e, src=src, dst=dst: e.tensor_copy(out=dst, in_=src), reads=[pb], writes=[XT])
                for ft in range(8):
                    pa = psf[ft % 2]; pu = psf[2 + ft % 2]; s_ = sa[ft % 2]
                    for kc in range(16):
                        P.op("pe", lambda e, pa=pa, kc=kc, ft=ft: e.matmul(pa[:, 0:ns], lhsT=Wg3[:, kc, ft * 128:(ft + 1) * 128], rhs=XT3[:, kc, 0:ns], start=(kc == 0), stop=(kc == 15)),
                             reads=[(Wg, kc), XT], writes=[pa])
                    for kc in range(16):
                        P.op("pe", lambda e, pu=pu, kc=kc, ft=ft: e.matmul(pu[:, 0:ns], lhsT=Wu3[:, kc, ft * 128:(ft + 1) * 128], rhs=XT3[:, kc, 0:ns], start=(kc == 0), stop=(kc == 15)),
                             reads=[(Wu, kc), XT], writes=[pu])
                    P.op("act", lambda e, pa=pa, s_=s_: e.activation(out=s_[:, 0:ns], in_=pa[:, 0:ns], func=AF.Silu), reads=[pa], writes=[s_])
                    P.op("dve", lambda e, pu=pu, s_=s_, ft=ft: e.tensor_tensor(out=AT3[:, ft, 0:ns], in0=s_[:, 0:ns], in1=pu[:, 0:ns], op=ALU.mult), reads=[s_, pu], writes=[(AT, ft)])

            def ffn_down(ydst, ns, AT):
                AT3 = AT[:].rearrange("p (k s) -> p k s", k=8)
                tiles = [(s, min(128, ns - s)) for s in range(0, ns, 128)]
                ATk = [(AT, ft) for ft in range(8)]
                for (s0, rows) in tiles:
                    y = ysb[cnt["y"] % 2]; cnt["y"] += 1
                    for nb in range(4):
                        pd = psf[4 + nb % 2]
                        for fc in range(8):
                            P.op("pe", lambda e, pd=pd, fc=fc, nb=nb, s0=s0, rows=rows: e.matmul(pd[:rows, :], lhsT=AT3[:, fc, s0:s0 + rows], rhs=Wd3[:, fc, nb * 512:(nb + 1) * 512], start=(fc == 0), stop=(fc == 7)),
                                 reads=ATk + [(Wd, fc)], writes=[pd])
                        if nb % 2 == 0:
                            P.op("act", lambda e, pd=pd, y=y, nb=nb, rows=rows: e.copy(out=y[:rows, nb * 512:(nb + 1) * 512], in_=pd[:rows, :]), reads=[pd], writes=[(y, nb)])
                        else:
                            P.op("dve", lambda e, pd=pd, y=y, nb=nb, rows=rows: e.tensor_copy(out=y[:rows, nb * 512:(nb + 1) * 512], in_=pd[:rows, :]), reads=[pd], writes=[(y, nb)])
                    P.dma(lambda e, y=y, s0=s0, rows=rows: e.dma_start(out=ydst[s0:s0 + rows, :], in_=y[:rows, :]), reads=[(y, nb) for nb in range(4)], writes=["yout"], final=True)

            for el in range(4):
                ex = 4 * g + el
                for kc in range(16):
                    P.dma(lambda e, kc=kc, ex=ex: e.dma_start(out=Wg3[:, kc, :], in_=wg[ex, kc * 128:(kc + 1) * 128, :]), eng="pool", writes=[(Wg, kc)])
                    P.dma(lambda e, kc=kc, ex=ex: e.dma_start(out=Wu3[:, kc, :], in_=wu[ex, kc * 128:(kc + 1) * 128, :]), eng="pool", writes=[(Wu, kc)])
                for kc in range(8):
                    P.dma(lambda e, kc=kc, ex=ex: e.dma_start(out=Wd3[:, kc, :], in_=wd[ex, kc * 128:(kc + 1) * 128, :]), eng="pool", writes=[(Wd, kc)])
                ffn_gu(xin_l[el][0:512, :], 512, ATa)
                ffn_gu(xin_l[el][512:1024, :], 512, ATb)
                ffn_gu(xin_c[el], CAPC, ATc)
                ffn_down(io["YL"][ex][0:512, :], 512, ATa)
                ffn_down(io["YL"][ex][512:1024, :], 512, ATb)
                ffn_down(io["YC"][ex], CAPC, ATc)
            P.end_phase()


BF = ml_dtypes.bfloat16
D = 2048; FF = 1024; N = 8192; NCTX = 256

def emit_k0(nc, P, pre, io):
    cT = io["cT"]; w = io["ada_w"]; b = io["ada_b"]; MOD = io["MOD"]
    with contextlib.ExitStack() as st:
        T = lambda name, shape, dt=F32: st.enter_context(nc.sbuf_tensor(pre + name, shape, dt))
        ps = [P.psum(st, pre + "ps%d" % i, [128, 512], F32) for i in range(2)]
        cs = T("cs", [128, 32]); ss = T("ss", [128, 32])
        wb = [T("wb%d" % i, [128, 16 * 512]) for i in range(3)]
        bt = [T("bt%d" % i, [2, 512]) for i in range(2)]; ot = [T("ot%d" % i, [2, 512]) for i in range(2)]
        zt = T("zt", [128, 2048])
        P.op("pool", lambda e: e.memset(zt[:], 0.0), writes=[zt])
        P.dma(lambda e: e.dma_start(out=io["AFF8"].rearrange("p i l -> p (i l)"), in_=zt[:]), reads=[zt], writes=["aff8"])
        P.dma(lambda e: e.dma_start(out=cs[:], in_=cT), writes=[cs])
        P.op("act", lambda e: e.activation(out=ss[:], in_=cs[:], func=AF.Silu), reads=[cs], writes=[ss])
        blk = 0
        for i in range(4):
            wv = w[i].rearrange("(kc p) n -> p kc n", p=128)
            for j in range(24):
                wt = wb[blk % 3]; pt = ps[blk % 2]; btj = bt[blk % 2]; otj = ot[blk % 2]
                c0 = j * 512
                P.dma(lambda e, wt=wt, wv=wv, c0=c0: e.dma_start(out=wt[:].rearrange("p (kc n) -> p kc n", kc=16), in_=wv[:, :, c0:c0 + 512]), writes=[wt])
                P.dma(lambda e, btj=btj, i=i, c0=c0: e.dma_start(out=btj[:], in_=b[i:i + 1, c0:c0 + 512].to_broadcast([2, 512])), writes=[btj])
                for kc in range(16):
                    P.op("pe", lambda e, pt=pt, wt=wt, kc=kc: e.matmul(pt[0:2, :], lhsT=ss[:, kc * 2:(kc + 1) * 2], rhs=wt[:, kc * 512:(kc + 1) * 512], start=(kc == 0), stop=(kc == 15)),
                         reads=[ss, wt], writes=[pt])
                P.op("dve", lambda e, pt=pt, btj=btj, otj=otj: e.tensor_tensor(out=otj[:], in0=pt[0:2, :], in1=btj[:], op=ALU.add), reads=[pt, btj], writes=[otj])
                P.dma(lambda e, otj=otj, i=i, c0=c0: e.dma_start(out=MOD[i, :, c0:c0 + 512], in_=otj[:]), reads=[otj], writes=["mod"])
                blk += 1
        P.end_phase()

def build_fused(nlayers=4):
    nc = bass.Bass("TRN2", target_bir_lowering=False)
    DI = lambda n, s, dt=F32: nc.dram_tensor(n, s, dt, kind="ExternalInput").ap()
    SC = lambda n, s, dt=F32: nc.dram_tensor(n, s, dt, kind="Internal").ap()
    x = DI("x", [N, D]); ctx = DI("ctx", [NCTX, D]); cT = DI("cT", [128, 32])
    ada_w = DI("ada_w", [4, D, 12288]); ada_b = DI("ada_b", [4, 12288]); norm_g = DI("norm_g", [4, 2, D]); final_g = DI("final_g", [1, D])
    ev_w_in = DI("ev_w_in", [2, D, 5152]); ev_gate_b = DI("ev_gate_b", [2, 4, 8]); ev_head_g = DI("ev_head_g", [2, 1024])
    ev_pool_w = DI("ev_pool_w", [2, 4, 256, 256]); ev_pool_scale = DI("ev_pool_scale", [2, 1024]); ev_w_out = DI("ev_w_out", [2, D, D])
    na_w_in = DI("na_w_in", [2, D, 6144]); na_bias = DI("na_bias", [2, 16, 128, 3200]); na_w_out = DI("na_w_out", [2, D, D])
    w_router = DI("moe_w_router", [4, D, 16]); w_gate = DI("moe_w_gate", [4, 16, D, FF]); w_up = DI("moe_w_up", [4, 16, D, FF]); w_down = DI("moe_w_down", [4, 16, FF, D])
    identb = DI("identb", [128, 128], BF16); identf = DI("identf", [128, 128]); tri = DI("tri", [2, 128, 128]); stri = DI("stri", [128, 128])
    tab = DI("tab", [66, 128, 256]); icl = DI("icl", [4, N]); icc = DI("icc", [4, NCTX]); capv = DI("capv", [128, 8])
    out = nc.dram_tensor("out", [N, D], F32, kind="ExternalOutput").ap()
    MOD = SC("MOD", [4, 2, 12288]); XL = SC("XL", [N, D]); XC = SC("XC", [NCTX, D])
    HT = SC("HT", [66, 128, 2048], BF16); MIXT = SC("MIXT", [66, 128, 16, 128], BF16)
    H2L = SC("H2L", [N, D], BF16); H2C = SC("H2C", [NCTX, D], BF16)
    AFF8 = SC("AFF8", [128, 64, 32]); SLOT8 = SC("SLOT8", [128, 64, 32], I32)
    YL = [SC("YL%d" % i, [1024, D]) for i in range(16)]; YC = [SC("YC%d" % i, [32, D]) for i in range(16)]
    XINL = [SC("XINL%d" % i, [1024, D], BF16) for i in range(4)]; XINC = [SC("XINC%d" % i, [32, D], BF16) for i in range(4)]
    P = Prog(nc)
    emit_k0(nc, P, "k0_", {"cT": cT, "ada_w": ada_w, "ada_b": ada_b, "MOD": MOD, "AFF8": AFF8})
    mrows = lambda i, r: MOD[i, r].rearrange("(s d) -> s d", s=6)

    def g1_io(i, q, comb, final):
        src_l, src_c = (x, ctx) if (i == 0 and not comb) else (XL, XC)
        io = {"xl": src_l[q * 2048:(q + 1) * 2048, :], "xc": src_c[q * 64:(q + 1) * 64, :], "ident": identb}
        if final:
            io["g"] = final_g; io["outl"] = out[q * 2048:(q + 1) * 2048, :]
        else:
            io["g"] = norm_g[i, 0:1, :]; io["modl"] = mrows(i, 0); io["modc"] = mrows(i, 1)
            io["hTl"] = [HT[2 + q * 16 + it] for it in range(16)]
            io["hTc"] = HT[q // 2].rearrange("p (k x) -> p k x", k=16)[:, :, (q % 2) * 64:(q % 2 + 1) * 64]
        if comb:
            ip = i - 1
            io.update({"yl": YL, "yc": YC, "slotl": [SLOT8[:, q * 16 + it, 0:16] for it in range(16)], "affl": [AFF8[:, q * 16 + it, 0:16] for it in range(16)],
                       "slotc": SLOT8[(q % 2) * 64:(q % 2 + 1) * 64, q // 2, 16:32], "affc": AFF8[(q % 2) * 64:(q % 2 + 1) * 64, q // 2, 16:32],
                       "gafl": mrows(ip, 0)[5:6, :], "gafc": mrows(ip, 1)[5:6, :], "xlo": XL[q * 2048:(q + 1) * 2048, :], "xco": XC[q * 64:(q + 1) * 64, :]})
        return io

    for i in range(nlayers):
        j = i // 2
        for q in range(4):
            emit_g1(nc, P, "L%dg1q%d_" % (i, q), g1_io(i, q, i > 0, False), i > 0, False)
        if i % 2 == 0:
            for hp in range(4):
                emit_mixe(nc, P, "L%dmxh%d_" % (i, hp), {"HT": HT, "MIXT": MIXT, "w_in": ev_w_in[j], "gate_b": ev_gate_b[j], "head_g": ev_head_g[j:j + 1, :],
                                                         "tab": tab, "identb": identb, "identf": identf, "tri": tri, "pool_w": ev_pool_w[j],
                                                         "pool_scale": ev_pool_scale[j], "icl": icl, "icc": icc}, hp)
            wout = ev_w_out[j]
        else:
            for hq in range(4):
                emit_mixo(nc, P, "L%dmxh%d_" % (i, hq), {"HT": HT, "MIXT": MIXT, "w_in": na_w_in[j], "bias": na_bias[j], "identb": identb}, hq, i != 3)
            wout = na_w_out[j]
        for q in range(4):
            src_l, src_c = (x, ctx) if i == 0 else (XL, XC)
            emit_g2(nc, P, "L%dg2q%d_" % (i, q), {
                "xl": src_l[q * 2048:(q + 1) * 2048, :], "xc": src_c[q * 64:(q + 1) * 64, :],
                "mixl": [MIXT[2 + q * 16 + it].rearrange("p k x -> p (k x)") for it in range(16)],
                "mixc": MIXT[q // 2][:, :, (q % 2) * 64:(q % 2 + 1) * 64], "wout": wout, "wr": w_router[i],
                "modl": mrows(i, 0), "modc": mrows(i, 1), "g": norm_g[i, 1:2, :], "identf": identf,
                "xmo_l": XL[q * 2048:(q + 1) * 2048, :], "xmo_c": XC[q * 64:(q + 1) * 64, :],
                "h2_l": H2L[q * 2048:(q + 1) * 2048, :], "h2_c": H2C[q * 64:(q + 1) * 64, :],
                "aff_l": [AFF8[:, q * 16 + it, 0:16] for it in range(16)], "aff_c": AFF8[(q % 2) * 64:(q % 2 + 1) * 64, q // 2, 16:32]})
        for g in range(4):
            emit_moe(nc, P, "L%dmoe%d_" % (i, g), {"AFF8": AFF8, "SLOT8": SLOT8, "capv": capv, "H2L": H2L, "H2C": H2C, "wg": w_gate[i], "wu": w_up[i], "wd": w_down[i],
                                                    "stri": stri, "identb": identb, "YL": YL, "YC": YC, "XINL": XINL, "XINC": XINC}, g)
    for q in range(4):
        emit_g1(nc, P, "Fg1q%d_" % q, g1_io(nlayers, q, True, True), True, True)
    P.flush(True)
    return nc

def host_consts(na_rpb):
    c = {}
    c["identb"] = np.eye(128, dtype=BF); c["identf"] = np.eye(128, dtype=np.float32)
    l = np.arange(128)
    c["tri"] = np.stack([(l[:, None] <= l[None, :]), (l[:, None] >= l[None, :])]).astype(np.float32)
    c["stri"] = (l[:, None] < l[None, :]).astype(np.float32)
    capv = np.zeros((128, 8), np.float32); capv[:, :4] = 1024; capv[:, 4:] = 32
    c["capv"] = capv
    tab = np.zeros((66, 128, 2, 2, 2, 32), np.float32); sc = 128.0 ** -0.5
    tab[0:2, :, 0, 0] = 1.0; tab[0:2, :, 0, 1] = sc
    inv = (10000.0 ** (-np.arange(32, dtype=np.float32) / 32)).astype(np.float32)
    t = np.arange(N); rows = (t // 64).astype(np.float32); cols = (t % 64).astype(np.float32)
    for blk, pos in ((0, rows), (1, cols)):
        ang = (pos[:, None] * inv[None, :]).astype(np.float32)
        cs = np.cos(ang).astype(np.float32).reshape(64, 128, 32); sn = np.sin(ang).astype(np.float32).reshape(64, 128, 32)
        tab[2:, :, 0, 0, blk] = cs; tab[2:, :, 1, 0, blk] = sn; tab[2:, :, 0, 1, blk] = cs * sc; tab[2:, :, 1, 1, blk] = sn * sc
    c["tab"] = tab.reshape(66, 128, 256)
    def inv_cnt(n):
        t = np.arange(n); o = np.zeros((4, n), np.float32)
        for g, w in enumerate((2, 4, 8, 16)):
            lo = np.clip(t - w // 2, 0, n); hi = np.clip(t + w // 2, 0, n); o[g] = 1.0 / (hi - lo)
        return o
    c["icl"] = inv_cnt(N); c["icc"] = inv_cnt(NCTX)
    per_m, idxmaps = na_patterns()
    nb = np.zeros((2, 16, 128, 5, 5, 128), np.float32)
    for j in range(2):
        rf = na_rpb[j].reshape(16, 15 * 31)
        for h in range(16):
            g = np.where(idxmaps >= 0, rf[h][np.maximum(idxmaps, 0)], np.float32(-30000.0))
            nb[j, h] = g.transpose(2, 0, 1, 3)
    c["na_bias"] = nb.reshape(2, 16, 128, 3200)
    return c


def kernel(x, c, ctx, c_ctx, ada_w, ada_b, norm_g, final_g, ev_w_in, ev_gate_b, ev_head_g, ev_pool_w,
           ev_pool_scale, ev_w_out, na_w_in, na_rpb, na_w_out, moe_w_router, moe_w_gate, moe_w_up, moe_w_down):
    A = lambda a: np.ascontiguousarray(np.asarray(a, dtype=np.float32))
    x = A(x); ctx = A(ctx); c = A(c); c_ctx = A(c_ctx)
    shared = {"ada_w": A(ada_w), "ada_b": A(ada_b), "norm_g": A(norm_g), "final_g": A(final_g)[None, :],
              "ev_w_in": A(ev_w_in), "ev_gate_b": A(ev_gate_b), "ev_head_g": A(ev_head_g), "ev_pool_w": A(ev_pool_w),
              "ev_pool_scale": A(ev_pool_scale), "ev_w_out": A(ev_w_out), "na_w_in": A(na_w_in), "na_w_out": A(na_w_out),
              "moe_w_router": A(moe_w_router), "moe_w_gate": A(moe_w_gate), "moe_w_up": A(moe_w_up), "moe_w_down": A(moe_w_down)}
    shared.update(host_consts(A(na_rpb)))
    nc = build_fused(4)
    in_maps = []
    for b in range(2):
        cv = np.stack([c[b], c_ctx])
        m = {"x": x[b], "ctx": ctx[b], "cT": np.ascontiguousarray(cv.reshape(2, 16, 128).transpose(2, 1, 0)).reshape(128, 32)}
        m.update(shared)
        in_maps.append(m)
    res = run_bass_kernel_spmd(nc, in_maps, core_ids=[0, 1])
    return np.stack([res.results[0]["out"], res.results[1]["out"]]).astype(np.float32)
```
